# Optimizing a Trainium2 kernel written in Bass

```python
import jax, jax.numpy as jnp
from jax import lax
import numpy as np

D_MODEL = 4096
BATCH = 4
SEQ = 4096
DEPTH = 1

MIX_WIDTH = D_MODEL
MLSTM_WIDTH = MIX_WIDTH // 2
MOBA_WIDTH = MIX_WIDTH - MLSTM_WIDTH
MLSTM_HEADS = 4
MLSTM_DV = MLSTM_WIDTH // MLSTM_HEADS
MLSTM_DK = MLSTM_DV // 2
MLSTM_QK_WIDTH = MLSTM_HEADS * MLSTM_DK
MLSTM_CHUNK = 128
CONV_WIDTH = 4
GATE_SOFTCAP = 15.0
MOBA_HEAD_DIM = 128
MOBA_HEADS = MOBA_WIDTH // MOBA_HEAD_DIM
MOBA_BLOCK = 256
MOBA_TOPK = 3
MOBA_QCHUNK = 16
ROPE_THETA = 10000.0
PEER_HEADS = 8
PEER_NKEYS = 128
PEER_EXPERTS = PEER_NKEYS * PEER_NKEYS
PEER_TOPK = 16
PEER_QDIM = 256
PEER_TCHUNK = 128
EPS = 1e-6

COL_MQ = 0
COL_MK = COL_MQ + MLSTM_QK_WIDTH
COL_MV = COL_MK + MLSTM_QK_WIDTH
COL_MO = COL_MV + MLSTM_WIDTH
COL_MI = COL_MO + MLSTM_WIDTH
COL_MF = COL_MI + MLSTM_HEADS
COL_AQ = COL_MF + MLSTM_HEADS
COL_AK = COL_AQ + MOBA_WIDTH
COL_AV = COL_AK + MOBA_WIDTH
IN_COLS = COL_AV + MOBA_WIDTH

kernel_name = "hymba_mlstm_moba_peer_adaln"


def rmsnorm(x, w):
    xf = x.astype(jnp.float32)
    y = xf * lax.rsqrt(jnp.mean(xf * xf, axis=-1, keepdims=True) + EPS)
    return (y * w.astype(jnp.float32)).astype(x.dtype)


def softcap(t):
    return GATE_SOFTCAP * jnp.tanh(t / GATE_SOFTCAP)


def rotary(t, positions):
    half = t.shape[-1] // 2
    inv = ROPE_THETA ** (-jnp.arange(half, dtype=jnp.float32) / half)
    ang = positions.astype(jnp.float32)[:, None] * inv[None, :]
    cos, sin = jnp.cos(ang), jnp.sin(ang)
    tf = t.astype(jnp.float32)
    t1, t2 = tf[..., :half], tf[..., half:]
    return jnp.concatenate([t1 * cos - t2 * sin, t2 * cos + t1 * sin], axis=-1).astype(t.dtype)


def causal_depthwise_conv(u, w, b):
    C = u.shape[-1]
    out = lax.conv_general_dilated(u, w[:, None, :].astype(u.dtype), window_strides=(1,),
                                   padding=[(CONV_WIDTH - 1, 0)],
                                   dimension_numbers=("NWC", "WIO", "NWC"),
                                   feature_group_count=C)
    return out + b.astype(u.dtype)


def mlstm_chunkwise(q, k, v, i_pre, f_pre):
    B, H, S, dk = q.shape
    L = MLSTM_CHUNK
    nc = S // L
    qf = q.astype(jnp.float32) * (dk ** -0.5)
    kf, vf = k.astype(jnp.float32), v.astype(jnp.float32)
    log_i = i_pre.astype(jnp.float32)
    log_f = jax.nn.log_sigmoid(f_pre.astype(jnp.float32))

    def chunks(t):
        return jnp.moveaxis(t.reshape(B, H, nc, L, *t.shape[3:]), 2, 0)

    causal = jnp.tril(jnp.ones((L, L), dtype=bool))

    def step(carry, inp):
        C, n, m = carry
        qc, kc, vc, li, lf = inp
        b = jnp.cumsum(lf, axis=-1)
        g = b[..., -1]
        dmat = b[..., :, None] - b[..., None, :] + li[..., None, :]
        dmat = jnp.where(causal, dmat, -jnp.inf)
        inter = b + m[..., None]
        m_t = jnp.maximum(inter, jnp.max(dmat, axis=-1))
        s = jnp.einsum("bhtd,bhsd->bhts", qc, kc) * jnp.exp(dmat - m_t[..., None])
        a_inter = jnp.exp(inter - m_t)
        num = jnp.einsum("bhts,bhsv->bhtv", s, vc) + a_inter[..., None] * jnp.einsum("bhtd,bhdv->bhtv", qc, C)
        den = jnp.sum(s, axis=-1) + a_inter * jnp.einsum("bhtd,bhd->bht", qc, n)
        h = num / jnp.maximum(jnp.abs(den), jnp.exp(-m_t))[..., None]
        wl = g[..., None] - b + li
        m_new = jnp.maximum(g + m, jnp.max(wl, axis=-1))
        ws = jnp.exp(wl - m_new[..., None])
        a = jnp.exp(g + m - m_new)
        C = a[..., None, None] * C + jnp.einsum("bhs,bhsd,bhsv->bhdv", ws, kc, vc)
        n = a[..., None] * n + jnp.einsum("bhs,bhsd->bhd", ws, kc)
        return (C, n, m_new), h

    init = (jnp.zeros((B, H, dk, v.shape[-1]), jnp.float32),
            jnp.zeros((B, H, dk), jnp.float32),
            jnp.zeros((B, H), jnp.float32))
    _, h = lax.scan(step, init, (chunks(qf), chunks(kf), chunks(vf), chunks(log_i), chunks(log_f)))
    return jnp.moveaxis(h, 0, 2).reshape(B, H, S, v.shape[-1])


def moba_attention(q, k, v):
    B, H, S, dh = q.shape
    s_pad = -(-S // MOBA_BLOCK) * MOBA_BLOCK
    pad = s_pad - S
    q, k, v = (jnp.pad(t, ((0, 0), (0, 0), (0, pad), (0, 0))) for t in (q, k, v))
    nb = s_pad // MOBA_BLOCK
    k_blocks = k.reshape(B, H, nb, MOBA_BLOCK, dh)
    v_blocks = v.reshape(B, H, nb, MOBA_BLOCK, dh)
    k_mean = jnp.mean(k_blocks.astype(jnp.float32), axis=3)
    gate = jnp.einsum("bhsd,bhnd->bhsn", q.astype(jnp.float32), k_mean)
    q_block = jnp.arange(s_pad) // MOBA_BLOCK
    past = jnp.arange(nb)[None, :] < q_block[:, None]
    gate = jnp.where(past, gate, -jnp.inf)
    n_sel = min(MOBA_TOPK, nb)
    _, sel = lax.top_k(gate, n_sel)
    sel_valid = sel < q_block[:, None]
    scale = dh ** -0.5
    nq = s_pad // MOBA_QCHUNK

    def to_chunks(t):
        return jnp.moveaxis(t.reshape(B, H, nq, MOBA_QCHUNK, *t.shape[3:]), 2, 0)

    b_idx = jnp.arange(B)[:, None, None, None]
    h_idx = jnp.arange(H)[None, :, None, None]

    def one_chunk(args):
        ci, qc, selc, validc = args
        start = ci * MOBA_QCHUNK
        own = start // MOBA_BLOCK
        k_own = lax.dynamic_index_in_dim(k_blocks, own, axis=2, keepdims=False)
        v_own = lax.dynamic_index_in_dim(v_blocks, own, axis=2, keepdims=False)
        k_sel = k_blocks[b_idx, h_idx, selc]
        v_sel = v_blocks[b_idx, h_idx, selc]
        s_sel = jnp.einsum("bhqd,bhqnkd->bhqnk", qc, k_sel, preferred_element_type=jnp.float32) * scale
        s_sel = jnp.where(validc[..., None], s_sel, -jnp.inf)
        s_own = jnp.einsum("bhqd,bhkd->bhqk", qc, k_own, preferred_element_type=jnp.float32) * scale
        q_pos = start + jnp.arange(MOBA_QCHUNK)
        k_pos = own * MOBA_BLOCK + jnp.arange(MOBA_BLOCK)
        s_own = jnp.where(k_pos[None, :] <= q_pos[:, None], s_own, -jnp.inf)
        s_all = jnp.concatenate([s_sel.reshape(B, H, MOBA_QCHUNK, n_sel * MOBA_BLOCK), s_own], axis=-1)
        p = jax.nn.softmax(s_all, axis=-1).astype(v.dtype)
        p_sel = p[..., :n_sel * MOBA_BLOCK].reshape(B, H, MOBA_QCHUNK, n_sel, MOBA_BLOCK)
        p_own = p[..., n_sel * MOBA_BLOCK:]
        return (jnp.einsum("bhqnk,bhqnkd->bhqd", p_sel, v_sel)
                + jnp.einsum("bhqk,bhkd->bhqd", p_own, v_own))

    outs = lax.map(one_chunk, (jnp.arange(nq), to_chunks(q), to_chunks(sel), to_chunks(sel_valid)))
    return jnp.moveaxis(outs, 0, 2).reshape(B, H, s_pad, dh)[:, :, :S]


def peer_ffn(xn, wq, subkeys, u, v):
    B, S, D = xn.shape
    T = B * S
    xt = xn.reshape(T, D)
    qry = (xt @ wq).reshape(T, PEER_HEADS, 2, PEER_QDIM // 2)
    s = jnp.einsum("thpd,hpnd->thpn", qry, subkeys, preferred_element_type=jnp.float32)
    s1, i1 = lax.top_k(s[:, :, 0], PEER_TOPK)
    s2, i2 = lax.top_k(s[:, :, 1], PEER_TOPK)
    cand = (s1[..., :, None] + s2[..., None, :]).reshape(T, PEER_HEADS, PEER_TOPK * PEER_TOPK)
    cand_id = (i1[..., :, None] * PEER_NKEYS + i2[..., None, :]).reshape(T, PEER_HEADS, PEER_TOPK * PEER_TOPK)
    top_s, top_pos = lax.top_k(cand, PEER_TOPK)
    eid = jnp.take_along_axis(cand_id, top_pos, axis=-1)
    g = jax.nn.softmax(top_s, axis=-1)
    nt = T // PEER_TCHUNK
    kk = PEER_HEADS * PEER_TOPK

    def one(args):
        xc, eidc, gc = args
        u_sel = u[eidc]
        act = jax.nn.gelu(jnp.einsum("td,tkd->tk", xc, u_sel, preferred_element_type=jnp.float32),
                          approximate=False)
        w = (gc * act).astype(xc.dtype)
        return jnp.einsum("tk,tkd->td", w, v[eidc])

    out = lax.map(one, (xt.reshape(nt, PEER_TCHUNK, D), eid.reshape(nt, PEER_TCHUNK, kk),
                        g.reshape(nt, PEER_TCHUNK, kk)))
    return out.reshape(B, S, D)


def setup_inputs(seed: int = 0) -> dict:
    key = jax.random.key(seed)
    ks = jax.random.split(key, 20)
    D = D_MODEL
    nrm = jax.random.normal
    f32 = jnp.float32
    x = nrm(ks[0], (BATCH, SEQ, D), f32)
    c = nrm(ks[1], (BATCH, D), f32)
    w_ada = nrm(ks[2], (DEPTH, D, 6 * D), f32) * (0.5 * D ** -0.5)
    b_ada = 0.01 * nrm(ks[3], (DEPTH, 6 * D), f32)
    norm1_w = 1.0 + 0.02 * nrm(ks[4], (DEPTH, D), f32)
    w_in = nrm(ks[5], (DEPTH, D, IN_COLS), f32) * D ** -0.5
    conv_w = nrm(ks[6], (DEPTH, CONV_WIDTH, 2 * MLSTM_QK_WIDTH), f32) * CONV_WIDTH ** -0.5
    conv_b = 0.01 * nrm(ks[7], (DEPTH, 2 * MLSTM_QK_WIDTH), f32)
    b_igate = 0.1 * nrm(ks[8], (DEPTH, MLSTM_HEADS), f32)
    b_fgate = jnp.linspace(3.0, 6.0, MLSTM_HEADS, dtype=f32)[None, :] + 0.1 * nrm(ks[9], (DEPTH, MLSTM_HEADS), f32)
    mlstm_norm_w = 1.0 + 0.02 * nrm(ks[10], (DEPTH, MLSTM_HEADS, MLSTM_DV), f32)
    w_out = nrm(ks[11], (DEPTH, MIX_WIDTH, D), f32) * MIX_WIDTH ** -0.5
    norm2_w = 1.0 + 0.02 * nrm(ks[12], (DEPTH, D), f32)
    peer_wq = nrm(ks[13], (DEPTH, D, PEER_HEADS * PEER_QDIM), f32) * D ** -0.5
    peer_subkeys = nrm(ks[14], (DEPTH, PEER_HEADS, 2, PEER_NKEYS, PEER_QDIM // 2), f32) * (PEER_QDIM // 2) ** -0.5
    peer_u = nrm(ks[15], (DEPTH, PEER_EXPERTS, D), f32) * D ** -0.5
    peer_v = nrm(ks[16], (DEPTH, PEER_EXPERTS, D), f32) * PEER_HEADS ** -0.5
    final_norm_w = 1.0 + 0.02 * nrm(ks[17], (D,), f32)
    return {"x": x, "c": c, "w_ada": w_ada, "b_ada": b_ada, "norm1_w": norm1_w, "w_in": w_in,
            "conv_w": conv_w, "conv_b": conv_b, "b_igate": b_igate, "b_fgate": b_fgate,
            "mlstm_norm_w": mlstm_norm_w, "w_out": w_out, "norm2_w": norm2_w, "peer_wq": peer_wq,
            "peer_subkeys": peer_subkeys, "peer_u": peer_u, "peer_v": peer_v, "final_norm_w": final_norm_w}


def reference(x, c, w_ada, b_ada, norm1_w, w_in, conv_w, conv_b, b_igate, b_fgate, mlstm_norm_w,
              w_out, norm2_w, peer_wq, peer_subkeys, peer_u, peer_v, final_norm_w):
    B, S, D = x.shape
    positions = jnp.arange(S)
    for l in range(DEPTH):
        mod = jax.nn.silu(c) @ w_ada[l] + b_ada[l]
        shift1, scale1, gate1, shift2, scale2, gate2 = (m[:, None, :] for m in jnp.split(mod, 6, axis=-1))

        xn = rmsnorm(x, norm1_w[l]) * (1 + scale1) + shift1
        p = xn @ w_in[l]

        qk = jax.nn.silu(causal_depthwise_conv(p[..., COL_MQ:COL_MV], conv_w[l], conv_b[l]))
        mq = qk[..., :MLSTM_QK_WIDTH].reshape(B, S, MLSTM_HEADS, MLSTM_DK).transpose(0, 2, 1, 3)
        mk = qk[..., MLSTM_QK_WIDTH:].reshape(B, S, MLSTM_HEADS, MLSTM_DK).transpose(0, 2, 1, 3)
        mv = p[..., COL_MV:COL_MO].reshape(B, S, MLSTM_HEADS, MLSTM_DV).transpose(0, 2, 1, 3)
        o_gate = jax.nn.sigmoid(p[..., COL_MO:COL_MI])
        i_pre = softcap(p[..., COL_MI:COL_MF] + b_igate[l]).transpose(0, 2, 1)
        f_pre = softcap(p[..., COL_MF:COL_AQ] + b_fgate[l]).transpose(0, 2, 1)
        h = mlstm_chunkwise(mq, mk, mv, i_pre, f_pre)
        h = h * lax.rsqrt(jnp.mean(h * h, axis=-1, keepdims=True) + EPS) * mlstm_norm_w[l][:, None, :].astype(jnp.float32)
        h_mlstm = h.astype(x.dtype).transpose(0, 2, 1, 3).reshape(B, S, MLSTM_WIDTH) * o_gate

        aq = p[..., COL_AQ:COL_AK].reshape(B, S, MOBA_HEADS, MOBA_HEAD_DIM).transpose(0, 2, 1, 3)
        ak = p[..., COL_AK:COL_AV].reshape(B, S, MOBA_HEADS, MOBA_HEAD_DIM).transpose(0, 2, 1, 3)
        av = p[..., COL_AV:IN_COLS].reshape(B, S, MOBA_HEADS, MOBA_HEAD_DIM).transpose(0, 2, 1, 3)
        h_moba = moba_attention(rotary(aq, positions), rotary(ak, positions), av)
        h_moba = h_moba.transpose(0, 2, 1, 3).reshape(B, S, MOBA_WIDTH)

        mixed = jnp.concatenate([h_mlstm, h_moba], axis=-1) @ w_out[l]
        x = x + gate1 * mixed

        xn2 = rmsnorm(x, norm2_w[l]) * (1 + scale2) + shift2
        x = x + gate2 * peer_ffn(xn2, peer_wq[l], peer_subkeys[l], peer_u[l], peer_v[l])
    return rmsnorm(x, final_norm_w)
```

```python
import numpy as np
from contextlib import ExitStack
import ml_dtypes
import concourse.bass as bass
import concourse.mybir as mybir
from concourse.bass_utils import run_bass_kernel_spmd

F32 = mybir.dt.float32
BF16 = mybir.dt.bfloat16
AF = mybir.ActivationFunctionType
ALU = mybir.AluOpType
AX = mybir.AxisListType

D = 4096
KC = 32
NDS = 24
EPS = 1e-6
NEG = -1.0e30


class Tk:
    __slots__ = ("w", "rs")

    def __init__(self):
        self.w = None
        self.rs = {}


class Tile:
    def __init__(self, t, n=1):
        self.t = t
        self.k = Tk()
        self.ks = [Tk() for _ in range(n)]

    def __getitem__(self, idx):
        return self.t[idx]


class KB:
    EPOCH = 20000

    def __init__(self, nc, st):
        self.nc = nc
        self.st = st
        self.eng = {"pe": nc.tensor, "act": nc.scalar, "dve": nc.vector, "pool": nc.gpsimd, "sp": nc.sync}
        self.cnt = {e: 0 for e in self.eng}
        self.sem = {e: self.newsem() for e in self.eng}
        self.last = {e: None for e in self.eng}
        self.seen = {}
        self.dsem = [self.newsem() for _ in range(NDS)]
        self.dval = [0] * NDS
        self.dnext = 0
        self.psum = []
        self.pnext = 0
        for i in range(8):
            self.psum.append(Tile(st.enter_context(nc.psum_tensor(f"ps{i}", [128, 512], F32))))
        self.pst = None

    def newsem(self):
        self._ns = getattr(self, "_ns", 0) + 1
        return self.st.enter_context(self.nc.semaphore(f"sem{self._ns}"))

    psum_rot = 8

    def ps(self):
        p = self.psum[self.pnext]
        self.pnext = (self.pnext + 1) % self.psum_rot
        return p

    def sb(self, shape, dtype, n=1, name=None):
        self._nm = getattr(self, "_nm", 0) + 1
        t = self.pst.enter_context(self.nc.sbuf_tensor(name or f"sb{self._nm}", list(shape), dtype))
        return Tile(t, n)

    def _wait(self, e, tk):
        sem, val = tk[0], tk[1]
        key = (e, id(sem))
        if self.seen.get(key, 0) >= val:
            return
        self.eng[e].wait_ge(sem, val)
        self.seen[key] = val

    def _deps(self, e, reads, writes):
        ticks = []
        for t in reads:
            if t.w is not None:
                ticks.append(t.w)
        for t in writes:
            if t.w is not None:
                ticks.append(t.w)
            ticks.extend(t.rs.values())
        for tk in ticks:
            if tk[2] == e and e == "pe":
                continue
            self._wait(e, tk)

    def _mark(self, tk, reads, writes):
        for t in reads:
            t.rs[id(tk[0])] = tk
        for t in writes:
            t.w = tk
            t.rs = {}

    @staticmethod
    def _tk(objs):
        out = []
        for o in objs:
            out.append(o.k if isinstance(o, Tile) else o)
        return out

    def op(self, e, fn, reads=(), writes=()):
        reads, writes = self._tk(reads), self._tk(writes)
        self._deps(e, reads, writes)
        if self.cnt[e] >= self.EPOCH:
            self.sem[e] = self.newsem()
            self.cnt[e] = 0
        self.cnt[e] += 1
        ins = fn(self.eng[e])
        ins.then_inc(self.sem[e], 1)
        tk = (self.sem[e], self.cnt[e], e)
        self.last[e] = tk
        self._mark(tk, reads, writes)

    def dma(self, q, out, in_, reads=(), writes=(), **kw):
        reads, writes = self._tk(reads), self._tk(writes)
        self._deps(q, reads, writes)
        i = self.dnext
        self.dnext = (self.dnext + 1) % NDS
        if self.dval[i] > 0:
            self._wait(q, (self.dsem[i], self.dval[i]))
        self.dval[i] += 16
        self.eng[q].dma_start(out=out, in_=in_, **kw).then_inc(self.dsem[i], 16)
        tk = (self.dsem[i], self.dval[i], "dma")
        self._mark(tk, reads, writes)

    def barrier(self):
        for e in self.eng:
            for e2 in self.eng:
                if e2 != e and self.last[e2] is not None:
                    self._wait(e, self.last[e2])
            for i in range(NDS):
                if self.dval[i] > 0:
                    self._wait(e, (self.dsem[i], self.dval[i]))

    def phase(self):
        kb = self

        class _P:
            def __enter__(s):
                kb.pst = ExitStack()
                kb.pst.__enter__()
                return kb

            def __exit__(s, *a):
                kb.barrier()
                kb.pst.__exit__(*a)
                kb.pst = None
                return False

        return _P()


class Cfg:
    def __init__(self, tok=2048, nk=128):
        self.TOK = tok
        self.T2 = 2 * tok
        self.NK = nk
        self.NE = nk * nk
        self.NB = self.T2 // 256
        self.NCH = self.T2 // 128


def build(cfg, upto=99):
    TOK, T2 = cfg.TOK, cfg.T2
    nc = bass.Bass("TRN2", target_bir_lowering=False)

    def din(name, shape, dt=F32):
        return nc.dram_tensor(name, list(shape), dt, kind="ExternalInput").ap()

    def dscr(name, shape, dt=F32):
        return nc.dram_tensor(name, list(shape), dt).ap()

    x_own = din("x_own", [TOK, D])
    x_past = din("x_past", [TOK, D])
    cT = din("cT", [128, KC])
    w_ada = din("w_ada", [D, 6 * D])
    b_adaT = din("b_adaT", [128, 192])
    n1wT = din("n1wT", [128, KC])
    n2wT = din("n2wT", [128, KC])
    fnw = din("fnw", [D])
    w_in = din("w_in", [D, 12296])
    flag = din("flag", [128, 1])
    ident_f = din("ident_f", [128, 128])
    y = nc.dram_tensor("y", [TOK, D], F32, kind="ExternalOutput").ap()
    NK, NE, NB, NCH = cfg.NK, cfg.NE, cfg.NB, cfg.NCH
    NBP = max(NB, 8)
    consts = din("consts", [128, 5, 128])
    conv_wT = din("conv_wT", [128, 16, 4])
    conv_bT = din("conv_bT", [128, 16])
    gbias = din("gbias", [128, NCH, 8])
    mnw = din("mnw", [2048])
    cs_tab = din("cs_tab", [T2, 128])
    pb_tab = din("pb_tab", [TOK, NBP])
    oneh = din("oneh", [NBP, NBP * 128])
    w_out = din("w_out", [D, D])
    wq = din("wq", [D, 2048])
    skT = din("skT", [128, 16, NK])
    uT = din("uT", [D, NE])
    vv = din("vv", [NE, D])
    qkc_d = dscr("qkc_d", [2048, T2], BF16)
    hcat_d = dscr("hcat_d", [TOK, D], BF16)
    x2_d = dscr("x2_d", [TOK, D])
    xn2T_d = dscr("xn2T_d", [128, KC, TOK], BF16)
    s_d = dscr("s_d", [TOK, 16, NK])
    peer_d = dscr("peer_d", [TOK, D])
    ans_d = dscr("ans_d", [TOK // 512, 3, 128, 4 * 8 * NK])

    mv_d = dscr("mv_d", [T2, 2048], BF16)
    o_d = dscr("o_d", [TOK, 2048])
    if_d = dscr("if_d", [T2, 8])
    aq_d = dscr("aq_d", [TOK, 2048])
    ak_d = dscr("ak_d", [T2, 2048])
    av_d = dscr("av_d", [T2, 2048], BF16)
    qkT_d = dscr("qkT_d", [2048, 4 + T2])
    mod_d = dscr("mod_d", [192, 128])
    dbg = {}

    with ExitStack() as st:
        kb = KB(nc, st)
        kb.pst = st
        modT = kb.sb([128, 192], F32, name="modT")
        A1 = kb.sb([128, KC], F32, name="A1")
        A1p = kb.sb([128, KC], F32, name="A1p")
        S1p = kb.sb([128, KC], F32, name="S1p")
        A2 = kb.sb([128, KC], F32, name="A2")
        identb = kb.sb([128, 128], BF16, name="identb")
        identf = kb.sb([128, 128], F32, name="identf")
        flg = kb.sb([128, 1], F32, name="flg")
        cst = kb.sb([128, 5, 128], F32, name="cst")
        onesb = kb.sb([128, 128], BF16, name="onesb")
        kb.pst = None
        kb.dma("sp", cst[:, :, :], consts[:, :, :], writes=[cst])
        kb.dma("pool", onesb[:, :], consts[:, 4, :], writes=[onesb])
        kb.dma("sp", identf[:, :], ident_f[:, :], writes=[identf])
        kb.dma("pool", identb[:, :], ident_f[:, :], writes=[identb])
        kb.dma("sp", flg[:, :], flag[:, :], writes=[flg])

        with kb.phase():
            c_sb = kb.sb([128, KC], F32)
            cs = kb.sb([128, KC], BF16)
            bad = kb.sb([128, 192], F32)
            n1 = kb.sb([128, KC], F32)
            n2 = kb.sb([128, KC], F32)
            wb = [kb.sb([128, KC, 512], BF16) for _ in range(2)]
            kb.dma("sp", c_sb[:, :], cT[:, :], writes=[c_sb])
            kb.dma("sp", bad[:, :], b_adaT[:, :], writes=[bad])
            kb.dma("sp", n1[:, :], n1wT[:, :], writes=[n1])
            kb.dma("sp", n2[:, :], n2wT[:, :], writes=[n2])
            kb.op("act", lambda e: e.activation(out=cs[:, :], in_=c_sb[:, :], func=AF.Silu), reads=[c_sb], writes=[cs])
            wv = w_ada.rearrange("(kc p) n -> p kc n", p=128)
            pacc = kb.ps()
            for cc in range(48):
                w = wb[cc % 2]
                kb.dma("pool", w[:, :, :], wv[:, :, cc * 512:(cc + 1) * 512], writes=[w])
                for sub in range(4):
                    j = cc * 4 + sub
                    for kc in range(KC):
                        kb.op("pe", lambda e, w=w, kc=kc, sub=sub, j=j: e.matmul(
                            pacc[:, j:j + 1], lhsT=w[:, kc, sub * 128:(sub + 1) * 128], rhs=cs[:, kc:kc + 1],
                            start=(kc == 0), stop=(kc == KC - 1)), reads=[w, cs], writes=[pacc])
            kb.op("dve", lambda e: e.tensor_tensor(out=modT[:, :], in0=pacc[:, 0:192], in1=bad[:, :], op=ALU.add),
                  reads=[pacc, bad], writes=[modT])
            kb.op("dve", lambda e: e.scalar_tensor_tensor(out=A1[:, :], in0=modT[:, 32:64], scalar=1.0, in1=n1[:, :],
                                                          op0=ALU.add, op1=ALU.mult), reads=[modT, n1], writes=[A1])
            kb.op("dve", lambda e: e.scalar_tensor_tensor(out=A2[:, :], in0=modT[:, 128:160], scalar=1.0, in1=n2[:, :],
                                                          op0=ALU.add, op1=ALU.mult), reads=[modT, n2], writes=[A2])
            kb.op("dve", lambda e: e.tensor_scalar(out=A1p[:, :], in0=A1[:, :], scalar1=flg[:, 0:1], scalar2=None,
                                                   op0=ALU.mult), reads=[A1, flg], writes=[A1p])
            kb.op("dve", lambda e: e.tensor_scalar(out=S1p[:, :], in0=modT[:, 0:32], scalar1=flg[:, 0:1], scalar2=None,
                                                   op0=ALU.mult), reads=[modT, flg], writes=[S1p])
            mrow = kb.sb([128, 2, 128], F32)
            for hh in range(2):
                pt = kb.ps()
                n = 128 if hh == 0 else 64
                kb.op("pe", lambda e, pt=pt, hh=hh, n=n: e.matmul(pt[0:n, 0:128], lhsT=modT[:, hh * 128:hh * 128 + n],
                                                                  rhs=identf[:, :], start=True, stop=True),
                      reads=[modT, identf], writes=[pt])
                kb.op("act", lambda e, pt=pt, hh=hh, n=n: e.activation(out=mrow[0:n, hh, :], in_=pt[0:n, 0:128], func=AF.Copy),
                      reads=[pt], writes=[mrow])
                kb.dma("sp", mod_d[hh * 128:hh * 128 + n, :], mrow[0:n, hh, :], reads=[mrow])
        if upto <= 1:
            dbg["modT"] = None

        TG = 512
        TGB = 1024 if TOK % 1024 == 0 else 512
        NG = T2 // TGB
        NTB = TGB // 128
        wiv = w_in.rearrange("(kc p) n -> p kc n", p=128)
        chunks = []
        for i in range(4):
            chunks.append((i * 512, 512, "qk"))
        for i in range(4):
            chunks.append((2048 + i * 512, 512, "mv"))
        for i in range(4):
            chunks.append((4096 + i * 512, 512, "o"))
        chunks.append((6144, 8, "if"))
        for i in range(4):
            chunks.append((6152 + i * 512, 512, "aq"))
        for i in range(4):
            chunks.append((8200 + i * 512, 512, "ak"))
        for i in range(4):
            chunks.append((10248 + i * 512, 512, "av"))
        with kb.phase():
            xt = [kb.sb([128, D], F32) for _ in range(2)]
            junk = kb.sb([128, D], BF16)
            xs = [kb.sb([128, D], BF16) for _ in range(2)]
            ss = [kb.sb([128, 1], F32) for _ in range(2)]
            xnT = kb.sb([128, KC, TGB], BF16, n=NTB)
            wb = [kb.sb([128, KC, 512], BF16) for _ in range(2)]
            stf = [kb.sb([128, 512], F32) for _ in range(3)]
            stb = [kb.sb([128, 512], BF16) for _ in range(3)]
            zpad = kb.sb([128, 4], F32)
            kb.op("dve", lambda e: e.memset(zpad[:, :], 0.0), writes=[zpad])
            for cc in range(16):
                kb.dma("sp", qkT_d[cc * 128:(cc + 1) * 128, 0:4], zpad[:, :], reads=[zpad])
            nst = 0
            nw = 0
            for g in range(NG):
                past = (g * TGB) < TOK
                for ti in range(NTB):
                    tok0 = g * TGB + ti * 128
                    src = x_past[tok0:tok0 + 128, :] if past else x_own[tok0 - TOK:tok0 - TOK + 128, :]
                    it = g * NTB + ti
                    x_t, xs_t, ss_t = xt[it % 2], xs[it % 2], ss[it % 2]
                    kb.dma("sp", x_t[:, :], src, writes=[x_t])
                    kb.op("act", lambda e, x_t=x_t, ss_t=ss_t: e.activation(out=junk[:, :], in_=x_t[:, :], func=AF.Square,
                                                                           accum_out=ss_t[:, 0:1]),
                          reads=[x_t], writes=[junk, ss_t])
                    kb.op("dve", lambda e, ss_t=ss_t: e.tensor_scalar(out=ss_t[:, :], in0=ss_t[:, :], scalar1=1.0 / D,
                                                                      scalar2=EPS, op0=ALU.mult, op1=ALU.add),
                          reads=[ss_t], writes=[ss_t])
                    kb.op("act", lambda e, ss_t=ss_t: e.activation(out=ss_t[:, :], in_=ss_t[:, :], func=AF.Sqrt),
                          reads=[ss_t], writes=[ss_t])
                    kb.op("dve", lambda e, ss_t=ss_t: e.reciprocal(out=ss_t[:, :], in_=ss_t[:, :]), reads=[ss_t], writes=[ss_t])
                    kb.op("dve", lambda e, x_t=x_t, xs_t=xs_t, ss_t=ss_t: e.tensor_scalar(
                        out=xs_t[:, :], in0=x_t[:, :], scalar1=ss_t[:, 0:1], scalar2=None, op0=ALU.mult),
                        reads=[x_t, ss_t], writes=[xs_t])
                    Asel, Ssel = (A1p, S1p) if past else (A1, None)
                    for k4 in range(KC // 4):
                        pt = kb.ps()
                        for q in range(4):
                            kc = k4 * 4 + q
                            kb.op("pe", lambda e, pt=pt, q=q, kc=kc, xs_t=xs_t: e.matmul(
                                pt[:, q * 128:(q + 1) * 128], lhsT=xs_t[:, kc * 128:(kc + 1) * 128], rhs=identb[:, :],
                                start=True, stop=True), reads=[xs_t, identb], writes=[pt])
                        for q in range(4):
                            kc = k4 * 4 + q
                            bias_ap = (S1p[:, kc:kc + 1] if past else modT[:, kc:kc + 1])
                            kb.op("act", lambda e, pt=pt, q=q, kc=kc, ti=ti, Asel=Asel, bias_ap=bias_ap: e.activation(
                                out=xnT[:, kc, ti * 128:(ti + 1) * 128], in_=pt[:, q * 128:(q + 1) * 128],
                                func=AF.Identity, scale=Asel[:, kc:kc + 1], bias=bias_ap),
                                reads=[pt, Asel, S1p if past else modT], writes=[xnT.ks[ti]])
                for (c0, ncol, kind) in chunks:
                    if past and kind in ("o", "aq"):
                        continue
                    w = wb[nw % 2]
                    nw += 1
                    kb.dma("pool", w[:, :, 0:ncol], wiv[:, :, c0:c0 + ncol], writes=[w])
                    if kind == "qk":
                        for sub, tb in [(a_, b_) for a_ in range(4) for b_ in range(TGB // 512)]:
                            pt = kb.ps()
                            for kc in range(KC):
                                kb.op("pe", lambda e, pt=pt, kc=kc, sub=sub, w=w, tb=tb: e.matmul(
                                    pt[:, :], lhsT=w[:, kc, sub * 128:(sub + 1) * 128], rhs=xnT[:, kc, tb * 512:(tb + 1) * 512],
                                    start=(kc == 0), stop=(kc == KC - 1)), reads=[w] + xnT.ks, writes=[pt])
                            s_t = stf[nst % 3]
                            ev = "act" if nst % 2 == 0 else "dve"
                            nst += 1
                            if ev == "act":
                                kb.op("act", lambda e, pt=pt, s_t=s_t: e.activation(out=s_t[:, :], in_=pt[:, :], func=AF.Copy),
                                      reads=[pt], writes=[s_t])
                            else:
                                kb.op("dve", lambda e, pt=pt, s_t=s_t: e.tensor_copy(out=s_t[:, :], in_=pt[:, :]),
                                      reads=[pt], writes=[s_t])
                            ch0 = c0 + sub * 128
                            kb.dma("sp", qkT_d[ch0:ch0 + 128, 4 + g * TGB + tb * 512:4 + g * TGB + (tb + 1) * 512], s_t[:, :], reads=[s_t])
                    else:
                        for ti in range(NTB):
                            tok0 = g * TGB + ti * 128
                            pt = kb.ps()
                            for kc in range(KC):
                                kb.op("pe", lambda e, pt=pt, kc=kc, ti=ti, w=w, ncol=ncol: e.matmul(
                                    pt[:, 0:ncol], lhsT=xnT[:, kc, ti * 128:(ti + 1) * 128], rhs=w[:, kc, 0:ncol],
                                    start=(kc == 0), stop=(kc == KC - 1)), reads=[w, xnT.ks[ti]], writes=[pt])
                            isb = kind in ("mv", "av")
                            s_t = (stb if isb else stf)[nst % 3]
                            ev = "act" if nst % 2 == 0 else "dve"
                            nst += 1
                            if ev == "act":
                                kb.op("act", lambda e, pt=pt, s_t=s_t, ncol=ncol: e.activation(out=s_t[:, 0:ncol], in_=pt[:, 0:ncol],
                                                                                              func=AF.Copy), reads=[pt], writes=[s_t])
                            else:
                                kb.op("dve", lambda e, pt=pt, s_t=s_t, ncol=ncol: e.tensor_copy(out=s_t[:, 0:ncol], in_=pt[:, 0:ncol]),
                                      reads=[pt], writes=[s_t])
                            if kind == "mv":
                                dst = mv_d[tok0:tok0 + 128, c0 - 2048:c0 - 2048 + 512]
                            elif kind == "o":
                                dst = o_d[tok0 - TOK:tok0 - TOK + 128, c0 - 4096:c0 - 4096 + 512]
                            elif kind == "if":
                                dst = if_d[tok0:tok0 + 128, 0:8]
                            elif kind == "aq":
                                dst = aq_d[tok0 - TOK:tok0 - TOK + 128, c0 - 6152:c0 - 6152 + 512]
                            elif kind == "ak":
                                dst = ak_d[tok0:tok0 + 128, c0 - 8200:c0 - 8200 + 512]
                            else:
                                dst = av_d[tok0:tok0 + 128, c0 - 10248:c0 - 10248 + 512]
                            kb.dma("sp", dst, s_t[:, 0:ncol], reads=[s_t])

        if upto <= 2:
            for nm, ap_, shp, dt in (("d_mod", mod_d, [192, 128], F32), ("d_qkT", qkT_d, [2048, 4 + T2], F32),
                                     ("d_if", if_d, [T2, 8], F32), ("d_ak", ak_d, [T2, 2048], F32),
                                     ("d_mv", mv_d, [T2, 2048], BF16), ("d_o", o_d, [TOK, 2048], F32)):
                o = nc.dram_tensor(nm, shp, dt, kind="ExternalOutput").ap()
                kb.dma("sp", o, ap_, )
            kb.barrier()
            return nc


        TRIu = cst[:, 0, :]
        CTS = cst[:, 1, :]
        CST = cst[:, 2, :]
        SEL127 = cst[:, 3, :]
        ONESF = cst[:, 4, :]

        def ev_copy(i, out, in_, rd, wr):
            if i % 2 == 0:
                kb.op("act", lambda e: e.activation(out=out, in_=in_, func=AF.Copy), reads=rd, writes=wr)
            else:
                kb.op("dve", lambda e: e.tensor_copy(out=out, in_=in_), reads=rd, writes=wr)

        with kb.phase():
            cw = kb.sb([128, 16, 4], F32)
            cb = kb.sb([128, 16], F32)
            kb.dma("sp", cw[:, :, :], conv_wT[:, :, :], writes=[cw])
            kb.dma("sp", cb[:, :], conv_bT[:, :], writes=[cb])
            ub = [kb.sb([128, 4 + T2], F32) for _ in range(2)]
            acc = kb.sb([128, T2], F32)
            ob = [kb.sb([128, T2], BF16) for _ in range(2)]
            for cc in range(16):
                u = ub[cc % 2]
                o_ = ob[cc % 2]
                kb.dma("sp", u[:, :], qkT_d[cc * 128:(cc + 1) * 128, :], writes=[u])
                kb.op("dve", lambda e, u=u, cc=cc: e.tensor_scalar(out=acc[:, :], in0=u[:, 1:1 + T2], scalar1=cw[:, cc, 0:1],
                                                                   scalar2=None, op0=ALU.mult), reads=[u, cw], writes=[acc])
                for j in range(1, 4):
                    kb.op("dve", lambda e, u=u, cc=cc, j=j: e.scalar_tensor_tensor(
                        out=acc[:, :], in0=u[:, 1 + j:1 + j + T2], scalar=cw[:, cc, j:j + 1], in1=acc[:, :],
                        op0=ALU.mult, op1=ALU.add), reads=[u, cw, acc], writes=[acc])
                kb.op("act", lambda e, o_=o_, cc=cc: e.activation(out=o_[:, :], in_=acc[:, :], func=AF.Silu, bias=cb[:, cc:cc + 1]),
                      reads=[acc, cb], writes=[o_])
                if cc < 8:
                    kb.op("dve", lambda e, o_=o_: e.tensor_scalar(out=o_[:, :], in0=o_[:, :], scalar1=0.0625, scalar2=None,
                                                                  op0=ALU.mult), reads=[o_], writes=[o_])
                kb.dma("sp", qkc_d[cc * 128:(cc + 1) * 128, :], o_[:, :], reads=[o_])

        with kb.phase():
            G = kb.sb([128, NCH, 8], F32)
            gb = kb.sb([128, NCH, 8], F32)
            LI = kb.sb([128, NCH, 4], F32)
            LF = kb.sb([128, NCH, 4], F32)
            Bc = kb.sb([128, NCH, 4], F32)
            U = kb.sb([128, NCH, 4], F32)
            kb.dma("sp", G[:, :, :], if_d.rearrange("(c p) g -> p c g", p=128), writes=[G])
            kb.dma("sp", gb[:, :, :], gbias[:, :, :], writes=[gb])
            kb.op("dve", lambda e: e.tensor_tensor(out=G[:, :, :], in0=G[:, :, :], in1=gb[:, :, :], op=ALU.add), reads=[G, gb], writes=[G])
            kb.op("act", lambda e: e.activation(out=G[:, :, :], in_=G[:, :, :], func=AF.Tanh, scale=1.0 / 15.0), reads=[G], writes=[G])
            kb.op("dve", lambda e: e.tensor_scalar(out=LI[:, :, :], in0=G[:, :, 0:4], scalar1=15.0, scalar2=None, op0=ALU.mult),
                  reads=[G], writes=[LI])
            kb.op("act", lambda e: e.activation(out=LF[:, :, :], in_=G[:, :, 4:8], func=AF.Exp, scale=-15.0), reads=[G], writes=[LF])
            kb.op("act", lambda e: e.activation(out=LF[:, :, :], in_=LF[:, :, :], func=AF.Ln, bias=1.0), reads=[LF], writes=[LF])
            kb.op("dve", lambda e: e.tensor_scalar(out=LF[:, :, :], in0=LF[:, :, :], scalar1=-1.0, scalar2=None, op0=ALU.mult),
                  reads=[LF], writes=[LF])
            pb_ = kb.ps()
            kb.op("pe", lambda e: e.matmul(pb_[:, 0:NCH * 4], lhsT=TRIu, rhs=LF[:, :, :].rearrange("p c h -> p (c h)"),
                                           start=True, stop=True), reads=[cst, LF], writes=[pb_])
            kb.op("dve", lambda e: e.tensor_copy(out=Bc[:, :, :].rearrange("p c h -> p (c h)"), in_=pb_[:, 0:NCH * 4]),
                  reads=[pb_], writes=[Bc])
            kb.op("dve", lambda e: e.tensor_tensor(out=U[:, :, :], in0=LI[:, :, :], in1=Bc[:, :, :], op=ALU.subtract),
                  reads=[LI, Bc], writes=[U])

            mnwb = kb.sb([128, 2048], F32)
            kb.dma("sp", mnwb[:, :], mnw.partition_broadcast(128), writes=[mnwb])
            qTh = kb.sb([128, 2, T2], BF16)
            kTh = kb.sb([128, 2, T2], BF16)
            vh = kb.sb([128, NCH, 512], BF16)
            Cst = kb.sb([128, 2, 512], F32)
            Cb = kb.sb([128, 2, 512], BF16)
            nst_ = kb.sb([128, 2], F32)
            nb_ = kb.sb([128, 2], BF16)
            mcol = kb.sb([128, 1], F32)
            dg = [kb.sb([128, 128], F32) for _ in range(2)]
            Um = kb.sb([128, 128], F32)
            Rm = kb.sb([128, 128], F32)
            ET = kb.sb([128, 128], F32)
            PT = kb.sb([128, 128], BF16)
            sm = [kb.sb([128, 16], F32) for _ in range(2)]
            mr = kb.sb([128, 2], F32)
            kw = kb.sb([128, 256], BF16)
            tmpn = kb.sb([128, 512], F32)
            num = kb.sb([128, 512], F32)
            hh_ = kb.sb([128, 512], F32)
            junk5 = kb.sb([128, 512], BF16)
            ot = kb.sb([128, 512], F32)
            hout = [kb.sb([128, 512], BF16) for _ in range(2)]
            nit = 0
            for h in range(4):
                kb.dma("sp", qTh[:, :, :], qkc_d[h * 256:(h + 1) * 256, :].rearrange("(j p) t -> p j t", p=128), writes=[qTh])
                kb.dma("sp", kTh[:, :, :], qkc_d[1024 + h * 256:1024 + (h + 1) * 256, :].rearrange("(j p) t -> p j t", p=128), writes=[kTh])
                kb.dma("sp", vh[:, :, :], mv_d[:, h * 512:(h + 1) * 512].rearrange("(c p) v -> p c v", p=128), writes=[vh])
                kb.op("dve", lambda e: e.memset(Cst[:, :, :], 0.0), writes=[Cst])
                kb.op("dve", lambda e: e.memset(Cb[:, :, :], 0.0), writes=[Cb])
                kb.op("dve", lambda e: e.memset(nst_[:, :], 0.0), writes=[nst_])
                kb.op("dve", lambda e: e.memset(nb_[:, :], 0.0), writes=[nb_])
                kb.op("dve", lambda e: e.memset(mcol[:, :], 0.0), writes=[mcol])
                for c in range(NCH):
                    own = c >= NCH // 2
                    if c == NCH // 2:
                        kb.op("dve", lambda e: e.tensor_scalar(out=Cst[:, :, :], in0=Cst[:, :, :], scalar1=flg[:, 0:1], scalar2=None, op0=ALU.mult),
                              reads=[Cst, flg], writes=[Cst])
                        kb.op("dve", lambda e: e.tensor_copy(out=Cb[:, :, :], in_=Cst[:, :, :]), reads=[Cst], writes=[Cb])
                        kb.op("dve", lambda e: e.tensor_scalar(out=nst_[:, :], in0=nst_[:, :], scalar1=flg[:, 0:1], scalar2=None, op0=ALU.mult),
                              reads=[nst_, flg], writes=[nst_])
                        kb.op("dve", lambda e: e.tensor_copy(out=nb_[:, :], in_=nst_[:, :]), reads=[nst_], writes=[nb_])
                        kb.op("dve", lambda e: e.tensor_scalar(out=mcol[:, :], in0=mcol[:, :], scalar1=flg[:, 0:1], scalar2=None, op0=ALU.mult),
                              reads=[mcol, flg], writes=[mcol])
                    cs_ = slice(c * 128, (c + 1) * 128)
                    ucol = U[:, c, h:h + 1]
                    bcol = Bc[:, c, h:h + 1]
                    s0 = sm[nit % 2]
                    nit += 1
                    d0 = dg[0]
                    kb.op("dve", lambda e, d0=d0, ucol=ucol: e.tensor_scalar(out=d0[:, :], in0=identf[:, :], scalar1=ucol, scalar2=None, op0=ALU.mult),
                          reads=[identf, U], writes=[d0])
                    pu = kb.ps()
                    kb.op("pe", lambda e, pu=pu, d0=d0: e.matmul(pu[:, 0:128], lhsT=ONESF, rhs=d0[:, :], start=True, stop=True),
                          reads=[cst, d0], writes=[pu])
                    kb.op("dve", lambda e, pu=pu: e.tensor_tensor(out=Um[:, :], in0=pu[:, 0:128], in1=CTS, op=ALU.add), reads=[pu, cst], writes=[Um])
                    kb.op("dve", lambda e, s0=s0: e.tensor_reduce(out=s0[:, 0:1], in_=Um[:, :], axis=AX.X, op=ALU.max), reads=[Um], writes=[s0])
                    kb.op("dve", lambda e, s0=s0: e.tensor_tensor(out=mr[:, 1:2], in0=s0[:, 0:1], in1=mcol[:, 0:1], op=ALU.max), reads=[s0, mcol], writes=[mr])
                    kb.op("dve", lambda e, bcol=bcol: e.tensor_tensor(out=mr[:, 0:1], in0=mr[:, 1:2], in1=bcol, op=ALU.add), reads=[mr, Bc], writes=[mr])
                    pm = kb.ps()
                    kb.op("pe", lambda e, pm=pm: e.matmul(pm[:, 0:2], lhsT=SEL127, rhs=mr[:, 0:2], start=True, stop=True), reads=[cst, mr], writes=[pm])
                    kb.op("dve", lambda e, pm=pm, s0=s0: e.tensor_copy(out=s0[:, 1:3], in_=pm[:, 0:2]), reads=[pm], writes=[s0])
                    if own:
                        d1 = dg[1]
                        kb.op("dve", lambda e, d1=d1: e.tensor_scalar(out=d1[:, :], in0=identf[:, :], scalar1=mr[:, 1:2], scalar2=None, op0=ALU.mult),
                              reads=[identf, mr], writes=[d1])
                        pr = kb.ps()
                        kb.op("pe", lambda e, pr=pr, d1=d1: e.matmul(pr[:, 0:128], lhsT=ONESF, rhs=d1[:, :], start=True, stop=True),
                              reads=[cst, d1], writes=[pr])
                        kb.op("dve", lambda e, pr=pr: e.tensor_tensor(out=Rm[:, :], in0=pr[:, 0:128], in1=CST, op=ALU.add), reads=[pr, cst], writes=[Rm])
                        kb.op("act", lambda e, ucol=ucol: e.activation(out=ET[:, :], in_=Rm[:, :], func=AF.Exp, scale=-1.0, bias=ucol),
                              reads=[Rm, U], writes=[ET])
                        pS = kb.ps()
                        for j in range(2):
                            kb.op("pe", lambda e, pS=pS, j=j, cs_=cs_: e.matmul(pS[:, 0:128], lhsT=kTh[:, j, cs_], rhs=qTh[:, j, cs_],
                                                                              start=(j == 0), stop=(j == 1)), reads=[kTh, qTh], writes=[pS])
                        kb.op("dve", lambda e, pS=pS: e.tensor_tensor(out=PT[:, :], in0=pS[:, 0:128], in1=ET[:, :], op=ALU.mult),
                              reads=[pS, ET], writes=[PT])
                        pnum = kb.ps()
                        kb.op("pe", lambda e, pnum=pnum, c=c: e.matmul(pnum[:, :], lhsT=PT[:, :], rhs=vh[:, c, :], start=True, stop=True),
                              reads=[PT, vh], writes=[pnum])
                        pden = kb.ps()
                        kb.op("pe", lambda e, pden=pden: e.matmul(pden[:, 0:1], lhsT=PT[:, :], rhs=onesb[:, 0:1], start=True, stop=True),
                              reads=[PT, onesb], writes=[pden])
                        pqc = kb.ps()
                        for j in range(2):
                            kb.op("pe", lambda e, pqc=pqc, j=j, cs_=cs_: e.matmul(pqc[:, :], lhsT=qTh[:, j, cs_], rhs=Cb[:, j, :],
                                                                                start=(j == 0), stop=(j == 1)), reads=[qTh, Cb], writes=[pqc])
                        for j in range(2):
                            kb.op("pe", lambda e, pden=pden, j=j, cs_=cs_: e.matmul(pden[:, 1:2], lhsT=qTh[:, j, cs_], rhs=nb_[:, j:j + 1],
                                                                                  start=(j == 0), stop=(j == 1)), reads=[qTh, nb_], writes=[pden])
                        kb.op("dve", lambda e, s0=s0: e.tensor_tensor(out=s0[:, 3:4], in0=mcol[:, 0:1], in1=mr[:, 1:2], op=ALU.subtract),
                              reads=[mcol, mr], writes=[s0])
                        kb.op("act", lambda e, s0=s0: e.activation(out=s0[:, 3:4], in_=s0[:, 3:4], func=AF.Exp), reads=[s0], writes=[s0])
                        kb.op("act", lambda e, pqc=pqc, s0=s0: e.activation(out=tmpn[:, :], in_=pqc[:, :], func=AF.Copy, scale=s0[:, 3:4]),
                              reads=[pqc, s0], writes=[tmpn])
                        kb.op("dve", lambda e, pnum=pnum: e.tensor_tensor(out=num[:, :], in0=pnum[:, :], in1=tmpn[:, :], op=ALU.add),
                              reads=[pnum, tmpn], writes=[num])
                        kb.op("dve", lambda e, pden=pden, s0=s0: e.tensor_copy(out=s0[:, 9:11], in_=pden[:, 0:2]), reads=[pden], writes=[s0])
                        kb.op("dve", lambda e, s0=s0: e.scalar_tensor_tensor(out=s0[:, 4:5], in0=s0[:, 10:11], scalar=s0[:, 3:4], in1=s0[:, 9:10],
                                                                             op0=ALU.mult, op1=ALU.add), reads=[s0], writes=[s0])
                        kb.op("dve", lambda e, s0=s0: e.tensor_scalar(out=s0[:, 5:6], in0=s0[:, 4:5], scalar1=-1.0, scalar2=None, op0=ALU.mult),
                              reads=[s0], writes=[s0])
                        kb.op("dve", lambda e, s0=s0: e.tensor_tensor(out=s0[:, 5:6], in0=s0[:, 5:6], in1=s0[:, 4:5], op=ALU.max),
                              reads=[s0], writes=[s0])
                        kb.op("act", lambda e, s0=s0: e.activation(out=s0[:, 6:7], in_=mr[:, 0:1], func=AF.Exp, scale=-1.0), reads=[mr], writes=[s0])
                        kb.op("dve", lambda e, s0=s0: e.tensor_tensor(out=s0[:, 7:8], in0=s0[:, 5:6], in1=s0[:, 6:7], op=ALU.max), reads=[s0], writes=[s0])
                        kb.op("dve", lambda e, s0=s0: e.reciprocal(out=s0[:, 8:9], in_=s0[:, 7:8]), reads=[s0], writes=[s0])
                        kb.op("dve", lambda e, s0=s0: e.tensor_scalar(out=hh_[:, :], in0=num[:, :], scalar1=s0[:, 8:9], scalar2=None, op0=ALU.mult),
                              reads=[num, s0], writes=[hh_])
                        kb.op("act", lambda e, s0=s0: e.activation(out=junk5[:, :], in_=hh_[:, :], func=AF.Square, accum_out=s0[:, 11:12]),
                              reads=[hh_], writes=[junk5, s0])
                        kb.op("dve", lambda e, s0=s0: e.tensor_scalar(out=s0[:, 11:12], in0=s0[:, 11:12], scalar1=1.0 / 512, scalar2=EPS,
                                                                      op0=ALU.mult, op1=ALU.add), reads=[s0], writes=[s0])
                        kb.op("act", lambda e, s0=s0: e.activation(out=s0[:, 11:12], in_=s0[:, 11:12], func=AF.Sqrt), reads=[s0], writes=[s0])
                        kb.op("dve", lambda e, s0=s0: e.reciprocal(out=s0[:, 12:13], in_=s0[:, 11:12]), reads=[s0], writes=[s0])
                        tok0 = (c - NCH // 2) * 128
                        kb.dma("sp", ot[:, :], o_d[tok0:tok0 + 128, h * 512:(h + 1) * 512], writes=[ot])
                        kb.op("act", lambda e: e.activation(out=ot[:, :], in_=ot[:, :], func=AF.Sigmoid), reads=[ot], writes=[ot])
                        kb.op("dve", lambda e, h=h: e.tensor_tensor(out=ot[:, :], in0=ot[:, :], in1=mnwb[:, h * 512:(h + 1) * 512], op=ALU.mult),
                              reads=[ot, mnwb], writes=[ot])
                        ho = hout[c % 2]
                        kb.op("dve", lambda e, s0=s0, ho=ho: e.scalar_tensor_tensor(out=ho[:, :], in0=hh_[:, :], scalar=s0[:, 12:13], in1=ot[:, :],
                                                                                   op0=ALU.mult, op1=ALU.mult), reads=[hh_, s0, ot], writes=[ho])
                        kb.dma("sp", hcat_d[tok0:tok0 + 128, h * 512:(h + 1) * 512], ho[:, :], reads=[ho])
                    kb.op("dve", lambda e, s0=s0, ucol=ucol: e.tensor_tensor(out=s0[:, 13:14], in0=ucol, in1=s0[:, 2:3], op=ALU.subtract),
                          reads=[U, s0], writes=[s0])
                    kb.op("dve", lambda e, s0=s0: e.tensor_tensor(out=s0[:, 14:15], in0=mcol[:, 0:1], in1=s0[:, 2:3], op=ALU.subtract),
                          reads=[mcol, s0], writes=[s0])
                    kb.op("act", lambda e, s0=s0: e.activation(out=s0[:, 13:15], in_=s0[:, 13:15], func=AF.Exp), reads=[s0], writes=[s0])
                    pk = kb.ps()
                    for j in range(2):
                        kb.op("pe", lambda e, pk=pk, j=j, cs_=cs_: e.matmul(pk[:, j * 128:(j + 1) * 128], lhsT=kTh[:, j, cs_], rhs=identb[:, :],
                                                                          start=True, stop=True), reads=[kTh, identb], writes=[pk])
                    kb.op("act", lambda e, pk=pk, s0=s0: e.activation(out=kw[:, :], in_=pk[:, 0:256], func=AF.Copy, scale=s0[:, 13:14]),
                          reads=[pk, s0], writes=[kw])
                    pn = kb.ps()
                    for j in range(2):
                        pkv = kb.ps()
                        kb.op("pe", lambda e, pkv=pkv, j=j, c=c: e.matmul(pkv[:, :], lhsT=kw[:, j * 128:(j + 1) * 128], rhs=vh[:, c, :],
                                                                         start=True, stop=True), reads=[kw, vh], writes=[pkv])
                        kb.op("pe", lambda e, pn=pn, j=j: e.matmul(pn[:, j:j + 1], lhsT=kw[:, j * 128:(j + 1) * 128], rhs=onesb[:, 0:1],
                                                                  start=True, stop=True), reads=[kw, onesb], writes=[pn])
                        kb.op("dve", lambda e, pkv=pkv, j=j, s0=s0: e.scalar_tensor_tensor(out=Cst[:, j, :], in0=Cst[:, j, :], scalar=s0[:, 14:15],
                                                                                          in1=pkv[:, :], op0=ALU.mult, op1=ALU.add),
                              reads=[Cst, s0, pkv], writes=[Cst])
                    kb.op("act", lambda e: e.activation(out=Cb[:, :, :], in_=Cst[:, :, :], func=AF.Copy), reads=[Cst], writes=[Cb])
                    kb.op("dve", lambda e, pn=pn, s0=s0: e.scalar_tensor_tensor(out=nst_[:, :], in0=nst_[:, :], scalar=s0[:, 14:15], in1=pn[:, 0:2],
                                                                               op0=ALU.mult, op1=ALU.add), reads=[nst_, s0, pn], writes=[nst_])
                    kb.op("dve", lambda e: e.tensor_copy(out=nb_[:, :], in_=nst_[:, :]), reads=[nst_], writes=[nb_])
                    kb.op("dve", lambda e, s0=s0: e.tensor_copy(out=mcol[:, :], in_=s0[:, 1:2]), reads=[s0], writes=[mcol])


        NTq = TOK // 128
        NTk = T2 // 128
        SCL = 128 ** -0.5
        with kb.phase():
            kb.psum_rot = 6
            kb.pnext = 0
            pO, pL = kb.psum[6], kb.psum[7]
            cst_ = kb.sb([128, NTk, 128], F32)
            kb.dma("sp", cst_[:, :, :], cs_tab.rearrange("(i p) d -> p i d", p=128), writes=[cst_])
            pbt = kb.sb([128, NTq, NBP], F32)
            kb.dma("sp", pbt[:, :, :], pb_tab.rearrange("(i p) n -> p i n", p=128), writes=[pbt])
            ohb = kb.sb([NBP, NBP * 128], BF16)
            kb.dma("pool", ohb[:, :], oneh[:, :], writes=[ohb])
            tri01 = kb.sb([128, 128], BF16)
            kb.op("dve", lambda e: e.tensor_copy(out=tri01[:, :], in_=TRIu), reads=[cst], writes=[tri01])
            xq = kb.sb([128, NTq, 128], F32)
            xk = kb.sb([128, NTk, 128], F32)
            rq = kb.sb([128, NTk, 128], F32)
            t1 = kb.sb([128, NTk, 64], F32)
            qT = kb.sb([128, TOK], BF16)
            kT = kb.sb([128, T2], BF16)
            vk = kb.sb([128, NTk, 128], BF16)
            kmT = kb.sb([128, NBP], BF16)
            kms = kb.sb([128, NBP], F32)
            gm = kb.sb([128, NBP], F32)
            t8 = kb.sb([128, 8], F32)
            selb = kb.sb([128, NBP], BF16)
            selT = kb.sb([NBP, TOK], BF16)
            PTm = [kb.sb([128, 128], BF16) for _ in range(3)]
            lrec = kb.sb([128, 2], F32)
            oo = [kb.sb([128, 128], BF16) for _ in range(2)]

            def rotary(X, R, nt):
                c_, s_ = cst_[:, NTk - nt:NTk, 0:64], cst_[:, NTk - nt:NTk, 64:128]
                x1, x2 = X[:, 0:nt, 0:64], X[:, 0:nt, 64:128]
                T1 = t1[:, 0:nt, :]
                kb.op("dve", lambda e: e.tensor_tensor(out=R[:, 0:nt, 0:64], in0=x1, in1=c_, op=ALU.mult), reads=[X, cst_], writes=[R])
                kb.op("dve", lambda e: e.tensor_tensor(out=T1, in0=x2, in1=s_, op=ALU.mult), reads=[X, cst_], writes=[t1])
                kb.op("dve", lambda e: e.tensor_tensor(out=R[:, 0:nt, 0:64], in0=R[:, 0:nt, 0:64], in1=T1, op=ALU.subtract), reads=[R, t1], writes=[R])
                kb.op("dve", lambda e: e.tensor_tensor(out=R[:, 0:nt, 64:128], in0=x2, in1=c_, op=ALU.mult), reads=[X, cst_], writes=[R])
                kb.op("dve", lambda e: e.tensor_tensor(out=T1, in0=x1, in1=s_, op=ALU.mult), reads=[X, cst_, t1], writes=[t1])
                kb.op("dve", lambda e: e.tensor_tensor(out=R[:, 0:nt, 64:128], in0=R[:, 0:nt, 64:128], in1=T1, op=ALU.add), reads=[R, t1], writes=[R])

            def transp(R, nt, dst):
                for i4 in range(nt // 4):
                    pt = kb.ps()
                    for q in range(4):
                        i = i4 * 4 + q
                        kb.op("pe", lambda e, pt=pt, q=q, i=i: e.matmul(pt[:, q * 128:(q + 1) * 128], lhsT=R[:, i, :], rhs=identf[:, :],
                                                                       start=True, stop=True), reads=[R, identf], writes=[pt])
                    ev_copy(i4, dst[:, i4 * 512:(i4 + 1) * 512], pt[:, :], [pt], [dst])

            nev = 0
            for h in range(16):
                hs = slice(h * 128, (h + 1) * 128)
                kb.dma("sp", xq[:, :, :], aq_d[:, hs].rearrange("(i p) d -> p i d", p=128), writes=[xq])
                kb.dma("sp", xk[:, :, :], ak_d[:, hs].rearrange("(i p) d -> p i d", p=128), writes=[xk])
                kb.dma("sp", vk[:, :, :], av_d[:, hs].rearrange("(i p) d -> p i d", p=128), writes=[vk])
                rotary(xq, rq, NTq)
                transp(rq, NTq, qT)
                rotary(xk, rq, NTk)
                transp(rq, NTk, kT)
                kb.op("dve", lambda e: e.memset(kms[:, :], 0.0), writes=[kms])
                kb.op("dve", lambda e: e.tensor_reduce(out=kms[:, 0:NB], in_=kT[:, :].rearrange("p (n k) -> p n k", k=256), axis=AX.X, op=ALU.add),
                      reads=[kT], writes=[kms])
                kb.op("dve", lambda e: e.tensor_scalar(out=kmT[:, :], in0=kms[:, :], scalar1=1.0 / 256, scalar2=None, op0=ALU.mult),
                      reads=[kms], writes=[kmT])
                for i in range(NTq):
                    pg = kb.ps()
                    kb.op("pe", lambda e, pg=pg, i=i: e.matmul(pg[:, 0:NBP], lhsT=qT[:, i * 128:(i + 1) * 128], rhs=kmT[:, :], start=True, stop=True),
                          reads=[qT, kmT], writes=[pg])
                    kb.op("dve", lambda e, pg=pg, i=i: e.tensor_tensor(out=gm[:, :], in0=pg[:, 0:NBP], in1=pbt[:, i, :], op=ALU.add),
                          reads=[pg, pbt], writes=[gm])
                    kb.op("dve", lambda e: e.max(out=t8[:, :], in_=gm[:, :]), reads=[gm], writes=[t8])
                    kb.op("dve", lambda e: e.tensor_scalar(out=t8[:, 2:3], in0=t8[:, 2:3], scalar1=-1.0e29, scalar2=None, op0=ALU.max),
                          reads=[t8], writes=[t8])
                    kb.op("dve", lambda e: e.tensor_scalar(out=gm[:, :], in0=gm[:, :], scalar1=t8[:, 2:3], scalar2=None, op0=ALU.is_ge),
                          reads=[gm, t8], writes=[gm])
                    kb.op("dve", lambda e: e.tensor_scalar(out=selb[:, :], in0=gm[:, :], scalar1=30000.0, scalar2=-30000.0, op0=ALU.mult, op1=ALU.add),
                          reads=[gm], writes=[selb])
                    pt = kb.ps()
                    kb.op("pe", lambda e, pt=pt: e.matmul(pt[0:NBP, 0:128], lhsT=selb[:, :], rhs=identb[:, :], start=True, stop=True),
                          reads=[selb, identb], writes=[pt])
                    kb.op("act", lambda e, pt=pt, i=i: e.activation(out=selT[:, i * 128:(i + 1) * 128], in_=pt[0:NBP, 0:128], func=AF.Copy),
                          reads=[pt], writes=[selT])
                for i in range(NTq):
                    qs = slice(i * 128, (i + 1) * 128)
                    jb = NB // 2 + i // 2
                    items = []
                    for n in range(jb):
                        for hf in range(2):
                            items.append((n, hf, "sel"))
                    for hf in range(2):
                        if hf < i % 2:
                            items.append((jb, hf, "full"))
                        elif hf == i % 2:
                            items.append((jb, hf, "tri"))
                    for idx, (n, hf, kind) in enumerate(items):
                        ks_ = slice(n * 256 + hf * 128, n * 256 + hf * 128 + 128)
                        ki = n * 2 + hf
                        pS = kb.ps()
                        kb.op("pe", lambda e, pS=pS, ks_=ks_, qs=qs, kind=kind: e.matmul(pS[:, 0:128], lhsT=kT[:, ks_], rhs=qT[:, qs], start=True,
                                                                                       stop=(kind != "sel")), reads=[kT, qT], writes=[pS])
                        if kind == "sel":
                            kb.op("pe", lambda e, pS=pS, n=n, qs=qs: e.matmul(pS[:, 0:128], lhsT=ohb[:, n * 128:(n + 1) * 128], rhs=selT[:, qs],
                                                                            start=False, stop=True), reads=[ohb, selT], writes=[pS])
                        P_ = PTm[nev % 3]
                        nev += 1
                        kb.op("act", lambda e, pS=pS, P_=P_: e.activation(out=P_[:, :], in_=pS[:, 0:128], func=AF.Exp, scale=SCL), reads=[pS], writes=[P_])
                        if kind == "tri":
                            kb.op("dve", lambda e, P_=P_: e.tensor_tensor(out=P_[:, :], in0=P_[:, :], in1=tri01[:, :], op=ALU.mult), reads=[P_, tri01], writes=[P_])
                        first, last = idx == 0, idx == len(items) - 1
                        kb.op("pe", lambda e, P_=P_, ki=ki, first=first, last=last: e.matmul(pO[:, 0:128], lhsT=P_[:, :], rhs=vk[:, ki, :], start=first, stop=last),
                              reads=[P_, vk], writes=[pO])
                        kb.op("pe", lambda e, P_=P_, first=first, last=last: e.matmul(pL[:, 0:1], lhsT=P_[:, :], rhs=onesb[:, 0:1], start=first, stop=last),
                              reads=[P_, onesb], writes=[pL])
                    kb.op("dve", lambda e: e.reciprocal(out=lrec[:, 0:1], in_=pL[:, 0:1]), reads=[pL], writes=[lrec])
                    o_ = oo[i % 2]
                    kb.op("act", lambda e, o_=o_: e.activation(out=o_[:, :], in_=pO[:, 0:128], func=AF.Copy, scale=lrec[:, 0:1]), reads=[pO, lrec], writes=[o_])
                    kb.dma("sp", hcat_d[i * 128:(i + 1) * 128, 2048 + h * 128:2048 + (h + 1) * 128], o_[:, :], reads=[o_])
            kb.psum_rot = 8
            kb.pnext = 0

        modflat = mod_d.rearrange("a b -> (a b)")

        def norm_T(x_t, junk_, xs_t, ssc, Acol, bias_fn, dstT, ti):
            kb.op("act", lambda e: e.activation(out=junk_[:, :], in_=x_t[:, :], func=AF.Square, accum_out=ssc[:, 0:1]), reads=[x_t], writes=[junk_, ssc])
            kb.op("dve", lambda e: e.tensor_scalar(out=ssc[:, 0:1], in0=ssc[:, 0:1], scalar1=1.0 / D, scalar2=EPS, op0=ALU.mult, op1=ALU.add), reads=[ssc], writes=[ssc])
            kb.op("act", lambda e: e.activation(out=ssc[:, 0:1], in_=ssc[:, 0:1], func=AF.Sqrt), reads=[ssc], writes=[ssc])
            kb.op("dve", lambda e: e.reciprocal(out=ssc[:, 1:2], in_=ssc[:, 0:1]), reads=[ssc], writes=[ssc])
            kb.op("dve", lambda e: e.tensor_scalar(out=xs_t[:, :], in0=x_t[:, :], scalar1=ssc[:, 1:2], scalar2=None, op0=ALU.mult), reads=[x_t, ssc], writes=[xs_t])
            for k4 in range(KC // 4):
                pt = kb.ps()
                for q in range(4):
                    kc = k4 * 4 + q
                    kb.op("pe", lambda e, pt=pt, q=q, kc=kc: e.matmul(pt[:, q * 128:(q + 1) * 128], lhsT=xs_t[:, kc * 128:(kc + 1) * 128], rhs=identb[:, :],
                                                                     start=True, stop=True), reads=[xs_t, identb], writes=[pt])
                for q in range(4):
                    kc = k4 * 4 + q
                    if Acol is None:
                        ev_copy(q, dstT[:, kc, ti * 128:(ti + 1) * 128], pt[:, q * 128:(q + 1) * 128], [pt], [dstT.ks[ti]])
                    else:
                        kb.op("act", lambda e, pt=pt, q=q, kc=kc: e.activation(out=dstT[:, kc, ti * 128:(ti + 1) * 128], in_=pt[:, q * 128:(q + 1) * 128],
                                                                              func=AF.Identity, scale=Acol[:, kc:kc + 1], bias=bias_fn(kc)),
                              reads=[pt, Acol, modT], writes=[dstT.ks[ti]])

        wov = w_out.rearrange("(kc p) n -> p kc n", p=128)
        with kb.phase():
            g1b = kb.sb([128, D], F32)
            kb.dma("sp", g1b[:, :], modflat[2 * D:3 * D].partition_broadcast(128), writes=[g1b])
            hT = kb.sb([128, KC, TG], BF16, n=4)
            hb = [kb.sb([128, D], BF16) for _ in range(2)]
            xg = [kb.sb([128, D], F32) for _ in range(4)]
            wb = [kb.sb([128, KC, 512], BF16) for _ in range(1)]
            tmpd = [kb.sb([128, 512], F32) for _ in range(2)]
            junk = kb.sb([128, D], BF16)
            xs2 = kb.sb([128, D], BF16)
            ssc = kb.sb([128, 2], F32)
            x2T = hT
            nw = 0
            for g in range(TOK // TG):
                for ti in range(4):
                    tok0 = g * TG + ti * 128
                    hb_ = hb[ti % 2]
                    kb.dma("sp", hb_[:, :], hcat_d[tok0:tok0 + 128, :], writes=[hb_])
                    kb.dma("sp", xg[ti][:, :], x_own[tok0:tok0 + 128, :], writes=[xg[ti]])
                    for k4 in range(KC // 4):
                        pt = kb.ps()
                        for q in range(4):
                            kc = k4 * 4 + q
                            kb.op("pe", lambda e, pt=pt, q=q, kc=kc, hb_=hb_: e.matmul(pt[:, q * 128:(q + 1) * 128], lhsT=hb_[:, kc * 128:(kc + 1) * 128],
                                                                                     rhs=identb[:, :], start=True, stop=True), reads=[hb_, identb], writes=[pt])
                        for q in range(4):
                            kc = k4 * 4 + q
                            ev_copy(q, hT[:, kc, ti * 128:(ti + 1) * 128], pt[:, q * 128:(q + 1) * 128], [pt], [hT.ks[ti]])
                for dc in range(8):
                    w = wb[0]
                    nw += 1
                    kb.dma("pool", w[:, :, :], wov[:, :, dc * 512:(dc + 1) * 512], writes=[w])
                    for ti in range(4):
                        pt = kb.ps()
                        for kc in range(KC):
                            kb.op("pe", lambda e, pt=pt, kc=kc, ti=ti, w=w: e.matmul(pt[:, :], lhsT=hT[:, kc, ti * 128:(ti + 1) * 128], rhs=w[:, kc, :],
                                                                                    start=(kc == 0), stop=(kc == KC - 1)), reads=[w, hT.ks[ti]], writes=[pt])
                        td = tmpd[ti % 2]
                        ds_ = slice(dc * 512, (dc + 1) * 512)
                        kb.op("dve", lambda e, pt=pt, td=td, ds_=ds_: e.tensor_tensor(out=td[:, :], in0=pt[:, :], in1=g1b[:, ds_], op=ALU.mult), reads=[pt, g1b], writes=[td])
                        kb.op("pool", lambda e, td=td, ds_=ds_, ti=ti: e.tensor_tensor(out=xg[ti][:, ds_], in0=xg[ti][:, ds_], in1=td[:, :], op=ALU.add),
                              reads=[xg[ti], td], writes=[xg[ti]])
                for ti in range(4):
                    tok0 = g * TG + ti * 128
                    kb.dma("sp", x2_d[tok0:tok0 + 128, :], xg[ti][:, :], reads=[xg[ti]])
                    norm_T(xg[ti], junk, xs2, ssc, A2, lambda kc: modT[:, 96 + kc:97 + kc], x2T, ti)
                kb.dma("sp", xn2T_d[:, :, g * TG:(g + 1) * TG], x2T[:, :, :], reads=x2T.ks)

        wqv = wq.rearrange("(kc p) n -> p kc n", p=128)
        uTv = uT.rearrange("(kc p) n -> p kc n", p=128)
        NGP = TOK // TG
        with kb.phase():
            xT = kb.sb([128, KC, TG], BF16)
            skb = kb.sb([128, 16, NK], BF16)
            kb.dma("pool", skb[:, :, :], skT[:, :, :], writes=[skb])
            wqb = [kb.sb([128, KC, 128], BF16) for _ in range(2)]
            qsb = [kb.sb([128, TG], BF16) for _ in range(2)]
            S2 = kb.sb([128, 4, 8, NK], F32)
            AA = kb.sb([128, 4, 8, NK], F32)
            NT = kb.sb([128, 4, 8, NK], F32)
            s1b = kb.sb([128, NK], F32)
            T1 = kb.sb([128, 16], F32)
            T2_ = kb.sb([128, 16], F32)
            cand = kb.sb([128, 256], F32)
            candb = kb.sb([128, 256], F32)
            c16 = kb.sb([128, 16], F32)
            e16 = kb.sb([128, 16], F32)
            zc = kb.sb([128, 4], F32)
            nwq = 0
            for g in range(NGP):
                kb.dma("sp", xT[:, :, :], xn2T_d[:, :, g * TG:(g + 1) * TG], writes=[xT])
                for hp in range(16):
                    h, half = hp // 2, hp % 2
                    w = wqb[nwq % 2]
                    qs_ = qsb[nwq % 2]
                    nwq += 1
                    kb.dma("pool", w[:, :, :], wqv[:, :, hp * 128:(hp + 1) * 128], writes=[w])
                    pq = kb.ps()
                    for kc in range(KC):
                        kb.op("pe", lambda e, pq=pq, kc=kc, w=w: e.matmul(pq[:, :], lhsT=w[:, kc, :], rhs=xT[:, kc, :], start=(kc == 0), stop=(kc == KC - 1)),
                              reads=[w, xT], writes=[pq])
                    kb.op("act", lambda e, pq=pq, qs_=qs_: e.activation(out=qs_[:, :], in_=pq[:, :], func=AF.Copy), reads=[pq], writes=[qs_])
                    for ti in range(4):
                        psc = kb.ps()
                        kb.op("pe", lambda e, psc=psc, ti=ti, hp=hp, qs_=qs_: e.matmul(psc[:, 0:NK], lhsT=qs_[:, ti * 128:(ti + 1) * 128], rhs=skb[:, hp, :], start=True, stop=True),
                              reads=[qs_, skb], writes=[psc])
                        if half == 0:
                            kb.op("dve", lambda e, psc=psc, ti=ti, h=h: e.tensor_copy(out=AA[:, ti, h, :], in_=psc[:, 0:NK]), reads=[psc], writes=[AA])
                        else:
                            kb.op("dve", lambda e, psc=psc, ti=ti, h=h: e.tensor_copy(out=S2[:, ti, h, :], in_=psc[:, 0:NK]), reads=[psc], writes=[S2])
                for ti in range(4):
                    for h in range(8):
                        for (src, dstT_) in ((AA[:, ti, h, :], T1), (S2[:, ti, h, :], T2_)):
                            kb.op("dve", lambda e, src=src, dstT_=dstT_: e.max(out=dstT_[:, 0:8], in_=src), reads=[AA, S2], writes=[dstT_])
                            kb.op("dve", lambda e, src=src, dstT_=dstT_: e.match_replace(out=s1b[:, :], in_to_replace=dstT_[:, 0:8], in_values=src, imm_value=NEG),
                                  reads=[AA, S2, dstT_], writes=[s1b])
                            kb.op("dve", lambda e, dstT_=dstT_: e.max(out=dstT_[:, 8:16], in_=s1b[:, :]), reads=[s1b], writes=[dstT_])
                        for a_ in range(16):
                            kb.op("dve", lambda e, a_=a_: e.tensor_scalar(out=cand[:, a_ * 16:(a_ + 1) * 16], in0=T2_[:, :], scalar1=T1[:, a_:a_ + 1], scalar2=None, op0=ALU.add),
                                  reads=[T1, T2_], writes=[cand])
                        kb.op("dve", lambda e: e.max(out=c16[:, 0:8], in_=cand[:, :]), reads=[cand], writes=[c16])
                        kb.op("dve", lambda e: e.match_replace(out=candb[:, :], in_to_replace=c16[:, 0:8], in_values=cand[:, :], imm_value=NEG), reads=[cand, c16], writes=[candb])
                        kb.op("dve", lambda e: e.max(out=c16[:, 8:16], in_=candb[:, :]), reads=[candb], writes=[c16])
                        kb.op("dve", lambda e: e.tensor_scalar(out=zc[:, 0:1], in0=c16[:, 0:1], scalar1=-1.0, scalar2=None, op0=ALU.mult), reads=[c16], writes=[zc])
                        kb.op("act", lambda e: e.activation(out=e16[:, :], in_=c16[:, :], func=AF.Exp, bias=zc[:, 0:1], accum_out=zc[:, 1:2]), reads=[c16, zc], writes=[e16, zc])
                        kb.op("act", lambda e: e.activation(out=zc[:, 2:3], in_=zc[:, 1:2], func=AF.Ln), reads=[zc], writes=[zc])
                        kb.op("dve", lambda e: e.tensor_tensor(out=zc[:, 2:3], in0=zc[:, 0:1], in1=zc[:, 2:3], op=ALU.subtract), reads=[zc], writes=[zc])
                        kb.op("dve", lambda e, ti=ti, h=h: e.tensor_scalar(out=NT[:, ti, h, :], in0=AA[:, ti, h, :], scalar1=-1.0, scalar2=c16[:, 15:16], op0=ALU.mult, op1=ALU.add),
                              reads=[AA, c16], writes=[NT])
                        kb.op("dve", lambda e, ti=ti, h=h: e.tensor_scalar(out=AA[:, ti, h, :], in0=AA[:, ti, h, :], scalar1=zc[:, 2:3], scalar2=None, op0=ALU.add),
                              reads=[AA, zc], writes=[AA])
                for j_, t_ in enumerate((S2, AA, NT)):
                    kb.dma("sp", ans_d[g, j_, :, :], t_[:, :, :, :].rearrange("p a b c -> p (a b c)"), reads=[t_])

        with kb.phase():
            kb.psum_rot = 4
            kb.pnext = 0
            pA_, pG_ = [kb.psum[4], kb.psum[5]], [kb.psum[6], kb.psum[7]]
            xT = kb.sb([128, KC, TG], BF16)
            S2 = kb.sb([128, 4, 8, NK], F32)
            AA = kb.sb([128, 4, 8, NK], F32)
            NT = kb.sb([128, 4, 8, NK], F32)
            NUB, NVB, NEB = 2, 3, 6
            Ub = [kb.sb([128, KC, NK], BF16) for _ in range(NUB)]
            Vb = [kb.sb([NK, D], BF16) for _ in range(NVB)]
            Eb = [kb.sb([128, NK], F32) for _ in range(NEB)]
            Gh = [kb.sb([128, NK], BF16) for _ in range(NEB)]
            gel = [kb.sb([NK, TG], F32) for _ in range(2)]
            wT = [kb.sb([NK, TG], BF16) for _ in range(4)]
            ACC = kb.sb([128, 4, D], F32, n=4)
            NP_ = NK // 2
            st_ = {"rot": 0}

            def v_load(c):
                V_ = Vb[c % NVB]
                kb.dma("pool", V_[:, :], vv[c * NK:(c + 1) * NK, :], writes=[V_], max_dma_last_dim=4096)

            def u_load(c):
                U_ = Ub[c % NUB]
                kb.dma("pool", U_[:, :, :], uTv[:, :, c * NK:(c + 1) * NK], writes=[U_])

            def s1_pa(p, part):
                q, hf = part // 2, part % 2
                c = 2 * p + q
                U_ = Ub[c % NUB]
                pA = pA_[q]
                for kc in range(hf * (KC // 2), (hf + 1) * (KC // 2)):
                    kb.op("pe", lambda e, pA=pA, kc=kc, U_=U_: e.matmul(pA[0:NK, :], lhsT=U_[:, kc, :], rhs=xT[:, kc, :], start=(kc == 0), stop=(kc == KC - 1)),
                          reads=[U_, xT], writes=[pA])
                if hf == 1:
                    kb.op("act", lambda e, q=q: e.activation(out=gel[q][:, :], in_=pA_[q][0:NK, :], func=AF.Gelu), reads=[pA_[q]], writes=[gel[q]])
                    if c + 2 < NK:
                        u_load(c + 2)

            def s1_gbuild(p, ti):
                for q in range(2):
                    i = 2 * p + q
                    pG = pG_[q]
                    for h in range(8):
                        E_ = Eb[st_["rot"] % NEB]
                        G_ = Gh[st_["rot"] % NEB]
                        st_["rot"] += 1
                        kb.op("act", lambda e, E_=E_, h=h, i=i: e.activation(out=E_[:, :], in_=S2[:, ti, h, :], func=AF.Exp, bias=AA[:, ti, h, i:i + 1]),
                              reads=[S2, AA], writes=[E_])
                        kb.op("dve", lambda e, E_=E_, G_=G_, h=h, i=i: e.scalar_tensor_tensor(out=G_[:, :], in0=S2[:, ti, h, :], scalar=NT[:, ti, h, i:i + 1], in1=E_[:, :],
                                                                                       op0=ALU.is_ge, op1=ALU.mult), reads=[S2, NT, E_], writes=[G_])
                        kb.op("pe", lambda e, G_=G_, pG=pG, h=h: e.matmul(pG[0:NK, ti * 128:(ti + 1) * 128], lhsT=G_[:, :], rhs=identb[:, :], start=(h == 0), stop=(h == 7)),
                              reads=[G_, identb], writes=[pG])

            def s1_end(p):
                for q in range(2):
                    c = 2 * p + q
                    w_ = wT[c % 4]
                    kb.op("dve", lambda e, w_=w_, q=q: e.tensor_tensor(out=w_[:, :], in0=pG_[q][0:NK, :], in1=gel[q][:, :], op=ALU.mult), reads=[pG_[q], gel[q]], writes=[w_])

            def s2_(p, ti):
                c0, c1 = 2 * p, 2 * p + 1
                for dc in range(8):
                    po = kb.ps()
                    ds_ = slice(dc * 512, (dc + 1) * 512)
                    kb.op("pe", lambda e, po=po, ds_=ds_: e.matmul(po[:, :], lhsT=wT[c0 % 4][:, ti * 128:(ti + 1) * 128], rhs=Vb[c0 % NVB][:, ds_], start=True, stop=False),
                          reads=[wT[c0 % 4], Vb[c0 % NVB]], writes=[po])
                    kb.op("pe", lambda e, po=po, ds_=ds_: e.matmul(po[:, :], lhsT=wT[c1 % 4][:, ti * 128:(ti + 1) * 128], rhs=Vb[c1 % NVB][:, ds_], start=False, stop=True),
                          reads=[wT[c1 % 4], Vb[c1 % NVB]], writes=[po])
                    if p == 0:
                        kb.op("dve", lambda e, po=po, ds_=ds_: e.tensor_copy(out=ACC[:, ti, ds_], in_=po[:, :]), reads=[po], writes=[ACC.ks[ti]])
                    else:
                        kb.op("dve", lambda e, po=po, ds_=ds_: e.tensor_tensor(out=ACC[:, ti, ds_], in0=ACC[:, ti, ds_], in1=po[:, :], op=ALU.add),
                              reads=[po, ACC.ks[ti]], writes=[ACC.ks[ti]])

            for g in range(NGP):
                kb.dma("sp", xT[:, :, :], xn2T_d[:, :, g * TG:(g + 1) * TG], writes=[xT])
                for j_, t_ in enumerate((S2, AA, NT)):
                    kb.dma("sp", t_[:, :, :, :].rearrange("p a b c -> p (a b c)"), ans_d[g, j_, :, :], writes=[t_])
                u_load(0)
                u_load(1)
                v_load(0)
                v_load(1)
                for part in range(4):
                    s1_pa(0, part)
                for ti in range(4):
                    s1_gbuild(0, ti)
                s1_end(0)
                for p in range(NP_):
                    nxt = p + 1 < NP_
                    if nxt:
                        v_load(2 * p + 2)
                    for ti in range(4):
                        s2_(p, ti)
                        if nxt:
                            s1_pa(p + 1, ti)
                            s1_gbuild(p + 1, ti)
                    if nxt:
                        s1_end(p + 1)
                        v_load(2 * p + 3)
                for ti in range(4):
                    tok0 = g * TG + ti * 128
                    kb.dma("sp", peer_d[tok0:tok0 + 128, :], ACC[:, ti, :], reads=[ACC.ks[ti]])
            kb.psum_rot = 8
            kb.pnext = 0

        with kb.phase():
            g2b = kb.sb([128, D], F32)
            fnb = kb.sb([128, D], F32)
            kb.dma("sp", g2b[:, :], modflat[5 * D:6 * D].partition_broadcast(128), writes=[g2b])
            kb.dma("sp", fnb[:, :], fnw.partition_broadcast(128), writes=[fnb])
            xa = [kb.sb([128, D], F32) for _ in range(2)]
            pa = [kb.sb([128, D], F32) for _ in range(2)]
            junk = kb.sb([128, D], BF16)
            ssf = [kb.sb([128, 2], F32) for _ in range(2)]
            for it in range(TOK // 128):
                x_, p_, ss_ = xa[it % 2], pa[it % 2], ssf[it % 2]
                rs_ = slice(it * 128, (it + 1) * 128)
                kb.dma("sp", x_[:, :], x2_d[rs_, :], writes=[x_])
                kb.dma("sp", p_[:, :], peer_d[rs_, :], writes=[p_])
                kb.op("dve", lambda e, p_=p_: e.tensor_tensor(out=p_[:, :], in0=p_[:, :], in1=g2b[:, :], op=ALU.mult), reads=[p_, g2b], writes=[p_])
                kb.op("pool", lambda e, p_=p_, x_=x_: e.tensor_tensor(out=x_[:, :], in0=x_[:, :], in1=p_[:, :], op=ALU.add), reads=[p_, x_], writes=[x_])
                kb.op("act", lambda e, x_=x_, ss_=ss_: e.activation(out=junk[:, :], in_=x_[:, :], func=AF.Square, accum_out=ss_[:, 0:1]), reads=[x_], writes=[junk, ss_])
                kb.op("dve", lambda e, ss_=ss_: e.tensor_scalar(out=ss_[:, 0:1], in0=ss_[:, 0:1], scalar1=1.0 / D, scalar2=EPS, op0=ALU.mult, op1=ALU.add), reads=[ss_], writes=[ss_])
                kb.op("act", lambda e, ss_=ss_: e.activation(out=ss_[:, 0:1], in_=ss_[:, 0:1], func=AF.Sqrt), reads=[ss_], writes=[ss_])
                kb.op("dve", lambda e, ss_=ss_: e.reciprocal(out=ss_[:, 1:2], in_=ss_[:, 0:1]), reads=[ss_], writes=[ss_])
                kb.op("dve", lambda e, x_=x_, p_=p_, ss_=ss_: e.scalar_tensor_tensor(out=p_[:, :], in0=x_[:, :], scalar=ss_[:, 1:2], in1=fnb[:, :], op0=ALU.mult, op1=ALU.mult),
                      reads=[x_, ss_, fnb], writes=[p_])
                kb.dma("sp", y[rs_, :], p_[:, :], reads=[p_])


        kb.barrier()
    return nc


def host_shared(cfg, inp):
    TOK, T2, NK, NB, NCH = cfg.TOK, cfg.T2, cfg.NK, cfg.NB, cfg.NCH
    NBP = max(NB, 8)
    m = {}
    idx = np.arange(128)
    le = (idx[:, None] <= idx[None, :])
    consts = np.zeros((128, 5, 128), np.float32)
    consts[:, 0, :] = le.astype(np.float32)
    consts[:, 1, :] = np.where(le.T, 0.0, NEG)
    consts[:, 2, :] = np.where(le, 0.0, 1.0e30)
    consts[127, 3, :] = 1.0
    consts[:, 4, :] = 1.0
    m["consts"] = consts
    m["ident_f"] = np.eye(128, dtype=np.float32)
    m["w_ada"] = inp["w_ada"][0]
    m["b_adaT"] = np.ascontiguousarray(inp["b_ada"][0].reshape(192, 128).T)
    m["n1wT"] = np.ascontiguousarray(inp["norm1_w"][0].reshape(KC, 128).T)
    m["n2wT"] = np.ascontiguousarray(inp["norm2_w"][0].reshape(KC, 128).T)
    m["fnw"] = np.ascontiguousarray(inp["final_norm_w"])
    m["w_in"] = inp["w_in"][0]
    m["conv_wT"] = np.ascontiguousarray(inp["conv_w"][0].T.reshape(16, 128, 4).transpose(1, 0, 2))
    m["conv_bT"] = np.ascontiguousarray(inp["conv_b"][0].reshape(16, 128).T)
    gb = np.concatenate([inp["b_igate"][0], inp["b_fgate"][0]]).astype(np.float32)
    m["gbias"] = np.ascontiguousarray(np.broadcast_to(gb[None, None, :], (128, NCH, 8)))
    m["mnw"] = np.ascontiguousarray(inp["mlstm_norm_w"][0].reshape(2048))
    oneh = np.zeros((NBP, NBP, 128), np.float32)
    for n in range(NBP):
        oneh[n, n, :] = 1.0
    m["oneh"] = oneh.reshape(NBP, NBP * 128)
    m["w_out"] = inp["w_out"][0]
    m["wq"] = inp["peer_wq"][0]
    m["skT"] = np.ascontiguousarray(inp["peer_subkeys"][0].reshape(16, NK, 128).transpose(2, 0, 1))
    m["uT"] = np.ascontiguousarray(inp["peer_u"][0].T)
    m["vv"] = inp["peer_v"][0]
    return m


def host_inputs(cfg, core, b, s, inp, shared):
    TOK, T2, NB = cfg.TOK, cfg.T2, cfg.NB
    NBP = max(NB, 8)
    x = inp["x"]
    m = dict(shared)
    own0 = s * TOK
    m["x_own"] = np.ascontiguousarray(x[b, own0:own0 + TOK])
    m["x_past"] = np.ascontiguousarray(x[b, 0:TOK]) if s == 1 else np.zeros((TOK, D), np.float32)
    m["cT"] = np.ascontiguousarray(inp["c"][b].reshape(KC, 128).T)
    m["flag"] = np.full((128, 1), float(s), np.float32)
    pos = np.concatenate([np.arange(TOK), s * TOK + np.arange(TOK)]).astype(np.float32)
    half = 64
    inv = (np.float32(10000.0) ** (-np.arange(half, dtype=np.float32) / np.float32(half))).astype(np.float32)
    ang = (pos[:, None] * inv[None, :]).astype(np.float32)
    m["cs_tab"] = np.concatenate([np.cos(ang), np.sin(ang)], axis=1).astype(np.float32)
    pb = np.full((TOK, NBP), NEG, np.float32)
    for q0 in range(0, TOK, 256):
        jb = NB // 2 + q0 // 256
        lo = 0 if s == 1 else NB // 2
        pb[q0:q0 + 256, lo:jb] = 0.0
    m["pb_tab"] = pb
    return m


_CACHE = {}


def kernel(**inp):
    inp = {k: np.asarray(v) for k, v in inp.items()}
    cfg = Cfg(tok=2048, nk=128)
    if "nc" not in _CACHE:
        _CACHE["nc"] = build(cfg)
    nc = _CACHE["nc"]
    shared = host_shared(cfg, inp)
    maps = []
    for core in range(8):
        b, s = core // 2, core % 2
        maps.append(host_inputs(cfg, core, b, s, inp, shared))
    res = run_bass_kernel_spmd(nc, maps, core_ids=list(range(8)))
    out = np.zeros((4, 4096, D), np.float32)
    for core in range(8):
        b, s = core // 2, core % 2
        out[b, s * cfg.TOK:(s + 1) * cfg.TOK] = res.results[core]["y"]
    return out
```

```python
import numpy as np
from contextlib import ExitStack
import ml_dtypes
import concourse.bass as bass
import concourse.mybir as mybir
from concourse.bass_utils import run_bass_kernel_spmd

F32 = mybir.dt.float32
BF16 = mybir.dt.bfloat16
AF = mybir.ActivationFunctionType
ALU = mybir.AluOpType
AX = mybir.AxisListType

D = 4096
KC = 32
NDS = 24
EPS = 1e-6
NEG = -1.0e30


class Tk:
    __slots__ = ("w", "rs")

    def __init__(self):
        self.w = None
        self.rs = {}


class Tile:
    def __init__(self, t, n=1):
        self.t = t
        self.k = Tk()
        self.ks = [Tk() for _ in range(n)]

    def __getitem__(self, idx):
        return self.t[idx]


class KB:
    EPOCH = 20000

    def __init__(self, nc, st):
        self.nc = nc
        self.st = st
        self.eng = {"pe": nc.tensor, "act": nc.scalar, "dve": nc.vector, "pool": nc.gpsimd, "sp": nc.sync}
        self.cnt = {e: 0 for e in self.eng}
        self.sem = {e: self.newsem() for e in self.eng}
        self.last = {e: None for e in self.eng}
        self.seen = {}
        self.dsem = [self.newsem() for _ in range(NDS)]
        self.dval = [0] * NDS
        self.dnext = 0
        self.psum = []
        self.pnext = 0
        for i in range(8):
            self.psum.append(Tile(st.enter_context(nc.psum_tensor(f"ps{i}", [128, 512], F32))))
        self.pst = None

    def newsem(self):
        self._ns = getattr(self, "_ns", 0) + 1
        return self.st.enter_context(self.nc.semaphore(f"sem{self._ns}"))

    psum_rot = 8

    def ps(self):
        p = self.psum[self.pnext]
        self.pnext = (self.pnext + 1) % self.psum_rot
        return p

    def sb(self, shape, dtype, n=1, name=None):
        self._nm = getattr(self, "_nm", 0) + 1
        t = self.pst.enter_context(self.nc.sbuf_tensor(name or f"sb{self._nm}", list(shape), dtype))
        return Tile(t, n)

    def _wait(self, e, tk):
        sem, val = tk[0], tk[1]
        key = (e, id(sem))
        if self.seen.get(key, 0) >= val:
            return
        self.eng[e].wait_ge(sem, val)
        self.seen[key] = val

    def _deps(self, e, reads, writes):
        ticks = []
        for t in reads:
            if t.w is not None:
                ticks.append(t.w)
        for t in writes:
            if t.w is not None:
                ticks.append(t.w)
            ticks.extend(t.rs.values())
        for tk in ticks:
            if tk[2] == e and e == "pe":
                continue
            self._wait(e, tk)

    def _mark(self, tk, reads, writes):
        for t in reads:
            t.rs[id(tk[0])] = tk
        for t in writes:
            t.w = tk
            t.rs = {}

    @staticmethod
    def _tk(objs):
        out = []
        for o in objs:
            out.append(o.k if isinstance(o, Tile) else o)
        return out

    def op(self, e, fn, reads=(), writes=()):
        reads, writes = self._tk(reads), self._tk(writes)
        self._deps(e, reads, writes)
        if self.cnt[e] >= self.EPOCH:
            self.sem[e] = self.newsem()
            self.cnt[e] = 0
        self.cnt[e] += 1
        ins = fn(self.eng[e])
        ins.then_inc(self.sem[e], 1)
        tk = (self.sem[e], self.cnt[e], e)
        self.last[e] = tk
        self._mark(tk, reads, writes)

    def dma(self, q, out, in_, reads=(), writes=(), **kw):
        reads, writes = self._tk(reads), self._tk(writes)
        self._deps(q, reads, writes)
        i = self.dnext
        self.dnext = (self.dnext + 1) % NDS
        if self.dval[i] > 0:
            self._wait(q, (self.dsem[i], self.dval[i]))
        self.dval[i] += 16
        self.eng[q].dma_start(out=out, in_=in_, **kw).then_inc(self.dsem[i], 16)
        tk = (self.dsem[i], self.dval[i], "dma")
        self._mark(tk, reads, writes)

    def barrier(self):
        for e in self.eng:
            for e2 in self.eng:
                if e2 != e and self.last[e2] is not None:
                    self._wait(e, self.last[e2])
            for i in range(NDS):
                if self.dval[i] > 0:
                    self._wait(e, (self.dsem[i], self.dval[i]))

    def phase(self):
        kb = self

        class _P:
            def __enter__(s):
                kb.pst = ExitStack()
                kb.pst.__enter__()
                return kb

            def __exit__(s, *a):
                kb.barrier()
                kb.pst.__exit__(*a)
                kb.pst = None
                return False

        return _P()


class Cfg:
    def __init__(self, tok=2048, nk=128):
        self.TOK = tok
        self.T2 = 2 * tok
        self.NK = nk
        self.NE = nk * nk
        self.NB = self.T2 // 256
        self.NCH = self.T2 // 128


def build(cfg, upto=99):
    TOK, T2 = cfg.TOK, cfg.T2
    nc = bass.Bass("TRN2", target_bir_lowering=False)

    def din(name, shape, dt=F32):
        return nc.dram_tensor(name, list(shape), dt, kind="ExternalInput").ap()

    def dscr(name, shape, dt=F32):
        return nc.dram_tensor(name, list(shape), dt).ap()

    x_own = din("x_own", [TOK, D])
    x_past = din("x_past", [TOK, D])
    cT = din("cT", [128, KC])
    w_ada = din("w_ada", [D, 6 * D])
    b_adaT = din("b_adaT", [128, 192])
    n1wT = din("n1wT", [128, KC])
    n2wT = din("n2wT", [128, KC])
    fnw = din("fnw", [D])
    w_in = din("w_in", [D, 12296])
    flag = din("flag", [128, 1])
    ident_f = din("ident_f", [128, 128])
    y = nc.dram_tensor("y", [TOK, D], F32, kind="ExternalOutput").ap()
    NK, NE, NB, NCH = cfg.NK, cfg.NE, cfg.NB, cfg.NCH
    NBP = max(NB, 8)
    consts = din("consts", [128, 5, 128])
    conv_wT = din("conv_wT", [128, 16, 4])
    conv_bT = din("conv_bT", [128, 16])
    gbias = din("gbias", [128, NCH, 8])
    mnw = din("mnw", [2048])
    cs_tab = din("cs_tab", [T2, 128])
    pb_tab = din("pb_tab", [TOK, NBP])
    oneh = din("oneh", [NBP, NBP * 128])
    w_out = din("w_out", [D, D])
    wq = din("wq", [D, 2048])
    skT = din("skT", [128, 16, NK])
    uT = din("uT", [D, NE])
    vv = din("vv", [NE, D])
    qkc_d = dscr("qkc_d", [2048, T2], BF16)
    hcat_d = dscr("hcat_d", [TOK, D], BF16)
    x2_d = dscr("x2_d", [TOK, D])
    xn2T_d = dscr("xn2T_d", [128, KC, TOK], BF16)
    s_d = dscr("s_d", [TOK, 16, NK])
    peer_d = dscr("peer_d", [TOK, D])
    ans_d = dscr("ans_d", [TOK // 512, 3, 128, 4 * 8 * NK])

    mv_d = dscr("mv_d", [T2, 2048], BF16)
    o_d = dscr("o_d", [TOK, 2048])
    if_d = dscr("if_d", [T2, 8])
    aq_d = dscr("aq_d", [TOK, 2048])
    ak_d = dscr("ak_d", [T2, 2048])
    av_d = dscr("av_d", [T2, 2048], BF16)
    qkT_d = dscr("qkT_d", [2048, 4 + T2])
    mod_d = dscr("mod_d", [192, 128])
    dbg = {}

    with ExitStack() as st:
        kb = KB(nc, st)
        kb.pst = st
        modT = kb.sb([128, 192], F32, name="modT")
        A1 = kb.sb([128, KC], F32, name="A1")
        A1p = kb.sb([128, KC], F32, name="A1p")
        S1p = kb.sb([128, KC], F32, name="S1p")
        A2 = kb.sb([128, KC], F32, name="A2")
        identb = kb.sb([128, 128], BF16, name="identb")
        identf = kb.sb([128, 128], F32, name="identf")
        flg = kb.sb([128, 1], F32, name="flg")
        cst = kb.sb([128, 5, 128], F32, name="cst")
        onesb = kb.sb([128, 128], BF16, name="onesb")
        kb.pst = None
        kb.dma("sp", cst[:, :, :], consts[:, :, :], writes=[cst])
        kb.dma("pool", onesb[:, :], consts[:, 4, :], writes=[onesb])
        kb.dma("sp", identf[:, :], ident_f[:, :], writes=[identf])
        kb.dma("pool", identb[:, :], ident_f[:, :], writes=[identb])
        kb.dma("sp", flg[:, :], flag[:, :], writes=[flg])

        with kb.phase():
            c_sb = kb.sb([128, KC], F32)
            cs = kb.sb([128, KC], BF16)
            bad = kb.sb([128, 192], F32)
            n1 = kb.sb([128, KC], F32)
            n2 = kb.sb([128, KC], F32)
            wb = [kb.sb([128, KC, 512], BF16) for _ in range(2)]
            kb.dma("sp", c_sb[:, :], cT[:, :], writes=[c_sb])
            kb.dma("sp", bad[:, :], b_adaT[:, :], writes=[bad])
            kb.dma("sp", n1[:, :], n1wT[:, :], writes=[n1])
            kb.dma("sp", n2[:, :], n2wT[:, :], writes=[n2])
            kb.op("act", lambda e: e.activation(out=cs[:, :], in_=c_sb[:, :], func=AF.Silu), reads=[c_sb], writes=[cs])
            wv = w_ada.rearrange("(kc p) n -> p kc n", p=128)
            pacc = kb.ps()
            for cc in range(48):
                w = wb[cc % 2]
                kb.dma("pool", w[:, :, :], wv[:, :, cc * 512:(cc + 1) * 512], writes=[w])
                for sub in range(4):
                    j = cc * 4 + sub
                    for kc in range(KC):
                        kb.op("pe", lambda e, w=w, kc=kc, sub=sub, j=j: e.matmul(
                            pacc[:, j:j + 1], lhsT=w[:, kc, sub * 128:(sub + 1) * 128], rhs=cs[:, kc:kc + 1],
                            start=(kc == 0), stop=(kc == KC - 1)), reads=[w, cs], writes=[pacc])
            kb.op("dve", lambda e: e.tensor_tensor(out=modT[:, :], in0=pacc[:, 0:192], in1=bad[:, :], op=ALU.add),
                  reads=[pacc, bad], writes=[modT])
            kb.op("dve", lambda e: e.scalar_tensor_tensor(out=A1[:, :], in0=modT[:, 32:64], scalar=1.0, in1=n1[:, :],
                                                          op0=ALU.add, op1=ALU.mult), reads=[modT, n1], writes=[A1])
            kb.op("dve", lambda e: e.scalar_tensor_tensor(out=A2[:, :], in0=modT[:, 128:160], scalar=1.0, in1=n2[:, :],
                                                          op0=ALU.add, op1=ALU.mult), reads=[modT, n2], writes=[A2])
            kb.op("dve", lambda e: e.tensor_scalar(out=A1p[:, :], in0=A1[:, :], scalar1=flg[:, 0:1], scalar2=None,
                                                   op0=ALU.mult), reads=[A1, flg], writes=[A1p])
            kb.op("dve", lambda e: e.tensor_scalar(out=S1p[:, :], in0=modT[:, 0:32], scalar1=flg[:, 0:1], scalar2=None,
                                                   op0=ALU.mult), reads=[modT, flg], writes=[S1p])
            mrow = kb.sb([128, 2, 128], F32)
            for hh in range(2):
                pt = kb.ps()
                n = 128 if hh == 0 else 64
                kb.op("pe", lambda e, pt=pt, hh=hh, n=n: e.matmul(pt[0:n, 0:128], lhsT=modT[:, hh * 128:hh * 128 + n],
                                                                  rhs=identf[:, :], start=True, stop=True),
                      reads=[modT, identf], writes=[pt])
                kb.op("act", lambda e, pt=pt, hh=hh, n=n: e.activation(out=mrow[0:n, hh, :], in_=pt[0:n, 0:128], func=AF.Copy),
                      reads=[pt], writes=[mrow])
                kb.dma("sp", mod_d[hh * 128:hh * 128 + n, :], mrow[0:n, hh, :], reads=[mrow])
        if upto <= 1:
            dbg["modT"] = None

        TG = 512
        TGB = 1024 if TOK % 1024 == 0 else 512
        NG = T2 // TGB
        NTB = TGB // 128
        wiv = w_in.rearrange("(kc p) n -> p kc n", p=128)
        chunks = []
        for i in range(4):
            chunks.append((i * 512, 512, "qk"))
        for i in range(4):
            chunks.append((2048 + i * 512, 512, "mv"))
        for i in range(4):
            chunks.append((4096 + i * 512, 512, "o"))
        chunks.append((6144, 8, "if"))
        for i in range(4):
            chunks.append((6152 + i * 512, 512, "aq"))
        for i in range(4):
            chunks.append((8200 + i * 512, 512, "ak"))
        for i in range(4):
            chunks.append((10248 + i * 512, 512, "av"))
        with kb.phase():
            xt = [kb.sb([128, D], F32) for _ in range(2)]
            junk = kb.sb([128, D], BF16)
            xs = [kb.sb([128, D], BF16) for _ in range(2)]
            ss = [kb.sb([128, 1], F32) for _ in range(2)]
            xnT = kb.sb([128, KC, TGB], BF16, n=NTB)
            wb = [kb.sb([128, KC, 512], BF16) for _ in range(2)]
            stf = [kb.sb([128, 512], F32) for _ in range(3)]
            stb = [kb.sb([128, 512], BF16) for _ in range(3)]
            zpad = kb.sb([128, 4], F32)
            kb.op("dve", lambda e: e.memset(zpad[:, :], 0.0), writes=[zpad])
            for cc in range(16):
                kb.dma("sp", qkT_d[cc * 128:(cc + 1) * 128, 0:4], zpad[:, :], reads=[zpad])
            nst = 0
            nw = 0
            for g in range(NG):
                past = (g * TGB) < TOK
                for ti in range(NTB):
                    tok0 = g * TGB + ti * 128
                    src = x_past[tok0:tok0 + 128, :] if past else x_own[tok0 - TOK:tok0 - TOK + 128, :]
                    it = g * NTB + ti
                    x_t, xs_t, ss_t = xt[it % 2], xs[it % 2], ss[it % 2]
                    kb.dma("sp", x_t[:, :], src, writes=[x_t])
                    kb.op("act", lambda e, x_t=x_t, ss_t=ss_t: e.activation(out=junk[:, :], in_=x_t[:, :], func=AF.Square,
                                                                           accum_out=ss_t[:, 0:1]),
                          reads=[x_t], writes=[junk, ss_t])
                    kb.op("dve", lambda e, ss_t=ss_t: e.tensor_scalar(out=ss_t[:, :], in0=ss_t[:, :], scalar1=1.0 / D,
                                                                      scalar2=EPS, op0=ALU.mult, op1=ALU.add),
                          reads=[ss_t], writes=[ss_t])
                    kb.op("act", lambda e, ss_t=ss_t: e.activation(out=ss_t[:, :], in_=ss_t[:, :], func=AF.Sqrt),
                          reads=[ss_t], writes=[ss_t])
                    kb.op("dve", lambda e, ss_t=ss_t: e.reciprocal(out=ss_t[:, :], in_=ss_t[:, :]), reads=[ss_t], writes=[ss_t])
                    kb.op("dve", lambda e, x_t=x_t, xs_t=xs_t, ss_t=ss_t: e.tensor_scalar(
                        out=xs_t[:, :], in0=x_t[:, :], scalar1=ss_t[:, 0:1], scalar2=None, op0=ALU.mult),
                        reads=[x_t, ss_t], writes=[xs_t])
                    Asel, Ssel = (A1p, S1p) if past else (A1, None)
                    for k4 in range(KC // 4):
                        pt = kb.ps()
                        for q in range(4):
                            kc = k4 * 4 + q
                            kb.op("pe", lambda e, pt=pt, q=q, kc=kc, xs_t=xs_t: e.matmul(
                                pt[:, q * 128:(q + 1) * 128], lhsT=xs_t[:, kc * 128:(kc + 1) * 128], rhs=identb[:, :],
                                start=True, stop=True), reads=[xs_t, identb], writes=[pt])
                        for q in range(4):
                            kc = k4 * 4 + q
                            bias_ap = (S1p[:, kc:kc + 1] if past else modT[:, kc:kc + 1])
                            kb.op("act", lambda e, pt=pt, q=q, kc=kc, ti=ti, Asel=Asel, bias_ap=bias_ap: e.activation(
                                out=xnT[:, kc, ti * 128:(ti + 1) * 128], in_=pt[:, q * 128:(q + 1) * 128],
                                func=AF.Identity, scale=Asel[:, kc:kc + 1], bias=bias_ap),
                                reads=[pt, Asel, S1p if past else modT], writes=[xnT.ks[ti]])
                for (c0, ncol, kind) in chunks:
                    if past and kind in ("o", "aq"):
                        continue
                    w = wb[nw % 2]
                    nw += 1
                    kb.dma("pool", w[:, :, 0:ncol], wiv[:, :, c0:c0 + ncol], writes=[w])
                    if kind == "qk":
                        for sub, tb in [(a_, b_) for a_ in range(4) for b_ in range(TGB // 512)]:
                            pt = kb.ps()
                            for kc in range(KC):
                                kb.op("pe", lambda e, pt=pt, kc=kc, sub=sub, w=w, tb=tb: e.matmul(
                                    pt[:, :], lhsT=w[:, kc, sub * 128:(sub + 1) * 128], rhs=xnT[:, kc, tb * 512:(tb + 1) * 512],
                                    start=(kc == 0), stop=(kc == KC - 1)), reads=[w] + xnT.ks, writes=[pt])
                            s_t = stf[nst % 3]
                            ev = "act" if nst % 2 == 0 else "dve"
                            nst += 1
                            if ev == "act":
                                kb.op("act", lambda e, pt=pt, s_t=s_t: e.activation(out=s_t[:, :], in_=pt[:, :], func=AF.Copy),
                                      reads=[pt], writes=[s_t])
                            else:
                                kb.op("dve", lambda e, pt=pt, s_t=s_t: e.tensor_copy(out=s_t[:, :], in_=pt[:, :]),
                                      reads=[pt], writes=[s_t])
                            ch0 = c0 + sub * 128
                            kb.dma("sp", qkT_d[ch0:ch0 + 128, 4 + g * TGB + tb * 512:4 + g * TGB + (tb + 1) * 512], s_t[:, :], reads=[s_t])
                    else:
                        for ti in range(NTB):
                            tok0 = g * TGB + ti * 128
                            pt = kb.ps()
                            for kc in range(KC):
                                kb.op("pe", lambda e, pt=pt, kc=kc, ti=ti, w=w, ncol=ncol: e.matmul(
                                    pt[:, 0:ncol], lhsT=xnT[:, kc, ti * 128:(ti + 1) * 128], rhs=w[:, kc, 0:ncol],
                                    start=(kc == 0), stop=(kc == KC - 1)), reads=[w, xnT.ks[ti]], writes=[pt])
                            isb = kind in ("mv", "av")
                            s_t = (stb if isb else stf)[nst % 3]
                            ev = "act" if nst % 2 == 0 else "dve"
                            nst += 1
                            if ev == "act":
                                kb.op("act", lambda e, pt=pt, s_t=s_t, ncol=ncol: e.activation(out=s_t[:, 0:ncol], in_=pt[:, 0:ncol],
                                                                                              func=AF.Copy), reads=[pt], writes=[s_t])
                            else:
                                kb.op("dve", lambda e, pt=pt, s_t=s_t, ncol=ncol: e.tensor_copy(out=s_t[:, 0:ncol], in_=pt[:, 0:ncol]),
                                      reads=[pt], writes=[s_t])
                            if kind == "mv":
                                dst = mv_d[tok0:tok0 + 128, c0 - 2048:c0 - 2048 + 512]
                            elif kind == "o":
                                dst = o_d[tok0 - TOK:tok0 - TOK + 128, c0 - 4096:c0 - 4096 + 512]
                            elif kind == "if":
                                dst = if_d[tok0:tok0 + 128, 0:8]
                            elif kind == "aq":
                                dst = aq_d[tok0 - TOK:tok0 - TOK + 128, c0 - 6152:c0 - 6152 + 512]
                            elif kind == "ak":
                                dst = ak_d[tok0:tok0 + 128, c0 - 8200:c0 - 8200 + 512]
                            else:
                                dst = av_d[tok0:tok0 + 128, c0 - 10248:c0 - 10248 + 512]
                            kb.dma("sp", dst, s_t[:, 0:ncol], reads=[s_t])

        if upto <= 2:
            for nm, ap_, shp, dt in (("d_mod", mod_d, [192, 128], F32), ("d_qkT", qkT_d, [2048, 4 + T2], F32),
                                     ("d_if", if_d, [T2, 8], F32), ("d_ak", ak_d, [T2, 2048], F32),
                                     ("d_mv", mv_d, [T2, 2048], BF16), ("d_o", o_d, [TOK, 2048], F32)):
                o = nc.dram_tensor(nm, shp, dt, kind="ExternalOutput").ap()
                kb.dma("sp", o, ap_, )
            kb.barrier()
            return nc


        TRIu = cst[:, 0, :]
        CTS = cst[:, 1, :]
        CST = cst[:, 2, :]
        SEL127 = cst[:, 3, :]
        ONESF = cst[:, 4, :]

        def ev_copy(i, out, in_, rd, wr):
            if i % 2 == 0:
                kb.op("act", lambda e: e.activation(out=out, in_=in_, func=AF.Copy), reads=rd, writes=wr)
            else:
                kb.op("dve", lambda e: e.tensor_copy(out=out, in_=in_), reads=rd, writes=wr)

        with kb.phase():
            cw = kb.sb([128, 16, 4], F32)
            cb = kb.sb([128, 16], F32)
            kb.dma("sp", cw[:, :, :], conv_wT[:, :, :], writes=[cw])
            kb.dma("sp", cb[:, :], conv_bT[:, :], writes=[cb])
            ub = [kb.sb([128, 4 + T2], F32) for _ in range(2)]
            acc = kb.sb([128, T2], F32)
            ob = [kb.sb([128, T2], BF16) for _ in range(2)]
            for cc in range(16):
                u = ub[cc % 2]
                o_ = ob[cc % 2]
                kb.dma("sp", u[:, :], qkT_d[cc * 128:(cc + 1) * 128, :], writes=[u])
                kb.op("dve", lambda e, u=u, cc=cc: e.tensor_scalar(out=acc[:, :], in0=u[:, 1:1 + T2], scalar1=cw[:, cc, 0:1],
                                                                   scalar2=None, op0=ALU.mult), reads=[u, cw], writes=[acc])
                for j in range(1, 4):
                    kb.op("dve", lambda e, u=u, cc=cc, j=j: e.scalar_tensor_tensor(
                        out=acc[:, :], in0=u[:, 1 + j:1 + j + T2], scalar=cw[:, cc, j:j + 1], in1=acc[:, :],
                        op0=ALU.mult, op1=ALU.add), reads=[u, cw, acc], writes=[acc])
                kb.op("act", lambda e, o_=o_, cc=cc: e.activation(out=o_[:, :], in_=acc[:, :], func=AF.Silu, bias=cb[:, cc:cc + 1]),
                      reads=[acc, cb], writes=[o_])
                if cc < 8:
                    kb.op("dve", lambda e, o_=o_: e.tensor_scalar(out=o_[:, :], in0=o_[:, :], scalar1=0.0625, scalar2=None,
                                                                  op0=ALU.mult), reads=[o_], writes=[o_])
                kb.dma("sp", qkc_d[cc * 128:(cc + 1) * 128, :], o_[:, :], reads=[o_])

        with kb.phase():
            G = kb.sb([128, NCH, 8], F32)
            gb = kb.sb([128, NCH, 8], F32)
            LI = kb.sb([128, NCH, 4], F32)
            LF = kb.sb([128, NCH, 4], F32)
            Bc = kb.sb([128, NCH, 4], F32)
            U = kb.sb([128, NCH, 4], F32)
            kb.dma("sp", G[:, :, :], if_d.rearrange("(c p) g -> p c g", p=128), writes=[G])
            kb.dma("sp", gb[:, :, :], gbias[:, :, :], writes=[gb])
            kb.op("dve", lambda e: e.tensor_tensor(out=G[:, :, :], in0=G[:, :, :], in1=gb[:, :, :], op=ALU.add), reads=[G, gb], writes=[G])
            kb.op("act", lambda e: e.activation(out=G[:, :, :], in_=G[:, :, :], func=AF.Tanh, scale=1.0 / 15.0), reads=[G], writes=[G])
            kb.op("dve", lambda e: e.tensor_scalar(out=LI[:, :, :], in0=G[:, :, 0:4], scalar1=15.0, scalar2=None, op0=ALU.mult),
                  reads=[G], writes=[LI])
            kb.op("act", lambda e: e.activation(out=LF[:, :, :], in_=G[:, :, 4:8], func=AF.Exp, scale=-15.0), reads=[G], writes=[LF])
            kb.op("act", lambda e: e.activation(out=LF[:, :, :], in_=LF[:, :, :], func=AF.Ln, bias=1.0), reads=[LF], writes=[LF])
            kb.op("dve", lambda e: e.tensor_scalar(out=LF[:, :, :], in0=LF[:, :, :], scalar1=-1.0, scalar2=None, op0=ALU.mult),
                  reads=[LF], writes=[LF])
            pb_ = kb.ps()
            kb.op("pe", lambda e: e.matmul(pb_[:, 0:NCH * 4], lhsT=TRIu, rhs=LF[:, :, :].rearrange("p c h -> p (c h)"),
                                           start=True, stop=True), reads=[cst, LF], writes=[pb_])
            kb.op("dve", lambda e: e.tensor_copy(out=Bc[:, :, :].rearrange("p c h -> p (c h)"), in_=pb_[:, 0:NCH * 4]),
                  reads=[pb_], writes=[Bc])
            kb.op("dve", lambda e: e.tensor_tensor(out=U[:, :, :], in0=LI[:, :, :], in1=Bc[:, :, :], op=ALU.subtract),
                  reads=[LI, Bc], writes=[U])

            mnwb = kb.sb([128, 2048], F32)
            kb.dma("sp", mnwb[:, :], mnw.partition_broadcast(128), writes=[mnwb])
            qTh = kb.sb([128, 2, T2], BF16)
            kTh = kb.sb([128, 2, T2], BF16)
            vh = kb.sb([128, NCH, 512], BF16)
            Cst = kb.sb([128, 2, 512], F32)
            Cb = kb.sb([128, 2, 512], BF16)
            nst_ = kb.sb([128, 2], F32)
            nb_ = kb.sb([128, 2], BF16)
            mcol = kb.sb([128, 1], F32)
            dg = [kb.sb([128, 128], F32) for _ in range(2)]
            Um = kb.sb([128, 128], F32)
            Rm = kb.sb([128, 128], F32)
            ET = kb.sb([128, 128], F32)
            PT = kb.sb([128, 128], BF16)
            sm = [kb.sb([128, 16], F32) for _ in range(2)]
            mr = kb.sb([128, 2], F32)
            kw = kb.sb([128, 256], BF16)
            tmpn = kb.sb([128, 512], F32)
            num = kb.sb([128, 512], F32)
            hh_ = kb.sb([128, 512], F32)
            junk5 = kb.sb([128, 512], BF16)
            ot = kb.sb([128, 512], F32)
            hout = [kb.sb([128, 512], BF16) for _ in range(2)]
            nit = 0
            for h in range(4):
                kb.dma("sp", qTh[:, :, :], qkc_d[h * 256:(h + 1) * 256, :].rearrange("(j p) t -> p j t", p=128), writes=[qTh])
                kb.dma("sp", kTh[:, :, :], qkc_d[1024 + h * 256:1024 + (h + 1) * 256, :].rearrange("(j p) t -> p j t", p=128), writes=[kTh])
                kb.dma("sp", vh[:, :, :], mv_d[:, h * 512:(h + 1) * 512].rearrange("(c p) v -> p c v", p=128), writes=[vh])
                kb.op("dve", lambda e: e.memset(Cst[:, :, :], 0.0), writes=[Cst])
                kb.op("dve", lambda e: e.memset(Cb[:, :, :], 0.0), writes=[Cb])
                kb.op("dve", lambda e: e.memset(nst_[:, :], 0.0), writes=[nst_])
                kb.op("dve", lambda e: e.memset(nb_[:, :], 0.0), writes=[nb_])
                kb.op("dve", lambda e: e.memset(mcol[:, :], 0.0), writes=[mcol])
                for c in range(NCH):
                    own = c >= NCH // 2
                    if c == NCH // 2:
                        kb.op("dve", lambda e: e.tensor_scalar(out=Cst[:, :, :], in0=Cst[:, :, :], scalar1=flg[:, 0:1], scalar2=None, op0=ALU.mult),
                              reads=[Cst, flg], writes=[Cst])
                        kb.op("dve", lambda e: e.tensor_copy(out=Cb[:, :, :], in_=Cst[:, :, :]), reads=[Cst], writes=[Cb])
                        kb.op("dve", lambda e: e.tensor_scalar(out=nst_[:, :], in0=nst_[:, :], scalar1=flg[:, 0:1], scalar2=None, op0=ALU.mult),
                              reads=[nst_, flg], writes=[nst_])
                        kb.op("dve", lambda e: e.tensor_copy(out=nb_[:, :], in_=nst_[:, :]), reads=[nst_], writes=[nb_])
                        kb.op("dve", lambda e: e.tensor_scalar(out=mcol[:, :], in0=mcol[:, :], scalar1=flg[:, 0:1], scalar2=None, op0=ALU.mult),
                              reads=[mcol, flg], writes=[mcol])
                    cs_ = slice(c * 128, (c + 1) * 128)
                    ucol = U[:, c, h:h + 1]
                    bcol = Bc[:, c, h:h + 1]
                    s0 = sm[nit % 2]
                    nit += 1
                    d0 = dg[0]
                    kb.op("dve", lambda e, d0=d0, ucol=ucol: e.tensor_scalar(out=d0[:, :], in0=identf[:, :], scalar1=ucol, scalar2=None, op0=ALU.mult),
                          reads=[identf, U], writes=[d0])
                    pu = kb.ps()
                    kb.op("pe", lambda e, pu=pu, d0=d0: e.matmul(pu[:, 0:128], lhsT=ONESF, rhs=d0[:, :], start=True, stop=True),
                          reads=[cst, d0], writes=[pu])
                    kb.op("dve", lambda e, pu=pu: e.tensor_tensor(out=Um[:, :], in0=pu[:, 0:128], in1=CTS, op=ALU.add), reads=[pu, cst], writes=[Um])
                    kb.op("dve", lambda e, s0=s0: e.tensor_reduce(out=s0[:, 0:1], in_=Um[:, :], axis=AX.X, op=ALU.max), reads=[Um], writes=[s0])
                    kb.op("dve", lambda e, s0=s0: e.tensor_tensor(out=mr[:, 1:2], in0=s0[:, 0:1], in1=mcol[:, 0:1], op=ALU.max), reads=[s0, mcol], writes=[mr])
                    kb.op("dve", lambda e, bcol=bcol: e.tensor_tensor(out=mr[:, 0:1], in0=mr[:, 1:2], in1=bcol, op=ALU.add), reads=[mr, Bc], writes=[mr])
                    pm = kb.ps()
                    kb.op("pe", lambda e, pm=pm: e.matmul(pm[:, 0:2], lhsT=SEL127, rhs=mr[:, 0:2], start=True, stop=True), reads=[cst, mr], writes=[pm])
                    kb.op("dve", lambda e, pm=pm, s0=s0: e.tensor_copy(out=s0[:, 1:3], in_=pm[:, 0:2]), reads=[pm], writes=[s0])
                    if own:
                        d1 = dg[1]
                        kb.op("dve", lambda e, d1=d1: e.tensor_scalar(out=d1[:, :], in0=identf[:, :], scalar1=mr[:, 1:2], scalar2=None, op0=ALU.mult),
                              reads=[identf, mr], writes=[d1])
                        pr = kb.ps()
                        kb.op("pe", lambda e, pr=pr, d1=d1: e.matmul(pr[:, 0:128], lhsT=ONESF, rhs=d1[:, :], start=True, stop=True),
                              reads=[cst, d1], writes=[pr])
                        kb.op("dve", lambda e, pr=pr: e.tensor_tensor(out=Rm[:, :], in0=pr[:, 0:128], in1=CST, op=ALU.add), reads=[pr, cst], writes=[Rm])
                        kb.op("act", lambda e, ucol=ucol: e.activation(out=ET[:, :], in_=Rm[:, :], func=AF.Exp, scale=-1.0, bias=ucol),
                              reads=[Rm, U], writes=[ET])
                        pS = kb.ps()
                        for j in range(2):
                            kb.op("pe", lambda e, pS=pS, j=j, cs_=cs_: e.matmul(pS[:, 0:128], lhsT=kTh[:, j, cs_], rhs=qTh[:, j, cs_],
                                                                              start=(j == 0), stop=(j == 1)), reads=[kTh, qTh], writes=[pS])
                        kb.op("dve", lambda e, pS=pS: e.tensor_tensor(out=PT[:, :], in0=pS[:, 0:128], in1=ET[:, :], op=ALU.mult),
                              reads=[pS, ET], writes=[PT])
                        pnum = kb.ps()
                        kb.op("pe", lambda e, pnum=pnum, c=c: e.matmul(pnum[:, :], lhsT=PT[:, :], rhs=vh[:, c, :], start=True, stop=True),
                              reads=[PT, vh], writes=[pnum])
                        pden = kb.ps()
                        kb.op("pe", lambda e, pden=pden: e.matmul(pden[:, 0:1], lhsT=PT[:, :], rhs=onesb[:, 0:1], start=True, stop=True),
                              reads=[PT, onesb], writes=[pden])
                        pqc = kb.ps()
                        for j in range(2):
                            kb.op("pe", lambda e, pqc=pqc, j=j, cs_=cs_: e.matmul(pqc[:, :], lhsT=qTh[:, j, cs_], rhs=Cb[:, j, :],
                                                                                start=(j == 0), stop=(j == 1)), reads=[qTh, Cb], writes=[pqc])
                        for j in range(2):
                            kb.op("pe", lambda e, pden=pden, j=j, cs_=cs_: e.matmul(pden[:, 1:2], lhsT=qTh[:, j, cs_], rhs=nb_[:, j:j + 1],
                                                                                  start=(j == 0), stop=(j == 1)), reads=[qTh, nb_], writes=[pden])
                        kb.op("dve", lambda e, s0=s0: e.tensor_tensor(out=s0[:, 3:4], in0=mcol[:, 0:1], in1=mr[:, 1:2], op=ALU.subtract),
                              reads=[mcol, mr], writes=[s0])
                        kb.op("act", lambda e, s0=s0: e.activation(out=s0[:, 3:4], in_=s0[:, 3:4], func=AF.Exp), reads=[s0], writes=[s0])
                        kb.op("act", lambda e, pqc=pqc, s0=s0: e.activation(out=tmpn[:, :], in_=pqc[:, :], func=AF.Copy, scale=s0[:, 3:4]),
                              reads=[pqc, s0], writes=[tmpn])
                        kb.op("dve", lambda e, pnum=pnum: e.tensor_tensor(out=num[:, :], in0=pnum[:, :], in1=tmpn[:, :], op=ALU.add),
                              reads=[pnum, tmpn], writes=[num])
                        kb.op("dve", lambda e, pden=pden, s0=s0: e.tensor_copy(out=s0[:, 9:11], in_=pden[:, 0:2]), reads=[pden], writes=[s0])
                        kb.op("dve", lambda e, s0=s0: e.scalar_tensor_tensor(out=s0[:, 4:5], in0=s0[:, 10:11], scalar=s0[:, 3:4], in1=s0[:, 9:10],
                                                                             op0=ALU.mult, op1=ALU.add), reads=[s0], writes=[s0])
                        kb.op("dve", lambda e, s0=s0: e.tensor_scalar(out=s0[:, 5:6], in0=s0[:, 4:5], scalar1=-1.0, scalar2=None, op0=ALU.mult),
                              reads=[s0], writes=[s0])
                        kb.op("dve", lambda e, s0=s0: e.tensor_tensor(out=s0[:, 5:6], in0=s0[:, 5:6], in1=s0[:, 4:5], op=ALU.max),
                              reads=[s0], writes=[s0])
                        kb.op("act", lambda e, s0=s0: e.activation(out=s0[:, 6:7], in_=mr[:, 0:1], func=AF.Exp, scale=-1.0), reads=[mr], writes=[s0])
                        kb.op("dve", lambda e, s0=s0: e.tensor_tensor(out=s0[:, 7:8], in0=s0[:, 5:6], in1=s0[:, 6:7], op=ALU.max), reads=[s0], writes=[s0])
                        kb.op("dve", lambda e, s0=s0: e.reciprocal(out=s0[:, 8:9], in_=s0[:, 7:8]), reads=[s0], writes=[s0])
                        kb.op("dve", lambda e, s0=s0: e.tensor_scalar(out=hh_[:, :], in0=num[:, :], scalar1=s0[:, 8:9], scalar2=None, op0=ALU.mult),
                              reads=[num, s0], writes=[hh_])
                        kb.op("act", lambda e, s0=s0: e.activation(out=junk5[:, :], in_=hh_[:, :], func=AF.Square, accum_out=s0[:, 11:12]),
                              reads=[hh_], writes=[junk5, s0])
                        kb.op("dve", lambda e, s0=s0: e.tensor_scalar(out=s0[:, 11:12], in0=s0[:, 11:12], scalar1=1.0 / 512, scalar2=EPS,
                                                                      op0=ALU.mult, op1=ALU.add), reads=[s0], writes=[s0])
                        kb.op("act", lambda e, s0=s0: e.activation(out=s0[:, 11:12], in_=s0[:, 11:12], func=AF.Sqrt), reads=[s0], writes=[s0])
                        kb.op("dve", lambda e, s0=s0: e.reciprocal(out=s0[:, 12:13], in_=s0[:, 11:12]), reads=[s0], writes=[s0])
                        tok0 = (c - NCH // 2) * 128
                        kb.dma("sp", ot[:, :], o_d[tok0:tok0 + 128, h * 512:(h + 1) * 512], writes=[ot])
                        kb.op("act", lambda e: e.activation(out=ot[:, :], in_=ot[:, :], func=AF.Sigmoid), reads=[ot], writes=[ot])
                        kb.op("dve", lambda e, h=h: e.tensor_tensor(out=ot[:, :], in0=ot[:, :], in1=mnwb[:, h * 512:(h + 1) * 512], op=ALU.mult),
                              reads=[ot, mnwb], writes=[ot])
                        ho = hout[c % 2]
                        kb.op("dve", lambda e, s0=s0, ho=ho: e.scalar_tensor_tensor(out=ho[:, :], in0=hh_[:, :], scalar=s0[:, 12:13], in1=ot[:, :],
                                                                                   op0=ALU.mult, op1=ALU.mult), reads=[hh_, s0, ot], writes=[ho])
                        kb.dma("sp", hcat_d[tok0:tok0 + 128, h * 512:(h + 1) * 512], ho[:, :], reads=[ho])
                    kb.op("dve", lambda e, s0=s0, ucol=ucol: e.tensor_tensor(out=s0[:, 13:14], in0=ucol, in1=s0[:, 2:3], op=ALU.subtract),
                          reads=[U, s0], writes=[s0])
                    kb.op("dve", lambda e, s0=s0: e.tensor_tensor(out=s0[:, 14:15], in0=mcol[:, 0:1], in1=s0[:, 2:3], op=ALU.subtract),
                          reads=[mcol, s0], writes=[s0])
                    kb.op("act", lambda e, s0=s0: e.activation(out=s0[:, 13:15], in_=s0[:, 13:15], func=AF.Exp), reads=[s0], writes=[s0])
                    pk = kb.ps()
                    for j in range(2):
                        kb.op("pe", lambda e, pk=pk, j=j, cs_=cs_: e.matmul(pk[:, j * 128:(j + 1) * 128], lhsT=kTh[:, j, cs_], rhs=identb[:, :],
                                                                          start=True, stop=True), reads=[kTh, identb], writes=[pk])
                    kb.op("act", lambda e, pk=pk, s0=s0: e.activation(out=kw[:, :], in_=pk[:, 0:256], func=AF.Copy, scale=s0[:, 13:14]),
                          reads=[pk, s0], writes=[kw])
                    pn = kb.ps()
                    for j in range(2):
                        pkv = kb.ps()
                        kb.op("pe", lambda e, pkv=pkv, j=j, c=c: e.matmul(pkv[:, :], lhsT=kw[:, j * 128:(j + 1) * 128], rhs=vh[:, c, :],
                                                                         start=True, stop=True), reads=[kw, vh], writes=[pkv])
                        kb.op("pe", lambda e, pn=pn, j=j: e.matmul(pn[:, j:j + 1], lhsT=kw[:, j * 128:(j + 1) * 128], rhs=onesb[:, 0:1],
                                                                  start=True, stop=True), reads=[kw, onesb], writes=[pn])
                        kb.op("dve", lambda e, pkv=pkv, j=j, s0=s0: e.scalar_tensor_tensor(out=Cst[:, j, :], in0=Cst[:, j, :], scalar=s0[:, 14:15],
                                                                                          in1=pkv[:, :], op0=ALU.mult, op1=ALU.add),
                              reads=[Cst, s0, pkv], writes=[Cst])
                    kb.op("act", lambda e: e.activation(out=Cb[:, :, :], in_=Cst[:, :, :], func=AF.Copy), reads=[Cst], writes=[Cb])
                    kb.op("dve", lambda e, pn=pn, s0=s0: e.scalar_tensor_tensor(out=nst_[:, :], in0=nst_[:, :], scalar=s0[:, 14:15], in1=pn[:, 0:2],
                                                                               op0=ALU.mult, op1=ALU.add), reads=[nst_, s0, pn], writes=[nst_])
                    kb.op("dve", lambda e: e.tensor_copy(out=nb_[:, :], in_=nst_[:, :]), reads=[nst_], writes=[nb_])
                    kb.op("dve", lambda e, s0=s0: e.tensor_copy(out=mcol[:, :], in_=s0[:, 1:2]), reads=[s0], writes=[mcol])


        NTq = TOK // 128
        NTk = T2 // 128
        SCL = 128 ** -0.5
        with kb.phase():
            kb.psum_rot = 6
            kb.pnext = 0
            pO, pL = kb.psum[6], kb.psum[7]
            cst_ = kb.sb([128, NTk, 128], F32)
            kb.dma("sp", cst_[:, :, :], cs_tab.rearrange("(i p) d -> p i d", p=128), writes=[cst_])
            pbt = kb.sb([128, NTq, NBP], F32)
            kb.dma("sp", pbt[:, :, :], pb_tab.rearrange("(i p) n -> p i n", p=128), writes=[pbt])
            ohb = kb.sb([NBP, NBP * 128], BF16)
            kb.dma("pool", ohb[:, :], oneh[:, :], writes=[ohb])
            tri01 = kb.sb([128, 128], BF16)
            kb.op("dve", lambda e: e.tensor_copy(out=tri01[:, :], in_=TRIu), reads=[cst], writes=[tri01])
            xq = kb.sb([128, NTq, 128], F32)
            xk = kb.sb([128, NTk, 128], F32)
            rq = kb.sb([128, NTk, 128], F32)
            t1 = kb.sb([128, NTk, 64], F32)
            qT = kb.sb([128, TOK], BF16)
            kT = kb.sb([128, T2], BF16)
            vk = kb.sb([128, NTk, 132], BF16)
            kb.op("dve", lambda e: e.memset(vk[:, :, :], 1.0), writes=[vk])
            kmT = kb.sb([128, NBP], BF16)
            kms = kb.sb([128, NBP], F32)
            gm = kb.sb([128, NBP], F32)
            t8 = kb.sb([128, 8], F32)
            selb = kb.sb([128, NBP], BF16)
            selT = kb.sb([NBP, TOK], BF16)
            PTm = [kb.sb([128, 512], BF16) for _ in range(3)]
            lrec = [kb.sb([128, 2], F32) for _ in range(2)]
            gmA = kb.sb([128, NTq, NBP], F32)
            t8A = kb.sb([128, NTq, 8], F32)
            selbA = kb.sb([128, NTq, NBP], BF16)
            st_m = {"rot": 0}
            pOs = [pO, pL]
            oo = [kb.sb([128, 128], BF16) for _ in range(2)]

            def rotary(X, R, nt):
                c_, s_ = cst_[:, NTk - nt:NTk, 0:64], cst_[:, NTk - nt:NTk, 64:128]
                x1, x2 = X[:, 0:nt, 0:64], X[:, 0:nt, 64:128]
                T1 = t1[:, 0:nt, :]
                kb.op("dve", lambda e: e.tensor_tensor(out=R[:, 0:nt, 0:64], in0=x1, in1=c_, op=ALU.mult), reads=[X, cst_], writes=[R])
                kb.op("dve", lambda e: e.tensor_tensor(out=T1, in0=x2, in1=s_, op=ALU.mult), reads=[X, cst_], writes=[t1])
                kb.op("dve", lambda e: e.tensor_tensor(out=R[:, 0:nt, 0:64], in0=R[:, 0:nt, 0:64], in1=T1, op=ALU.subtract), reads=[R, t1], writes=[R])
                kb.op("dve", lambda e: e.tensor_tensor(out=R[:, 0:nt, 64:128], in0=x2, in1=c_, op=ALU.mult), reads=[X, cst_], writes=[R])
                kb.op("dve", lambda e: e.tensor_tensor(out=T1, in0=x1, in1=s_, op=ALU.mult), reads=[X, cst_, t1], writes=[t1])
                kb.op("dve", lambda e: e.tensor_tensor(out=R[:, 0:nt, 64:128], in0=R[:, 0:nt, 64:128], in1=T1, op=ALU.add), reads=[R, t1], writes=[R])

            def transp(R, nt, dst):
                for i4 in range(nt // 4):
                    pt = kb.ps()
                    for q in range(4):
                        i = i4 * 4 + q
                        kb.op("pe", lambda e, pt=pt, q=q, i=i: e.matmul(pt[:, q * 128:(q + 1) * 128], lhsT=R[:, i, :], rhs=identf[:, :],
                                                                       start=True, stop=True), reads=[R, identf], writes=[pt])
                    ev_copy(i4, dst[:, i4 * 512:(i4 + 1) * 512], pt[:, :], [pt], [dst])

            nev = 0
            for h in range(16):
                hs = slice(h * 128, (h + 1) * 128)
                kb.dma("sp", xq[:, :, :], aq_d[:, hs].rearrange("(i p) d -> p i d", p=128), writes=[xq])
                kb.dma("sp", xk[:, :, :], ak_d[:, hs].rearrange("(i p) d -> p i d", p=128), writes=[xk])
                kb.dma("sp", vk[:, :, 0:128], av_d[:, hs].rearrange("(i p) d -> p i d", p=128), writes=[vk])
                rotary(xq, rq, NTq)
                transp(rq, NTq, qT)
                rotary(xk, rq, NTk)
                transp(rq, NTk, kT)
                kb.op("dve", lambda e: e.memset(kms[:, :], 0.0), writes=[kms])
                kb.op("dve", lambda e: e.tensor_reduce(out=kms[:, 0:NB], in_=kT[:, :].rearrange("p (n k) -> p n k", k=256), axis=AX.X, op=ALU.add),
                      reads=[kT], writes=[kms])
                kb.op("dve", lambda e: e.tensor_scalar(out=kmT[:, :], in0=kms[:, :], scalar1=1.0 / 256, scalar2=None, op0=ALU.mult),
                      reads=[kms], writes=[kmT])
                pg = kb.ps()
                for i in range(NTq):
                    kb.op("pe", lambda e, i=i: e.matmul(pg[:, i * NBP:(i + 1) * NBP], lhsT=qT[:, i * 128:(i + 1) * 128], rhs=kmT[:, :], start=True, stop=True),
                          reads=[qT, kmT], writes=[pg])
                kb.op("dve", lambda e: e.tensor_tensor(out=gmA[:, :, :].rearrange("p a b -> p (a b)"), in0=pg[:, 0:NTq * NBP],
                                                       in1=pbt[:, :, :].rearrange("p a b -> p (a b)"), op=ALU.add), reads=[pg, pbt], writes=[gmA])
                for i in range(NTq):
                    kb.op("dve", lambda e, i=i: e.max(out=t8A[:, i, :], in_=gmA[:, i, :]), reads=[gmA], writes=[t8A])
                kb.op("dve", lambda e: e.tensor_scalar(out=t8A[:, :, 2:3], in0=t8A[:, :, 2:3], scalar1=-1.0e29, scalar2=None, op0=ALU.max), reads=[t8A], writes=[t8A])
                for i in range(NTq):
                    kb.op("dve", lambda e, i=i: e.tensor_scalar(out=gmA[:, i, :], in0=gmA[:, i, :], scalar1=t8A[:, i, 2:3], scalar2=None, op0=ALU.is_ge),
                          reads=[gmA, t8A], writes=[gmA])
                kb.op("dve", lambda e: e.tensor_scalar(out=selbA[:, :, :], in0=gmA[:, :, :], scalar1=30000.0, scalar2=-30000.0, op0=ALU.mult, op1=ALU.add),
                      reads=[gmA], writes=[selbA])
                for i4 in range(NTq // 4):
                    pt = kb.ps()
                    for q in range(4):
                        i = i4 * 4 + q
                        kb.op("pe", lambda e, pt=pt, q=q, i=i: e.matmul(pt[0:NBP, q * 128:(q + 1) * 128], lhsT=selbA[:, i, :], rhs=identb[:, :], start=True, stop=True),
                              reads=[selbA, identb], writes=[pt])
                    kb.op("act", lambda e, pt=pt, i4=i4: e.activation(out=selT[:, i4 * 512:(i4 + 1) * 512], in_=pt[0:NBP, 0:512], func=AF.Copy), reads=[pt], writes=[selT])
                for jb in range(TOK // 256):
                    q2 = slice(jb * 256, (jb + 1) * 256)
                    gb_ = NB // 2 + jb
                    started = [False, False]

                    def score_past(n):
                        pS = kb.ps()
                        for hf in range(2):
                            ks_ = slice(n * 256 + hf * 128, n * 256 + hf * 128 + 128)
                            kb.op("pe", lambda e, pS=pS, hf=hf, ks_=ks_: e.matmul(pS[:, hf * 256:(hf + 1) * 256], lhsT=kT[:, ks_], rhs=qT[:, q2], start=True, stop=False),
                                  reads=[kT, qT], writes=[pS])
                            kb.op("pe", lambda e, pS=pS, hf=hf, n=n: e.matmul(pS[:, hf * 256:(hf + 1) * 256], lhsT=ohb[:, n * 128:(n + 1) * 128], rhs=selT[:, q2], start=False, stop=True),
                                  reads=[ohb, selT], writes=[pS])
                        P_ = PTm[st_m["rot"] % 3]
                        st_m["rot"] += 1
                        kb.op("act", lambda e, pS=pS, P_=P_: e.activation(out=P_[:, :], in_=pS[:, :], func=AF.Exp, scale=SCL), reads=[pS], writes=[P_])
                        return [(P_, hf * 256 + qt * 128, n * 2 + hf, qt) for hf in range(2) for qt in range(2)]

                    def score_own(qt, hf, tri):
                        pS = kb.ps()
                        ks_ = slice(gb_ * 256 + hf * 128, gb_ * 256 + hf * 128 + 128)
                        qs_ = slice(jb * 256 + qt * 128, jb * 256 + (qt + 1) * 128)
                        kb.op("pe", lambda e, pS=pS: e.matmul(pS[:, 0:128], lhsT=kT[:, ks_], rhs=qT[:, qs_], start=True, stop=True), reads=[kT, qT], writes=[pS])
                        P_ = PTm[st_m["rot"] % 3]
                        st_m["rot"] += 1
                        kb.op("act", lambda e, pS=pS, P_=P_: e.activation(out=P_[:, 0:128], in_=pS[:, 0:128], func=AF.Exp, scale=SCL), reads=[pS], writes=[P_])
                        if tri:
                            kb.op("dve", lambda e, P_=P_: e.tensor_tensor(out=P_[:, 0:128], in0=P_[:, 0:128], in1=tri01[:, :], op=ALU.mult), reads=[P_, tri01], writes=[P_])
                        return [(P_, 0, gb_ * 2 + hf, qt)]

                    def pv(lst, lastflags):
                        for (P_, c0_, ki, qt), last in zip(lst, lastflags):
                            first = not started[qt]
                            started[qt] = True
                            kb.op("pe", lambda e, P_=P_, c0_=c0_, ki=ki, qt=qt, first=first, last=last: e.matmul(
                                pOs[qt][:, 0:129], lhsT=P_[:, c0_:c0_ + 128], rhs=vk[:, ki, 0:129], start=first, stop=last), reads=[P_, vk], writes=[pOs[qt]])

                    prev = None
                    for n in range(gb_):
                        cur = score_past(n)
                        if prev is not None:
                            pv(prev, [False] * 4)
                        prev = cur
                    o0 = score_own(0, 0, True)
                    pv(prev, [False] * 4)
                    o1 = score_own(1, 0, False)
                    pv(o0, [True])
                    o2 = score_own(1, 1, True)
                    pv(o1, [False])
                    pv(o2, [True])
                    for qt in range(2):
                        i = jb * 2 + qt
                        lr = lrec[i % 2]
                        kb.op("dve", lambda e, qt=qt, lr=lr: e.reciprocal(out=lr[:, 0:1], in_=pOs[qt][:, 128:129]), reads=[pOs[qt]], writes=[lr])
                        o_ = oo[i % 2]
                        kb.op("act", lambda e, o_=o_, qt=qt, lr=lr: e.activation(out=o_[:, :], in_=pOs[qt][:, 0:128], func=AF.Copy, scale=lr[:, 0:1]), reads=[pOs[qt], lr], writes=[o_])
                        kb.dma("sp", hcat_d[i * 128:(i + 1) * 128, 2048 + h * 128:2048 + (h + 1) * 128], o_[:, :], reads=[o_])
            kb.psum_rot = 8
            kb.pnext = 0

        modflat = mod_d.rearrange("a b -> (a b)")

        def norm_T(x_t, junk_, xs_t, ssc, Acol, bias_fn, dstT, ti):
            kb.op("act", lambda e: e.activation(out=junk_[:, :], in_=x_t[:, :], func=AF.Square, accum_out=ssc[:, 0:1]), reads=[x_t], writes=[junk_, ssc])
            kb.op("dve", lambda e: e.tensor_scalar(out=ssc[:, 0:1], in0=ssc[:, 0:1], scalar1=1.0 / D, scalar2=EPS, op0=ALU.mult, op1=ALU.add), reads=[ssc], writes=[ssc])
            kb.op("act", lambda e: e.activation(out=ssc[:, 0:1], in_=ssc[:, 0:1], func=AF.Sqrt), reads=[ssc], writes=[ssc])
            kb.op("dve", lambda e: e.reciprocal(out=ssc[:, 1:2], in_=ssc[:, 0:1]), reads=[ssc], writes=[ssc])
            kb.op("dve", lambda e: e.tensor_scalar(out=xs_t[:, :], in0=x_t[:, :], scalar1=ssc[:, 1:2], scalar2=None, op0=ALU.mult), reads=[x_t, ssc], writes=[xs_t])
            for k4 in range(KC // 4):
                pt = kb.ps()
                for q in range(4):
                    kc = k4 * 4 + q
                    kb.op("pe", lambda e, pt=pt, q=q, kc=kc: e.matmul(pt[:, q * 128:(q + 1) * 128], lhsT=xs_t[:, kc * 128:(kc + 1) * 128], rhs=identb[:, :],
                                                                     start=True, stop=True), reads=[xs_t, identb], writes=[pt])
                for q in range(4):
                    kc = k4 * 4 + q
                    if Acol is None:
                        ev_copy(q, dstT[:, kc, ti * 128:(ti + 1) * 128], pt[:, q * 128:(q + 1) * 128], [pt], [dstT.ks[ti]])
                    else:
                        kb.op("act", lambda e, pt=pt, q=q, kc=kc: e.activation(out=dstT[:, kc, ti * 128:(ti + 1) * 128], in_=pt[:, q * 128:(q + 1) * 128],
                                                                              func=AF.Identity, scale=Acol[:, kc:kc + 1], bias=bias_fn(kc)),
                              reads=[pt, Acol, modT], writes=[dstT.ks[ti]])

        wov = w_out.rearrange("(kc p) n -> p kc n", p=128)
        with kb.phase():
            g1b = kb.sb([128, D], F32)
            kb.dma("sp", g1b[:, :], modflat[2 * D:3 * D].partition_broadcast(128), writes=[g1b])
            hT = kb.sb([128, KC, TG], BF16, n=4)
            hb = [kb.sb([128, D], BF16) for _ in range(2)]
            xg = [kb.sb([128, D], F32) for _ in range(4)]
            wb = [kb.sb([128, KC, 512], BF16) for _ in range(1)]
            tmpd = [kb.sb([128, 512], F32) for _ in range(2)]
            junk = kb.sb([128, D], BF16)
            xs2 = kb.sb([128, D], BF16)
            ssc = kb.sb([128, 2], F32)
            x2T = hT
            nw = 0
            for g in range(TOK // TG):
                for ti in range(4):
                    tok0 = g * TG + ti * 128
                    hb_ = hb[ti % 2]
                    kb.dma("sp", hb_[:, :], hcat_d[tok0:tok0 + 128, :], writes=[hb_])
                    kb.dma("sp", xg[ti][:, :], x_own[tok0:tok0 + 128, :], writes=[xg[ti]])
                    for k4 in range(KC // 4):
                        pt = kb.ps()
                        for q in range(4):
                            kc = k4 * 4 + q
                            kb.op("pe", lambda e, pt=pt, q=q, kc=kc, hb_=hb_: e.matmul(pt[:, q * 128:(q + 1) * 128], lhsT=hb_[:, kc * 128:(kc + 1) * 128],
                                                                                     rhs=identb[:, :], start=True, stop=True), reads=[hb_, identb], writes=[pt])
                        for q in range(4):
                            kc = k4 * 4 + q
                            ev_copy(q, hT[:, kc, ti * 128:(ti + 1) * 128], pt[:, q * 128:(q + 1) * 128], [pt], [hT.ks[ti]])
                for dc in range(8):
                    w = wb[0]
                    nw += 1
                    kb.dma("pool", w[:, :, :], wov[:, :, dc * 512:(dc + 1) * 512], writes=[w])
                    for ti in range(4):
                        pt = kb.ps()
                        for kc in range(KC):
                            kb.op("pe", lambda e, pt=pt, kc=kc, ti=ti, w=w: e.matmul(pt[:, :], lhsT=hT[:, kc, ti * 128:(ti + 1) * 128], rhs=w[:, kc, :],
                                                                                    start=(kc == 0), stop=(kc == KC - 1)), reads=[w, hT.ks[ti]], writes=[pt])
                        td = tmpd[ti % 2]
                        ds_ = slice(dc * 512, (dc + 1) * 512)
                        kb.op("dve", lambda e, pt=pt, td=td, ds_=ds_: e.tensor_tensor(out=td[:, :], in0=pt[:, :], in1=g1b[:, ds_], op=ALU.mult), reads=[pt, g1b], writes=[td])
                        kb.op("pool", lambda e, td=td, ds_=ds_, ti=ti: e.tensor_tensor(out=xg[ti][:, ds_], in0=xg[ti][:, ds_], in1=td[:, :], op=ALU.add),
                              reads=[xg[ti], td], writes=[xg[ti]])
                for ti in range(4):
                    tok0 = g * TG + ti * 128
                    kb.dma("sp", x2_d[tok0:tok0 + 128, :], xg[ti][:, :], reads=[xg[ti]])
                    norm_T(xg[ti], junk, xs2, ssc, A2, lambda kc: modT[:, 96 + kc:97 + kc], x2T, ti)
                kb.dma("sp", xn2T_d[:, :, g * TG:(g + 1) * TG], x2T[:, :, :], reads=x2T.ks)

        wqv = wq.rearrange("(kc p) n -> p kc n", p=128)
        uTv = uT.rearrange("(kc p) n -> p kc n", p=128)
        NGP = TOK // TG
        with kb.phase():
            xT = kb.sb([128, KC, TG], BF16)
            skb = kb.sb([128, 16, NK], BF16)
            kb.dma("pool", skb[:, :, :], skT[:, :, :], writes=[skb])
            wqb = [kb.sb([128, KC, 128], BF16) for _ in range(2)]
            qsb = [kb.sb([128, TG], BF16) for _ in range(2)]
            S2 = kb.sb([128, 4, 8, NK], F32)
            AA = kb.sb([128, 4, 8, NK], F32)
            NT = kb.sb([128, 4, 8, NK], F32)
            s1b = kb.sb([128, NK], F32)
            T1 = kb.sb([128, 16], F32)
            T2_ = kb.sb([128, 16], F32)
            cand = kb.sb([128, 256], F32)
            candb = kb.sb([128, 256], F32)
            c16 = kb.sb([128, 16], F32)
            e16 = kb.sb([128, 16], F32)
            zc = kb.sb([128, 4], F32)
            nwq = 0
            for g in range(NGP):
                kb.dma("sp", xT[:, :, :], xn2T_d[:, :, g * TG:(g + 1) * TG], writes=[xT])
                for hp in range(16):
                    h, half = hp // 2, hp % 2
                    w = wqb[nwq % 2]
                    qs_ = qsb[nwq % 2]
                    nwq += 1
                    kb.dma("pool", w[:, :, :], wqv[:, :, hp * 128:(hp + 1) * 128], writes=[w])
                    pq = kb.ps()
                    for kc in range(KC):
                        kb.op("pe", lambda e, pq=pq, kc=kc, w=w: e.matmul(pq[:, :], lhsT=w[:, kc, :], rhs=xT[:, kc, :], start=(kc == 0), stop=(kc == KC - 1)),
                              reads=[w, xT], writes=[pq])
                    kb.op("act", lambda e, pq=pq, qs_=qs_: e.activation(out=qs_[:, :], in_=pq[:, :], func=AF.Copy), reads=[pq], writes=[qs_])
                    for ti in range(4):
                        psc = kb.ps()
                        kb.op("pe", lambda e, psc=psc, ti=ti, hp=hp, qs_=qs_: e.matmul(psc[:, 0:NK], lhsT=qs_[:, ti * 128:(ti + 1) * 128], rhs=skb[:, hp, :], start=True, stop=True),
                              reads=[qs_, skb], writes=[psc])
                        if half == 0:
                            kb.op("dve", lambda e, psc=psc, ti=ti, h=h: e.tensor_copy(out=AA[:, ti, h, :], in_=psc[:, 0:NK]), reads=[psc], writes=[AA])
                        else:
                            kb.op("dve", lambda e, psc=psc, ti=ti, h=h: e.tensor_copy(out=S2[:, ti, h, :], in_=psc[:, 0:NK]), reads=[psc], writes=[S2])
                for ti in range(4):
                    for h in range(8):
                        for (src, dstT_) in ((AA[:, ti, h, :], T1), (S2[:, ti, h, :], T2_)):
                            kb.op("dve", lambda e, src=src, dstT_=dstT_: e.max(out=dstT_[:, 0:8], in_=src), reads=[AA, S2], writes=[dstT_])
                            kb.op("dve", lambda e, src=src, dstT_=dstT_: e.match_replace(out=s1b[:, :], in_to_replace=dstT_[:, 0:8], in_values=src, imm_value=NEG),
                                  reads=[AA, S2, dstT_], writes=[s1b])
                            kb.op("dve", lambda e, dstT_=dstT_: e.max(out=dstT_[:, 8:16], in_=s1b[:, :]), reads=[s1b], writes=[dstT_])
                        for a_ in range(16):
                            kb.op("dve", lambda e, a_=a_: e.tensor_scalar(out=cand[:, a_ * 16:(a_ + 1) * 16], in0=T2_[:, :], scalar1=T1[:, a_:a_ + 1], scalar2=None, op0=ALU.add),
                                  reads=[T1, T2_], writes=[cand])
                        kb.op("dve", lambda e: e.max(out=c16[:, 0:8], in_=cand[:, :]), reads=[cand], writes=[c16])
                        kb.op("dve", lambda e: e.match_replace(out=candb[:, :], in_to_replace=c16[:, 0:8], in_values=cand[:, :], imm_value=NEG), reads=[cand, c16], writes=[candb])
                        kb.op("dve", lambda e: e.max(out=c16[:, 8:16], in_=candb[:, :]), reads=[candb], writes=[c16])
                        kb.op("dve", lambda e: e.tensor_scalar(out=zc[:, 0:1], in0=c16[:, 0:1], scalar1=-1.0, scalar2=None, op0=ALU.mult), reads=[c16], writes=[zc])
                        kb.op("act", lambda e: e.activation(out=e16[:, :], in_=c16[:, :], func=AF.Exp, bias=zc[:, 0:1], accum_out=zc[:, 1:2]), reads=[c16, zc], writes=[e16, zc])
                        kb.op("act", lambda e: e.activation(out=zc[:, 2:3], in_=zc[:, 1:2], func=AF.Ln), reads=[zc], writes=[zc])
                        kb.op("dve", lambda e: e.tensor_tensor(out=zc[:, 2:3], in0=zc[:, 0:1], in1=zc[:, 2:3], op=ALU.subtract), reads=[zc], writes=[zc])
                        kb.op("dve", lambda e, ti=ti, h=h: e.tensor_scalar(out=NT[:, ti, h, :], in0=AA[:, ti, h, :], scalar1=-1.0, scalar2=c16[:, 15:16], op0=ALU.mult, op1=ALU.add),
                              reads=[AA, c16], writes=[NT])
                        kb.op("dve", lambda e, ti=ti, h=h: e.tensor_scalar(out=AA[:, ti, h, :], in0=AA[:, ti, h, :], scalar1=zc[:, 2:3], scalar2=None, op0=ALU.add),
                              reads=[AA, zc], writes=[AA])
                for j_, t_ in enumerate((S2, AA, NT)):
                    kb.dma("sp", ans_d[g, j_, :, :], t_[:, :, :, :].rearrange("p a b c -> p (a b c)"), reads=[t_])

        with kb.phase():
            kb.psum_rot = 4
            kb.pnext = 0
            pA_, pG_ = [kb.psum[4], kb.psum[5]], [kb.psum[6], kb.psum[7]]
            xT = kb.sb([128, KC, TG], BF16)
            S2 = kb.sb([128, 4, 8, NK], F32)
            AA = kb.sb([128, 4, 8, NK], F32)
            NT = kb.sb([128, 4, 8, NK], F32)
            NUB, NVB, NEB = 2, 3, 10
            Ub = [kb.sb([128, KC, NK], BF16) for _ in range(NUB)]
            Vb = [kb.sb([NK, D], BF16) for _ in range(NVB)]
            Eb = [kb.sb([128, NK], F32) for _ in range(NEB)]
            Gh = [kb.sb([128, NK], BF16) for _ in range(NEB)]
            gel = [kb.sb([NK, TG], F32) for _ in range(2)]
            wT = [kb.sb([NK, TG], BF16) for _ in range(4)]
            ACC = kb.sb([128, 4, D], F32, n=4)
            NP_ = NK // 2
            st_ = {"rot": 0}

            def v_load(c):
                V_ = Vb[c % NVB]
                kb.dma("pool", V_[:, :], vv[c * NK:(c + 1) * NK, :], writes=[V_], max_dma_last_dim=4096)

            def u_load(c):
                U_ = Ub[c % NUB]
                kb.dma("pool", U_[:, :, :], uTv[:, :, c * NK:(c + 1) * NK], writes=[U_])

            def block(ps2, ps1, ti):
                pend = []
                for k in range(8):
                    if ps2 is not None:
                        c0, c1 = 2 * ps2, 2 * ps2 + 1
                        po = kb.ps()
                        ds_ = slice(k * 512, (k + 1) * 512)
                        kb.op("pe", lambda e, po=po, ds_=ds_: e.matmul(po[:, :], lhsT=wT[c0 % 4][:, ti * 128:(ti + 1) * 128], rhs=Vb[c0 % NVB][:, ds_], start=True, stop=False),
                              reads=[wT[c0 % 4], Vb[c0 % NVB]], writes=[po])
                        kb.op("pe", lambda e, po=po, ds_=ds_: e.matmul(po[:, :], lhsT=wT[c1 % 4][:, ti * 128:(ti + 1) * 128], rhs=Vb[c1 % NVB][:, ds_], start=False, stop=True),
                              reads=[wT[c1 % 4], Vb[c1 % NVB]], writes=[po])
                        if ps2 == 0:
                            kb.op("dve", lambda e, po=po, ds_=ds_: e.tensor_copy(out=ACC[:, ti, ds_], in_=po[:, :]), reads=[po], writes=[ACC.ks[ti]])
                        else:
                            kb.op("dve", lambda e, po=po, ds_=ds_: e.tensor_tensor(out=ACC[:, ti, ds_], in0=ACC[:, ti, ds_], in1=po[:, :], op=ALU.add),
                                  reads=[po, ACC.ks[ti]], writes=[ACC.ks[ti]])
                    if ps1 is not None:
                        q_, hf = ti // 2, ti % 2
                        c = 2 * ps1 + q_
                        U_ = Ub[c % NUB]
                        pA = pA_[q_]
                        for kc in (hf * 16 + 2 * k, hf * 16 + 2 * k + 1):
                            kb.op("pe", lambda e, pA=pA, kc=kc, U_=U_: e.matmul(pA[0:NK, :], lhsT=U_[:, kc, :], rhs=xT[:, kc, :], start=(kc == 0), stop=(kc == KC - 1)),
                                  reads=[U_, xT], writes=[pA])
                        for fn in pend:
                            fn()
                        pend = []
                        h = k
                        for q in range(2):
                            i = 2 * ps1 + q
                            pG = pG_[q]
                            E_ = Eb[st_["rot"] % NEB]
                            G_ = Gh[st_["rot"] % NEB]
                            st_["rot"] += 1
                            kb.op("act", lambda e, E_=E_, h=h, i=i: e.activation(out=E_[:, :], in_=S2[:, ti, h, :], func=AF.Exp, bias=AA[:, ti, h, i:i + 1]),
                                  reads=[S2, AA], writes=[E_])
                            kb.op("dve", lambda e, E_=E_, G_=G_, h=h, i=i: e.scalar_tensor_tensor(out=G_[:, :], in0=S2[:, ti, h, :], scalar=NT[:, ti, h, i:i + 1], in1=E_[:, :],
                                                                                           op0=ALU.is_ge, op1=ALU.mult), reads=[S2, NT, E_], writes=[G_])
                            pend.append(lambda G_=G_, pG=pG, h=h: kb.op(
                                "pe", lambda e: e.matmul(pG[0:NK, ti * 128:(ti + 1) * 128], lhsT=G_[:, :], rhs=identb[:, :], start=(h == 0), stop=(h == 7)),
                                reads=[G_, identb], writes=[pG]))
                for fn in pend:
                    fn()
                if ps1 is not None and ti % 2 == 1:
                    q_ = ti // 2
                    c = 2 * ps1 + q_
                    kb.op("act", lambda e, q_=q_: e.activation(out=gel[q_][:, :], in_=pA_[q_][0:NK, :], func=AF.Gelu), reads=[pA_[q_]], writes=[gel[q_]])
                    if c + 2 < NK:
                        u_load(c + 2)

            def s1_gbuild(p, ti):
                for q in range(2):
                    i = 2 * p + q
                    pG = pG_[q]
                    for h in range(8):
                        E_ = Eb[st_["rot"] % NEB]
                        G_ = Gh[st_["rot"] % NEB]
                        st_["rot"] += 1
                        kb.op("act", lambda e, E_=E_, h=h, i=i: e.activation(out=E_[:, :], in_=S2[:, ti, h, :], func=AF.Exp, bias=AA[:, ti, h, i:i + 1]),
                              reads=[S2, AA], writes=[E_])
                        kb.op("dve", lambda e, E_=E_, G_=G_, h=h, i=i: e.scalar_tensor_tensor(out=G_[:, :], in0=S2[:, ti, h, :], scalar=NT[:, ti, h, i:i + 1], in1=E_[:, :],
                                                                                       op0=ALU.is_ge, op1=ALU.mult), reads=[S2, NT, E_], writes=[G_])
                        kb.op("pe", lambda e, G_=G_, pG=pG, h=h: e.matmul(pG[0:NK, ti * 128:(ti + 1) * 128], lhsT=G_[:, :], rhs=identb[:, :], start=(h == 0), stop=(h == 7)),
                              reads=[G_, identb], writes=[pG])

            def s1_end(p):
                for q in range(2):
                    c = 2 * p + q
                    w_ = wT[c % 4]
                    kb.op("dve", lambda e, w_=w_, q=q: e.tensor_tensor(out=w_[:, :], in0=pG_[q][0:NK, :], in1=gel[q][:, :], op=ALU.mult), reads=[pG_[q], gel[q]], writes=[w_])

            def s2_(p, ti):
                c0, c1 = 2 * p, 2 * p + 1
                for dc in range(8):
                    po = kb.ps()
                    ds_ = slice(dc * 512, (dc + 1) * 512)
                    kb.op("pe", lambda e, po=po, ds_=ds_: e.matmul(po[:, :], lhsT=wT[c0 % 4][:, ti * 128:(ti + 1) * 128], rhs=Vb[c0 % NVB][:, ds_], start=True, stop=False),
                          reads=[wT[c0 % 4], Vb[c0 % NVB]], writes=[po])
                    kb.op("pe", lambda e, po=po, ds_=ds_: e.matmul(po[:, :], lhsT=wT[c1 % 4][:, ti * 128:(ti + 1) * 128], rhs=Vb[c1 % NVB][:, ds_], start=False, stop=True),
                          reads=[wT[c1 % 4], Vb[c1 % NVB]], writes=[po])
                    if p == 0:
                        kb.op("dve", lambda e, po=po, ds_=ds_: e.tensor_copy(out=ACC[:, ti, ds_], in_=po[:, :]), reads=[po], writes=[ACC.ks[ti]])
                    else:
                        kb.op("dve", lambda e, po=po, ds_=ds_: e.tensor_tensor(out=ACC[:, ti, ds_], in0=ACC[:, ti, ds_], in1=po[:, :], op=ALU.add),
                              reads=[po, ACC.ks[ti]], writes=[ACC.ks[ti]])

            for g in range(NGP):
                kb.dma("sp", xT[:, :, :], xn2T_d[:, :, g * TG:(g + 1) * TG], writes=[xT])
                for j_, t_ in enumerate((S2, AA, NT)):
                    kb.dma("sp", t_[:, :, :, :].rearrange("p a b c -> p (a b c)"), ans_d[g, j_, :, :], writes=[t_])
                u_load(0)
                u_load(1)
                v_load(0)
                v_load(1)
                for ti in range(4):
                    block(None, 0, ti)
                s1_end(0)
                for p in range(NP_):
                    nxt = p + 1 < NP_
                    if nxt:
                        v_load(2 * p + 2)
                    for ti in range(4):
                        block(p, p + 1 if nxt else None, ti)
                    if nxt:
                        s1_end(p + 1)
                        v_load(2 * p + 3)
                for ti in range(4):
                    tok0 = g * TG + ti * 128
                    kb.dma("sp", peer_d[tok0:tok0 + 128, :], ACC[:, ti, :], reads=[ACC.ks[ti]])
            kb.psum_rot = 8
            kb.pnext = 0

        with kb.phase():
            g2b = kb.sb([128, D], F32)
            fnb = kb.sb([128, D], F32)
            kb.dma("sp", g2b[:, :], modflat[5 * D:6 * D].partition_broadcast(128), writes=[g2b])
            kb.dma("sp", fnb[:, :], fnw.partition_broadcast(128), writes=[fnb])
            xa = [kb.sb([128, D], F32) for _ in range(2)]
            pa = [kb.sb([128, D], F32) for _ in range(2)]
            junk = kb.sb([128, D], BF16)
            ssf = [kb.sb([128, 2], F32) for _ in range(2)]
            for it in range(TOK // 128):
                x_, p_, ss_ = xa[it % 2], pa[it % 2], ssf[it % 2]
                rs_ = slice(it * 128, (it + 1) * 128)
                kb.dma("sp", x_[:, :], x2_d[rs_, :], writes=[x_])
                kb.dma("sp", p_[:, :], peer_d[rs_, :], writes=[p_])
                kb.op("dve", lambda e, p_=p_: e.tensor_tensor(out=p_[:, :], in0=p_[:, :], in1=g2b[:, :], op=ALU.mult), reads=[p_, g2b], writes=[p_])
                kb.op("pool", lambda e, p_=p_, x_=x_: e.tensor_tensor(out=x_[:, :], in0=x_[:, :], in1=p_[:, :], op=ALU.add), reads=[p_, x_], writes=[x_])
                kb.op("act", lambda e, x_=x_, ss_=ss_: e.activation(out=junk[:, :], in_=x_[:, :], func=AF.Square, accum_out=ss_[:, 0:1]), reads=[x_], writes=[junk, ss_])
                kb.op("dve", lambda e, ss_=ss_: e.tensor_scalar(out=ss_[:, 0:1], in0=ss_[:, 0:1], scalar1=1.0 / D, scalar2=EPS, op0=ALU.mult, op1=ALU.add), reads=[ss_], writes=[ss_])
                kb.op("act", lambda e, ss_=ss_: e.activation(out=ss_[:, 0:1], in_=ss_[:, 0:1], func=AF.Sqrt), reads=[ss_], writes=[ss_])
                kb.op("dve", lambda e, ss_=ss_: e.reciprocal(out=ss_[:, 1:2], in_=ss_[:, 0:1]), reads=[ss_], writes=[ss_])
                kb.op("dve", lambda e, x_=x_, p_=p_, ss_=ss_: e.scalar_tensor_tensor(out=p_[:, :], in0=x_[:, :], scalar=ss_[:, 1:2], in1=fnb[:, :], op0=ALU.mult, op1=ALU.mult),
                      reads=[x_, ss_, fnb], writes=[p_])
                kb.dma("sp", y[rs_, :], p_[:, :], reads=[p_])


        kb.barrier()
    return nc


def host_shared(cfg, inp):
    TOK, T2, NK, NB, NCH = cfg.TOK, cfg.T2, cfg.NK, cfg.NB, cfg.NCH
    NBP = max(NB, 8)
    m = {}
    idx = np.arange(128)
    le = (idx[:, None] <= idx[None, :])
    consts = np.zeros((128, 5, 128), np.float32)
    consts[:, 0, :] = le.astype(np.float32)
    consts[:, 1, :] = np.where(le.T, 0.0, NEG)
    consts[:, 2, :] = np.where(le, 0.0, 1.0e30)
    consts[127, 3, :] = 1.0
    consts[:, 4, :] = 1.0
    m["consts"] = consts
    m["ident_f"] = np.eye(128, dtype=np.float32)
    m["w_ada"] = inp["w_ada"][0]
    m["b_adaT"] = np.ascontiguousarray(inp["b_ada"][0].reshape(192, 128).T)
    m["n1wT"] = np.ascontiguousarray(inp["norm1_w"][0].reshape(KC, 128).T)
    m["n2wT"] = np.ascontiguousarray(inp["norm2_w"][0].reshape(KC, 128).T)
    m["fnw"] = np.ascontiguousarray(inp["final_norm_w"])
    m["w_in"] = inp["w_in"][0]
    m["conv_wT"] = np.ascontiguousarray(inp["conv_w"][0].T.reshape(16, 128, 4).transpose(1, 0, 2))
    m["conv_bT"] = np.ascontiguousarray(inp["conv_b"][0].reshape(16, 128).T)
    gb = np.concatenate([inp["b_igate"][0], inp["b_fgate"][0]]).astype(np.float32)
    m["gbias"] = np.ascontiguousarray(np.broadcast_to(gb[None, None, :], (128, NCH, 8)))
    m["mnw"] = np.ascontiguousarray(inp["mlstm_norm_w"][0].reshape(2048))
    oneh = np.zeros((NBP, NBP, 128), np.float32)
    for n in range(NBP):
        oneh[n, n, :] = 1.0
    m["oneh"] = oneh.reshape(NBP, NBP * 128)
    m["w_out"] = inp["w_out"][0]
    m["wq"] = inp["peer_wq"][0]
    m["skT"] = np.ascontiguousarray(inp["peer_subkeys"][0].reshape(16, NK, 128).transpose(2, 0, 1))
    m["uT"] = np.ascontiguousarray(inp["peer_u"][0].T)
    m["vv"] = inp["peer_v"][0]
    return m


def host_inputs(cfg, core, b, s, inp, shared):
    TOK, T2, NB = cfg.TOK, cfg.T2, cfg.NB
    NBP = max(NB, 8)
    x = inp["x"]
    m = dict(shared)
    own0 = s * TOK
    m["x_own"] = np.ascontiguousarray(x[b, own0:own0 + TOK])
    m["x_past"] = np.ascontiguousarray(x[b, 0:TOK]) if s == 1 else np.zeros((TOK, D), np.float32)
    m["cT"] = np.ascontiguousarray(inp["c"][b].reshape(KC, 128).T)
    m["flag"] = np.full((128, 1), float(s), np.float32)
    pos = np.concatenate([np.arange(TOK), s * TOK + np.arange(TOK)]).astype(np.float32)
    half = 64
    inv = (np.float32(10000.0) ** (-np.arange(half, dtype=np.float32) / np.float32(half))).astype(np.float32)
    ang = (pos[:, None] * inv[None, :]).astype(np.float32)
    m["cs_tab"] = np.concatenate([np.cos(ang), np.sin(ang)], axis=1).astype(np.float32)
    pb = np.full((TOK, NBP), NEG, np.float32)
    for q0 in range(0, TOK, 256):
        jb = NB // 2 + q0 // 256
        lo = 0 if s == 1 else NB // 2
        pb[q0:q0 + 256, lo:jb] = 0.0
    m["pb_tab"] = pb
    return m


_CACHE = {}


def kernel(**inp):
    inp = {k: np.asarray(v) for k, v in inp.items()}
    cfg = Cfg(tok=2048, nk=128)
    if "nc" not in _CACHE:
        _CACHE["nc"] = build(cfg)
    nc = _CACHE["nc"]
    shared = host_shared(cfg, inp)
    maps = []
    for core in range(8):
        b, s = core // 2, core % 2
        maps.append(host_inputs(cfg, core, b, s, inp, shared))
    res = run_bass_kernel_spmd(nc, maps, core_ids=list(range(8)))
    out = np.zeros((4, 4096, D), np.float32)
    for core in range(8):
        b, s = core // 2, core % 2
        out[b, s * cfg.TOK:(s + 1) * cfg.TOK] = res.results[core]["y"]
    return out
```

```python
import numpy as np
from contextlib import ExitStack
import ml_dtypes
import concourse.bass as bass
import concourse.mybir as mybir
from concourse.bass_utils import run_bass_kernel_spmd

F32 = mybir.dt.float32
BF16 = mybir.dt.bfloat16
AF = mybir.ActivationFunctionType
ALU = mybir.AluOpType
AX = mybir.AxisListType

D = 4096
KC = 32
NDS = 24
EPS = 1e-6
NEG = -1.0e30


class Tk:
    __slots__ = ("w", "rs")

    def __init__(self):
        self.w = None
        self.rs = {}


class Tile:
    def __init__(self, t, n=1):
        self.t = t
        self.k = Tk()
        self.ks = [Tk() for _ in range(n)]

    def __getitem__(self, idx):
        return self.t[idx]


class KB:
    EPOCH = 20000

    def __init__(self, nc, st):
        self.nc = nc
        self.st = st
        self.eng = {"pe": nc.tensor, "act": nc.scalar, "dve": nc.vector, "pool": nc.gpsimd, "sp": nc.sync}
        self.cnt = {e: 0 for e in self.eng}
        self.sem = {e: self.newsem() for e in self.eng}
        self.last = {e: None for e in self.eng}
        self.seen = {}
        self.dsem = [self.newsem() for _ in range(NDS)]
        self.dval = [0] * NDS
        self.dnext = 0
        self.psum = []
        self.pnext = 0
        for i in range(8):
            self.psum.append(Tile(st.enter_context(nc.psum_tensor(f"ps{i}", [128, 512], F32))))
        self.pst = None

    def newsem(self):
        self._ns = getattr(self, "_ns", 0) + 1
        return self.st.enter_context(self.nc.semaphore(f"sem{self._ns}"))

    psum_rot = 8

    def ps(self):
        p = self.psum[self.pnext]
        self.pnext = (self.pnext + 1) % self.psum_rot
        return p

    def sb(self, shape, dtype, n=1, name=None):
        self._nm = getattr(self, "_nm", 0) + 1
        t = self.pst.enter_context(self.nc.sbuf_tensor(name or f"sb{self._nm}", list(shape), dtype))
        return Tile(t, n)

    def _wait(self, e, tk):
        sem, val = tk[0], tk[1]
        key = (e, id(sem))
        if self.seen.get(key, 0) >= val:
            return
        self.eng[e].wait_ge(sem, val)
        self.seen[key] = val

    def _deps(self, e, reads, writes):
        ticks = []
        for t in reads:
            if t.w is not None:
                ticks.append(t.w)
        for t in writes:
            if t.w is not None:
                ticks.append(t.w)
            ticks.extend(t.rs.values())
        for tk in ticks:
            if tk[2] == e and e == "pe":
                continue
            self._wait(e, tk)

    def _mark(self, tk, reads, writes):
        for t in reads:
            t.rs[id(tk[0])] = tk
        for t in writes:
            t.w = tk
            t.rs = {}

    @staticmethod
    def _tk(objs):
        out = []
        for o in objs:
            out.append(o.k if isinstance(o, Tile) else o)
        return out

    def op(self, e, fn, reads=(), writes=()):
        reads, writes = self._tk(reads), self._tk(writes)
        self._deps(e, reads, writes)
        if self.cnt[e] >= self.EPOCH:
            self.sem[e] = self.newsem()
            self.cnt[e] = 0
        self.cnt[e] += 1
        ins = fn(self.eng[e])
        ins.then_inc(self.sem[e], 1)
        tk = (self.sem[e], self.cnt[e], e)
        self.last[e] = tk
        self._mark(tk, reads, writes)

    def dma(self, q, out, in_, reads=(), writes=(), **kw):
        reads, writes = self._tk(reads), self._tk(writes)
        self._deps(q, reads, writes)
        i = self.dnext
        self.dnext = (self.dnext + 1) % NDS
        if self.dval[i] > 0:
            self._wait(q, (self.dsem[i], self.dval[i]))
        self.dval[i] += 16
        self.eng[q].dma_start(out=out, in_=in_, **kw).then_inc(self.dsem[i], 16)
        tk = (self.dsem[i], self.dval[i], "dma")
        self._mark(tk, reads, writes)

    def barrier(self):
        for e in self.eng:
            for e2 in self.eng:
                if e2 != e and self.last[e2] is not None:
                    self._wait(e, self.last[e2])
            for i in range(NDS):
                if self.dval[i] > 0:
                    self._wait(e, (self.dsem[i], self.dval[i]))

    def phase(self):
        kb = self

        class _P:
            def __enter__(s):
                kb.pst = ExitStack()
                kb.pst.__enter__()
                return kb

            def __exit__(s, *a):
                kb.barrier()
                kb.pst.__exit__(*a)
                kb.pst = None
                return False

        return _P()


class Cfg:
    def __init__(self, tok=2048, nk=128):
        self.TOK = tok
        self.T2 = 2 * tok
        self.NK = nk
        self.NE = nk * nk
        self.NB = self.T2 // 256
        self.NCH = self.T2 // 128


def build(cfg, upto=99):
    TOK, T2 = cfg.TOK, cfg.T2
    nc = bass.Bass("TRN2", target_bir_lowering=False)

    def din(name, shape, dt=F32):
        return nc.dram_tensor(name, list(shape), dt, kind="ExternalInput").ap()

    def dscr(name, shape, dt=F32):
        return nc.dram_tensor(name, list(shape), dt).ap()

    x_own = din("x_own", [TOK, D])
    x_past = din("x_past", [TOK, D])
    cT = din("cT", [128, KC])
    w_ada = din("w_ada", [D, 6 * D])
    b_adaT = din("b_adaT", [128, 192])
    n1wT = din("n1wT", [128, KC])
    n2wT = din("n2wT", [128, KC])
    fnw = din("fnw", [D])
    w_in = din("w_in", [D, 12296])
    flag = din("flag", [128, 1])
    ident_f = din("ident_f", [128, 128])
    y = nc.dram_tensor("y", [TOK, D], F32, kind="ExternalOutput").ap()
    NK, NE, NB, NCH = cfg.NK, cfg.NE, cfg.NB, cfg.NCH
    NBP = max(NB, 8)
    consts = din("consts", [128, 5, 128])
    conv_wT = din("conv_wT", [128, 16, 4])
    conv_bT = din("conv_bT", [128, 16])
    gbias = din("gbias", [128, NCH, 8])
    mnw = din("mnw", [2048])
    cs_tab = din("cs_tab", [T2, 128])
    pb_tab = din("pb_tab", [TOK, NBP])
    oneh = din("oneh", [NBP, NBP * 128])
    w_out = din("w_out", [D, D])
    wqc = din("wqc", [16, 128, KC * 128])
    skT = din("skT", [128, 16, NK])
    uTc = din("uTc", [NK, 128, KC * NK])
    vv = din("vv", [NE, D])
    qkc_d = dscr("qkc_d", [2048, T2], BF16)
    hcat_d = dscr("hcat_d", [TOK, D], BF16)
    x2_d = dscr("x2_d", [TOK, D])
    xn2T_d = dscr("xn2T_d", [128, KC, TOK], BF16)
    s_d = dscr("s_d", [TOK, 16, NK])
    peer_d = dscr("peer_d", [TOK, D])
    ans_d = dscr("ans_d", [TOK // 512, 3, 128, 4 * 8 * NK])

    mv_d = dscr("mv_d", [T2, 2048], BF16)
    o_d = dscr("o_d", [TOK, 2048])
    if_d = dscr("if_d", [T2, 8])
    aq_d = dscr("aq_d", [TOK, 2048])
    ak_d = dscr("ak_d", [T2, 2048])
    av_d = dscr("av_d", [T2, 2048], BF16)
    qkT_d = dscr("qkT_d", [2048, 4 + T2])
    mod_d = dscr("mod_d", [192, 128])
    dbg = {}

    with ExitStack() as st:
        kb = KB(nc, st)
        kb.pst = st
        modT = kb.sb([128, 192], F32, name="modT")
        A1 = kb.sb([128, KC], F32, name="A1")
        A1p = kb.sb([128, KC], F32, name="A1p")
        S1p = kb.sb([128, KC], F32, name="S1p")
        A2 = kb.sb([128, KC], F32, name="A2")
        identb = kb.sb([128, 128], BF16, name="identb")
        identf = kb.sb([128, 128], F32, name="identf")
        flg = kb.sb([128, 1], F32, name="flg")
        cst = kb.sb([128, 5, 128], F32, name="cst")
        onesb = kb.sb([128, 128], BF16, name="onesb")
        kb.pst = None
        kb.dma("sp", cst[:, :, :], consts[:, :, :], writes=[cst])
        kb.dma("pool", onesb[:, :], consts[:, 4, :], writes=[onesb])
        kb.dma("sp", identf[:, :], ident_f[:, :], writes=[identf])
        kb.dma("pool", identb[:, :], ident_f[:, :], writes=[identb])
        kb.dma("sp", flg[:, :], flag[:, :], writes=[flg])

        with kb.phase():
            c_sb = kb.sb([128, KC], F32)
            cs = kb.sb([128, KC], BF16)
            bad = kb.sb([128, 192], F32)
            n1 = kb.sb([128, KC], F32)
            n2 = kb.sb([128, KC], F32)
            wb = [kb.sb([128, KC, 512], BF16) for _ in range(2)]
            kb.dma("sp", c_sb[:, :], cT[:, :], writes=[c_sb])
            kb.dma("sp", bad[:, :], b_adaT[:, :], writes=[bad])
            kb.dma("sp", n1[:, :], n1wT[:, :], writes=[n1])
            kb.dma("sp", n2[:, :], n2wT[:, :], writes=[n2])
            kb.op("act", lambda e: e.activation(out=cs[:, :], in_=c_sb[:, :], func=AF.Silu), reads=[c_sb], writes=[cs])
            wv = w_ada.rearrange("(kc p) n -> p kc n", p=128)
            pacc = kb.ps()
            for cc in range(48):
                w = wb[cc % 2]
                kb.dma("pool", w[:, :, :], wv[:, :, cc * 512:(cc + 1) * 512], writes=[w])
                for sub in range(4):
                    j = cc * 4 + sub
                    for kc in range(KC):
                        kb.op("pe", lambda e, w=w, kc=kc, sub=sub, j=j: e.matmul(
                            pacc[:, j:j + 1], lhsT=w[:, kc, sub * 128:(sub + 1) * 128], rhs=cs[:, kc:kc + 1],
                            start=(kc == 0), stop=(kc == KC - 1)), reads=[w, cs], writes=[pacc])
            kb.op("dve", lambda e: e.tensor_tensor(out=modT[:, :], in0=pacc[:, 0:192], in1=bad[:, :], op=ALU.add),
                  reads=[pacc, bad], writes=[modT])
            kb.op("dve", lambda e: e.scalar_tensor_tensor(out=A1[:, :], in0=modT[:, 32:64], scalar=1.0, in1=n1[:, :],
                                                          op0=ALU.add, op1=ALU.mult), reads=[modT, n1], writes=[A1])
            kb.op("dve", lambda e: e.scalar_tensor_tensor(out=A2[:, :], in0=modT[:, 128:160], scalar=1.0, in1=n2[:, :],
                                                          op0=ALU.add, op1=ALU.mult), reads=[modT, n2], writes=[A2])
            kb.op("dve", lambda e: e.tensor_scalar(out=A1p[:, :], in0=A1[:, :], scalar1=flg[:, 0:1], scalar2=None,
                                                   op0=ALU.mult), reads=[A1, flg], writes=[A1p])
            kb.op("dve", lambda e: e.tensor_scalar(out=S1p[:, :], in0=modT[:, 0:32], scalar1=flg[:, 0:1], scalar2=None,
                                                   op0=ALU.mult), reads=[modT, flg], writes=[S1p])
            mrow = kb.sb([128, 2, 128], F32)
            for hh in range(2):
                pt = kb.ps()
                n = 128 if hh == 0 else 64
                kb.op("pe", lambda e, pt=pt, hh=hh, n=n: e.matmul(pt[0:n, 0:128], lhsT=modT[:, hh * 128:hh * 128 + n],
                                                                  rhs=identf[:, :], start=True, stop=True),
                      reads=[modT, identf], writes=[pt])
                kb.op("act", lambda e, pt=pt, hh=hh, n=n: e.activation(out=mrow[0:n, hh, :], in_=pt[0:n, 0:128], func=AF.Copy),
                      reads=[pt], writes=[mrow])
                kb.dma("sp", mod_d[hh * 128:hh * 128 + n, :], mrow[0:n, hh, :], reads=[mrow])
        if upto <= 1:
            dbg["modT"] = None

        TG = 512
        TGB = 1024 if TOK % 1024 == 0 else 512
        NG = T2 // TGB
        NTB = TGB // 128
        wiv = w_in.rearrange("(kc p) n -> p kc n", p=128)
        chunks = []
        for i in range(4):
            chunks.append((i * 512, 512, "qk"))
        for i in range(4):
            chunks.append((2048 + i * 512, 512, "mv"))
        for i in range(4):
            chunks.append((4096 + i * 512, 512, "o"))
        chunks.append((6144, 8, "if"))
        for i in range(4):
            chunks.append((6152 + i * 512, 512, "aq"))
        for i in range(4):
            chunks.append((8200 + i * 512, 512, "ak"))
        for i in range(4):
            chunks.append((10248 + i * 512, 512, "av"))
        with kb.phase():
            xt = [kb.sb([128, D], F32) for _ in range(2)]
            junk = kb.sb([128, D], BF16)
            xs = [kb.sb([128, D], BF16) for _ in range(2)]
            ss = [kb.sb([128, 1], F32) for _ in range(2)]
            xnT = kb.sb([128, KC, TGB], BF16, n=NTB)
            wb = [kb.sb([128, KC, 512], BF16) for _ in range(2)]
            stf = [kb.sb([128, 512], F32) for _ in range(3)]
            stb = [kb.sb([128, 512], BF16) for _ in range(3)]
            zpad = kb.sb([128, 4], F32)
            kb.op("dve", lambda e: e.memset(zpad[:, :], 0.0), writes=[zpad])
            for cc in range(16):
                kb.dma("sp", qkT_d[cc * 128:(cc + 1) * 128, 0:4], zpad[:, :], reads=[zpad])
            nst = 0
            nw = 0
            for g in range(NG):
                past = (g * TGB) < TOK
                for ti in range(NTB):
                    tok0 = g * TGB + ti * 128
                    src = x_past[tok0:tok0 + 128, :] if past else x_own[tok0 - TOK:tok0 - TOK + 128, :]
                    it = g * NTB + ti
                    x_t, xs_t, ss_t = xt[it % 2], xs[it % 2], ss[it % 2]
                    kb.dma("sp", x_t[:, :], src, writes=[x_t])
                    kb.op("act", lambda e, x_t=x_t, ss_t=ss_t: e.activation(out=junk[:, :], in_=x_t[:, :], func=AF.Square,
                                                                           accum_out=ss_t[:, 0:1]),
                          reads=[x_t], writes=[junk, ss_t])
                    kb.op("dve", lambda e, ss_t=ss_t: e.tensor_scalar(out=ss_t[:, :], in0=ss_t[:, :], scalar1=1.0 / D,
                                                                      scalar2=EPS, op0=ALU.mult, op1=ALU.add),
                          reads=[ss_t], writes=[ss_t])
                    kb.op("act", lambda e, ss_t=ss_t: e.activation(out=ss_t[:, :], in_=ss_t[:, :], func=AF.Sqrt),
                          reads=[ss_t], writes=[ss_t])
                    kb.op("dve", lambda e, ss_t=ss_t: e.reciprocal(out=ss_t[:, :], in_=ss_t[:, :]), reads=[ss_t], writes=[ss_t])
                    kb.op("dve", lambda e, x_t=x_t, xs_t=xs_t, ss_t=ss_t: e.tensor_scalar(
                        out=xs_t[:, :], in0=x_t[:, :], scalar1=ss_t[:, 0:1], scalar2=None, op0=ALU.mult),
                        reads=[x_t, ss_t], writes=[xs_t])
                    Asel, Ssel = (A1p, S1p) if past else (A1, None)
                    for k4 in range(KC // 4):
                        pt = kb.ps()
                        for q in range(4):
                            kc = k4 * 4 + q
                            kb.op("pe", lambda e, pt=pt, q=q, kc=kc, xs_t=xs_t: e.matmul(
                                pt[:, q * 128:(q + 1) * 128], lhsT=xs_t[:, kc * 128:(kc + 1) * 128], rhs=identb[:, :],
                                start=True, stop=True), reads=[xs_t, identb], writes=[pt])
                        for q in range(4):
                            kc = k4 * 4 + q
                            bias_ap = (S1p[:, kc:kc + 1] if past else modT[:, kc:kc + 1])
                            kb.op("act", lambda e, pt=pt, q=q, kc=kc, ti=ti, Asel=Asel, bias_ap=bias_ap: e.activation(
                                out=xnT[:, kc, ti * 128:(ti + 1) * 128], in_=pt[:, q * 128:(q + 1) * 128],
                                func=AF.Identity, scale=Asel[:, kc:kc + 1], bias=bias_ap),
                                reads=[pt, Asel, S1p if past else modT], writes=[xnT.ks[ti]])
                for (c0, ncol, kind) in chunks:
                    if past and kind in ("o", "aq"):
                        continue
                    w = wb[nw % 2]
                    nw += 1
                    kb.dma("pool", w[:, :, 0:ncol], wiv[:, :, c0:c0 + ncol], writes=[w])
                    if kind == "qk":
                        for sub, tb in [(a_, b_) for a_ in range(4) for b_ in range(TGB // 512)]:
                            pt = kb.ps()
                            for kc in range(KC):
                                kb.op("pe", lambda e, pt=pt, kc=kc, sub=sub, w=w, tb=tb: e.matmul(
                                    pt[:, :], lhsT=w[:, kc, sub * 128:(sub + 1) * 128], rhs=xnT[:, kc, tb * 512:(tb + 1) * 512],
                                    start=(kc == 0), stop=(kc == KC - 1)), reads=[w] + xnT.ks, writes=[pt])
                            s_t = stf[nst % 3]
                            ev = "act" if nst % 2 == 0 else "dve"
                            nst += 1
                            if ev == "act":
                                kb.op("act", lambda e, pt=pt, s_t=s_t: e.activation(out=s_t[:, :], in_=pt[:, :], func=AF.Copy),
                                      reads=[pt], writes=[s_t])
                            else:
                                kb.op("dve", lambda e, pt=pt, s_t=s_t: e.tensor_copy(out=s_t[:, :], in_=pt[:, :]),
                                      reads=[pt], writes=[s_t])
                            ch0 = c0 + sub * 128
                            kb.dma("sp", qkT_d[ch0:ch0 + 128, 4 + g * TGB + tb * 512:4 + g * TGB + (tb + 1) * 512], s_t[:, :], reads=[s_t])
                    else:
                        for ti in range(NTB):
                            tok0 = g * TGB + ti * 128
                            pt = kb.ps()
                            for kc in range(KC):
                                kb.op("pe", lambda e, pt=pt, kc=kc, ti=ti, w=w, ncol=ncol: e.matmul(
                                    pt[:, 0:ncol], lhsT=xnT[:, kc, ti * 128:(ti + 1) * 128], rhs=w[:, kc, 0:ncol],
                                    start=(kc == 0), stop=(kc == KC - 1)), reads=[w, xnT.ks[ti]], writes=[pt])
                            isb = kind in ("mv", "av")
                            s_t = (stb if isb else stf)[nst % 3]
                            ev = "act" if nst % 2 == 0 else "dve"
                            nst += 1
                            if ev == "act":
                                kb.op("act", lambda e, pt=pt, s_t=s_t, ncol=ncol: e.activation(out=s_t[:, 0:ncol], in_=pt[:, 0:ncol],
                                                                                              func=AF.Copy), reads=[pt], writes=[s_t])
                            else:
                                kb.op("dve", lambda e, pt=pt, s_t=s_t, ncol=ncol: e.tensor_copy(out=s_t[:, 0:ncol], in_=pt[:, 0:ncol]),
                                      reads=[pt], writes=[s_t])
                            if kind == "mv":
                                dst = mv_d[tok0:tok0 + 128, c0 - 2048:c0 - 2048 + 512]
                            elif kind == "o":
                                dst = o_d[tok0 - TOK:tok0 - TOK + 128, c0 - 4096:c0 - 4096 + 512]
                            elif kind == "if":
                                dst = if_d[tok0:tok0 + 128, 0:8]
                            elif kind == "aq":
                                dst = aq_d[tok0 - TOK:tok0 - TOK + 128, c0 - 6152:c0 - 6152 + 512]
                            elif kind == "ak":
                                dst = ak_d[tok0:tok0 + 128, c0 - 8200:c0 - 8200 + 512]
                            else:
                                dst = av_d[tok0:tok0 + 128, c0 - 10248:c0 - 10248 + 512]
                            kb.dma("sp", dst, s_t[:, 0:ncol], reads=[s_t])

        if upto <= 2:
            for nm, ap_, shp, dt in (("d_mod", mod_d, [192, 128], F32), ("d_qkT", qkT_d, [2048, 4 + T2], F32),
                                     ("d_if", if_d, [T2, 8], F32), ("d_ak", ak_d, [T2, 2048], F32),
                                     ("d_mv", mv_d, [T2, 2048], BF16), ("d_o", o_d, [TOK, 2048], F32)):
                o = nc.dram_tensor(nm, shp, dt, kind="ExternalOutput").ap()
                kb.dma("sp", o, ap_, )
            kb.barrier()
            return nc


        TRIu = cst[:, 0, :]
        CTS = cst[:, 1, :]
        CST = cst[:, 2, :]
        SEL127 = cst[:, 3, :]
        ONESF = cst[:, 4, :]

        def ev_copy(i, out, in_, rd, wr):
            if i % 2 == 0:
                kb.op("act", lambda e: e.activation(out=out, in_=in_, func=AF.Copy), reads=rd, writes=wr)
            else:
                kb.op("dve", lambda e: e.tensor_copy(out=out, in_=in_), reads=rd, writes=wr)

        with kb.phase():
            cw = kb.sb([128, 16, 4], F32)
            cb = kb.sb([128, 16], F32)
            kb.dma("sp", cw[:, :, :], conv_wT[:, :, :], writes=[cw])
            kb.dma("sp", cb[:, :], conv_bT[:, :], writes=[cb])
            ub = [kb.sb([128, 4 + T2], F32) for _ in range(2)]
            acc = kb.sb([128, T2], F32)
            ob = [kb.sb([128, T2], BF16) for _ in range(2)]
            for cc in range(16):
                u = ub[cc % 2]
                o_ = ob[cc % 2]
                kb.dma("sp", u[:, :], qkT_d[cc * 128:(cc + 1) * 128, :], writes=[u])
                kb.op("dve", lambda e, u=u, cc=cc: e.tensor_scalar(out=acc[:, :], in0=u[:, 1:1 + T2], scalar1=cw[:, cc, 0:1],
                                                                   scalar2=None, op0=ALU.mult), reads=[u, cw], writes=[acc])
                for j in range(1, 4):
                    kb.op("dve", lambda e, u=u, cc=cc, j=j: e.scalar_tensor_tensor(
                        out=acc[:, :], in0=u[:, 1 + j:1 + j + T2], scalar=cw[:, cc, j:j + 1], in1=acc[:, :],
                        op0=ALU.mult, op1=ALU.add), reads=[u, cw, acc], writes=[acc])
                kb.op("act", lambda e, o_=o_, cc=cc: e.activation(out=o_[:, :], in_=acc[:, :], func=AF.Silu, bias=cb[:, cc:cc + 1]),
                      reads=[acc, cb], writes=[o_])
                if cc < 8:
                    kb.op("dve", lambda e, o_=o_: e.tensor_scalar(out=o_[:, :], in0=o_[:, :], scalar1=0.0625, scalar2=None,
                                                                  op0=ALU.mult), reads=[o_], writes=[o_])
                kb.dma("sp", qkc_d[cc * 128:(cc + 1) * 128, :], o_[:, :], reads=[o_])

        with kb.phase():
            G = kb.sb([128, NCH, 8], F32)
            gb = kb.sb([128, NCH, 8], F32)
            LI = kb.sb([128, NCH, 4], F32)
            LF = kb.sb([128, NCH, 4], F32)
            Bc = kb.sb([128, NCH, 4], F32)
            U = kb.sb([128, NCH, 4], F32)
            kb.dma("sp", G[:, :, :], if_d.rearrange("(c p) g -> p c g", p=128), writes=[G])
            kb.dma("sp", gb[:, :, :], gbias[:, :, :], writes=[gb])
            kb.op("dve", lambda e: e.tensor_tensor(out=G[:, :, :], in0=G[:, :, :], in1=gb[:, :, :], op=ALU.add), reads=[G, gb], writes=[G])
            kb.op("act", lambda e: e.activation(out=G[:, :, :], in_=G[:, :, :], func=AF.Tanh, scale=1.0 / 15.0), reads=[G], writes=[G])
            kb.op("dve", lambda e: e.tensor_scalar(out=LI[:, :, :], in0=G[:, :, 0:4], scalar1=15.0, scalar2=None, op0=ALU.mult),
                  reads=[G], writes=[LI])
            kb.op("act", lambda e: e.activation(out=LF[:, :, :], in_=G[:, :, 4:8], func=AF.Exp, scale=-15.0), reads=[G], writes=[LF])
            kb.op("act", lambda e: e.activation(out=LF[:, :, :], in_=LF[:, :, :], func=AF.Ln, bias=1.0), reads=[LF], writes=[LF])
            kb.op("dve", lambda e: e.tensor_scalar(out=LF[:, :, :], in0=LF[:, :, :], scalar1=-1.0, scalar2=None, op0=ALU.mult),
                  reads=[LF], writes=[LF])
            pb_ = kb.ps()
            kb.op("pe", lambda e: e.matmul(pb_[:, 0:NCH * 4], lhsT=TRIu, rhs=LF[:, :, :].rearrange("p c h -> p (c h)"),
                                           start=True, stop=True), reads=[cst, LF], writes=[pb_])
            kb.op("dve", lambda e: e.tensor_copy(out=Bc[:, :, :].rearrange("p c h -> p (c h)"), in_=pb_[:, 0:NCH * 4]),
                  reads=[pb_], writes=[Bc])
            kb.op("dve", lambda e: e.tensor_tensor(out=U[:, :, :], in0=LI[:, :, :], in1=Bc[:, :, :], op=ALU.subtract),
                  reads=[LI, Bc], writes=[U])

            mnwb = kb.sb([128, 2048], F32)
            kb.dma("sp", mnwb[:, :], mnw.partition_broadcast(128), writes=[mnwb])
            qTh = kb.sb([128, 2, T2], BF16)
            kTh = kb.sb([128, 2, T2], BF16)
            vh = kb.sb([128, NCH, 512], BF16)
            Cst = kb.sb([128, 2, 512], F32)
            Cb = kb.sb([128, 2, 512], BF16)
            nst_ = kb.sb([128, 2], F32)
            nb_ = kb.sb([128, 2], BF16)
            mcol = kb.sb([128, 1], F32)
            dg = [kb.sb([128, 128], F32) for _ in range(2)]
            Um = kb.sb([128, 128], F32)
            Rm = kb.sb([128, 128], F32)
            ET = kb.sb([128, 128], F32)
            PT = kb.sb([128, 128], BF16)
            sm = [kb.sb([128, 16], F32) for _ in range(2)]
            mr = kb.sb([128, 2], F32)
            kw = kb.sb([128, 256], BF16)
            tmpn = kb.sb([128, 512], F32)
            num = kb.sb([128, 512], F32)
            hh_ = kb.sb([128, 512], F32)
            junk5 = kb.sb([128, 512], BF16)
            ot = kb.sb([128, 512], F32)
            hout = [kb.sb([128, 512], BF16) for _ in range(2)]
            nit = 0
            for h in range(4):
                kb.dma("sp", qTh[:, :, :], qkc_d[h * 256:(h + 1) * 256, :].rearrange("(j p) t -> p j t", p=128), writes=[qTh])
                kb.dma("sp", kTh[:, :, :], qkc_d[1024 + h * 256:1024 + (h + 1) * 256, :].rearrange("(j p) t -> p j t", p=128), writes=[kTh])
                kb.dma("sp", vh[:, :, :], mv_d[:, h * 512:(h + 1) * 512].rearrange("(c p) v -> p c v", p=128), writes=[vh])
                kb.op("dve", lambda e: e.memset(Cst[:, :, :], 0.0), writes=[Cst])
                kb.op("dve", lambda e: e.memset(Cb[:, :, :], 0.0), writes=[Cb])
                kb.op("dve", lambda e: e.memset(nst_[:, :], 0.0), writes=[nst_])
                kb.op("dve", lambda e: e.memset(nb_[:, :], 0.0), writes=[nb_])
                kb.op("dve", lambda e: e.memset(mcol[:, :], 0.0), writes=[mcol])
                for c in range(NCH):
                    own = c >= NCH // 2
                    if c == NCH // 2:
                        kb.op("dve", lambda e: e.tensor_scalar(out=Cst[:, :, :], in0=Cst[:, :, :], scalar1=flg[:, 0:1], scalar2=None, op0=ALU.mult),
                              reads=[Cst, flg], writes=[Cst])
                        kb.op("dve", lambda e: e.tensor_copy(out=Cb[:, :, :], in_=Cst[:, :, :]), reads=[Cst], writes=[Cb])
                        kb.op("dve", lambda e: e.tensor_scalar(out=nst_[:, :], in0=nst_[:, :], scalar1=flg[:, 0:1], scalar2=None, op0=ALU.mult),
                              reads=[nst_, flg], writes=[nst_])
                        kb.op("dve", lambda e: e.tensor_copy(out=nb_[:, :], in_=nst_[:, :]), reads=[nst_], writes=[nb_])
                        kb.op("dve", lambda e: e.tensor_scalar(out=mcol[:, :], in0=mcol[:, :], scalar1=flg[:, 0:1], scalar2=None, op0=ALU.mult),
                              reads=[mcol, flg], writes=[mcol])
                    cs_ = slice(c * 128, (c + 1) * 128)
                    ucol = U[:, c, h:h + 1]
                    bcol = Bc[:, c, h:h + 1]
                    s0 = sm[nit % 2]
                    nit += 1
                    d0 = dg[0]
                    kb.op("dve", lambda e, d0=d0, ucol=ucol: e.tensor_scalar(out=d0[:, :], in0=identf[:, :], scalar1=ucol, scalar2=None, op0=ALU.mult),
                          reads=[identf, U], writes=[d0])
                    pu = kb.ps()
                    kb.op("pe", lambda e, pu=pu, d0=d0: e.matmul(pu[:, 0:128], lhsT=ONESF, rhs=d0[:, :], start=True, stop=True),
                          reads=[cst, d0], writes=[pu])
                    kb.op("dve", lambda e, pu=pu: e.tensor_tensor(out=Um[:, :], in0=pu[:, 0:128], in1=CTS, op=ALU.add), reads=[pu, cst], writes=[Um])
                    kb.op("dve", lambda e, s0=s0: e.tensor_reduce(out=s0[:, 0:1], in_=Um[:, :], axis=AX.X, op=ALU.max), reads=[Um], writes=[s0])
                    kb.op("dve", lambda e, s0=s0: e.tensor_tensor(out=mr[:, 1:2], in0=s0[:, 0:1], in1=mcol[:, 0:1], op=ALU.max), reads=[s0, mcol], writes=[mr])
                    kb.op("dve", lambda e, bcol=bcol: e.tensor_tensor(out=mr[:, 0:1], in0=mr[:, 1:2], in1=bcol, op=ALU.add), reads=[mr, Bc], writes=[mr])
                    pm = kb.ps()
                    kb.op("pe", lambda e, pm=pm: e.matmul(pm[:, 0:2], lhsT=SEL127, rhs=mr[:, 0:2], start=True, stop=True), reads=[cst, mr], writes=[pm])
                    kb.op("dve", lambda e, pm=pm, s0=s0: e.tensor_copy(out=s0[:, 1:3], in_=pm[:, 0:2]), reads=[pm], writes=[s0])
                    if own:
                        d1 = dg[1]
                        kb.op("dve", lambda e, d1=d1: e.tensor_scalar(out=d1[:, :], in0=identf[:, :], scalar1=mr[:, 1:2], scalar2=None, op0=ALU.mult),
                              reads=[identf, mr], writes=[d1])
                        pr = kb.ps()
                        kb.op("pe", lambda e, pr=pr, d1=d1: e.matmul(pr[:, 0:128], lhsT=ONESF, rhs=d1[:, :], start=True, stop=True),
                              reads=[cst, d1], writes=[pr])
                        kb.op("dve", lambda e, pr=pr: e.tensor_tensor(out=Rm[:, :], in0=pr[:, 0:128], in1=CST, op=ALU.add), reads=[pr, cst], writes=[Rm])
                        kb.op("act", lambda e, ucol=ucol: e.activation(out=ET[:, :], in_=Rm[:, :], func=AF.Exp, scale=-1.0, bias=ucol),
                              reads=[Rm, U], writes=[ET])
                        pS = kb.ps()
                        for j in range(2):
                            kb.op("pe", lambda e, pS=pS, j=j, cs_=cs_: e.matmul(pS[:, 0:128], lhsT=kTh[:, j, cs_], rhs=qTh[:, j, cs_],
                                                                              start=(j == 0), stop=(j == 1)), reads=[kTh, qTh], writes=[pS])
                        kb.op("dve", lambda e, pS=pS: e.tensor_tensor(out=PT[:, :], in0=pS[:, 0:128], in1=ET[:, :], op=ALU.mult),
                              reads=[pS, ET], writes=[PT])
                        pnum = kb.ps()
                        kb.op("pe", lambda e, pnum=pnum, c=c: e.matmul(pnum[:, :], lhsT=PT[:, :], rhs=vh[:, c, :], start=True, stop=True),
                              reads=[PT, vh], writes=[pnum])
                        pden = kb.ps()
                        kb.op("pe", lambda e, pden=pden: e.matmul(pden[:, 0:1], lhsT=PT[:, :], rhs=onesb[:, 0:1], start=True, stop=True),
                              reads=[PT, onesb], writes=[pden])
                        pqc = kb.ps()
                        for j in range(2):
                            kb.op("pe", lambda e, pqc=pqc, j=j, cs_=cs_: e.matmul(pqc[:, :], lhsT=qTh[:, j, cs_], rhs=Cb[:, j, :],
                                                                                start=(j == 0), stop=(j == 1)), reads=[qTh, Cb], writes=[pqc])
                        for j in range(2):
                            kb.op("pe", lambda e, pden=pden, j=j, cs_=cs_: e.matmul(pden[:, 1:2], lhsT=qTh[:, j, cs_], rhs=nb_[:, j:j + 1],
                                                                                  start=(j == 0), stop=(j == 1)), reads=[qTh, nb_], writes=[pden])
                        kb.op("dve", lambda e, s0=s0: e.tensor_tensor(out=s0[:, 3:4], in0=mcol[:, 0:1], in1=mr[:, 1:2], op=ALU.subtract),
                              reads=[mcol, mr], writes=[s0])
                        kb.op("act", lambda e, s0=s0: e.activation(out=s0[:, 3:4], in_=s0[:, 3:4], func=AF.Exp), reads=[s0], writes=[s0])
                        kb.op("act", lambda e, pqc=pqc, s0=s0: e.activation(out=tmpn[:, :], in_=pqc[:, :], func=AF.Copy, scale=s0[:, 3:4]),
                              reads=[pqc, s0], writes=[tmpn])
                        kb.op("dve", lambda e, pnum=pnum: e.tensor_tensor(out=num[:, :], in0=pnum[:, :], in1=tmpn[:, :], op=ALU.add),
                              reads=[pnum, tmpn], writes=[num])
                        kb.op("dve", lambda e, pden=pden, s0=s0: e.tensor_copy(out=s0[:, 9:11], in_=pden[:, 0:2]), reads=[pden], writes=[s0])
                        kb.op("dve", lambda e, s0=s0: e.scalar_tensor_tensor(out=s0[:, 4:5], in0=s0[:, 10:11], scalar=s0[:, 3:4], in1=s0[:, 9:10],
                                                                             op0=ALU.mult, op1=ALU.add), reads=[s0], writes=[s0])
                        kb.op("dve", lambda e, s0=s0: e.tensor_scalar(out=s0[:, 5:6], in0=s0[:, 4:5], scalar1=-1.0, scalar2=None, op0=ALU.mult),
                              reads=[s0], writes=[s0])
                        kb.op("dve", lambda e, s0=s0: e.tensor_tensor(out=s0[:, 5:6], in0=s0[:, 5:6], in1=s0[:, 4:5], op=ALU.max),
                              reads=[s0], writes=[s0])
                        kb.op("act", lambda e, s0=s0: e.activation(out=s0[:, 6:7], in_=mr[:, 0:1], func=AF.Exp, scale=-1.0), reads=[mr], writes=[s0])
                        kb.op("dve", lambda e, s0=s0: e.tensor_tensor(out=s0[:, 7:8], in0=s0[:, 5:6], in1=s0[:, 6:7], op=ALU.max), reads=[s0], writes=[s0])
                        kb.op("dve", lambda e, s0=s0: e.reciprocal(out=s0[:, 8:9], in_=s0[:, 7:8]), reads=[s0], writes=[s0])
                        kb.op("dve", lambda e, s0=s0: e.tensor_scalar(out=hh_[:, :], in0=num[:, :], scalar1=s0[:, 8:9], scalar2=None, op0=ALU.mult),
                              reads=[num, s0], writes=[hh_])
                        kb.op("act", lambda e, s0=s0: e.activation(out=junk5[:, :], in_=hh_[:, :], func=AF.Square, accum_out=s0[:, 11:12]),
                              reads=[hh_], writes=[junk5, s0])
                        kb.op("dve", lambda e, s0=s0: e.tensor_scalar(out=s0[:, 11:12], in0=s0[:, 11:12], scalar1=1.0 / 512, scalar2=EPS,
                                                                      op0=ALU.mult, op1=ALU.add), reads=[s0], writes=[s0])
                        kb.op("act", lambda e, s0=s0: e.activation(out=s0[:, 11:12], in_=s0[:, 11:12], func=AF.Sqrt), reads=[s0], writes=[s0])
                        kb.op("dve", lambda e, s0=s0: e.reciprocal(out=s0[:, 12:13], in_=s0[:, 11:12]), reads=[s0], writes=[s0])
                        tok0 = (c - NCH // 2) * 128
                        kb.dma("sp", ot[:, :], o_d[tok0:tok0 + 128, h * 512:(h + 1) * 512], writes=[ot])
                        kb.op("act", lambda e: e.activation(out=ot[:, :], in_=ot[:, :], func=AF.Sigmoid), reads=[ot], writes=[ot])
                        kb.op("dve", lambda e, h=h: e.tensor_tensor(out=ot[:, :], in0=ot[:, :], in1=mnwb[:, h * 512:(h + 1) * 512], op=ALU.mult),
                              reads=[ot, mnwb], writes=[ot])
                        ho = hout[c % 2]
                        kb.op("dve", lambda e, s0=s0, ho=ho: e.scalar_tensor_tensor(out=ho[:, :], in0=hh_[:, :], scalar=s0[:, 12:13], in1=ot[:, :],
                                                                                   op0=ALU.mult, op1=ALU.mult), reads=[hh_, s0, ot], writes=[ho])
                        kb.dma("sp", hcat_d[tok0:tok0 + 128, h * 512:(h + 1) * 512], ho[:, :], reads=[ho])
                    kb.op("dve", lambda e, s0=s0, ucol=ucol: e.tensor_tensor(out=s0[:, 13:14], in0=ucol, in1=s0[:, 2:3], op=ALU.subtract),
                          reads=[U, s0], writes=[s0])
                    kb.op("dve", lambda e, s0=s0: e.tensor_tensor(out=s0[:, 14:15], in0=mcol[:, 0:1], in1=s0[:, 2:3], op=ALU.subtract),
                          reads=[mcol, s0], writes=[s0])
                    kb.op("act", lambda e, s0=s0: e.activation(out=s0[:, 13:15], in_=s0[:, 13:15], func=AF.Exp), reads=[s0], writes=[s0])
                    pk = kb.ps()
                    for j in range(2):
                        kb.op("pe", lambda e, pk=pk, j=j, cs_=cs_: e.matmul(pk[:, j * 128:(j + 1) * 128], lhsT=kTh[:, j, cs_], rhs=identb[:, :],
                                                                          start=True, stop=True), reads=[kTh, identb], writes=[pk])
                    kb.op("act", lambda e, pk=pk, s0=s0: e.activation(out=kw[:, :], in_=pk[:, 0:256], func=AF.Copy, scale=s0[:, 13:14]),
                          reads=[pk, s0], writes=[kw])
                    pn = kb.ps()
                    for j in range(2):
                        pkv = kb.ps()
                        kb.op("pe", lambda e, pkv=pkv, j=j, c=c: e.matmul(pkv[:, :], lhsT=kw[:, j * 128:(j + 1) * 128], rhs=vh[:, c, :],
                                                                         start=True, stop=True), reads=[kw, vh], writes=[pkv])
                        kb.op("pe", lambda e, pn=pn, j=j: e.matmul(pn[:, j:j + 1], lhsT=kw[:, j * 128:(j + 1) * 128], rhs=onesb[:, 0:1],
                                                                  start=True, stop=True), reads=[kw, onesb], writes=[pn])
                        kb.op("dve", lambda e, pkv=pkv, j=j, s0=s0: e.scalar_tensor_tensor(out=Cst[:, j, :], in0=Cst[:, j, :], scalar=s0[:, 14:15],
                                                                                          in1=pkv[:, :], op0=ALU.mult, op1=ALU.add),
                              reads=[Cst, s0, pkv], writes=[Cst])
                    kb.op("act", lambda e: e.activation(out=Cb[:, :, :], in_=Cst[:, :, :], func=AF.Copy), reads=[Cst], writes=[Cb])
                    kb.op("dve", lambda e, pn=pn, s0=s0: e.scalar_tensor_tensor(out=nst_[:, :], in0=nst_[:, :], scalar=s0[:, 14:15], in1=pn[:, 0:2],
                                                                               op0=ALU.mult, op1=ALU.add), reads=[nst_, s0, pn], writes=[nst_])
                    kb.op("dve", lambda e: e.tensor_copy(out=nb_[:, :], in_=nst_[:, :]), reads=[nst_], writes=[nb_])
                    kb.op("dve", lambda e, s0=s0: e.tensor_copy(out=mcol[:, :], in_=s0[:, 1:2]), reads=[s0], writes=[mcol])


        NTq = TOK // 128
        NTk = T2 // 128
        SCL = 128 ** -0.5
        with kb.phase():
            kb.psum_rot = 6
            kb.pnext = 0
            pO, pL = kb.psum[6], kb.psum[7]
            cst_ = kb.sb([128, NTk, 128], F32)
            kb.dma("sp", cst_[:, :, :], cs_tab.rearrange("(i p) d -> p i d", p=128), writes=[cst_])
            pbt = kb.sb([128, NTq, NBP], F32)
            kb.dma("sp", pbt[:, :, :], pb_tab.rearrange("(i p) n -> p i n", p=128), writes=[pbt])
            ohb = kb.sb([NBP, NBP * 128], BF16)
            kb.dma("pool", ohb[:, :], oneh[:, :], writes=[ohb])
            tri01 = kb.sb([128, 128], BF16)
            kb.op("dve", lambda e: e.tensor_copy(out=tri01[:, :], in_=TRIu), reads=[cst], writes=[tri01])
            xq = kb.sb([128, NTq, 128], F32)
            xk = kb.sb([128, NTk, 128], F32)
            rq = kb.sb([128, NTk, 128], F32)
            t1 = kb.sb([128, NTk, 64], F32)
            qT = kb.sb([128, TOK], BF16)
            kT = kb.sb([128, T2], BF16)
            vk = kb.sb([128, NTk, 132], BF16)
            kb.op("dve", lambda e: e.memset(vk[:, :, :], 1.0), writes=[vk])
            kmT = kb.sb([128, NBP], BF16)
            kms = kb.sb([128, NBP], F32)
            gm = kb.sb([128, NBP], F32)
            t8 = kb.sb([128, 8], F32)
            selb = kb.sb([128, NBP], BF16)
            selT = kb.sb([NBP, TOK], BF16)
            PTm = [kb.sb([128, 512], BF16) for _ in range(3)]
            lrec = [kb.sb([128, 2], F32) for _ in range(2)]
            gmA = kb.sb([128, NTq, NBP], F32)
            t8A = kb.sb([128, NTq, 8], F32)
            selbA = kb.sb([128, NTq, NBP], BF16)
            st_m = {"rot": 0}
            pOs = [pO, pL]
            oo = [kb.sb([128, 128], BF16) for _ in range(2)]

            def rotary(X, R, nt):
                c_, s_ = cst_[:, NTk - nt:NTk, 0:64], cst_[:, NTk - nt:NTk, 64:128]
                x1, x2 = X[:, 0:nt, 0:64], X[:, 0:nt, 64:128]
                T1 = t1[:, 0:nt, :]
                kb.op("dve", lambda e: e.tensor_tensor(out=R[:, 0:nt, 0:64], in0=x1, in1=c_, op=ALU.mult), reads=[X, cst_], writes=[R])
                kb.op("dve", lambda e: e.tensor_tensor(out=T1, in0=x2, in1=s_, op=ALU.mult), reads=[X, cst_], writes=[t1])
                kb.op("dve", lambda e: e.tensor_tensor(out=R[:, 0:nt, 0:64], in0=R[:, 0:nt, 0:64], in1=T1, op=ALU.subtract), reads=[R, t1], writes=[R])
                kb.op("dve", lambda e: e.tensor_tensor(out=R[:, 0:nt, 64:128], in0=x2, in1=c_, op=ALU.mult), reads=[X, cst_], writes=[R])
                kb.op("dve", lambda e: e.tensor_tensor(out=T1, in0=x1, in1=s_, op=ALU.mult), reads=[X, cst_, t1], writes=[t1])
                kb.op("dve", lambda e: e.tensor_tensor(out=R[:, 0:nt, 64:128], in0=R[:, 0:nt, 64:128], in1=T1, op=ALU.add), reads=[R, t1], writes=[R])

            def transp(R, nt, dst):
                for i4 in range(nt // 4):
                    pt = kb.ps()
                    for q in range(4):
                        i = i4 * 4 + q
                        kb.op("pe", lambda e, pt=pt, q=q, i=i: e.matmul(pt[:, q * 128:(q + 1) * 128], lhsT=R[:, i, :], rhs=identf[:, :],
                                                                       start=True, stop=True), reads=[R, identf], writes=[pt])
                    ev_copy(i4, dst[:, i4 * 512:(i4 + 1) * 512], pt[:, :], [pt], [dst])

            nev = 0
            for h in range(16):
                hs = slice(h * 128, (h + 1) * 128)
                kb.dma("sp", xq[:, :, :], aq_d[:, hs].rearrange("(i p) d -> p i d", p=128), writes=[xq])
                kb.dma("sp", xk[:, :, :], ak_d[:, hs].rearrange("(i p) d -> p i d", p=128), writes=[xk])
                kb.dma("sp", vk[:, :, 0:128], av_d[:, hs].rearrange("(i p) d -> p i d", p=128), writes=[vk])
                rotary(xq, rq, NTq)
                transp(rq, NTq, qT)
                rotary(xk, rq, NTk)
                transp(rq, NTk, kT)
                kb.op("dve", lambda e: e.memset(kms[:, :], 0.0), writes=[kms])
                kb.op("dve", lambda e: e.tensor_reduce(out=kms[:, 0:NB], in_=kT[:, :].rearrange("p (n k) -> p n k", k=256), axis=AX.X, op=ALU.add),
                      reads=[kT], writes=[kms])
                kb.op("dve", lambda e: e.tensor_scalar(out=kmT[:, :], in0=kms[:, :], scalar1=1.0 / 256, scalar2=None, op0=ALU.mult),
                      reads=[kms], writes=[kmT])
                pg = kb.ps()
                for i in range(NTq):
                    kb.op("pe", lambda e, i=i: e.matmul(pg[:, i * NBP:(i + 1) * NBP], lhsT=qT[:, i * 128:(i + 1) * 128], rhs=kmT[:, :], start=True, stop=True),
                          reads=[qT, kmT], writes=[pg])
                kb.op("dve", lambda e: e.tensor_tensor(out=gmA[:, :, :].rearrange("p a b -> p (a b)"), in0=pg[:, 0:NTq * NBP],
                                                       in1=pbt[:, :, :].rearrange("p a b -> p (a b)"), op=ALU.add), reads=[pg, pbt], writes=[gmA])
                for i in range(NTq):
                    kb.op("dve", lambda e, i=i: e.max(out=t8A[:, i, :], in_=gmA[:, i, :]), reads=[gmA], writes=[t8A])
                kb.op("dve", lambda e: e.tensor_scalar(out=t8A[:, :, 2:3], in0=t8A[:, :, 2:3], scalar1=-1.0e29, scalar2=None, op0=ALU.max), reads=[t8A], writes=[t8A])
                for i in range(NTq):
                    kb.op("dve", lambda e, i=i: e.tensor_scalar(out=gmA[:, i, :], in0=gmA[:, i, :], scalar1=t8A[:, i, 2:3], scalar2=None, op0=ALU.is_ge),
                          reads=[gmA, t8A], writes=[gmA])
                kb.op("dve", lambda e: e.tensor_scalar(out=selbA[:, :, :], in0=gmA[:, :, :], scalar1=30000.0, scalar2=-30000.0, op0=ALU.mult, op1=ALU.add),
                      reads=[gmA], writes=[selbA])
                for i4 in range(NTq // 4):
                    pt = kb.ps()
                    for q in range(4):
                        i = i4 * 4 + q
                        kb.op("pe", lambda e, pt=pt, q=q, i=i: e.matmul(pt[0:NBP, q * 128:(q + 1) * 128], lhsT=selbA[:, i, :], rhs=identb[:, :], start=True, stop=True),
                              reads=[selbA, identb], writes=[pt])
                    kb.op("act", lambda e, pt=pt, i4=i4: e.activation(out=selT[:, i4 * 512:(i4 + 1) * 512], in_=pt[0:NBP, 0:512], func=AF.Copy), reads=[pt], writes=[selT])
                for jb in range(TOK // 256):
                    q2 = slice(jb * 256, (jb + 1) * 256)
                    gb_ = NB // 2 + jb
                    started = [False, False]

                    def score_past(n):
                        pS = kb.ps()
                        for hf in range(2):
                            ks_ = slice(n * 256 + hf * 128, n * 256 + hf * 128 + 128)
                            kb.op("pe", lambda e, pS=pS, hf=hf, ks_=ks_: e.matmul(pS[:, hf * 256:(hf + 1) * 256], lhsT=kT[:, ks_], rhs=qT[:, q2], start=True, stop=False),
                                  reads=[kT, qT], writes=[pS])
                            kb.op("pe", lambda e, pS=pS, hf=hf, n=n: e.matmul(pS[:, hf * 256:(hf + 1) * 256], lhsT=ohb[:, n * 128:(n + 1) * 128], rhs=selT[:, q2], start=False, stop=True),
                                  reads=[ohb, selT], writes=[pS])
                        P_ = PTm[st_m["rot"] % 3]
                        st_m["rot"] += 1
                        kb.op("act", lambda e, pS=pS, P_=P_: e.activation(out=P_[:, :], in_=pS[:, :], func=AF.Exp, scale=SCL), reads=[pS], writes=[P_])
                        return [(P_, hf * 256 + qt * 128, n * 2 + hf, qt) for hf in range(2) for qt in range(2)]

                    def score_own(qt, hf, tri):
                        pS = kb.ps()
                        ks_ = slice(gb_ * 256 + hf * 128, gb_ * 256 + hf * 128 + 128)
                        qs_ = slice(jb * 256 + qt * 128, jb * 256 + (qt + 1) * 128)
                        kb.op("pe", lambda e, pS=pS: e.matmul(pS[:, 0:128], lhsT=kT[:, ks_], rhs=qT[:, qs_], start=True, stop=True), reads=[kT, qT], writes=[pS])
                        P_ = PTm[st_m["rot"] % 3]
                        st_m["rot"] += 1
                        kb.op("act", lambda e, pS=pS, P_=P_: e.activation(out=P_[:, 0:128], in_=pS[:, 0:128], func=AF.Exp, scale=SCL), reads=[pS], writes=[P_])
                        if tri:
                            kb.op("dve", lambda e, P_=P_: e.tensor_tensor(out=P_[:, 0:128], in0=P_[:, 0:128], in1=tri01[:, :], op=ALU.mult), reads=[P_, tri01], writes=[P_])
                        return [(P_, 0, gb_ * 2 + hf, qt)]

                    def pv(lst, lastflags):
                        for (P_, c0_, ki, qt), last in zip(lst, lastflags):
                            first = not started[qt]
                            started[qt] = True
                            kb.op("pe", lambda e, P_=P_, c0_=c0_, ki=ki, qt=qt, first=first, last=last: e.matmul(
                                pOs[qt][:, 0:129], lhsT=P_[:, c0_:c0_ + 128], rhs=vk[:, ki, 0:129], start=first, stop=last), reads=[P_, vk], writes=[pOs[qt]])

                    prev = None
                    for n in range(gb_):
                        cur = score_past(n)
                        if prev is not None:
                            pv(prev, [False] * 4)
                        prev = cur
                    o0 = score_own(0, 0, True)
                    pv(prev, [False] * 4)
                    o1 = score_own(1, 0, False)
                    pv(o0, [True])
                    o2 = score_own(1, 1, True)
                    pv(o1, [False])
                    pv(o2, [True])
                    for qt in range(2):
                        i = jb * 2 + qt
                        lr = lrec[i % 2]
                        kb.op("dve", lambda e, qt=qt, lr=lr: e.reciprocal(out=lr[:, 0:1], in_=pOs[qt][:, 128:129]), reads=[pOs[qt]], writes=[lr])
                        o_ = oo[i % 2]
                        kb.op("act", lambda e, o_=o_, qt=qt, lr=lr: e.activation(out=o_[:, :], in_=pOs[qt][:, 0:128], func=AF.Copy, scale=lr[:, 0:1]), reads=[pOs[qt], lr], writes=[o_])
                        kb.dma("sp", hcat_d[i * 128:(i + 1) * 128, 2048 + h * 128:2048 + (h + 1) * 128], o_[:, :], reads=[o_])
            kb.psum_rot = 8
            kb.pnext = 0

        modflat = mod_d.rearrange("a b -> (a b)")

        def norm_T(x_t, junk_, xs_t, ssc, Acol, bias_fn, dstT, ti):
            kb.op("act", lambda e: e.activation(out=junk_[:, :], in_=x_t[:, :], func=AF.Square, accum_out=ssc[:, 0:1]), reads=[x_t], writes=[junk_, ssc])
            kb.op("dve", lambda e: e.tensor_scalar(out=ssc[:, 0:1], in0=ssc[:, 0:1], scalar1=1.0 / D, scalar2=EPS, op0=ALU.mult, op1=ALU.add), reads=[ssc], writes=[ssc])
            kb.op("act", lambda e: e.activation(out=ssc[:, 0:1], in_=ssc[:, 0:1], func=AF.Sqrt), reads=[ssc], writes=[ssc])
            kb.op("dve", lambda e: e.reciprocal(out=ssc[:, 1:2], in_=ssc[:, 0:1]), reads=[ssc], writes=[ssc])
            kb.op("dve", lambda e: e.tensor_scalar(out=xs_t[:, :], in0=x_t[:, :], scalar1=ssc[:, 1:2], scalar2=None, op0=ALU.mult), reads=[x_t, ssc], writes=[xs_t])
            for k4 in range(KC // 4):
                pt = kb.ps()
                for q in range(4):
                    kc = k4 * 4 + q
                    kb.op("pe", lambda e, pt=pt, q=q, kc=kc: e.matmul(pt[:, q * 128:(q + 1) * 128], lhsT=xs_t[:, kc * 128:(kc + 1) * 128], rhs=identb[:, :],
                                                                     start=True, stop=True), reads=[xs_t, identb], writes=[pt])
                for q in range(4):
                    kc = k4 * 4 + q
                    if Acol is None:
                        ev_copy(q, dstT[:, kc, ti * 128:(ti + 1) * 128], pt[:, q * 128:(q + 1) * 128], [pt], [dstT.ks[ti]])
                    else:
                        kb.op("act", lambda e, pt=pt, q=q, kc=kc: e.activation(out=dstT[:, kc, ti * 128:(ti + 1) * 128], in_=pt[:, q * 128:(q + 1) * 128],
                                                                              func=AF.Identity, scale=Acol[:, kc:kc + 1], bias=bias_fn(kc)),
                              reads=[pt, Acol, modT], writes=[dstT.ks[ti]])

        wov = w_out.rearrange("(kc p) n -> p kc n", p=128)
        with kb.phase():
            g1b = kb.sb([128, D], F32)
            kb.dma("sp", g1b[:, :], modflat[2 * D:3 * D].partition_broadcast(128), writes=[g1b])
            hT = kb.sb([128, KC, TG], BF16, n=4)
            hb = [kb.sb([128, D], BF16) for _ in range(2)]
            xg = [kb.sb([128, D], F32) for _ in range(4)]
            wb = [kb.sb([128, KC, 512], BF16) for _ in range(2)]
            tmpd = [kb.sb([128, 512], F32) for _ in range(2)]
            junk = hb[0]
            xs2 = hb[1]
            ssc = kb.sb([128, 2], F32)
            x2T = hT
            nw = 0
            for g in range(TOK // TG):
                for ti in range(4):
                    tok0 = g * TG + ti * 128
                    hb_ = hb[ti % 2]
                    kb.dma("sp", hb_[:, :], hcat_d[tok0:tok0 + 128, :], writes=[hb_])
                    kb.dma("sp", xg[ti][:, :], x_own[tok0:tok0 + 128, :], writes=[xg[ti]])
                    for k4 in range(KC // 4):
                        pt = kb.ps()
                        for q in range(4):
                            kc = k4 * 4 + q
                            kb.op("pe", lambda e, pt=pt, q=q, kc=kc, hb_=hb_: e.matmul(pt[:, q * 128:(q + 1) * 128], lhsT=hb_[:, kc * 128:(kc + 1) * 128],
                                                                                     rhs=identb[:, :], start=True, stop=True), reads=[hb_, identb], writes=[pt])
                        for q in range(4):
                            kc = k4 * 4 + q
                            ev_copy(q, hT[:, kc, ti * 128:(ti + 1) * 128], pt[:, q * 128:(q + 1) * 128], [pt], [hT.ks[ti]])
                for dc in range(8):
                    w = wb[nw % 2]
                    nw += 1
                    kb.dma("pool", w[:, :, :], wov[:, :, dc * 512:(dc + 1) * 512], writes=[w])
                    for ti in range(4):
                        pt = kb.ps()
                        for kc in range(KC):
                            kb.op("pe", lambda e, pt=pt, kc=kc, ti=ti, w=w: e.matmul(pt[:, :], lhsT=hT[:, kc, ti * 128:(ti + 1) * 128], rhs=w[:, kc, :],
                                                                                    start=(kc == 0), stop=(kc == KC - 1)), reads=[w, hT.ks[ti]], writes=[pt])
                        td = tmpd[ti % 2]
                        ds_ = slice(dc * 512, (dc + 1) * 512)
                        kb.op("dve", lambda e, pt=pt, td=td, ds_=ds_: e.tensor_tensor(out=td[:, :], in0=pt[:, :], in1=g1b[:, ds_], op=ALU.mult), reads=[pt, g1b], writes=[td])
                        kb.op("pool", lambda e, td=td, ds_=ds_, ti=ti: e.tensor_tensor(out=xg[ti][:, ds_], in0=xg[ti][:, ds_], in1=td[:, :], op=ALU.add),
                              reads=[xg[ti], td], writes=[xg[ti]])
                for ti in range(4):
                    tok0 = g * TG + ti * 128
                    kb.dma("sp", x2_d[tok0:tok0 + 128, :], xg[ti][:, :], reads=[xg[ti]])
                    norm_T(xg[ti], junk, xs2, ssc, A2, lambda kc: modT[:, 96 + kc:97 + kc], x2T, ti)
                kb.dma("sp", xn2T_d[:, :, g * TG:(g + 1) * TG], x2T[:, :, :], reads=x2T.ks)

        NGP = TOK // TG
        with kb.phase():
            xT = kb.sb([128, KC, TG], BF16)
            skb = kb.sb([128, 16, NK], BF16)
            kb.dma("pool", skb[:, :, :], skT[:, :, :], writes=[skb])
            wqb = [kb.sb([128, KC, 128], BF16) for _ in range(2)]
            qsb = [kb.sb([128, TG], BF16) for _ in range(2)]
            S2 = kb.sb([128, 4, 8, NK], F32)
            AA = kb.sb([128, 4, 8, NK], F32)
            NT = kb.sb([128, 4, 8, NK], F32)
            s1b = kb.sb([128, NK], F32)
            T1 = kb.sb([128, 16], F32)
            T2_ = kb.sb([128, 16], F32)
            cand = kb.sb([128, 256], F32)
            candb = kb.sb([128, 256], F32)
            c16 = kb.sb([128, 16], F32)
            e16 = kb.sb([128, 16], F32)
            zc = kb.sb([128, 4], F32)
            nwq = 0
            for g in range(NGP):
                kb.dma("sp", xT[:, :, :], xn2T_d[:, :, g * TG:(g + 1) * TG], writes=[xT])
                for hp in range(16):
                    h, half = hp // 2, hp % 2
                    w = wqb[nwq % 2]
                    qs_ = qsb[nwq % 2]
                    nwq += 1
                    kb.dma("pool", w[:, :, :], wqc[hp].rearrange("p (kc e) -> p kc e", e=128), writes=[w], max_dma_last_dim=4096)
                    pq = kb.ps()
                    for kc in range(KC):
                        kb.op("pe", lambda e, pq=pq, kc=kc, w=w: e.matmul(pq[:, :], lhsT=w[:, kc, :], rhs=xT[:, kc, :], start=(kc == 0), stop=(kc == KC - 1)),
                              reads=[w, xT], writes=[pq])
                    kb.op("act", lambda e, pq=pq, qs_=qs_: e.activation(out=qs_[:, :], in_=pq[:, :], func=AF.Copy), reads=[pq], writes=[qs_])
                    for ti in range(4):
                        psc = kb.ps()
                        kb.op("pe", lambda e, psc=psc, ti=ti, hp=hp, qs_=qs_: e.matmul(psc[:, 0:NK], lhsT=qs_[:, ti * 128:(ti + 1) * 128], rhs=skb[:, hp, :], start=True, stop=True),
                              reads=[qs_, skb], writes=[psc])
                        if half == 0:
                            kb.op("dve", lambda e, psc=psc, ti=ti, h=h: e.tensor_copy(out=AA[:, ti, h, :], in_=psc[:, 0:NK]), reads=[psc], writes=[AA])
                        else:
                            kb.op("dve", lambda e, psc=psc, ti=ti, h=h: e.tensor_copy(out=S2[:, ti, h, :], in_=psc[:, 0:NK]), reads=[psc], writes=[S2])
                for ti in range(4):
                    for h in range(8):
                        for (src, dstT_) in ((AA[:, ti, h, :], T1), (S2[:, ti, h, :], T2_)):
                            kb.op("dve", lambda e, src=src, dstT_=dstT_: e.max(out=dstT_[:, 0:8], in_=src), reads=[AA, S2], writes=[dstT_])
                            kb.op("dve", lambda e, src=src, dstT_=dstT_: e.match_replace(out=s1b[:, :], in_to_replace=dstT_[:, 0:8], in_values=src, imm_value=NEG),
                                  reads=[AA, S2, dstT_], writes=[s1b])
                            kb.op("dve", lambda e, dstT_=dstT_: e.max(out=dstT_[:, 8:16], in_=s1b[:, :]), reads=[s1b], writes=[dstT_])
                        for a_ in range(16):
                            kb.op("dve", lambda e, a_=a_: e.tensor_scalar(out=cand[:, a_ * 16:(a_ + 1) * 16], in0=T2_[:, :], scalar1=T1[:, a_:a_ + 1], scalar2=None, op0=ALU.add),
                                  reads=[T1, T2_], writes=[cand])
                        kb.op("dve", lambda e: e.max(out=c16[:, 0:8], in_=cand[:, :]), reads=[cand], writes=[c16])
                        kb.op("dve", lambda e: e.match_replace(out=candb[:, :], in_to_replace=c16[:, 0:8], in_values=cand[:, :], imm_value=NEG), reads=[cand, c16], writes=[candb])
                        kb.op("dve", lambda e: e.max(out=c16[:, 8:16], in_=candb[:, :]), reads=[candb], writes=[c16])
                        kb.op("dve", lambda e: e.tensor_scalar(out=zc[:, 0:1], in0=c16[:, 0:1], scalar1=-1.0, scalar2=None, op0=ALU.mult), reads=[c16], writes=[zc])
                        kb.op("act", lambda e: e.activation(out=e16[:, :], in_=c16[:, :], func=AF.Exp, bias=zc[:, 0:1], accum_out=zc[:, 1:2]), reads=[c16, zc], writes=[e16, zc])
                        kb.op("act", lambda e: e.activation(out=zc[:, 2:3], in_=zc[:, 1:2], func=AF.Ln), reads=[zc], writes=[zc])
                        kb.op("dve", lambda e: e.tensor_tensor(out=zc[:, 2:3], in0=zc[:, 0:1], in1=zc[:, 2:3], op=ALU.subtract), reads=[zc], writes=[zc])
                        kb.op("dve", lambda e, ti=ti, h=h: e.tensor_scalar(out=NT[:, ti, h, :], in0=AA[:, ti, h, :], scalar1=-1.0, scalar2=c16[:, 15:16], op0=ALU.mult, op1=ALU.add),
                              reads=[AA, c16], writes=[NT])
                        kb.op("dve", lambda e, ti=ti, h=h: e.tensor_scalar(out=AA[:, ti, h, :], in0=AA[:, ti, h, :], scalar1=zc[:, 2:3], scalar2=None, op0=ALU.add),
                              reads=[AA, zc], writes=[AA])
                for j_, t_ in enumerate((S2, AA, NT)):
                    kb.dma("sp", ans_d[g, j_, :, :], t_[:, :, :, :].rearrange("p a b c -> p (a b c)"), reads=[t_])

        with kb.phase():
            kb.psum_rot = 4
            kb.pnext = 0
            pA_, pG_ = [kb.psum[4], kb.psum[5]], [kb.psum[6], kb.psum[7]]
            xT = kb.sb([128, KC, TG], BF16)
            S2 = kb.sb([128, 4, 8, NK], F32)
            AA = kb.sb([128, 4, 8, NK], F32)
            NT = kb.sb([128, 4, 8, NK], F32)
            NUB, NVB, NEB = 2, 3, 10
            Ub = [kb.sb([128, KC, NK], BF16) for _ in range(NUB)]
            Vb = [kb.sb([NK, D], BF16) for _ in range(NVB)]
            Eb = [kb.sb([128, NK], F32) for _ in range(NEB)]
            Gh = [kb.sb([128, NK], BF16) for _ in range(NEB)]
            gel = [kb.sb([NK, TG], F32) for _ in range(2)]
            wT = [kb.sb([NK, TG], BF16) for _ in range(4)]
            ACC = kb.sb([128, 4, D], F32, n=32)
            NP_ = NK // 2
            st_ = {"rot": 0}

            def v_load(c):
                V_ = Vb[c % NVB]
                kb.dma("pool", V_[:, :], vv[c * NK:(c + 1) * NK, :], writes=[V_], max_dma_last_dim=4096)

            def u_load(c):
                U_ = Ub[c % NUB]
                kb.dma("pool", U_[:, :, :], uTc[c].rearrange("p (kc e) -> p kc e", e=NK), writes=[U_], max_dma_last_dim=4096)

            def block(ps2, ps1, ti):
                pend = []
                for k in range(8):
                    if ps2 is not None:
                        c0, c1 = 2 * ps2, 2 * ps2 + 1
                        po = kb.ps()
                        ds_ = slice(k * 512, (k + 1) * 512)
                        kb.op("pe", lambda e, po=po, ds_=ds_: e.matmul(po[:, :], lhsT=wT[c0 % 4][:, ti * 128:(ti + 1) * 128], rhs=Vb[c0 % NVB][:, ds_], start=True, stop=False),
                              reads=[wT[c0 % 4], Vb[c0 % NVB]], writes=[po])
                        kb.op("pe", lambda e, po=po, ds_=ds_: e.matmul(po[:, :], lhsT=wT[c1 % 4][:, ti * 128:(ti + 1) * 128], rhs=Vb[c1 % NVB][:, ds_], start=False, stop=True),
                              reads=[wT[c1 % 4], Vb[c1 % NVB]], writes=[po])
                        if ps2 == 0:
                            kb.op("dve", lambda e, po=po, ds_=ds_: e.tensor_copy(out=ACC[:, ti, ds_], in_=po[:, :]), reads=[po], writes=[ACC.ks[ti * 8 + k]])
                        else:
                            kb.op("dve", lambda e, po=po, ds_=ds_: e.tensor_tensor(out=ACC[:, ti, ds_], in0=ACC[:, ti, ds_], in1=po[:, :], op=ALU.add),
                                  reads=[po, ACC.ks[ti * 8 + k]], writes=[ACC.ks[ti * 8 + k]])
                    if ps1 is not None:
                        q_, hf = ti // 2, ti % 2
                        c = 2 * ps1 + q_
                        U_ = Ub[c % NUB]
                        pA = pA_[q_]
                        for kc in (hf * 16 + 2 * k, hf * 16 + 2 * k + 1):
                            kb.op("pe", lambda e, pA=pA, kc=kc, U_=U_: e.matmul(pA[0:NK, :], lhsT=U_[:, kc, :], rhs=xT[:, kc, :], start=(kc == 0), stop=(kc == KC - 1)),
                                  reads=[U_, xT], writes=[pA])
                        for fn in pend:
                            fn()
                        pend = []
                        h = k
                        for q in range(2):
                            i = 2 * ps1 + q
                            pG = pG_[q]
                            E_ = Eb[st_["rot"] % NEB]
                            G_ = Gh[st_["rot"] % NEB]
                            st_["rot"] += 1
                            kb.op("act", lambda e, E_=E_, h=h, i=i: e.activation(out=E_[:, :], in_=S2[:, ti, h, :], func=AF.Exp, bias=AA[:, ti, h, i:i + 1]),
                                  reads=[S2, AA], writes=[E_])
                            kb.op("dve", lambda e, E_=E_, G_=G_, h=h, i=i: e.scalar_tensor_tensor(out=G_[:, :], in0=S2[:, ti, h, :], scalar=NT[:, ti, h, i:i + 1], in1=E_[:, :],
                                                                                           op0=ALU.is_ge, op1=ALU.mult), reads=[S2, NT, E_], writes=[G_])
                            pend.append(lambda G_=G_, pG=pG, h=h: kb.op(
                                "pe", lambda e: e.matmul(pG[0:NK, ti * 128:(ti + 1) * 128], lhsT=G_[:, :], rhs=identb[:, :], start=(h == 0), stop=(h == 7)),
                                reads=[G_, identb], writes=[pG]))
                for fn in pend:
                    fn()
                if ps1 is not None and ti % 2 == 1:
                    q_ = ti // 2
                    c = 2 * ps1 + q_
                    kb.op("act", lambda e, q_=q_: e.activation(out=gel[q_][:, :], in_=pA_[q_][0:NK, :], func=AF.Gelu), reads=[pA_[q_]], writes=[gel[q_]])
                    if c + 2 < NK:
                        u_load(c + 2)

            def s1_gbuild(p, ti):
                for q in range(2):
                    i = 2 * p + q
                    pG = pG_[q]
                    for h in range(8):
                        E_ = Eb[st_["rot"] % NEB]
                        G_ = Gh[st_["rot"] % NEB]
                        st_["rot"] += 1
                        kb.op("act", lambda e, E_=E_, h=h, i=i: e.activation(out=E_[:, :], in_=S2[:, ti, h, :], func=AF.Exp, bias=AA[:, ti, h, i:i + 1]),
                              reads=[S2, AA], writes=[E_])
                        kb.op("dve", lambda e, E_=E_, G_=G_, h=h, i=i: e.scalar_tensor_tensor(out=G_[:, :], in0=S2[:, ti, h, :], scalar=NT[:, ti, h, i:i + 1], in1=E_[:, :],
                                                                                       op0=ALU.is_ge, op1=ALU.mult), reads=[S2, NT, E_], writes=[G_])
                        kb.op("pe", lambda e, G_=G_, pG=pG, h=h: e.matmul(pG[0:NK, ti * 128:(ti + 1) * 128], lhsT=G_[:, :], rhs=identb[:, :], start=(h == 0), stop=(h == 7)),
                              reads=[G_, identb], writes=[pG])

            def s1_end(p):
                for q in range(2):
                    c = 2 * p + q
                    w_ = wT[c % 4]
                    kb.op("dve", lambda e, w_=w_, q=q: e.tensor_tensor(out=w_[:, :], in0=pG_[q][0:NK, :], in1=gel[q][:, :], op=ALU.mult), reads=[pG_[q], gel[q]], writes=[w_])

            def s2_(p, ti):
                c0, c1 = 2 * p, 2 * p + 1
                for dc in range(8):
                    po = kb.ps()
                    ds_ = slice(dc * 512, (dc + 1) * 512)
                    kb.op("pe", lambda e, po=po, ds_=ds_: e.matmul(po[:, :], lhsT=wT[c0 % 4][:, ti * 128:(ti + 1) * 128], rhs=Vb[c0 % NVB][:, ds_], start=True, stop=False),
                          reads=[wT[c0 % 4], Vb[c0 % NVB]], writes=[po])
                    kb.op("pe", lambda e, po=po, ds_=ds_: e.matmul(po[:, :], lhsT=wT[c1 % 4][:, ti * 128:(ti + 1) * 128], rhs=Vb[c1 % NVB][:, ds_], start=False, stop=True),
                          reads=[wT[c1 % 4], Vb[c1 % NVB]], writes=[po])
                    if p == 0:
                        kb.op("dve", lambda e, po=po, ds_=ds_: e.tensor_copy(out=ACC[:, ti, ds_], in_=po[:, :]), reads=[po], writes=[ACC.ks[ti]])
                    else:
                        kb.op("dve", lambda e, po=po, ds_=ds_: e.tensor_tensor(out=ACC[:, ti, ds_], in0=ACC[:, ti, ds_], in1=po[:, :], op=ALU.add),
                              reads=[po, ACC.ks[ti]], writes=[ACC.ks[ti]])

            for g in range(NGP):
                kb.dma("sp", xT[:, :, :], xn2T_d[:, :, g * TG:(g + 1) * TG], writes=[xT])
                for j_, t_ in enumerate((S2, AA, NT)):
                    kb.dma("sp", t_[:, :, :, :].rearrange("p a b c -> p (a b c)"), ans_d[g, j_, :, :], writes=[t_])
                u_load(0)
                u_load(1)
                v_load(0)
                v_load(1)
                for ti in range(4):
                    block(None, 0, ti)
                s1_end(0)
                for p in range(NP_):
                    nxt = p + 1 < NP_
                    if nxt:
                        v_load(2 * p + 2)
                    for ti in range(4):
                        block(p, p + 1 if nxt else None, ti)
                    if nxt:
                        s1_end(p + 1)
                        v_load(2 * p + 3)
                for ti in range(4):
                    tok0 = g * TG + ti * 128
                    kb.dma("sp", peer_d[tok0:tok0 + 128, :], ACC[:, ti, :], reads=ACC.ks[ti * 8:(ti + 1) * 8])
            kb.psum_rot = 8
            kb.pnext = 0

        with kb.phase():
            g2b = kb.sb([128, D], F32)
            fnb = kb.sb([128, D], F32)
            kb.dma("sp", g2b[:, :], modflat[5 * D:6 * D].partition_broadcast(128), writes=[g2b])
            kb.dma("sp", fnb[:, :], fnw.partition_broadcast(128), writes=[fnb])
            xa = [kb.sb([128, D], F32) for _ in range(2)]
            pa = [kb.sb([128, D], F32) for _ in range(2)]
            junk = kb.sb([128, D], BF16)
            ssf = [kb.sb([128, 2], F32) for _ in range(2)]
            for it in range(TOK // 128):
                x_, p_, ss_ = xa[it % 2], pa[it % 2], ssf[it % 2]
                rs_ = slice(it * 128, (it + 1) * 128)
                kb.dma("sp", x_[:, :], x2_d[rs_, :], writes=[x_])
                kb.dma("sp", p_[:, :], peer_d[rs_, :], writes=[p_])
                kb.op("dve", lambda e, p_=p_: e.tensor_tensor(out=p_[:, :], in0=p_[:, :], in1=g2b[:, :], op=ALU.mult), reads=[p_, g2b], writes=[p_])
                kb.op("pool", lambda e, p_=p_, x_=x_: e.tensor_tensor(out=x_[:, :], in0=x_[:, :], in1=p_[:, :], op=ALU.add), reads=[p_, x_], writes=[x_])
                kb.op("act", lambda e, x_=x_, ss_=ss_: e.activation(out=junk[:, :], in_=x_[:, :], func=AF.Square, accum_out=ss_[:, 0:1]), reads=[x_], writes=[junk, ss_])
                kb.op("dve", lambda e, ss_=ss_: e.tensor_scalar(out=ss_[:, 0:1], in0=ss_[:, 0:1], scalar1=1.0 / D, scalar2=EPS, op0=ALU.mult, op1=ALU.add), reads=[ss_], writes=[ss_])
                kb.op("act", lambda e, ss_=ss_: e.activation(out=ss_[:, 0:1], in_=ss_[:, 0:1], func=AF.Sqrt), reads=[ss_], writes=[ss_])
                kb.op("dve", lambda e, ss_=ss_: e.reciprocal(out=ss_[:, 1:2], in_=ss_[:, 0:1]), reads=[ss_], writes=[ss_])
                kb.op("dve", lambda e, x_=x_, p_=p_, ss_=ss_: e.scalar_tensor_tensor(out=p_[:, :], in0=x_[:, :], scalar=ss_[:, 1:2], in1=fnb[:, :], op0=ALU.mult, op1=ALU.mult),
                      reads=[x_, ss_, fnb], writes=[p_])
                kb.dma("sp", y[rs_, :], p_[:, :], reads=[p_])


        kb.barrier()
    return nc


def host_shared(cfg, inp):
    TOK, T2, NK, NB, NCH = cfg.TOK, cfg.T2, cfg.NK, cfg.NB, cfg.NCH
    NBP = max(NB, 8)
    m = {}
    idx = np.arange(128)
    le = (idx[:, None] <= idx[None, :])
    consts = np.zeros((128, 5, 128), np.float32)
    consts[:, 0, :] = le.astype(np.float32)
    consts[:, 1, :] = np.where(le.T, 0.0, NEG)
    consts[:, 2, :] = np.where(le, 0.0, 1.0e30)
    consts[127, 3, :] = 1.0
    consts[:, 4, :] = 1.0
    m["consts"] = consts
    m["ident_f"] = np.eye(128, dtype=np.float32)
    m["w_ada"] = inp["w_ada"][0]
    m["b_adaT"] = np.ascontiguousarray(inp["b_ada"][0].reshape(192, 128).T)
    m["n1wT"] = np.ascontiguousarray(inp["norm1_w"][0].reshape(KC, 128).T)
    m["n2wT"] = np.ascontiguousarray(inp["norm2_w"][0].reshape(KC, 128).T)
    m["fnw"] = np.ascontiguousarray(inp["final_norm_w"])
    m["w_in"] = inp["w_in"][0]
    m["conv_wT"] = np.ascontiguousarray(inp["conv_w"][0].T.reshape(16, 128, 4).transpose(1, 0, 2))
    m["conv_bT"] = np.ascontiguousarray(inp["conv_b"][0].reshape(16, 128).T)
    gb = np.concatenate([inp["b_igate"][0], inp["b_fgate"][0]]).astype(np.float32)
    m["gbias"] = np.ascontiguousarray(np.broadcast_to(gb[None, None, :], (128, NCH, 8)))
    m["mnw"] = np.ascontiguousarray(inp["mlstm_norm_w"][0].reshape(2048))
    oneh = np.zeros((NBP, NBP, 128), np.float32)
    for n in range(NBP):
        oneh[n, n, :] = 1.0
    m["oneh"] = oneh.reshape(NBP, NBP * 128)
    m["w_out"] = inp["w_out"][0]
    m["wqc"] = np.ascontiguousarray(inp["peer_wq"][0].reshape(KC, 128, 16, 128).transpose(2, 1, 0, 3)).reshape(16, 128, KC * 128)
    m["skT"] = np.ascontiguousarray(inp["peer_subkeys"][0].reshape(16, NK, 128).transpose(2, 0, 1))
    m["uTc"] = np.ascontiguousarray(inp["peer_u"][0].reshape(NK, NK, KC, 128).transpose(0, 3, 2, 1)).reshape(NK, 128, KC * NK)
    m["vv"] = inp["peer_v"][0]
    return m


def host_inputs(cfg, core, b, s, inp, shared):
    TOK, T2, NB = cfg.TOK, cfg.T2, cfg.NB
    NBP = max(NB, 8)
    x = inp["x"]
    m = dict(shared)
    own0 = s * TOK
    m["x_own"] = np.ascontiguousarray(x[b, own0:own0 + TOK])
    m["x_past"] = np.ascontiguousarray(x[b, 0:TOK]) if s == 1 else np.zeros((TOK, D), np.float32)
    m["cT"] = np.ascontiguousarray(inp["c"][b].reshape(KC, 128).T)
    m["flag"] = np.full((128, 1), float(s), np.float32)
    pos = np.concatenate([np.arange(TOK), s * TOK + np.arange(TOK)]).astype(np.float32)
    half = 64
    inv = (np.float32(10000.0) ** (-np.arange(half, dtype=np.float32) / np.float32(half))).astype(np.float32)
    ang = (pos[:, None] * inv[None, :]).astype(np.float32)
    m["cs_tab"] = np.concatenate([np.cos(ang), np.sin(ang)], axis=1).astype(np.float32)
    pb = np.full((TOK, NBP), NEG, np.float32)
    for q0 in range(0, TOK, 256):
        jb = NB // 2 + q0 // 256
        lo = 0 if s == 1 else NB // 2
        pb[q0:q0 + 256, lo:jb] = 0.0
    m["pb_tab"] = pb
    return m


_CACHE = {}


def kernel(**inp):
    inp = {k: np.asarray(v) for k, v in inp.items()}
    cfg = Cfg(tok=2048, nk=128)
    if "nc" not in _CACHE:
        _CACHE["nc"] = build(cfg)
    nc = _CACHE["nc"]
    shared = host_shared(cfg, inp)
    maps = []
    for core in range(8):
        b, s = core // 2, core % 2
        maps.append(host_inputs(cfg, core, b, s, inp, shared))
    res = run_bass_kernel_spmd(nc, maps, core_ids=list(range(8)))
    out = np.zeros((4, 4096, D), np.float32)
    for core in range(8):
        b, s = core // 2, core % 2
        out[b, s * cfg.TOK:(s + 1) * cfg.TOK] = res.results[core]["y"]
    return out
```

```python
import numpy as np
from contextlib import ExitStack
import ml_dtypes
import concourse.bass as bass
import concourse.mybir as mybir
from concourse.bass_utils import run_bass_kernel_spmd

F32 = mybir.dt.float32
BF16 = mybir.dt.bfloat16
AF = mybir.ActivationFunctionType
ALU = mybir.AluOpType
AX = mybir.AxisListType

D = 4096
KC = 32
NDS = 24
EPS = 1e-6
NEG = -1.0e30


class Tk:
    __slots__ = ("w", "rs")

    def __init__(self):
        self.w = None
        self.rs = {}


class Tile:
    def __init__(self, t, n=1):
        self.t = t
        self.k = Tk()
        self.ks = [Tk() for _ in range(n)]

    def __getitem__(self, idx):
        return self.t[idx]


class KB:
    EPOCH = 20000

    def __init__(self, nc, st):
        self.nc = nc
        self.st = st
        self.eng = {"pe": nc.tensor, "act": nc.scalar, "dve": nc.vector, "pool": nc.gpsimd, "sp": nc.sync}
        self.cnt = {e: 0 for e in self.eng}
        self.sem = {e: self.newsem() for e in self.eng}
        self.last = {e: None for e in self.eng}
        self.seen = {}
        self.dsem = [self.newsem() for _ in range(NDS)]
        self.dval = [0] * NDS
        self.dnext = 0
        self.psum = []
        self.pnext = 0
        for i in range(8):
            self.psum.append(Tile(st.enter_context(nc.psum_tensor(f"ps{i}", [128, 512], F32))))
        self.pst = None

    def newsem(self):
        self._ns = getattr(self, "_ns", 0) + 1
        return self.st.enter_context(self.nc.semaphore(f"sem{self._ns}"))

    psum_rot = 8

    def ps(self):
        p = self.psum[self.pnext]
        self.pnext = (self.pnext + 1) % self.psum_rot
        return p

    def sb(self, shape, dtype, n=1, name=None):
        self._nm = getattr(self, "_nm", 0) + 1
        t = self.pst.enter_context(self.nc.sbuf_tensor(name or f"sb{self._nm}", list(shape), dtype))
        return Tile(t, n)

    def _wait(self, e, tk):
        sem, val = tk[0], tk[1]
        key = (e, id(sem))
        if self.seen.get(key, 0) >= val:
            return
        self.eng[e].wait_ge(sem, val)
        self.seen[key] = val

    def _deps(self, e, reads, writes):
        ticks = []
        for t in reads:
            if t.w is not None:
                ticks.append(t.w)
        for t in writes:
            if t.w is not None:
                ticks.append(t.w)
            ticks.extend(t.rs.values())
        for tk in ticks:
            if tk[2] == e and e == "pe":
                continue
            self._wait(e, tk)

    def _mark(self, tk, reads, writes):
        for t in reads:
            t.rs[id(tk[0])] = tk
        for t in writes:
            t.w = tk
            t.rs = {}

    @staticmethod
    def _tk(objs):
        out = []
        for o in objs:
            out.append(o.k if isinstance(o, Tile) else o)
        return out

    def op(self, e, fn, reads=(), writes=()):
        reads, writes = self._tk(reads), self._tk(writes)
        self._deps(e, reads, writes)
        if self.cnt[e] >= self.EPOCH:
            self.sem[e] = self.newsem()
            self.cnt[e] = 0
        self.cnt[e] += 1
        ins = fn(self.eng[e])
        ins.then_inc(self.sem[e], 1)
        tk = (self.sem[e], self.cnt[e], e)
        self.last[e] = tk
        self._mark(tk, reads, writes)

    def dma(self, q, out, in_, reads=(), writes=(), **kw):
        reads, writes = self._tk(reads), self._tk(writes)
        self._deps(q, reads, writes)
        i = self.dnext
        self.dnext = (self.dnext + 1) % NDS
        if self.dval[i] > 0:
            self._wait(q, (self.dsem[i], self.dval[i]))
        self.dval[i] += 16
        self.eng[q].dma_start(out=out, in_=in_, **kw).then_inc(self.dsem[i], 16)
        tk = (self.dsem[i], self.dval[i], "dma")
        self._mark(tk, reads, writes)

    def barrier(self):
        for e in self.eng:
            for e2 in self.eng:
                if e2 != e and self.last[e2] is not None:
                    self._wait(e, self.last[e2])
            for i in range(NDS):
                if self.dval[i] > 0:
                    self._wait(e, (self.dsem[i], self.dval[i]))

    def phase(self):
        kb = self

        class _P:
            def __enter__(s):
                kb.pst = ExitStack()
                kb.pst.__enter__()
                return kb

            def __exit__(s, *a):
                kb.barrier()
                kb.pst.__exit__(*a)
                kb.pst = None
                return False

        return _P()


class Cfg:
    def __init__(self, tok=2048, nk=128):
        self.TOK = tok
        self.T2 = 2 * tok
        self.NK = nk
        self.NE = nk * nk
        self.NB = self.T2 // 256
        self.NCH = self.T2 // 128


def build(cfg, upto=99):
    TOK, T2 = cfg.TOK, cfg.T2
    nc = bass.Bass("TRN2", target_bir_lowering=False)

    def din(name, shape, dt=F32):
        return nc.dram_tensor(name, list(shape), dt, kind="ExternalInput").ap()

    def dscr(name, shape, dt=F32):
        return nc.dram_tensor(name, list(shape), dt).ap()

    x_own = din("x_own", [TOK, D])
    x_past = din("x_past", [TOK, D])
    cT = din("cT", [128, KC])
    w_ada = din("w_ada", [D, 6 * D])
    b_adaT = din("b_adaT", [128, 192])
    n1wT = din("n1wT", [128, KC])
    n2wT = din("n2wT", [128, KC])
    fnw = din("fnw", [D])
    w_in = din("w_in", [D, 12296])
    flag = din("flag", [128, 1])
    ident_f = din("ident_f", [128, 128])
    y = nc.dram_tensor("y", [TOK, D], F32, kind="ExternalOutput").ap()
    NK, NE, NB, NCH = cfg.NK, cfg.NE, cfg.NB, cfg.NCH
    NBP = max(NB, 8)
    consts = din("consts", [128, 5, 128])
    conv_wT = din("conv_wT", [128, 16, 4])
    conv_bT = din("conv_bT", [128, 16])
    gbias = din("gbias", [128, NCH, 8])
    mnw = din("mnw", [2048])
    cs_tab = din("cs_tab", [T2, 128])
    pb_tab = din("pb_tab", [TOK, NBP])
    oneh = din("oneh", [NBP, NBP * 128])
    w_out = din("w_out", [D, D])
    wqc = din("wqc", [16, 128, KC * 128])
    skT = din("skT", [128, 16, NK])
    uTc = din("uTc", [NK, 128, KC * NK])
    vv = din("vv", [NE, D])
    qkc_d = dscr("qkc_d", [2048, T2], BF16)
    hcat_d = dscr("hcat_d", [TOK, D], BF16)
    x2_d = dscr("x2_d", [TOK, D])
    xn2T_d = dscr("xn2T_d", [128, KC, TOK], BF16)
    s_d = dscr("s_d", [TOK, 16, NK])
    peer_d = dscr("peer_d", [TOK, D])
    ans_d = dscr("ans_d", [TOK // 512, 3, 128, 4 * 8 * NK])

    mv_d = dscr("mv_d", [T2, 2048], BF16)
    o_d = dscr("o_d", [TOK, 2048])
    if_d = dscr("if_d", [T2, 8])
    aq_d = dscr("aq_d", [TOK, 2048])
    ak_d = dscr("ak_d", [T2, 2048])
    av_d = dscr("av_d", [T2, 2048], BF16)
    qkT_d = dscr("qkT_d", [2048, 4 + T2])
    mod_d = dscr("mod_d", [192, 128])
    dbg = {}

    with ExitStack() as st:
        kb = KB(nc, st)
        kb.pst = st
        modT = kb.sb([128, 192], F32, name="modT")
        A1 = kb.sb([128, KC], F32, name="A1")
        A1p = kb.sb([128, KC], F32, name="A1p")
        S1p = kb.sb([128, KC], F32, name="S1p")
        A2 = kb.sb([128, KC], F32, name="A2")
        identb = kb.sb([128, 128], BF16, name="identb")
        identf = kb.sb([128, 128], F32, name="identf")
        flg = kb.sb([128, 1], F32, name="flg")
        cst = kb.sb([128, 5, 128], F32, name="cst")
        onesb = kb.sb([128, 128], BF16, name="onesb")
        kb.pst = None
        kb.dma("sp", cst[:, :, :], consts[:, :, :], writes=[cst])
        kb.dma("pool", onesb[:, :], consts[:, 4, :], writes=[onesb])
        kb.dma("sp", identf[:, :], ident_f[:, :], writes=[identf])
        kb.dma("pool", identb[:, :], ident_f[:, :], writes=[identb])
        kb.dma("sp", flg[:, :], flag[:, :], writes=[flg])

        with kb.phase():
            c_sb = kb.sb([128, KC], F32)
            cs = kb.sb([128, KC], BF16)
            bad = kb.sb([128, 192], F32)
            n1 = kb.sb([128, KC], F32)
            n2 = kb.sb([128, KC], F32)
            wb = [kb.sb([128, KC, 512], BF16) for _ in range(2)]
            kb.dma("sp", c_sb[:, :], cT[:, :], writes=[c_sb])
            kb.dma("sp", bad[:, :], b_adaT[:, :], writes=[bad])
            kb.dma("sp", n1[:, :], n1wT[:, :], writes=[n1])
            kb.dma("sp", n2[:, :], n2wT[:, :], writes=[n2])
            kb.op("act", lambda e: e.activation(out=cs[:, :], in_=c_sb[:, :], func=AF.Silu), reads=[c_sb], writes=[cs])
            wv = w_ada.rearrange("(kc p) n -> p kc n", p=128)
            pacc = kb.ps()
            for cc in range(48):
                w = wb[cc % 2]
                kb.dma("pool", w[:, :, :], wv[:, :, cc * 512:(cc + 1) * 512], writes=[w])
                for sub in range(4):
                    j = cc * 4 + sub
                    for kc in range(KC):
                        kb.op("pe", lambda e, w=w, kc=kc, sub=sub, j=j: e.matmul(
                            pacc[:, j:j + 1], lhsT=w[:, kc, sub * 128:(sub + 1) * 128], rhs=cs[:, kc:kc + 1],
                            start=(kc == 0), stop=(kc == KC - 1)), reads=[w, cs], writes=[pacc])
            kb.op("dve", lambda e: e.tensor_tensor(out=modT[:, :], in0=pacc[:, 0:192], in1=bad[:, :], op=ALU.add),
                  reads=[pacc, bad], writes=[modT])
            kb.op("dve", lambda e: e.scalar_tensor_tensor(out=A1[:, :], in0=modT[:, 32:64], scalar=1.0, in1=n1[:, :],
                                                          op0=ALU.add, op1=ALU.mult), reads=[modT, n1], writes=[A1])
            kb.op("dve", lambda e: e.scalar_tensor_tensor(out=A2[:, :], in0=modT[:, 128:160], scalar=1.0, in1=n2[:, :],
                                                          op0=ALU.add, op1=ALU.mult), reads=[modT, n2], writes=[A2])
            kb.op("dve", lambda e: e.tensor_scalar(out=A1p[:, :], in0=A1[:, :], scalar1=flg[:, 0:1], scalar2=None,
                                                   op0=ALU.mult), reads=[A1, flg], writes=[A1p])
            kb.op("dve", lambda e: e.tensor_scalar(out=S1p[:, :], in0=modT[:, 0:32], scalar1=flg[:, 0:1], scalar2=None,
                                                   op0=ALU.mult), reads=[modT, flg], writes=[S1p])
            mrow = kb.sb([128, 2, 128], F32)
            for hh in range(2):
                pt = kb.ps()
                n = 128 if hh == 0 else 64
                kb.op("pe", lambda e, pt=pt, hh=hh, n=n: e.matmul(pt[0:n, 0:128], lhsT=modT[:, hh * 128:hh * 128 + n],
                                                                  rhs=identf[:, :], start=True, stop=True),
                      reads=[modT, identf], writes=[pt])
                kb.op("act", lambda e, pt=pt, hh=hh, n=n: e.activation(out=mrow[0:n, hh, :], in_=pt[0:n, 0:128], func=AF.Copy),
                      reads=[pt], writes=[mrow])
                kb.dma("sp", mod_d[hh * 128:hh * 128 + n, :], mrow[0:n, hh, :], reads=[mrow])
        if upto <= 1:
            dbg["modT"] = None

        TG = 512
        TGB = 1024 if TOK % 1024 == 0 else 512
        NG = T2 // TGB
        NTB = TGB // 128
        wiv = w_in.rearrange("(kc p) n -> p kc n", p=128)
        chunks = []
        for i in range(4):
            chunks.append((i * 512, 512, "qk"))
        for i in range(4):
            chunks.append((2048 + i * 512, 512, "mv"))
        for i in range(4):
            chunks.append((4096 + i * 512, 512, "o"))
        chunks.append((6144, 8, "if"))
        for i in range(4):
            chunks.append((6152 + i * 512, 512, "aq"))
        for i in range(4):
            chunks.append((8200 + i * 512, 512, "ak"))
        for i in range(4):
            chunks.append((10248 + i * 512, 512, "av"))
        with kb.phase():
            xt = [kb.sb([128, D], F32) for _ in range(2)]
            junk = kb.sb([128, D], BF16)
            xs = [kb.sb([128, D], BF16) for _ in range(2)]
            ss = [kb.sb([128, 1], F32) for _ in range(2)]
            xnT = kb.sb([128, KC, TGB], BF16, n=NTB)
            wb = [kb.sb([128, KC, 512], BF16) for _ in range(2)]
            stf = [kb.sb([128, 512], F32) for _ in range(3)]
            stb = [kb.sb([128, 512], BF16) for _ in range(3)]
            zpad = kb.sb([128, 4], F32)
            kb.op("dve", lambda e: e.memset(zpad[:, :], 0.0), writes=[zpad])
            for cc in range(16):
                kb.dma("sp", qkT_d[cc * 128:(cc + 1) * 128, 0:4], zpad[:, :], reads=[zpad])
            nst = 0
            nw = 0
            for g in range(NG):
                past = (g * TGB) < TOK
                for ti in range(NTB):
                    tok0 = g * TGB + ti * 128
                    src = x_past[tok0:tok0 + 128, :] if past else x_own[tok0 - TOK:tok0 - TOK + 128, :]
                    it = g * NTB + ti
                    x_t, xs_t, ss_t = xt[it % 2], xs[it % 2], ss[it % 2]
                    kb.dma("sp", x_t[:, :], src, writes=[x_t])
                    kb.op("act", lambda e, x_t=x_t, ss_t=ss_t: e.activation(out=junk[:, :], in_=x_t[:, :], func=AF.Square,
                                                                           accum_out=ss_t[:, 0:1]),
                          reads=[x_t], writes=[junk, ss_t])
                    kb.op("dve", lambda e, ss_t=ss_t: e.tensor_scalar(out=ss_t[:, :], in0=ss_t[:, :], scalar1=1.0 / D,
                                                                      scalar2=EPS, op0=ALU.mult, op1=ALU.add),
                          reads=[ss_t], writes=[ss_t])
                    kb.op("act", lambda e, ss_t=ss_t: e.activation(out=ss_t[:, :], in_=ss_t[:, :], func=AF.Sqrt),
                          reads=[ss_t], writes=[ss_t])
                    kb.op("dve", lambda e, ss_t=ss_t: e.reciprocal(out=ss_t[:, :], in_=ss_t[:, :]), reads=[ss_t], writes=[ss_t])
                    kb.op("dve", lambda e, x_t=x_t, xs_t=xs_t, ss_t=ss_t: e.tensor_scalar(
                        out=xs_t[:, :], in0=x_t[:, :], scalar1=ss_t[:, 0:1], scalar2=None, op0=ALU.mult),
                        reads=[x_t, ss_t], writes=[xs_t])
                    Asel, Ssel = (A1p, S1p) if past else (A1, None)
                    for k4 in range(KC // 4):
                        pt = kb.ps()
                        for q in range(4):
                            kc = k4 * 4 + q
                            kb.op("pe", lambda e, pt=pt, q=q, kc=kc, xs_t=xs_t: e.matmul(
                                pt[:, q * 128:(q + 1) * 128], lhsT=xs_t[:, kc * 128:(kc + 1) * 128], rhs=identb[:, :],
                                start=True, stop=True), reads=[xs_t, identb], writes=[pt])
                        for q in range(4):
                            kc = k4 * 4 + q
                            bias_ap = (S1p[:, kc:kc + 1] if past else modT[:, kc:kc + 1])
                            kb.op("act", lambda e, pt=pt, q=q, kc=kc, ti=ti, Asel=Asel, bias_ap=bias_ap: e.activation(
                                out=xnT[:, kc, ti * 128:(ti + 1) * 128], in_=pt[:, q * 128:(q + 1) * 128],
                                func=AF.Identity, scale=Asel[:, kc:kc + 1], bias=bias_ap),
                                reads=[pt, Asel, S1p if past else modT], writes=[xnT.ks[ti]])
                for (c0, ncol, kind) in chunks:
                    if past and kind in ("o", "aq"):
                        continue
                    w = wb[nw % 2]
                    nw += 1
                    kb.dma("pool", w[:, :, 0:ncol], wiv[:, :, c0:c0 + ncol], writes=[w])
                    if kind == "qk":
                        for sub, tb in [(a_, b_) for a_ in range(4) for b_ in range(TGB // 512)]:
                            pt = kb.ps()
                            for kc in range(KC):
                                kb.op("pe", lambda e, pt=pt, kc=kc, sub=sub, w=w, tb=tb: e.matmul(
                                    pt[:, :], lhsT=w[:, kc, sub * 128:(sub + 1) * 128], rhs=xnT[:, kc, tb * 512:(tb + 1) * 512],
                                    start=(kc == 0), stop=(kc == KC - 1)), reads=[w] + xnT.ks, writes=[pt])
                            s_t = stf[nst % 3]
                            ev = "act" if nst % 2 == 0 else "dve"
                            nst += 1
                            if ev == "act":
                                kb.op("act", lambda e, pt=pt, s_t=s_t: e.activation(out=s_t[:, :], in_=pt[:, :], func=AF.Copy),
                                      reads=[pt], writes=[s_t])
                            else:
                                kb.op("dve", lambda e, pt=pt, s_t=s_t: e.tensor_copy(out=s_t[:, :], in_=pt[:, :]),
                                      reads=[pt], writes=[s_t])
                            ch0 = c0 + sub * 128
                            kb.dma("sp", qkT_d[ch0:ch0 + 128, 4 + g * TGB + tb * 512:4 + g * TGB + (tb + 1) * 512], s_t[:, :], reads=[s_t])
                    else:
                        for ti in range(NTB):
                            tok0 = g * TGB + ti * 128
                            pt = kb.ps()
                            for kc in range(KC):
                                kb.op("pe", lambda e, pt=pt, kc=kc, ti=ti, w=w, ncol=ncol: e.matmul(
                                    pt[:, 0:ncol], lhsT=xnT[:, kc, ti * 128:(ti + 1) * 128], rhs=w[:, kc, 0:ncol],
                                    start=(kc == 0), stop=(kc == KC - 1)), reads=[w, xnT.ks[ti]], writes=[pt])
                            isb = kind in ("mv", "av")
                            s_t = (stb if isb else stf)[nst % 3]
                            ev = "act" if nst % 2 == 0 else "dve"
                            nst += 1
                            if ev == "act":
                                kb.op("act", lambda e, pt=pt, s_t=s_t, ncol=ncol: e.activation(out=s_t[:, 0:ncol], in_=pt[:, 0:ncol],
                                                                                              func=AF.Copy), reads=[pt], writes=[s_t])
                            else:
                                kb.op("dve", lambda e, pt=pt, s_t=s_t, ncol=ncol: e.tensor_copy(out=s_t[:, 0:ncol], in_=pt[:, 0:ncol]),
                                      reads=[pt], writes=[s_t])
                            if kind == "mv":
                                dst = mv_d[tok0:tok0 + 128, c0 - 2048:c0 - 2048 + 512]
                            elif kind == "o":
                                dst = o_d[tok0 - TOK:tok0 - TOK + 128, c0 - 4096:c0 - 4096 + 512]
                            elif kind == "if":
                                dst = if_d[tok0:tok0 + 128, 0:8]
                            elif kind == "aq":
                                dst = aq_d[tok0 - TOK:tok0 - TOK + 128, c0 - 6152:c0 - 6152 + 512]
                            elif kind == "ak":
                                dst = ak_d[tok0:tok0 + 128, c0 - 8200:c0 - 8200 + 512]
                            else:
                                dst = av_d[tok0:tok0 + 128, c0 - 10248:c0 - 10248 + 512]
                            kb.dma("sp", dst, s_t[:, 0:ncol], reads=[s_t])

        if upto <= 2:
            for nm, ap_, shp, dt in (("d_mod", mod_d, [192, 128], F32), ("d_qkT", qkT_d, [2048, 4 + T2], F32),
                                     ("d_if", if_d, [T2, 8], F32), ("d_ak", ak_d, [T2, 2048], F32),
                                     ("d_mv", mv_d, [T2, 2048], BF16), ("d_o", o_d, [TOK, 2048], F32)):
                o = nc.dram_tensor(nm, shp, dt, kind="ExternalOutput").ap()
                kb.dma("sp", o, ap_, )
            kb.barrier()
            return nc


        TRIu = cst[:, 0, :]
        CTS = cst[:, 1, :]
        CST = cst[:, 2, :]
        SEL127 = cst[:, 3, :]
        ONESF = cst[:, 4, :]

        def ev_copy(i, out, in_, rd, wr):
            if i % 2 == 0:
                kb.op("act", lambda e: e.activation(out=out, in_=in_, func=AF.Copy), reads=rd, writes=wr)
            else:
                kb.op("dve", lambda e: e.tensor_copy(out=out, in_=in_), reads=rd, writes=wr)

        with kb.phase():
            cw = kb.sb([128, 16, 4], F32)
            cb = kb.sb([128, 16], F32)
            kb.dma("sp", cw[:, :, :], conv_wT[:, :, :], writes=[cw])
            kb.dma("sp", cb[:, :], conv_bT[:, :], writes=[cb])
            ub = [kb.sb([128, 4 + T2], F32) for _ in range(2)]
            acc = kb.sb([128, T2], F32)
            ob = [kb.sb([128, T2], BF16) for _ in range(2)]
            for cc in range(16):
                u = ub[cc % 2]
                o_ = ob[cc % 2]
                kb.dma("sp", u[:, :], qkT_d[cc * 128:(cc + 1) * 128, :], writes=[u])
                kb.op("dve", lambda e, u=u, cc=cc: e.tensor_scalar(out=acc[:, :], in0=u[:, 1:1 + T2], scalar1=cw[:, cc, 0:1],
                                                                   scalar2=None, op0=ALU.mult), reads=[u, cw], writes=[acc])
                for j in range(1, 4):
                    kb.op("dve", lambda e, u=u, cc=cc, j=j: e.scalar_tensor_tensor(
                        out=acc[:, :], in0=u[:, 1 + j:1 + j + T2], scalar=cw[:, cc, j:j + 1], in1=acc[:, :],
                        op0=ALU.mult, op1=ALU.add), reads=[u, cw, acc], writes=[acc])
                kb.op("act", lambda e, o_=o_, cc=cc: e.activation(out=o_[:, :], in_=acc[:, :], func=AF.Silu, bias=cb[:, cc:cc + 1]),
                      reads=[acc, cb], writes=[o_])
                if cc < 8:
                    kb.op("dve", lambda e, o_=o_: e.tensor_scalar(out=o_[:, :], in0=o_[:, :], scalar1=0.0625, scalar2=None,
                                                                  op0=ALU.mult), reads=[o_], writes=[o_])
                kb.dma("sp", qkc_d[cc * 128:(cc + 1) * 128, :], o_[:, :], reads=[o_])

        with kb.phase():
            G = kb.sb([128, NCH, 8], F32)
            gb = kb.sb([128, NCH, 8], F32)
            LI = kb.sb([128, NCH, 4], F32)
            LF = kb.sb([128, NCH, 4], F32)
            Bc = kb.sb([128, NCH, 4], F32)
            U = kb.sb([128, NCH, 4], F32)
            kb.dma("sp", G[:, :, :], if_d.rearrange("(c p) g -> p c g", p=128), writes=[G])
            kb.dma("sp", gb[:, :, :], gbias[:, :, :], writes=[gb])
            kb.op("dve", lambda e: e.tensor_tensor(out=G[:, :, :], in0=G[:, :, :], in1=gb[:, :, :], op=ALU.add), reads=[G, gb], writes=[G])
            kb.op("act", lambda e: e.activation(out=G[:, :, :], in_=G[:, :, :], func=AF.Tanh, scale=1.0 / 15.0), reads=[G], writes=[G])
            kb.op("dve", lambda e: e.tensor_scalar(out=LI[:, :, :], in0=G[:, :, 0:4], scalar1=15.0, scalar2=None, op0=ALU.mult),
                  reads=[G], writes=[LI])
            kb.op("act", lambda e: e.activation(out=LF[:, :, :], in_=G[:, :, 4:8], func=AF.Exp, scale=-15.0), reads=[G], writes=[LF])
            kb.op("act", lambda e: e.activation(out=LF[:, :, :], in_=LF[:, :, :], func=AF.Ln, bias=1.0), reads=[LF], writes=[LF])
            kb.op("dve", lambda e: e.tensor_scalar(out=LF[:, :, :], in0=LF[:, :, :], scalar1=-1.0, scalar2=None, op0=ALU.mult),
                  reads=[LF], writes=[LF])
            pb_ = kb.ps()
            kb.op("pe", lambda e: e.matmul(pb_[:, 0:NCH * 4], lhsT=TRIu, rhs=LF[:, :, :].rearrange("p c h -> p (c h)"),
                                           start=True, stop=True), reads=[cst, LF], writes=[pb_])
            kb.op("dve", lambda e: e.tensor_copy(out=Bc[:, :, :].rearrange("p c h -> p (c h)"), in_=pb_[:, 0:NCH * 4]),
                  reads=[pb_], writes=[Bc])
            kb.op("dve", lambda e: e.tensor_tensor(out=U[:, :, :], in0=LI[:, :, :], in1=Bc[:, :, :], op=ALU.subtract),
                  reads=[LI, Bc], writes=[U])

            mnwb = kb.sb([128, 2048], F32)
            kb.dma("sp", mnwb[:, :], mnw.partition_broadcast(128), writes=[mnwb])
            qTh = kb.sb([128, 2, T2], BF16)
            kTh = kb.sb([128, 2, T2], BF16)
            vh = kb.sb([128, NCH, 512], BF16)
            Cst = kb.sb([128, 2, 512], F32)
            Cb = kb.sb([128, 2, 512], BF16)
            nst_ = kb.sb([128, 2], F32)
            nb_ = kb.sb([128, 2], BF16)
            mcol = kb.sb([128, 1], F32)
            dg = [kb.sb([128, 128], F32) for _ in range(2)]
            Um = kb.sb([128, 128], F32)
            Rm = kb.sb([128, 128], F32)
            ET = kb.sb([128, 128], F32)
            PT = kb.sb([128, 128], BF16)
            sm = [kb.sb([128, 16], F32) for _ in range(2)]
            mr = kb.sb([128, 2], F32)
            kw = kb.sb([128, 256], BF16)
            tmpn = kb.sb([128, 512], F32)
            num = kb.sb([128, 512], F32)
            hh_ = kb.sb([128, 512], F32)
            junk5 = kb.sb([128, 512], BF16)
            ot = kb.sb([128, 512], F32)
            hout = [kb.sb([128, 512], BF16) for _ in range(2)]
            nit = 0
            for h in range(4):
                kb.dma("sp", qTh[:, :, :], qkc_d[h * 256:(h + 1) * 256, :].rearrange("(j p) t -> p j t", p=128), writes=[qTh])
                kb.dma("sp", kTh[:, :, :], qkc_d[1024 + h * 256:1024 + (h + 1) * 256, :].rearrange("(j p) t -> p j t", p=128), writes=[kTh])
                kb.dma("sp", vh[:, :, :], mv_d[:, h * 512:(h + 1) * 512].rearrange("(c p) v -> p c v", p=128), writes=[vh])
                kb.op("dve", lambda e: e.memset(Cst[:, :, :], 0.0), writes=[Cst])
                kb.op("dve", lambda e: e.memset(Cb[:, :, :], 0.0), writes=[Cb])
                kb.op("dve", lambda e: e.memset(nst_[:, :], 0.0), writes=[nst_])
                kb.op("dve", lambda e: e.memset(nb_[:, :], 0.0), writes=[nb_])
                kb.op("dve", lambda e: e.memset(mcol[:, :], 0.0), writes=[mcol])
                for c in range(NCH):
                    own = c >= NCH // 2
                    if c == NCH // 2:
                        kb.op("dve", lambda e: e.tensor_scalar(out=Cst[:, :, :], in0=Cst[:, :, :], scalar1=flg[:, 0:1], scalar2=None, op0=ALU.mult),
                              reads=[Cst, flg], writes=[Cst])
                        kb.op("dve", lambda e: e.tensor_copy(out=Cb[:, :, :], in_=Cst[:, :, :]), reads=[Cst], writes=[Cb])
                        kb.op("dve", lambda e: e.tensor_scalar(out=nst_[:, :], in0=nst_[:, :], scalar1=flg[:, 0:1], scalar2=None, op0=ALU.mult),
                              reads=[nst_, flg], writes=[nst_])
                        kb.op("dve", lambda e: e.tensor_copy(out=nb_[:, :], in_=nst_[:, :]), reads=[nst_], writes=[nb_])
                        kb.op("dve", lambda e: e.tensor_scalar(out=mcol[:, :], in0=mcol[:, :], scalar1=flg[:, 0:1], scalar2=None, op0=ALU.mult),
                              reads=[mcol, flg], writes=[mcol])
                    cs_ = slice(c * 128, (c + 1) * 128)
                    ucol = U[:, c, h:h + 1]
                    bcol = Bc[:, c, h:h + 1]
                    s0 = sm[nit % 2]
                    nit += 1
                    d0 = dg[0]
                    kb.op("dve", lambda e, d0=d0, ucol=ucol: e.tensor_scalar(out=d0[:, :], in0=identf[:, :], scalar1=ucol, scalar2=None, op0=ALU.mult),
                          reads=[identf, U], writes=[d0])
                    pu = kb.ps()
                    kb.op("pe", lambda e, pu=pu, d0=d0: e.matmul(pu[:, 0:128], lhsT=ONESF, rhs=d0[:, :], start=True, stop=True),
                          reads=[cst, d0], writes=[pu])
                    kb.op("dve", lambda e, pu=pu: e.tensor_tensor(out=Um[:, :], in0=pu[:, 0:128], in1=CTS, op=ALU.add), reads=[pu, cst], writes=[Um])
                    kb.op("dve", lambda e, s0=s0: e.tensor_reduce(out=s0[:, 0:1], in_=Um[:, :], axis=AX.X, op=ALU.max), reads=[Um], writes=[s0])
                    kb.op("dve", lambda e, s0=s0: e.tensor_tensor(out=mr[:, 1:2], in0=s0[:, 0:1], in1=mcol[:, 0:1], op=ALU.max), reads=[s0, mcol], writes=[mr])
                    kb.op("dve", lambda e, bcol=bcol: e.tensor_tensor(out=mr[:, 0:1], in0=mr[:, 1:2], in1=bcol, op=ALU.add), reads=[mr, Bc], writes=[mr])
                    pm = kb.ps()
                    kb.op("pe", lambda e, pm=pm: e.matmul(pm[:, 0:2], lhsT=SEL127, rhs=mr[:, 0:2], start=True, stop=True), reads=[cst, mr], writes=[pm])
                    kb.op("dve", lambda e, pm=pm, s0=s0: e.tensor_copy(out=s0[:, 1:3], in_=pm[:, 0:2]), reads=[pm], writes=[s0])
                    if own:
                        d1 = dg[1]
                        kb.op("dve", lambda e, d1=d1: e.tensor_scalar(out=d1[:, :], in0=identf[:, :], scalar1=mr[:, 1:2], scalar2=None, op0=ALU.mult),
                              reads=[identf, mr], writes=[d1])
                        pr = kb.ps()
                        kb.op("pe", lambda e, pr=pr, d1=d1: e.matmul(pr[:, 0:128], lhsT=ONESF, rhs=d1[:, :], start=True, stop=True),
                              reads=[cst, d1], writes=[pr])
                        kb.op("dve", lambda e, pr=pr: e.tensor_tensor(out=Rm[:, :], in0=pr[:, 0:128], in1=CST, op=ALU.add), reads=[pr, cst], writes=[Rm])
                        kb.op("act", lambda e, ucol=ucol: e.activation(out=ET[:, :], in_=Rm[:, :], func=AF.Exp, scale=-1.0, bias=ucol),
                              reads=[Rm, U], writes=[ET])
                        pS = kb.ps()
                        for j in range(2):
                            kb.op("pe", lambda e, pS=pS, j=j, cs_=cs_: e.matmul(pS[:, 0:128], lhsT=kTh[:, j, cs_], rhs=qTh[:, j, cs_],
                                                                              start=(j == 0), stop=(j == 1)), reads=[kTh, qTh], writes=[pS])
                        kb.op("dve", lambda e, pS=pS: e.tensor_tensor(out=PT[:, :], in0=pS[:, 0:128], in1=ET[:, :], op=ALU.mult),
                              reads=[pS, ET], writes=[PT])
                        pnum = kb.ps()
                        kb.op("pe", lambda e, pnum=pnum, c=c: e.matmul(pnum[:, :], lhsT=PT[:, :], rhs=vh[:, c, :], start=True, stop=True),
                              reads=[PT, vh], writes=[pnum])
                        pden = kb.ps()
                        kb.op("pe", lambda e, pden=pden: e.matmul(pden[:, 0:1], lhsT=PT[:, :], rhs=onesb[:, 0:1], start=True, stop=True),
                              reads=[PT, onesb], writes=[pden])
                        pqc = kb.ps()
                        for j in range(2):
                            kb.op("pe", lambda e, pqc=pqc, j=j, cs_=cs_: e.matmul(pqc[:, :], lhsT=qTh[:, j, cs_], rhs=Cb[:, j, :],
                                                                                start=(j == 0), stop=(j == 1)), reads=[qTh, Cb], writes=[pqc])
                        for j in range(2):
                            kb.op("pe", lambda e, pden=pden, j=j, cs_=cs_: e.matmul(pden[:, 1:2], lhsT=qTh[:, j, cs_], rhs=nb_[:, j:j + 1],
                                                                                  start=(j == 0), stop=(j == 1)), reads=[qTh, nb_], writes=[pden])
                        kb.op("dve", lambda e, s0=s0: e.tensor_tensor(out=s0[:, 3:4], in0=mcol[:, 0:1], in1=mr[:, 1:2], op=ALU.subtract),
                              reads=[mcol, mr], writes=[s0])
                        kb.op("act", lambda e, s0=s0: e.activation(out=s0[:, 3:4], in_=s0[:, 3:4], func=AF.Exp), reads=[s0], writes=[s0])
                        kb.op("act", lambda e, pqc=pqc, s0=s0: e.activation(out=tmpn[:, :], in_=pqc[:, :], func=AF.Copy, scale=s0[:, 3:4]),
                              reads=[pqc, s0], writes=[tmpn])
                        kb.op("dve", lambda e, pnum=pnum: e.tensor_tensor(out=num[:, :], in0=pnum[:, :], in1=tmpn[:, :], op=ALU.add),
                              reads=[pnum, tmpn], writes=[num])
                        kb.op("dve", lambda e, pden=pden, s0=s0: e.tensor_copy(out=s0[:, 9:11], in_=pden[:, 0:2]), reads=[pden], writes=[s0])
                        kb.op("dve", lambda e, s0=s0: e.scalar_tensor_tensor(out=s0[:, 4:5], in0=s0[:, 10:11], scalar=s0[:, 3:4], in1=s0[:, 9:10],
                                                                             op0=ALU.mult, op1=ALU.add), reads=[s0], writes=[s0])
                        kb.op("dve", lambda e, s0=s0: e.tensor_scalar(out=s0[:, 5:6], in0=s0[:, 4:5], scalar1=-1.0, scalar2=None, op0=ALU.mult),
                              reads=[s0], writes=[s0])
                        kb.op("dve", lambda e, s0=s0: e.tensor_tensor(out=s0[:, 5:6], in0=s0[:, 5:6], in1=s0[:, 4:5], op=ALU.max),
                              reads=[s0], writes=[s0])
                        kb.op("act", lambda e, s0=s0: e.activation(out=s0[:, 6:7], in_=mr[:, 0:1], func=AF.Exp, scale=-1.0), reads=[mr], writes=[s0])
                        kb.op("dve", lambda e, s0=s0: e.tensor_tensor(out=s0[:, 7:8], in0=s0[:, 5:6], in1=s0[:, 6:7], op=ALU.max), reads=[s0], writes=[s0])
                        kb.op("dve", lambda e, s0=s0: e.reciprocal(out=s0[:, 8:9], in_=s0[:, 7:8]), reads=[s0], writes=[s0])
                        kb.op("dve", lambda e, s0=s0: e.tensor_scalar(out=hh_[:, :], in0=num[:, :], scalar1=s0[:, 8:9], scalar2=None, op0=ALU.mult),
                              reads=[num, s0], writes=[hh_])
                        kb.op("act", lambda e, s0=s0: e.activation(out=junk5[:, :], in_=hh_[:, :], func=AF.Square, accum_out=s0[:, 11:12]),
                              reads=[hh_], writes=[junk5, s0])
                        kb.op("dve", lambda e, s0=s0: e.tensor_scalar(out=s0[:, 11:12], in0=s0[:, 11:12], scalar1=1.0 / 512, scalar2=EPS,
                                                                      op0=ALU.mult, op1=ALU.add), reads=[s0], writes=[s0])
                        kb.op("act", lambda e, s0=s0: e.activation(out=s0[:, 11:12], in_=s0[:, 11:12], func=AF.Sqrt), reads=[s0], writes=[s0])
                        kb.op("dve", lambda e, s0=s0: e.reciprocal(out=s0[:, 12:13], in_=s0[:, 11:12]), reads=[s0], writes=[s0])
                        tok0 = (c - NCH // 2) * 128
                        kb.dma("sp", ot[:, :], o_d[tok0:tok0 + 128, h * 512:(h + 1) * 512], writes=[ot])
                        kb.op("act", lambda e: e.activation(out=ot[:, :], in_=ot[:, :], func=AF.Sigmoid), reads=[ot], writes=[ot])
                        kb.op("dve", lambda e, h=h: e.tensor_tensor(out=ot[:, :], in0=ot[:, :], in1=mnwb[:, h * 512:(h + 1) * 512], op=ALU.mult),
                              reads=[ot, mnwb], writes=[ot])
                        ho = hout[c % 2]
                        kb.op("dve", lambda e, s0=s0, ho=ho: e.scalar_tensor_tensor(out=ho[:, :], in0=hh_[:, :], scalar=s0[:, 12:13], in1=ot[:, :],
                                                                                   op0=ALU.mult, op1=ALU.mult), reads=[hh_, s0, ot], writes=[ho])
                        kb.dma("sp", hcat_d[tok0:tok0 + 128, h * 512:(h + 1) * 512], ho[:, :], reads=[ho])
                    kb.op("dve", lambda e, s0=s0, ucol=ucol: e.tensor_tensor(out=s0[:, 13:14], in0=ucol, in1=s0[:, 2:3], op=ALU.subtract),
                          reads=[U, s0], writes=[s0])
                    kb.op("dve", lambda e, s0=s0: e.tensor_tensor(out=s0[:, 14:15], in0=mcol[:, 0:1], in1=s0[:, 2:3], op=ALU.subtract),
                          reads=[mcol, s0], writes=[s0])
                    kb.op("act", lambda e, s0=s0: e.activation(out=s0[:, 13:15], in_=s0[:, 13:15], func=AF.Exp), reads=[s0], writes=[s0])
                    pk = kb.ps()
                    for j in range(2):
                        kb.op("pe", lambda e, pk=pk, j=j, cs_=cs_: e.matmul(pk[:, j * 128:(j + 1) * 128], lhsT=kTh[:, j, cs_], rhs=identb[:, :],
                                                                          start=True, stop=True), reads=[kTh, identb], writes=[pk])
                    kb.op("act", lambda e, pk=pk, s0=s0: e.activation(out=kw[:, :], in_=pk[:, 0:256], func=AF.Copy, scale=s0[:, 13:14]),
                          reads=[pk, s0], writes=[kw])
                    pn = kb.ps()
                    for j in range(2):
                        pkv = kb.ps()
                        kb.op("pe", lambda e, pkv=pkv, j=j, c=c: e.matmul(pkv[:, :], lhsT=kw[:, j * 128:(j + 1) * 128], rhs=vh[:, c, :],
                                                                         start=True, stop=True), reads=[kw, vh], writes=[pkv])
                        kb.op("pe", lambda e, pn=pn, j=j: e.matmul(pn[:, j:j + 1], lhsT=kw[:, j * 128:(j + 1) * 128], rhs=onesb[:, 0:1],
                                                                  start=True, stop=True), reads=[kw, onesb], writes=[pn])
                        kb.op("dve", lambda e, pkv=pkv, j=j, s0=s0: e.scalar_tensor_tensor(out=Cst[:, j, :], in0=Cst[:, j, :], scalar=s0[:, 14:15],
                                                                                          in1=pkv[:, :], op0=ALU.mult, op1=ALU.add),
                              reads=[Cst, s0, pkv], writes=[Cst])
                    kb.op("act", lambda e: e.activation(out=Cb[:, :, :], in_=Cst[:, :, :], func=AF.Copy), reads=[Cst], writes=[Cb])
                    kb.op("dve", lambda e, pn=pn, s0=s0: e.scalar_tensor_tensor(out=nst_[:, :], in0=nst_[:, :], scalar=s0[:, 14:15], in1=pn[:, 0:2],
                                                                               op0=ALU.mult, op1=ALU.add), reads=[nst_, s0, pn], writes=[nst_])
                    kb.op("dve", lambda e: e.tensor_copy(out=nb_[:, :], in_=nst_[:, :]), reads=[nst_], writes=[nb_])
                    kb.op("dve", lambda e, s0=s0: e.tensor_copy(out=mcol[:, :], in_=s0[:, 1:2]), reads=[s0], writes=[mcol])


        NTq = TOK // 128
        NTk = T2 // 128
        SCL = 128 ** -0.5
        with kb.phase():
            kb.psum_rot = 6
            kb.pnext = 0
            pO, pL = kb.psum[6], kb.psum[7]
            cst_ = kb.sb([128, NTk, 128], F32)
            kb.dma("sp", cst_[:, :, :], cs_tab.rearrange("(i p) d -> p i d", p=128), writes=[cst_])
            pbt = kb.sb([128, NTq, NBP], F32)
            kb.dma("sp", pbt[:, :, :], pb_tab.rearrange("(i p) n -> p i n", p=128), writes=[pbt])
            ohb = kb.sb([NBP, NBP * 128], BF16)
            kb.dma("pool", ohb[:, :], oneh[:, :], writes=[ohb])
            tri01 = kb.sb([128, 128], BF16)
            kb.op("dve", lambda e: e.tensor_copy(out=tri01[:, :], in_=TRIu), reads=[cst], writes=[tri01])
            xq = kb.sb([128, NTq, 128], F32)
            xk = kb.sb([128, NTk, 128], F32)
            rq = kb.sb([128, NTk, 128], F32)
            t1 = kb.sb([128, NTk, 64], F32)
            qT = kb.sb([128, TOK], BF16)
            kT = kb.sb([128, T2], BF16)
            vk = kb.sb([128, NTk, 132], BF16)
            kb.op("dve", lambda e: e.memset(vk[:, :, :], 1.0), writes=[vk])
            kmT = kb.sb([128, NBP], BF16)
            kms = kb.sb([128, NBP], F32)
            gm = kb.sb([128, NBP], F32)
            t8 = kb.sb([128, 8], F32)
            selb = kb.sb([128, NBP], BF16)
            selT = kb.sb([NBP, TOK], BF16)
            PTm = [kb.sb([128, 512], BF16) for _ in range(3)]
            lrec = [kb.sb([128, 2], F32) for _ in range(2)]
            gmA = kb.sb([128, NTq, NBP], F32)
            t8A = kb.sb([128, NTq, 8], F32)
            selbA = kb.sb([128, NTq, NBP], BF16)
            st_m = {"rot": 0}
            pOs = [pO, pL]
            oo = [kb.sb([128, 128], BF16) for _ in range(2)]

            def rotary(X, R, nt):
                c_, s_ = cst_[:, NTk - nt:NTk, 0:64], cst_[:, NTk - nt:NTk, 64:128]
                x1, x2 = X[:, 0:nt, 0:64], X[:, 0:nt, 64:128]
                T1 = t1[:, 0:nt, :]
                kb.op("dve", lambda e: e.tensor_tensor(out=R[:, 0:nt, 0:64], in0=x1, in1=c_, op=ALU.mult), reads=[X, cst_], writes=[R])
                kb.op("dve", lambda e: e.tensor_tensor(out=T1, in0=x2, in1=s_, op=ALU.mult), reads=[X, cst_], writes=[t1])
                kb.op("dve", lambda e: e.tensor_tensor(out=R[:, 0:nt, 0:64], in0=R[:, 0:nt, 0:64], in1=T1, op=ALU.subtract), reads=[R, t1], writes=[R])
                kb.op("dve", lambda e: e.tensor_tensor(out=R[:, 0:nt, 64:128], in0=x2, in1=c_, op=ALU.mult), reads=[X, cst_], writes=[R])
                kb.op("dve", lambda e: e.tensor_tensor(out=T1, in0=x1, in1=s_, op=ALU.mult), reads=[X, cst_, t1], writes=[t1])
                kb.op("dve", lambda e: e.tensor_tensor(out=R[:, 0:nt, 64:128], in0=R[:, 0:nt, 64:128], in1=T1, op=ALU.add), reads=[R, t1], writes=[R])

            def transp(R, nt, dst):
                for i4 in range(nt // 4):
                    pt = kb.ps()
                    for q in range(4):
                        i = i4 * 4 + q
                        kb.op("pe", lambda e, pt=pt, q=q, i=i: e.matmul(pt[:, q * 128:(q + 1) * 128], lhsT=R[:, i, :], rhs=identf[:, :],
                                                                       start=True, stop=True), reads=[R, identf], writes=[pt])
                    ev_copy(i4, dst[:, i4 * 512:(i4 + 1) * 512], pt[:, :], [pt], [dst])

            nev = 0
            for h in range(16):
                hs = slice(h * 128, (h + 1) * 128)
                kb.dma("sp", xq[:, :, :], aq_d[:, hs].rearrange("(i p) d -> p i d", p=128), writes=[xq])
                kb.dma("sp", xk[:, :, :], ak_d[:, hs].rearrange("(i p) d -> p i d", p=128), writes=[xk])
                kb.dma("sp", vk[:, :, 0:128], av_d[:, hs].rearrange("(i p) d -> p i d", p=128), writes=[vk])
                rotary(xq, rq, NTq)
                transp(rq, NTq, qT)
                rotary(xk, rq, NTk)
                transp(rq, NTk, kT)
                kb.op("dve", lambda e: e.memset(kms[:, :], 0.0), writes=[kms])
                kb.op("dve", lambda e: e.tensor_reduce(out=kms[:, 0:NB], in_=kT[:, :].rearrange("p (n k) -> p n k", k=256), axis=AX.X, op=ALU.add),
                      reads=[kT], writes=[kms])
                kb.op("dve", lambda e: e.tensor_scalar(out=kmT[:, :], in0=kms[:, :], scalar1=1.0 / 256, scalar2=None, op0=ALU.mult),
                      reads=[kms], writes=[kmT])
                pg = kb.ps()
                for i in range(NTq):
                    kb.op("pe", lambda e, i=i: e.matmul(pg[:, i * NBP:(i + 1) * NBP], lhsT=qT[:, i * 128:(i + 1) * 128], rhs=kmT[:, :], start=True, stop=True),
                          reads=[qT, kmT], writes=[pg])
                kb.op("dve", lambda e: e.tensor_tensor(out=gmA[:, :, :].rearrange("p a b -> p (a b)"), in0=pg[:, 0:NTq * NBP],
                                                       in1=pbt[:, :, :].rearrange("p a b -> p (a b)"), op=ALU.add), reads=[pg, pbt], writes=[gmA])
                for i in range(NTq):
                    kb.op("dve", lambda e, i=i: e.max(out=t8A[:, i, :], in_=gmA[:, i, :]), reads=[gmA], writes=[t8A])
                kb.op("dve", lambda e: e.tensor_scalar(out=t8A[:, :, 2:3], in0=t8A[:, :, 2:3], scalar1=-1.0e29, scalar2=None, op0=ALU.max), reads=[t8A], writes=[t8A])
                for i in range(NTq):
                    kb.op("dve", lambda e, i=i: e.tensor_scalar(out=gmA[:, i, :], in0=gmA[:, i, :], scalar1=t8A[:, i, 2:3], scalar2=None, op0=ALU.is_ge),
                          reads=[gmA, t8A], writes=[gmA])
                kb.op("dve", lambda e: e.tensor_scalar(out=selbA[:, :, :], in0=gmA[:, :, :], scalar1=30000.0, scalar2=-30000.0, op0=ALU.mult, op1=ALU.add),
                      reads=[gmA], writes=[selbA])
                for i4 in range(NTq // 4):
                    pt = kb.ps()
                    for q in range(4):
                        i = i4 * 4 + q
                        kb.op("pe", lambda e, pt=pt, q=q, i=i: e.matmul(pt[0:NBP, q * 128:(q + 1) * 128], lhsT=selbA[:, i, :], rhs=identb[:, :], start=True, stop=True),
                              reads=[selbA, identb], writes=[pt])
                    kb.op("act", lambda e, pt=pt, i4=i4: e.activation(out=selT[:, i4 * 512:(i4 + 1) * 512], in_=pt[0:NBP, 0:512], func=AF.Copy), reads=[pt], writes=[selT])
                for jb in range(TOK // 256):
                    q2 = slice(jb * 256, (jb + 1) * 256)
                    gb_ = NB // 2 + jb
                    started = [False, False]

                    def score_past(n):
                        pS = kb.ps()
                        for hf in range(2):
                            ks_ = slice(n * 256 + hf * 128, n * 256 + hf * 128 + 128)
                            kb.op("pe", lambda e, pS=pS, hf=hf, ks_=ks_: e.matmul(pS[:, hf * 256:(hf + 1) * 256], lhsT=kT[:, ks_], rhs=qT[:, q2], start=True, stop=False),
                                  reads=[kT, qT], writes=[pS])
                            kb.op("pe", lambda e, pS=pS, hf=hf, n=n: e.matmul(pS[:, hf * 256:(hf + 1) * 256], lhsT=ohb[:, n * 128:(n + 1) * 128], rhs=selT[:, q2], start=False, stop=True),
                                  reads=[ohb, selT], writes=[pS])
                        P_ = PTm[st_m["rot"] % 3]
                        st_m["rot"] += 1
                        kb.op("act", lambda e, pS=pS, P_=P_: e.activation(out=P_[:, :], in_=pS[:, :], func=AF.Exp, scale=SCL), reads=[pS], writes=[P_])
                        return [(P_, hf * 256 + qt * 128, n * 2 + hf, qt) for hf in range(2) for qt in range(2)]

                    def score_own(qt, hf, tri):
                        pS = kb.ps()
                        ks_ = slice(gb_ * 256 + hf * 128, gb_ * 256 + hf * 128 + 128)
                        qs_ = slice(jb * 256 + qt * 128, jb * 256 + (qt + 1) * 128)
                        kb.op("pe", lambda e, pS=pS: e.matmul(pS[:, 0:128], lhsT=kT[:, ks_], rhs=qT[:, qs_], start=True, stop=True), reads=[kT, qT], writes=[pS])
                        P_ = PTm[st_m["rot"] % 3]
                        st_m["rot"] += 1
                        kb.op("act", lambda e, pS=pS, P_=P_: e.activation(out=P_[:, 0:128], in_=pS[:, 0:128], func=AF.Exp, scale=SCL), reads=[pS], writes=[P_])
                        if tri:
                            kb.op("dve", lambda e, P_=P_: e.tensor_tensor(out=P_[:, 0:128], in0=P_[:, 0:128], in1=tri01[:, :], op=ALU.mult), reads=[P_, tri01], writes=[P_])
                        return [(P_, 0, gb_ * 2 + hf, qt)]

                    def pv(lst, lastflags):
                        for (P_, c0_, ki, qt), last in zip(lst, lastflags):
                            first = not started[qt]
                            started[qt] = True
                            kb.op("pe", lambda e, P_=P_, c0_=c0_, ki=ki, qt=qt, first=first, last=last: e.matmul(
                                pOs[qt][:, 0:129], lhsT=P_[:, c0_:c0_ + 128], rhs=vk[:, ki, 0:129], start=first, stop=last), reads=[P_, vk], writes=[pOs[qt]])

                    prev = None
                    for n in range(gb_):
                        cur = score_past(n)
                        if prev is not None:
                            pv(prev, [False] * 4)
                        prev = cur
                    o0 = score_own(0, 0, True)
                    pv(prev, [False] * 4)
                    o1 = score_own(1, 0, False)
                    pv(o0, [True])
                    o2 = score_own(1, 1, True)
                    pv(o1, [False])
                    pv(o2, [True])
                    for qt in range(2):
                        i = jb * 2 + qt
                        lr = lrec[i % 2]
                        kb.op("dve", lambda e, qt=qt, lr=lr: e.reciprocal(out=lr[:, 0:1], in_=pOs[qt][:, 128:129]), reads=[pOs[qt]], writes=[lr])
                        o_ = oo[i % 2]
                        kb.op("act", lambda e, o_=o_, qt=qt, lr=lr: e.activation(out=o_[:, :], in_=pOs[qt][:, 0:128], func=AF.Copy, scale=lr[:, 0:1]), reads=[pOs[qt], lr], writes=[o_])
                        kb.dma("sp", hcat_d[i * 128:(i + 1) * 128, 2048 + h * 128:2048 + (h + 1) * 128], o_[:, :], reads=[o_])
            kb.psum_rot = 8
            kb.pnext = 0

        modflat = mod_d.rearrange("a b -> (a b)")

        def norm_T(x_t, junk_, xs_t, ssc, Acol, bias_fn, dstT, ti):
            kb.op("act", lambda e: e.activation(out=junk_[:, :], in_=x_t[:, :], func=AF.Square, accum_out=ssc[:, 0:1]), reads=[x_t], writes=[junk_, ssc])
            kb.op("dve", lambda e: e.tensor_scalar(out=ssc[:, 0:1], in0=ssc[:, 0:1], scalar1=1.0 / D, scalar2=EPS, op0=ALU.mult, op1=ALU.add), reads=[ssc], writes=[ssc])
            kb.op("act", lambda e: e.activation(out=ssc[:, 0:1], in_=ssc[:, 0:1], func=AF.Sqrt), reads=[ssc], writes=[ssc])
            kb.op("dve", lambda e: e.reciprocal(out=ssc[:, 1:2], in_=ssc[:, 0:1]), reads=[ssc], writes=[ssc])
            kb.op("dve", lambda e: e.tensor_scalar(out=xs_t[:, :], in0=x_t[:, :], scalar1=ssc[:, 1:2], scalar2=None, op0=ALU.mult), reads=[x_t, ssc], writes=[xs_t])
            for k4 in range(KC // 4):
                pt = kb.ps()
                for q in range(4):
                    kc = k4 * 4 + q
                    kb.op("pe", lambda e, pt=pt, q=q, kc=kc: e.matmul(pt[:, q * 128:(q + 1) * 128], lhsT=xs_t[:, kc * 128:(kc + 1) * 128], rhs=identb[:, :],
                                                                     start=True, stop=True), reads=[xs_t, identb], writes=[pt])
                for q in range(4):
                    kc = k4 * 4 + q
                    if Acol is None:
                        ev_copy(q, dstT[:, kc, ti * 128:(ti + 1) * 128], pt[:, q * 128:(q + 1) * 128], [pt], [dstT.ks[ti]])
                    else:
                        kb.op("act", lambda e, pt=pt, q=q, kc=kc: e.activation(out=dstT[:, kc, ti * 128:(ti + 1) * 128], in_=pt[:, q * 128:(q + 1) * 128],
                                                                              func=AF.Identity, scale=Acol[:, kc:kc + 1], bias=bias_fn(kc)),
                              reads=[pt, Acol, modT], writes=[dstT.ks[ti]])

        wov = w_out.rearrange("(kc p) n -> p kc n", p=128)
        with kb.phase():
            g1b = kb.sb([128, D], F32)
            kb.dma("sp", g1b[:, :], modflat[2 * D:3 * D].partition_broadcast(128), writes=[g1b])
            hT = kb.sb([128, KC, TG], BF16, n=4)
            hb = [kb.sb([128, D], BF16) for _ in range(2)]
            xg = [kb.sb([128, D], F32) for _ in range(4)]
            wb = [kb.sb([128, KC, 512], BF16) for _ in range(2)]
            tmpd = [kb.sb([128, 512], F32) for _ in range(2)]
            junk = hb[0]
            xs2 = hb[1]
            ssc = kb.sb([128, 2], F32)
            x2T = hT
            nw = 0
            for g in range(TOK // TG):
                for ti in range(4):
                    tok0 = g * TG + ti * 128
                    hb_ = hb[ti % 2]
                    kb.dma("sp", hb_[:, :], hcat_d[tok0:tok0 + 128, :], writes=[hb_])
                    kb.dma("sp", xg[ti][:, :], x_own[tok0:tok0 + 128, :], writes=[xg[ti]])
                    for k4 in range(KC // 4):
                        pt = kb.ps()
                        for q in range(4):
                            kc = k4 * 4 + q
                            kb.op("pe", lambda e, pt=pt, q=q, kc=kc, hb_=hb_: e.matmul(pt[:, q * 128:(q + 1) * 128], lhsT=hb_[:, kc * 128:(kc + 1) * 128],
                                                                                     rhs=identb[:, :], start=True, stop=True), reads=[hb_, identb], writes=[pt])
                        for q in range(4):
                            kc = k4 * 4 + q
                            ev_copy(q, hT[:, kc, ti * 128:(ti + 1) * 128], pt[:, q * 128:(q + 1) * 128], [pt], [hT.ks[ti]])
                for dc in range(8):
                    w = wb[nw % 2]
                    nw += 1
                    kb.dma("pool", w[:, :, :], wov[:, :, dc * 512:(dc + 1) * 512], writes=[w])
                    for ti in range(4):
                        pt = kb.ps()
                        for kc in range(KC):
                            kb.op("pe", lambda e, pt=pt, kc=kc, ti=ti, w=w: e.matmul(pt[:, :], lhsT=hT[:, kc, ti * 128:(ti + 1) * 128], rhs=w[:, kc, :],
                                                                                    start=(kc == 0), stop=(kc == KC - 1)), reads=[w, hT.ks[ti]], writes=[pt])
                        td = tmpd[ti % 2]
                        ds_ = slice(dc * 512, (dc + 1) * 512)
                        kb.op("dve", lambda e, pt=pt, td=td, ds_=ds_: e.tensor_tensor(out=td[:, :], in0=pt[:, :], in1=g1b[:, ds_], op=ALU.mult), reads=[pt, g1b], writes=[td])
                        kb.op("dve", lambda e, td=td, ds_=ds_, ti=ti: e.tensor_tensor(out=xg[ti][:, ds_], in0=xg[ti][:, ds_], in1=td[:, :], op=ALU.add),
                              reads=[xg[ti], td], writes=[xg[ti]])
                for ti in range(4):
                    tok0 = g * TG + ti * 128
                    kb.dma("sp", x2_d[tok0:tok0 + 128, :], xg[ti][:, :], reads=[xg[ti]])
                    norm_T(xg[ti], junk, xs2, ssc, A2, lambda kc: modT[:, 96 + kc:97 + kc], x2T, ti)
                kb.dma("sp", xn2T_d[:, :, g * TG:(g + 1) * TG], x2T[:, :, :], reads=x2T.ks)

        NGP = TOK // TG
        with kb.phase():
            xT = kb.sb([128, KC, TG], BF16)
            skb = kb.sb([128, 16, NK], BF16)
            kb.dma("pool", skb[:, :, :], skT[:, :, :], writes=[skb])
            wqb = [kb.sb([128, KC, 128], BF16) for _ in range(2)]
            qsb = [kb.sb([128, TG], BF16) for _ in range(2)]
            S2 = kb.sb([128, 4, 8, NK], F32)
            AA = kb.sb([128, 4, 8, NK], F32)
            NT = kb.sb([128, 4, 8, NK], F32)
            s1b = kb.sb([128, NK], F32)
            T1 = kb.sb([128, 16], F32)
            T2_ = kb.sb([128, 16], F32)
            cand = kb.sb([128, 256], F32)
            candb = kb.sb([128, 256], F32)
            c16 = kb.sb([128, 16], F32)
            e16 = kb.sb([128, 16], F32)
            zc = kb.sb([128, 4], F32)
            nwq = 0
            for g in range(NGP):
                kb.dma("sp", xT[:, :, :], xn2T_d[:, :, g * TG:(g + 1) * TG], writes=[xT])
                for hp in range(16):
                    h, half = hp // 2, hp % 2
                    w = wqb[nwq % 2]
                    qs_ = qsb[nwq % 2]
                    nwq += 1
                    kb.dma("pool", w[:, :, :], wqc[hp].rearrange("p (kc e) -> p kc e", e=128), writes=[w], max_dma_last_dim=4096)
                    pq = kb.ps()
                    for kc in range(KC):
                        kb.op("pe", lambda e, pq=pq, kc=kc, w=w: e.matmul(pq[:, :], lhsT=w[:, kc, :], rhs=xT[:, kc, :], start=(kc == 0), stop=(kc == KC - 1)),
                              reads=[w, xT], writes=[pq])
                    kb.op("act", lambda e, pq=pq, qs_=qs_: e.activation(out=qs_[:, :], in_=pq[:, :], func=AF.Copy), reads=[pq], writes=[qs_])
                    for ti in range(4):
                        psc = kb.ps()
                        kb.op("pe", lambda e, psc=psc, ti=ti, hp=hp, qs_=qs_: e.matmul(psc[:, 0:NK], lhsT=qs_[:, ti * 128:(ti + 1) * 128], rhs=skb[:, hp, :], start=True, stop=True),
                              reads=[qs_, skb], writes=[psc])
                        if half == 0:
                            kb.op("dve", lambda e, psc=psc, ti=ti, h=h: e.tensor_copy(out=AA[:, ti, h, :], in_=psc[:, 0:NK]), reads=[psc], writes=[AA])
                        else:
                            kb.op("dve", lambda e, psc=psc, ti=ti, h=h: e.tensor_copy(out=S2[:, ti, h, :], in_=psc[:, 0:NK]), reads=[psc], writes=[S2])
                for ti in range(4):
                    for h in range(8):
                        for (src, dstT_) in ((AA[:, ti, h, :], T1), (S2[:, ti, h, :], T2_)):
                            kb.op("dve", lambda e, src=src, dstT_=dstT_: e.max(out=dstT_[:, 0:8], in_=src), reads=[AA, S2], writes=[dstT_])
                            kb.op("dve", lambda e, src=src, dstT_=dstT_: e.match_replace(out=s1b[:, :], in_to_replace=dstT_[:, 0:8], in_values=src, imm_value=NEG),
                                  reads=[AA, S2, dstT_], writes=[s1b])
                            kb.op("dve", lambda e, dstT_=dstT_: e.max(out=dstT_[:, 8:16], in_=s1b[:, :]), reads=[s1b], writes=[dstT_])
                        for a_ in range(16):
                            kb.op("dve", lambda e, a_=a_: e.tensor_scalar(out=cand[:, a_ * 16:(a_ + 1) * 16], in0=T2_[:, :], scalar1=T1[:, a_:a_ + 1], scalar2=None, op0=ALU.add),
                                  reads=[T1, T2_], writes=[cand])
                        kb.op("dve", lambda e: e.max(out=c16[:, 0:8], in_=cand[:, :]), reads=[cand], writes=[c16])
                        kb.op("dve", lambda e: e.match_replace(out=candb[:, :], in_to_replace=c16[:, 0:8], in_values=cand[:, :], imm_value=NEG), reads=[cand, c16], writes=[candb])
                        kb.op("dve", lambda e: e.max(out=c16[:, 8:16], in_=candb[:, :]), reads=[candb], writes=[c16])
                        kb.op("dve", lambda e: e.tensor_scalar(out=zc[:, 0:1], in0=c16[:, 0:1], scalar1=-1.0, scalar2=None, op0=ALU.mult), reads=[c16], writes=[zc])
                        kb.op("act", lambda e: e.activation(out=e16[:, :], in_=c16[:, :], func=AF.Exp, bias=zc[:, 0:1], accum_out=zc[:, 1:2]), reads=[c16, zc], writes=[e16, zc])
                        kb.op("act", lambda e: e.activation(out=zc[:, 2:3], in_=zc[:, 1:2], func=AF.Ln), reads=[zc], writes=[zc])
                        kb.op("dve", lambda e: e.tensor_tensor(out=zc[:, 2:3], in0=zc[:, 0:1], in1=zc[:, 2:3], op=ALU.subtract), reads=[zc], writes=[zc])
                        kb.op("dve", lambda e, ti=ti, h=h: e.tensor_scalar(out=NT[:, ti, h, :], in0=AA[:, ti, h, :], scalar1=-1.0, scalar2=c16[:, 15:16], op0=ALU.mult, op1=ALU.add),
                              reads=[AA, c16], writes=[NT])
                        kb.op("dve", lambda e, ti=ti, h=h: e.tensor_scalar(out=AA[:, ti, h, :], in0=AA[:, ti, h, :], scalar1=zc[:, 2:3], scalar2=None, op0=ALU.add),
                              reads=[AA, zc], writes=[AA])
                for j_, t_ in enumerate((S2, AA, NT)):
                    kb.dma("sp", ans_d[g, j_, :, :], t_[:, :, :, :].rearrange("p a b c -> p (a b c)"), reads=[t_])

        with kb.phase():
            kb.psum_rot = 4
            kb.pnext = 0
            pA_, pG_ = [kb.psum[4], kb.psum[5]], [kb.psum[6], kb.psum[7]]
            xT = kb.sb([128, KC, TG], BF16)
            S2 = kb.sb([128, 4, 8, NK], F32)
            AA = kb.sb([128, 4, 8, NK], F32)
            NT = kb.sb([128, 4, 8, NK], F32)
            NUB, NVB, NEB = 2, 4, 5
            Ub = [kb.sb([128, KC, NK], BF16) for _ in range(NUB)]
            Vb = [kb.sb([NK, D], BF16) for _ in range(NVB)]
            Eb = [kb.sb([128, NK], BF16) for _ in range(NEB)]
            Gh = [kb.sb([128, NK], BF16) for _ in range(NEB)]
            gel = [kb.sb([NK, TG], BF16) for _ in range(4)]
            wT = [kb.sb([NK, TG], BF16) for _ in range(4)]
            ACC = kb.sb([128, 4, D], F32, n=32)
            NP_ = NK // 2
            st_ = {"rot": 0}

            def v_load(c):
                V_ = Vb[c % NVB]
                kb.dma("pool", V_[:, :], vv[c * NK:(c + 1) * NK, :], writes=[V_], max_dma_last_dim=4096)

            def u_load(c):
                U_ = Ub[c % NUB]
                kb.dma("pool", U_[:, :, :], uTc[c].rearrange("p (kc e) -> p kc e", e=NK), writes=[U_], max_dma_last_dim=4096)

            def block(ps2, ps1, ti, ppa):
                pend = []
                for k in range(8):
                    if ps2 is not None:
                        c0, c1 = 2 * ps2, 2 * ps2 + 1
                        po = kb.ps()
                        ds_ = slice(k * 512, (k + 1) * 512)
                        kb.op("pe", lambda e, po=po, ds_=ds_: e.matmul(po[:, :], lhsT=wT[c0 % 4][:, ti * 128:(ti + 1) * 128], rhs=Vb[c0 % NVB][:, ds_], start=True, stop=False),
                              reads=[wT[c0 % 4], Vb[c0 % NVB]], writes=[po])
                        kb.op("pe", lambda e, po=po, ds_=ds_: e.matmul(po[:, :], lhsT=wT[c1 % 4][:, ti * 128:(ti + 1) * 128], rhs=Vb[c1 % NVB][:, ds_], start=False, stop=True),
                              reads=[wT[c1 % 4], Vb[c1 % NVB]], writes=[po])
                        if ps2 == 0:
                            kb.op("dve", lambda e, po=po, ds_=ds_: e.tensor_copy(out=ACC[:, ti, ds_], in_=po[:, :]), reads=[po], writes=[ACC.ks[ti * 8 + k]])
                        else:
                            kb.op("dve", lambda e, po=po, ds_=ds_: e.tensor_tensor(out=ACC[:, ti, ds_], in0=ACC[:, ti, ds_], in1=po[:, :], op=ALU.add),
                                  reads=[po, ACC.ks[ti * 8 + k]], writes=[ACC.ks[ti * 8 + k]])
                    if ppa is not None:
                        q_, hf = ti // 2, ti % 2
                        c = 2 * ppa + q_
                        U_ = Ub[c % NUB]
                        pA = pA_[q_]
                        for kc in (hf * 16 + 2 * k, hf * 16 + 2 * k + 1):
                            kb.op("pe", lambda e, pA=pA, kc=kc, U_=U_: e.matmul(pA[0:NK, :], lhsT=U_[:, kc, :], rhs=xT[:, kc, :], start=(kc == 0), stop=(kc == KC - 1)),
                                  reads=[U_, xT], writes=[pA])
                    if ps1 is not None:
                        for fn in pend:
                            fn()
                        pend = []
                        h = k
                        for q in range(2):
                            i = 2 * ps1 + q
                            pG = pG_[q]
                            E_ = Eb[st_["rot"] % NEB]
                            G_ = Gh[st_["rot"] % NEB]
                            st_["rot"] += 1
                            kb.op("act", lambda e, E_=E_, h=h, i=i: e.activation(out=E_[:, :], in_=S2[:, ti, h, :], func=AF.Exp, bias=AA[:, ti, h, i:i + 1]),
                                  reads=[S2, AA], writes=[E_])
                            kb.op("dve", lambda e, E_=E_, G_=G_, h=h, i=i: e.scalar_tensor_tensor(out=G_[:, :], in0=S2[:, ti, h, :], scalar=NT[:, ti, h, i:i + 1], in1=E_[:, :],
                                                                                           op0=ALU.is_ge, op1=ALU.mult), reads=[S2, NT, E_], writes=[G_])
                            pend.append(lambda G_=G_, pG=pG, h=h: kb.op(
                                "pe", lambda e: e.matmul(pG[0:NK, ti * 128:(ti + 1) * 128], lhsT=G_[:, :], rhs=identb[:, :], start=(h == 0), stop=(h == 7)),
                                reads=[G_, identb], writes=[pG]))
                for fn in pend:
                    fn()
                if ppa is not None and ti % 2 == 1:
                    q_ = ti // 2
                    c = 2 * ppa + q_
                    g_ = gel[(ppa % 2) * 2 + q_]
                    kb.op("act", lambda e, q_=q_, g_=g_: e.activation(out=g_[:, :], in_=pA_[q_][0:NK, :], func=AF.Gelu), reads=[pA_[q_]], writes=[g_])
                    if c + 2 < NK:
                        u_load(c + 2)

            def s1_gbuild(p, ti):
                for q in range(2):
                    i = 2 * p + q
                    pG = pG_[q]
                    for h in range(8):
                        E_ = Eb[st_["rot"] % NEB]
                        G_ = Gh[st_["rot"] % NEB]
                        st_["rot"] += 1
                        kb.op("act", lambda e, E_=E_, h=h, i=i: e.activation(out=E_[:, :], in_=S2[:, ti, h, :], func=AF.Exp, bias=AA[:, ti, h, i:i + 1]),
                              reads=[S2, AA], writes=[E_])
                        kb.op("dve", lambda e, E_=E_, G_=G_, h=h, i=i: e.scalar_tensor_tensor(out=G_[:, :], in0=S2[:, ti, h, :], scalar=NT[:, ti, h, i:i + 1], in1=E_[:, :],
                                                                                       op0=ALU.is_ge, op1=ALU.mult), reads=[S2, NT, E_], writes=[G_])
                        kb.op("pe", lambda e, G_=G_, pG=pG, h=h: e.matmul(pG[0:NK, ti * 128:(ti + 1) * 128], lhsT=G_[:, :], rhs=identb[:, :], start=(h == 0), stop=(h == 7)),
                              reads=[G_, identb], writes=[pG])

            def s1_end(p):
                for q in range(2):
                    c = 2 * p + q
                    w_ = wT[c % 4]
                    g_ = gel[(p % 2) * 2 + q]
                    kb.op("dve", lambda e, w_=w_, q=q, g_=g_: e.tensor_tensor(out=w_[:, :], in0=pG_[q][0:NK, :], in1=g_[:, :], op=ALU.mult), reads=[pG_[q], g_], writes=[w_])

            def s2_(p, ti):
                c0, c1 = 2 * p, 2 * p + 1
                for dc in range(8):
                    po = kb.ps()
                    ds_ = slice(dc * 512, (dc + 1) * 512)
                    kb.op("pe", lambda e, po=po, ds_=ds_: e.matmul(po[:, :], lhsT=wT[c0 % 4][:, ti * 128:(ti + 1) * 128], rhs=Vb[c0 % NVB][:, ds_], start=True, stop=False),
                          reads=[wT[c0 % 4], Vb[c0 % NVB]], writes=[po])
                    kb.op("pe", lambda e, po=po, ds_=ds_: e.matmul(po[:, :], lhsT=wT[c1 % 4][:, ti * 128:(ti + 1) * 128], rhs=Vb[c1 % NVB][:, ds_], start=False, stop=True),
                          reads=[wT[c1 % 4], Vb[c1 % NVB]], writes=[po])
                    if p == 0:
                        kb.op("dve", lambda e, po=po, ds_=ds_: e.tensor_copy(out=ACC[:, ti, ds_], in_=po[:, :]), reads=[po], writes=[ACC.ks[ti]])
                    else:
                        kb.op("dve", lambda e, po=po, ds_=ds_: e.tensor_tensor(out=ACC[:, ti, ds_], in0=ACC[:, ti, ds_], in1=po[:, :], op=ALU.add),
                              reads=[po, ACC.ks[ti]], writes=[ACC.ks[ti]])

            for g in range(NGP):
                kb.dma("sp", xT[:, :, :], xn2T_d[:, :, g * TG:(g + 1) * TG], writes=[xT])
                for j_, t_ in enumerate((S2, AA, NT)):
                    kb.dma("sp", t_[:, :, :, :].rearrange("p a b c -> p (a b c)"), ans_d[g, j_, :, :], writes=[t_])
                u_load(0)
                u_load(1)
                v_load(0)
                v_load(1)
                for ti in range(4):
                    block(None, None, ti, 0)
                for ti in range(4):
                    block(None, 0, ti, 1 if NP_ > 1 else None)
                s1_end(0)
                for p in range(NP_):
                    nxt = p + 1 < NP_
                    if nxt:
                        v_load(2 * p + 2)
                        v_load(2 * p + 3)
                    for ti in range(4):
                        block(p, p + 1 if nxt else None, ti, p + 2 if p + 2 < NP_ else None)
                    if nxt:
                        s1_end(p + 1)
                for ti in range(4):
                    tok0 = g * TG + ti * 128
                    kb.dma("sp", peer_d[tok0:tok0 + 128, :], ACC[:, ti, :], reads=ACC.ks[ti * 8:(ti + 1) * 8])
            kb.psum_rot = 8
            kb.pnext = 0

        with kb.phase():
            g2b = kb.sb([128, D], F32)
            fnb = kb.sb([128, D], F32)
            kb.dma("sp", g2b[:, :], modflat[5 * D:6 * D].partition_broadcast(128), writes=[g2b])
            kb.dma("sp", fnb[:, :], fnw.partition_broadcast(128), writes=[fnb])
            xa = [kb.sb([128, D], F32) for _ in range(2)]
            pa = [kb.sb([128, D], F32) for _ in range(2)]
            junk = kb.sb([128, D], BF16)
            ssf = [kb.sb([128, 2], F32) for _ in range(2)]
            for it in range(TOK // 128):
                x_, p_, ss_ = xa[it % 2], pa[it % 2], ssf[it % 2]
                rs_ = slice(it * 128, (it + 1) * 128)
                kb.dma("sp", x_[:, :], x2_d[rs_, :], writes=[x_])
                kb.dma("sp", p_[:, :], peer_d[rs_, :], writes=[p_])
                kb.op("dve", lambda e, p_=p_: e.tensor_tensor(out=p_[:, :], in0=p_[:, :], in1=g2b[:, :], op=ALU.mult), reads=[p_, g2b], writes=[p_])
                kb.op("pool", lambda e, p_=p_, x_=x_: e.tensor_tensor(out=x_[:, :], in0=x_[:, :], in1=p_[:, :], op=ALU.add), reads=[p_, x_], writes=[x_])
                kb.op("act", lambda e, x_=x_, ss_=ss_: e.activation(out=junk[:, :], in_=x_[:, :], func=AF.Square, accum_out=ss_[:, 0:1]), reads=[x_], writes=[junk, ss_])
                kb.op("dve", lambda e, ss_=ss_: e.tensor_scalar(out=ss_[:, 0:1], in0=ss_[:, 0:1], scalar1=1.0 / D, scalar2=EPS, op0=ALU.mult, op1=ALU.add), reads=[ss_], writes=[ss_])
                kb.op("act", lambda e, ss_=ss_: e.activation(out=ss_[:, 0:1], in_=ss_[:, 0:1], func=AF.Sqrt), reads=[ss_], writes=[ss_])
                kb.op("dve", lambda e, ss_=ss_: e.reciprocal(out=ss_[:, 1:2], in_=ss_[:, 0:1]), reads=[ss_], writes=[ss_])
                kb.op("dve", lambda e, x_=x_, p_=p_, ss_=ss_: e.scalar_tensor_tensor(out=p_[:, :], in0=x_[:, :], scalar=ss_[:, 1:2], in1=fnb[:, :], op0=ALU.mult, op1=ALU.mult),
                      reads=[x_, ss_, fnb], writes=[p_])
                kb.dma("sp", y[rs_, :], p_[:, :], reads=[p_])


        kb.barrier()
    return nc


def host_shared(cfg, inp):
    TOK, T2, NK, NB, NCH = cfg.TOK, cfg.T2, cfg.NK, cfg.NB, cfg.NCH
    NBP = max(NB, 8)
    m = {}
    idx = np.arange(128)
    le = (idx[:, None] <= idx[None, :])
    consts = np.zeros((128, 5, 128), np.float32)
    consts[:, 0, :] = le.astype(np.float32)
    consts[:, 1, :] = np.where(le.T, 0.0, NEG)
    consts[:, 2, :] = np.where(le, 0.0, 1.0e30)
    consts[127, 3, :] = 1.0
    consts[:, 4, :] = 1.0
    m["consts"] = consts
    m["ident_f"] = np.eye(128, dtype=np.float32)
    m["w_ada"] = inp["w_ada"][0]
    m["b_adaT"] = np.ascontiguousarray(inp["b_ada"][0].reshape(192, 128).T)
    m["n1wT"] = np.ascontiguousarray(inp["norm1_w"][0].reshape(KC, 128).T)
    m["n2wT"] = np.ascontiguousarray(inp["norm2_w"][0].reshape(KC, 128).T)
    m["fnw"] = np.ascontiguousarray(inp["final_norm_w"])
    m["w_in"] = inp["w_in"][0]
    m["conv_wT"] = np.ascontiguousarray(inp["conv_w"][0].T.reshape(16, 128, 4).transpose(1, 0, 2))
    m["conv_bT"] = np.ascontiguousarray(inp["conv_b"][0].reshape(16, 128).T)
    gb = np.concatenate([inp["b_igate"][0], inp["b_fgate"][0]]).astype(np.float32)
    m["gbias"] = np.ascontiguousarray(np.broadcast_to(gb[None, None, :], (128, NCH, 8)))
    m["mnw"] = np.ascontiguousarray(inp["mlstm_norm_w"][0].reshape(2048))
    oneh = np.zeros((NBP, NBP, 128), np.float32)
    for n in range(NBP):
        oneh[n, n, :] = 1.0
    m["oneh"] = oneh.reshape(NBP, NBP * 128)
    m["w_out"] = inp["w_out"][0]
    m["wqc"] = np.ascontiguousarray(inp["peer_wq"][0].reshape(KC, 128, 16, 128).transpose(2, 1, 0, 3)).reshape(16, 128, KC * 128)
    m["skT"] = np.ascontiguousarray(inp["peer_subkeys"][0].reshape(16, NK, 128).transpose(2, 0, 1))
    m["uTc"] = np.ascontiguousarray(inp["peer_u"][0].reshape(NK, NK, KC, 128).transpose(0, 3, 2, 1)).reshape(NK, 128, KC * NK)
    m["vv"] = inp["peer_v"][0]
    return m


def host_inputs(cfg, core, b, s, inp, shared):
    TOK, T2, NB = cfg.TOK, cfg.T2, cfg.NB
    NBP = max(NB, 8)
    x = inp["x"]
    m = dict(shared)
    own0 = s * TOK
    m["x_own"] = np.ascontiguousarray(x[b, own0:own0 + TOK])
    m["x_past"] = np.ascontiguousarray(x[b, 0:TOK]) if s == 1 else np.zeros((TOK, D), np.float32)
    m["cT"] = np.ascontiguousarray(inp["c"][b].reshape(KC, 128).T)
    m["flag"] = np.full((128, 1), float(s), np.float32)
    pos = np.concatenate([np.arange(TOK), s * TOK + np.arange(TOK)]).astype(np.float32)
    half = 64
    inv = (np.float32(10000.0) ** (-np.arange(half, dtype=np.float32) / np.float32(half))).astype(np.float32)
    ang = (pos[:, None] * inv[None, :]).astype(np.float32)
    m["cs_tab"] = np.concatenate([np.cos(ang), np.sin(ang)], axis=1).astype(np.float32)
    pb = np.full((TOK, NBP), NEG, np.float32)
    for q0 in range(0, TOK, 256):
        jb = NB // 2 + q0 // 256
        lo = 0 if s == 1 else NB // 2
        pb[q0:q0 + 256, lo:jb] = 0.0
    m["pb_tab"] = pb
    return m


_CACHE = {}


def kernel(**inp):
    inp = {k: np.asarray(v) for k, v in inp.items()}
    cfg = Cfg(tok=2048, nk=128)
    if "nc" not in _CACHE:
        _CACHE["nc"] = build(cfg)
    nc = _CACHE["nc"]
    shared = host_shared(cfg, inp)
    maps = []
    for core in range(8):
        b, s = core // 2, core % 2
        maps.append(host_inputs(cfg, core, b, s, inp, shared))
    res = run_bass_kernel_spmd(nc, maps, core_ids=list(range(8)))
    out = np.zeros((4, 4096, D), np.float32)
    for core in range(8):
        b, s = core // 2, core % 2
        out[b, s * cfg.TOK:(s + 1) * cfg.TOK] = res.results[core]["y"]
    return out
```

```python
import numpy as np
from contextlib import ExitStack
import ml_dtypes
import concourse.bass as bass
import concourse.mybir as mybir
from concourse.bass_utils import run_bass_kernel_spmd

F32 = mybir.dt.float32
BF16 = mybir.dt.bfloat16
AF = mybir.ActivationFunctionType
ALU = mybir.AluOpType
AX = mybir.AxisListType

D = 4096
KC = 32
NDS = 24
EPS = 1e-6
NEG = -1.0e30


class Tk:
    __slots__ = ("w", "rs")

    def __init__(self):
        self.w = None
        self.rs = {}


class Tile:
    def __init__(self, t, n=1):
        self.t = t
        self.k = Tk()
        self.ks = [Tk() for _ in range(n)]

    def __getitem__(self, idx):
        return self.t[idx]


class KB:
    EPOCH = 20000

    def __init__(self, nc, st):
        self.nc = nc
        self.st = st
        self.eng = {"pe": nc.tensor, "act": nc.scalar, "dve": nc.vector, "pool": nc.gpsimd, "sp": nc.sync}
        self.cnt = {e: 0 for e in self.eng}
        self.sem = {e: self.newsem() for e in self.eng}
        self.last = {e: None for e in self.eng}
        self.seen = {}
        self.dsem = [self.newsem() for _ in range(NDS)]
        self.dval = [0] * NDS
        self.dnext = 0
        self.psum = []
        self.pnext = 0
        for i in range(8):
            self.psum.append(Tile(st.enter_context(nc.psum_tensor(f"ps{i}", [128, 512], F32))))
        self.pst = None

    def newsem(self):
        self._ns = getattr(self, "_ns", 0) + 1
        return self.st.enter_context(self.nc.semaphore(f"sem{self._ns}"))

    psum_rot = 8

    def ps(self):
        p = self.psum[self.pnext]
        self.pnext = (self.pnext + 1) % self.psum_rot
        return p

    def sb(self, shape, dtype, n=1, name=None):
        self._nm = getattr(self, "_nm", 0) + 1
        t = self.pst.enter_context(self.nc.sbuf_tensor(name or f"sb{self._nm}", list(shape), dtype))
        return Tile(t, n)

    def _wait(self, e, tk):
        sem, val = tk[0], tk[1]
        key = (e, id(sem))
        if self.seen.get(key, 0) >= val:
            return
        self.eng[e].wait_ge(sem, val)
        self.seen[key] = val

    def _deps(self, e, reads, writes):
        ticks = []
        for t in reads:
            if t.w is not None:
                ticks.append(t.w)
        for t in writes:
            if t.w is not None:
                ticks.append(t.w)
            ticks.extend(t.rs.values())
        for tk in ticks:
            if tk[2] == e and e == "pe":
                continue
            self._wait(e, tk)

    def _mark(self, tk, reads, writes):
        for t in reads:
            t.rs[id(tk[0])] = tk
        for t in writes:
            t.w = tk
            t.rs = {}

    @staticmethod
    def _tk(objs):
        out = []
        for o in objs:
            out.append(o.k if isinstance(o, Tile) else o)
        return out

    def op(self, e, fn, reads=(), writes=()):
        reads, writes = self._tk(reads), self._tk(writes)
        self._deps(e, reads, writes)
        if self.cnt[e] >= self.EPOCH:
            self.sem[e] = self.newsem()
            self.cnt[e] = 0
        self.cnt[e] += 1
        ins = fn(self.eng[e])
        ins.then_inc(self.sem[e], 1)
        tk = (self.sem[e], self.cnt[e], e)
        self.last[e] = tk
        self._mark(tk, reads, writes)

    def dma(self, q, out, in_, reads=(), writes=(), **kw):
        reads, writes = self._tk(reads), self._tk(writes)
        self._deps(q, reads, writes)
        i = self.dnext
        self.dnext = (self.dnext + 1) % NDS
        if self.dval[i] > 0:
            self._wait(q, (self.dsem[i], self.dval[i]))
        self.dval[i] += 16
        self.eng[q].dma_start(out=out, in_=in_, **kw).then_inc(self.dsem[i], 16)
        tk = (self.dsem[i], self.dval[i], "dma")
        self._mark(tk, reads, writes)

    def barrier(self):
        for e in self.eng:
            for e2 in self.eng:
                if e2 != e and self.last[e2] is not None:
                    self._wait(e, self.last[e2])
            for i in range(NDS):
                if self.dval[i] > 0:
                    self._wait(e, (self.dsem[i], self.dval[i]))

    def phase(self):
        kb = self

        class _P:
            def __enter__(s):
                kb.pst = ExitStack()
                kb.pst.__enter__()
                return kb

            def __exit__(s, *a):
                kb.barrier()
                kb.pst.__exit__(*a)
                kb.pst = None
                return False

        return _P()


class Cfg:
    def __init__(self, tok=2048, nk=128):
        self.TOK = tok
        self.T2 = 2 * tok
        self.NK = nk
        self.NE = nk * nk
        self.NB = self.T2 // 256
        self.NCH = self.T2 // 128


def build(cfg, upto=99):
    TOK, T2 = cfg.TOK, cfg.T2
    nc = bass.Bass("TRN2", target_bir_lowering=False)

    def din(name, shape, dt=F32):
        return nc.dram_tensor(name, list(shape), dt, kind="ExternalInput").ap()

    def dscr(name, shape, dt=F32):
        return nc.dram_tensor(name, list(shape), dt).ap()

    x_own = din("x_own", [TOK, D])
    x_past = din("x_past", [TOK, D])
    cT = din("cT", [128, KC])
    w_ada = din("w_ada", [D, 6 * D])
    b_adaT = din("b_adaT", [128, 192])
    n1wT = din("n1wT", [128, KC])
    n2wT = din("n2wT", [128, KC])
    fnw = din("fnw", [D])
    w_in = din("w_in", [D, 12296])
    flag = din("flag", [128, 1])
    ident_f = din("ident_f", [128, 128])
    y = nc.dram_tensor("y", [TOK, D], F32, kind="ExternalOutput").ap()
    NK, NE, NB, NCH = cfg.NK, cfg.NE, cfg.NB, cfg.NCH
    NBP = max(NB, 8)
    consts = din("consts", [128, 5, 128])
    conv_wT = din("conv_wT", [128, 16, 4])
    conv_bT = din("conv_bT", [128, 16])
    gbias = din("gbias", [128, NCH, 8])
    mnw = din("mnw", [2048])
    cs_tab = din("cs_tab", [T2, 128])
    pb_tab = din("pb_tab", [TOK, NBP])
    oneh = din("oneh", [NBP, NBP * 128])
    w_out = din("w_out", [D, D])
    wqc = din("wqc", [16, 128, KC * 128])
    skT = din("skT", [128, 16, NK])
    uTc = din("uTc", [NK, 128, KC * NK])
    vv = din("vv", [NE, D])
    qkc_d = dscr("qkc_d", [2048, T2], BF16)
    hcat_d = dscr("hcat_d", [TOK, D], BF16)
    x2_d = dscr("x2_d", [TOK, D])
    xn2T_d = dscr("xn2T_d", [128, KC, TOK], BF16)
    s_d = dscr("s_d", [TOK, 16, NK])
    peer_d = dscr("peer_d", [TOK, D])
    ans_d = dscr("ans_d", [TOK // 512, 3, 128, 4 * 8 * NK])

    mv_d = dscr("mv_d", [T2, 2048], BF16)
    o_d = dscr("o_d", [TOK, 2048])
    if_d = dscr("if_d", [T2, 8])
    aq_d = dscr("aq_d", [TOK, 2048])
    ak_d = dscr("ak_d", [T2, 2048])
    av_d = dscr("av_d", [T2, 2048], BF16)
    qkT_d = dscr("qkT_d", [2048, 4 + T2])
    mod_d = dscr("mod_d", [192, 128])
    dbg = {}

    with ExitStack() as st:
        kb = KB(nc, st)
        kb.pst = st
        modT = kb.sb([128, 192], F32, name="modT")
        A1 = kb.sb([128, KC], F32, name="A1")
        A1p = kb.sb([128, KC], F32, name="A1p")
        S1p = kb.sb([128, KC], F32, name="S1p")
        A2 = kb.sb([128, KC], F32, name="A2")
        identb = kb.sb([128, 128], BF16, name="identb")
        identf = kb.sb([128, 128], F32, name="identf")
        flg = kb.sb([128, 1], F32, name="flg")
        cst = kb.sb([128, 5, 128], F32, name="cst")
        onesb = kb.sb([128, 128], BF16, name="onesb")
        kb.pst = None
        kb.dma("sp", cst[:, :, :], consts[:, :, :], writes=[cst])
        kb.dma("pool", onesb[:, :], consts[:, 4, :], writes=[onesb])
        kb.dma("sp", identf[:, :], ident_f[:, :], writes=[identf])
        kb.dma("pool", identb[:, :], ident_f[:, :], writes=[identb])
        kb.dma("sp", flg[:, :], flag[:, :], writes=[flg])

        with kb.phase():
            c_sb = kb.sb([128, KC], F32)
            cs = kb.sb([128, KC], BF16)
            bad = kb.sb([128, 192], F32)
            n1 = kb.sb([128, KC], F32)
            n2 = kb.sb([128, KC], F32)
            wb = [kb.sb([128, KC, 512], BF16) for _ in range(2)]
            kb.dma("sp", c_sb[:, :], cT[:, :], writes=[c_sb])
            kb.dma("sp", bad[:, :], b_adaT[:, :], writes=[bad])
            kb.dma("sp", n1[:, :], n1wT[:, :], writes=[n1])
            kb.dma("sp", n2[:, :], n2wT[:, :], writes=[n2])
            kb.op("act", lambda e: e.activation(out=cs[:, :], in_=c_sb[:, :], func=AF.Silu), reads=[c_sb], writes=[cs])
            wv = w_ada.rearrange("(kc p) n -> p kc n", p=128)
            pacc = kb.ps()
            for cc in range(48):
                w = wb[cc % 2]
                kb.dma("pool", w[:, :, :], wv[:, :, cc * 512:(cc + 1) * 512], writes=[w])
                for sub in range(4):
                    j = cc * 4 + sub
                    for kc in range(KC):
                        kb.op("pe", lambda e, w=w, kc=kc, sub=sub, j=j: e.matmul(
                            pacc[:, j:j + 1], lhsT=w[:, kc, sub * 128:(sub + 1) * 128], rhs=cs[:, kc:kc + 1],
                            start=(kc == 0), stop=(kc == KC - 1)), reads=[w, cs], writes=[pacc])
            kb.op("dve", lambda e: e.tensor_tensor(out=modT[:, :], in0=pacc[:, 0:192], in1=bad[:, :], op=ALU.add),
                  reads=[pacc, bad], writes=[modT])
            kb.op("dve", lambda e: e.scalar_tensor_tensor(out=A1[:, :], in0=modT[:, 32:64], scalar=1.0, in1=n1[:, :],
                                                          op0=ALU.add, op1=ALU.mult), reads=[modT, n1], writes=[A1])
            kb.op("dve", lambda e: e.scalar_tensor_tensor(out=A2[:, :], in0=modT[:, 128:160], scalar=1.0, in1=n2[:, :],
                                                          op0=ALU.add, op1=ALU.mult), reads=[modT, n2], writes=[A2])
            kb.op("dve", lambda e: e.tensor_scalar(out=A1p[:, :], in0=A1[:, :], scalar1=flg[:, 0:1], scalar2=None,
                                                   op0=ALU.mult), reads=[A1, flg], writes=[A1p])
            kb.op("dve", lambda e: e.tensor_scalar(out=S1p[:, :], in0=modT[:, 0:32], scalar1=flg[:, 0:1], scalar2=None,
                                                   op0=ALU.mult), reads=[modT, flg], writes=[S1p])
            mrow = kb.sb([128, 2, 128], F32)
            for hh in range(2):
                pt = kb.ps()
                n = 128 if hh == 0 else 64
                kb.op("pe", lambda e, pt=pt, hh=hh, n=n: e.matmul(pt[0:n, 0:128], lhsT=modT[:, hh * 128:hh * 128 + n],
                                                                  rhs=identf[:, :], start=True, stop=True),
                      reads=[modT, identf], writes=[pt])
                kb.op("act", lambda e, pt=pt, hh=hh, n=n: e.activation(out=mrow[0:n, hh, :], in_=pt[0:n, 0:128], func=AF.Copy),
                      reads=[pt], writes=[mrow])
                kb.dma("sp", mod_d[hh * 128:hh * 128 + n, :], mrow[0:n, hh, :], reads=[mrow])
        if upto <= 1:
            dbg["modT"] = None

        TG = 512
        TGB = 1024 if TOK % 1024 == 0 else 512
        NG = T2 // TGB
        NTB = TGB // 128
        wiv = w_in.rearrange("(kc p) n -> p kc n", p=128)
        chunks = []
        for i in range(4):
            chunks.append((i * 512, 512, "qk"))
        for i in range(4):
            chunks.append((2048 + i * 512, 512, "mv"))
        for i in range(4):
            chunks.append((4096 + i * 512, 512, "o"))
        chunks.append((6144, 8, "if"))
        for i in range(4):
            chunks.append((6152 + i * 512, 512, "aq"))
        for i in range(4):
            chunks.append((8200 + i * 512, 512, "ak"))
        for i in range(4):
            chunks.append((10248 + i * 512, 512, "av"))
        with kb.phase():
            xt = [kb.sb([128, D], F32) for _ in range(2)]
            junk = kb.sb([128, D], BF16)
            xs = [kb.sb([128, D], BF16) for _ in range(2)]
            ss = [kb.sb([128, 1], F32) for _ in range(2)]
            xnT = kb.sb([128, KC, TGB], BF16, n=NTB)
            wb = [kb.sb([128, KC, 512], BF16) for _ in range(2)]
            stf = [kb.sb([128, 512], F32) for _ in range(3)]
            stb = [kb.sb([128, 512], BF16) for _ in range(3)]
            zpad = kb.sb([128, 4], F32)
            kb.op("dve", lambda e: e.memset(zpad[:, :], 0.0), writes=[zpad])
            for cc in range(16):
                kb.dma("sp", qkT_d[cc * 128:(cc + 1) * 128, 0:4], zpad[:, :], reads=[zpad])
            nst = 0
            nw = 0
            for g in range(NG):
                past = (g * TGB) < TOK
                for ti in range(NTB):
                    tok0 = g * TGB + ti * 128
                    src = x_past[tok0:tok0 + 128, :] if past else x_own[tok0 - TOK:tok0 - TOK + 128, :]
                    it = g * NTB + ti
                    x_t, xs_t, ss_t = xt[it % 2], xs[it % 2], ss[it % 2]
                    kb.dma("sp", x_t[:, :], src, writes=[x_t])
                    kb.op("act", lambda e, x_t=x_t, ss_t=ss_t: e.activation(out=junk[:, :], in_=x_t[:, :], func=AF.Square,
                                                                           accum_out=ss_t[:, 0:1]),
                          reads=[x_t], writes=[junk, ss_t])
                    kb.op("dve", lambda e, ss_t=ss_t: e.tensor_scalar(out=ss_t[:, :], in0=ss_t[:, :], scalar1=1.0 / D,
                                                                      scalar2=EPS, op0=ALU.mult, op1=ALU.add),
                          reads=[ss_t], writes=[ss_t])
                    kb.op("act", lambda e, ss_t=ss_t: e.activation(out=ss_t[:, :], in_=ss_t[:, :], func=AF.Sqrt),
                          reads=[ss_t], writes=[ss_t])
                    kb.op("dve", lambda e, ss_t=ss_t: e.reciprocal(out=ss_t[:, :], in_=ss_t[:, :]), reads=[ss_t], writes=[ss_t])
                    kb.op("dve", lambda e, x_t=x_t, xs_t=xs_t, ss_t=ss_t: e.tensor_scalar(
                        out=xs_t[:, :], in0=x_t[:, :], scalar1=ss_t[:, 0:1], scalar2=None, op0=ALU.mult),
                        reads=[x_t, ss_t], writes=[xs_t])
                    Asel, Ssel = (A1p, S1p) if past else (A1, None)
                    for k4 in range(KC // 4):
                        pt = kb.ps()
                        for q in range(4):
                            kc = k4 * 4 + q
                            kb.op("pe", lambda e, pt=pt, q=q, kc=kc, xs_t=xs_t: e.matmul(
                                pt[:, q * 128:(q + 1) * 128], lhsT=xs_t[:, kc * 128:(kc + 1) * 128], rhs=identb[:, :],
                                start=True, stop=True), reads=[xs_t, identb], writes=[pt])
                        for q in range(4):
                            kc = k4 * 4 + q
                            bias_ap = (S1p[:, kc:kc + 1] if past else modT[:, kc:kc + 1])
                            kb.op("act", lambda e, pt=pt, q=q, kc=kc, ti=ti, Asel=Asel, bias_ap=bias_ap: e.activation(
                                out=xnT[:, kc, ti * 128:(ti + 1) * 128], in_=pt[:, q * 128:(q + 1) * 128],
                                func=AF.Identity, scale=Asel[:, kc:kc + 1], bias=bias_ap),
                                reads=[pt, Asel, S1p if past else modT], writes=[xnT.ks[ti]])
                for (c0, ncol, kind) in chunks:
                    if past and kind in ("o", "aq"):
                        continue
                    w = wb[nw % 2]
                    nw += 1
                    kb.dma("pool", w[:, :, 0:ncol], wiv[:, :, c0:c0 + ncol], writes=[w])
                    if kind == "qk":
                        for sub, tb in [(a_, b_) for a_ in range(4) for b_ in range(TGB // 512)]:
                            pt = kb.ps()
                            for kc in range(KC):
                                kb.op("pe", lambda e, pt=pt, kc=kc, sub=sub, w=w, tb=tb: e.matmul(
                                    pt[:, :], lhsT=w[:, kc, sub * 128:(sub + 1) * 128], rhs=xnT[:, kc, tb * 512:(tb + 1) * 512],
                                    start=(kc == 0), stop=(kc == KC - 1)), reads=[w] + xnT.ks, writes=[pt])
                            s_t = stf[nst % 3]
                            ev = "act" if nst % 2 == 0 else "dve"
                            nst += 1
                            if ev == "act":
                                kb.op("act", lambda e, pt=pt, s_t=s_t: e.activation(out=s_t[:, :], in_=pt[:, :], func=AF.Copy),
                                      reads=[pt], writes=[s_t])
                            else:
                                kb.op("dve", lambda e, pt=pt, s_t=s_t: e.tensor_copy(out=s_t[:, :], in_=pt[:, :]),
                                      reads=[pt], writes=[s_t])
                            ch0 = c0 + sub * 128
                            kb.dma("sp", qkT_d[ch0:ch0 + 128, 4 + g * TGB + tb * 512:4 + g * TGB + (tb + 1) * 512], s_t[:, :], reads=[s_t])
                    else:
                        for ti in range(NTB):
                            tok0 = g * TGB + ti * 128
                            pt = kb.ps()
                            for kc in range(KC):
                                kb.op("pe", lambda e, pt=pt, kc=kc, ti=ti, w=w, ncol=ncol: e.matmul(
                                    pt[:, 0:ncol], lhsT=xnT[:, kc, ti * 128:(ti + 1) * 128], rhs=w[:, kc, 0:ncol],
                                    start=(kc == 0), stop=(kc == KC - 1)), reads=[w, xnT.ks[ti]], writes=[pt])
                            isb = kind in ("mv", "av")
                            s_t = (stb if isb else stf)[nst % 3]
                            ev = "act" if nst % 2 == 0 else "dve"
                            nst += 1
                            if ev == "act":
                                kb.op("act", lambda e, pt=pt, s_t=s_t, ncol=ncol: e.activation(out=s_t[:, 0:ncol], in_=pt[:, 0:ncol],
                                                                                              func=AF.Copy), reads=[pt], writes=[s_t])
                            else:
                                kb.op("dve", lambda e, pt=pt, s_t=s_t, ncol=ncol: e.tensor_copy(out=s_t[:, 0:ncol], in_=pt[:, 0:ncol]),
                                      reads=[pt], writes=[s_t])
                            if kind == "mv":
                                dst = mv_d[tok0:tok0 + 128, c0 - 2048:c0 - 2048 + 512]
                            elif kind == "o":
                                dst = o_d[tok0 - TOK:tok0 - TOK + 128, c0 - 4096:c0 - 4096 + 512]
                            elif kind == "if":
                                dst = if_d[tok0:tok0 + 128, 0:8]
                            elif kind == "aq":
                                dst = aq_d[tok0 - TOK:tok0 - TOK + 128, c0 - 6152:c0 - 6152 + 512]
                            elif kind == "ak":
                                dst = ak_d[tok0:tok0 + 128, c0 - 8200:c0 - 8200 + 512]
                            else:
                                dst = av_d[tok0:tok0 + 128, c0 - 10248:c0 - 10248 + 512]
                            kb.dma("sp", dst, s_t[:, 0:ncol], reads=[s_t])

        if upto <= 2:
            for nm, ap_, shp, dt in (("d_mod", mod_d, [192, 128], F32), ("d_qkT", qkT_d, [2048, 4 + T2], F32),
                                     ("d_if", if_d, [T2, 8], F32), ("d_ak", ak_d, [T2, 2048], F32),
                                     ("d_mv", mv_d, [T2, 2048], BF16), ("d_o", o_d, [TOK, 2048], F32)):
                o = nc.dram_tensor(nm, shp, dt, kind="ExternalOutput").ap()
                kb.dma("sp", o, ap_, )
            kb.barrier()
            return nc


        TRIu = cst[:, 0, :]
        CTS = cst[:, 1, :]
        CST = cst[:, 2, :]
        SEL127 = cst[:, 3, :]
        ONESF = cst[:, 4, :]

        def ev_copy(i, out, in_, rd, wr):
            if i % 2 == 0:
                kb.op("act", lambda e: e.activation(out=out, in_=in_, func=AF.Copy), reads=rd, writes=wr)
            else:
                kb.op("dve", lambda e: e.tensor_copy(out=out, in_=in_), reads=rd, writes=wr)

        with kb.phase():
            cw = kb.sb([128, 16, 4], F32)
            cb = kb.sb([128, 16], F32)
            kb.dma("sp", cw[:, :, :], conv_wT[:, :, :], writes=[cw])
            kb.dma("sp", cb[:, :], conv_bT[:, :], writes=[cb])
            ub = [kb.sb([128, 4 + T2], F32) for _ in range(2)]
            acc = kb.sb([128, T2], F32)
            ob = [kb.sb([128, T2], BF16) for _ in range(2)]
            for cc in range(16):
                u = ub[cc % 2]
                o_ = ob[cc % 2]
                kb.dma("sp", u[:, :], qkT_d[cc * 128:(cc + 1) * 128, :], writes=[u])
                kb.op("dve", lambda e, u=u, cc=cc: e.tensor_scalar(out=acc[:, :], in0=u[:, 1:1 + T2], scalar1=cw[:, cc, 0:1],
                                                                   scalar2=None, op0=ALU.mult), reads=[u, cw], writes=[acc])
                for j in range(1, 4):
                    kb.op("dve", lambda e, u=u, cc=cc, j=j: e.scalar_tensor_tensor(
                        out=acc[:, :], in0=u[:, 1 + j:1 + j + T2], scalar=cw[:, cc, j:j + 1], in1=acc[:, :],
                        op0=ALU.mult, op1=ALU.add), reads=[u, cw, acc], writes=[acc])
                kb.op("act", lambda e, o_=o_, cc=cc: e.activation(out=o_[:, :], in_=acc[:, :], func=AF.Silu, bias=cb[:, cc:cc + 1]),
                      reads=[acc, cb], writes=[o_])
                if cc < 8:
                    kb.op("dve", lambda e, o_=o_: e.tensor_scalar(out=o_[:, :], in0=o_[:, :], scalar1=0.0625, scalar2=None,
                                                                  op0=ALU.mult), reads=[o_], writes=[o_])
                kb.dma("sp", qkc_d[cc * 128:(cc + 1) * 128, :], o_[:, :], reads=[o_])

        with kb.phase():
            G = kb.sb([128, NCH, 8], F32)
            gb = kb.sb([128, NCH, 8], F32)
            LI = kb.sb([128, NCH, 4], F32)
            LF = kb.sb([128, NCH, 4], F32)
            Bc = kb.sb([128, NCH, 4], F32)
            U = kb.sb([128, NCH, 4], F32)
            kb.dma("sp", G[:, :, :], if_d.rearrange("(c p) g -> p c g", p=128), writes=[G])
            kb.dma("sp", gb[:, :, :], gbias[:, :, :], writes=[gb])
            kb.op("dve", lambda e: e.tensor_tensor(out=G[:, :, :], in0=G[:, :, :], in1=gb[:, :, :], op=ALU.add), reads=[G, gb], writes=[G])
            kb.op("act", lambda e: e.activation(out=G[:, :, :], in_=G[:, :, :], func=AF.Tanh, scale=1.0 / 15.0), reads=[G], writes=[G])
            kb.op("dve", lambda e: e.tensor_scalar(out=LI[:, :, :], in0=G[:, :, 0:4], scalar1=15.0, scalar2=None, op0=ALU.mult),
                  reads=[G], writes=[LI])
            kb.op("act", lambda e: e.activation(out=LF[:, :, :], in_=G[:, :, 4:8], func=AF.Exp, scale=-15.0), reads=[G], writes=[LF])
            kb.op("act", lambda e: e.activation(out=LF[:, :, :], in_=LF[:, :, :], func=AF.Ln, bias=1.0), reads=[LF], writes=[LF])
            kb.op("dve", lambda e: e.tensor_scalar(out=LF[:, :, :], in0=LF[:, :, :], scalar1=-1.0, scalar2=None, op0=ALU.mult),
                  reads=[LF], writes=[LF])
            pb_ = kb.ps()
            kb.op("pe", lambda e: e.matmul(pb_[:, 0:NCH * 4], lhsT=TRIu, rhs=LF[:, :, :].rearrange("p c h -> p (c h)"),
                                           start=True, stop=True), reads=[cst, LF], writes=[pb_])
            kb.op("dve", lambda e: e.tensor_copy(out=Bc[:, :, :].rearrange("p c h -> p (c h)"), in_=pb_[:, 0:NCH * 4]),
                  reads=[pb_], writes=[Bc])
            kb.op("dve", lambda e: e.tensor_tensor(out=U[:, :, :], in0=LI[:, :, :], in1=Bc[:, :, :], op=ALU.subtract),
                  reads=[LI, Bc], writes=[U])

            mnwb = kb.sb([128, 2048], F32)
            kb.dma("sp", mnwb[:, :], mnw.partition_broadcast(128), writes=[mnwb])
            def mkset():
                qTh = kb.sb([128, 2, T2], BF16)
                kTh = kb.sb([128, 2, T2], BF16)
                vh = kb.sb([128, NCH, 512], BF16)
                Cst = kb.sb([128, 2, 512], F32)
                Cb = kb.sb([128, 2, 512], BF16)
                nst_ = kb.sb([128, 2], F32)
                nb_ = kb.sb([128, 2], BF16)
                mcol = kb.sb([128, 1], F32)
                dg = [kb.sb([128, 128], F32) for _ in range(2)]
                Um = kb.sb([128, 128], F32)
                Rm = kb.sb([128, 128], F32)
                ET = kb.sb([128, 128], F32)
                PT = kb.sb([128, 128], BF16)
                sm = [kb.sb([128, 16], F32) for _ in range(2)]
                mr = kb.sb([128, 2], F32)
                kw = kb.sb([128, 256], BF16)
                tmpn = kb.sb([128, 512], F32)
                num = kb.sb([128, 512], F32)
                hh_ = kb.sb([128, 512], F32)
                junk5 = kb.sb([128, 512], BF16)
                ot = kb.sb([128, 512], F32)
                hout = [kb.sb([128, 512], BF16) for _ in range(2)]
                return dict(qTh=qTh, kTh=kTh, vh=vh, Cst=Cst, Cb=Cb, nst_=nst_, nb_=nb_, mcol=mcol, dg=dg, Um=Um, Rm=Rm, ET=ET, PT=PT, sm=sm, mr=mr, kw=kw, tmpn=tmpn, num=num, hh_=hh_, junk5=junk5, ot=ot, hout=hout, nit=0)

            sets = [mkset(), mkset()]

            def head_init(h, T):
                qTh, kTh, vh, Cst, Cb, nst_, nb_, mcol, dg, Um, Rm, ET, PT, sm, mr, kw, tmpn, num, hh_, junk5, ot, hout = T['qTh'], T['kTh'], T['vh'], T['Cst'], T['Cb'], T['nst_'], T['nb_'], T['mcol'], T['dg'], T['Um'], T['Rm'], T['ET'], T['PT'], T['sm'], T['mr'], T['kw'], T['tmpn'], T['num'], T['hh_'], T['junk5'], T['ot'], T['hout']
                kb.dma("sp", qTh[:, :, :], qkc_d[h * 256:(h + 1) * 256, :].rearrange("(j p) t -> p j t", p=128), writes=[qTh])
                kb.dma("sp", kTh[:, :, :], qkc_d[1024 + h * 256:1024 + (h + 1) * 256, :].rearrange("(j p) t -> p j t", p=128), writes=[kTh])
                kb.dma("sp", vh[:, :, :], mv_d[:, h * 512:(h + 1) * 512].rearrange("(c p) v -> p c v", p=128), writes=[vh])
                kb.op("dve", lambda e: e.memset(Cst[:, :, :], 0.0), writes=[Cst])
                kb.op("dve", lambda e: e.memset(Cb[:, :, :], 0.0), writes=[Cb])
                kb.op("dve", lambda e: e.memset(nst_[:, :], 0.0), writes=[nst_])
                kb.op("dve", lambda e: e.memset(nb_[:, :], 0.0), writes=[nb_])
                kb.op("dve", lambda e: e.memset(mcol[:, :], 0.0), writes=[mcol])

            def chunk(h, c, T):
                qTh, kTh, vh, Cst, Cb, nst_, nb_, mcol, dg, Um, Rm, ET, PT, sm, mr, kw, tmpn, num, hh_, junk5, ot, hout = T['qTh'], T['kTh'], T['vh'], T['Cst'], T['Cb'], T['nst_'], T['nb_'], T['mcol'], T['dg'], T['Um'], T['Rm'], T['ET'], T['PT'], T['sm'], T['mr'], T['kw'], T['tmpn'], T['num'], T['hh_'], T['junk5'], T['ot'], T['hout']
                own = c >= NCH // 2
                if c == NCH // 2:
                    kb.op("dve", lambda e: e.tensor_scalar(out=Cst[:, :, :], in0=Cst[:, :, :], scalar1=flg[:, 0:1], scalar2=None, op0=ALU.mult),
                          reads=[Cst, flg], writes=[Cst])
                    kb.op("dve", lambda e: e.tensor_copy(out=Cb[:, :, :], in_=Cst[:, :, :]), reads=[Cst], writes=[Cb])
                    kb.op("dve", lambda e: e.tensor_scalar(out=nst_[:, :], in0=nst_[:, :], scalar1=flg[:, 0:1], scalar2=None, op0=ALU.mult),
                          reads=[nst_, flg], writes=[nst_])
                    kb.op("dve", lambda e: e.tensor_copy(out=nb_[:, :], in_=nst_[:, :]), reads=[nst_], writes=[nb_])
                    kb.op("dve", lambda e: e.tensor_scalar(out=mcol[:, :], in0=mcol[:, :], scalar1=flg[:, 0:1], scalar2=None, op0=ALU.mult),
                          reads=[mcol, flg], writes=[mcol])
                cs_ = slice(c * 128, (c + 1) * 128)
                ucol = U[:, c, h:h + 1]
                bcol = Bc[:, c, h:h + 1]
                s0 = sm[T['nit'] % 2]
                T['nit'] += 1
                d0 = dg[0]
                kb.op("dve", lambda e, d0=d0, ucol=ucol: e.tensor_scalar(out=d0[:, :], in0=identf[:, :], scalar1=ucol, scalar2=None, op0=ALU.mult),
                      reads=[identf, U], writes=[d0])
                pu = kb.ps()
                kb.op("pe", lambda e, pu=pu, d0=d0: e.matmul(pu[:, 0:128], lhsT=ONESF, rhs=d0[:, :], start=True, stop=True),
                      reads=[cst, d0], writes=[pu])
                kb.op("dve", lambda e, pu=pu: e.tensor_tensor(out=Um[:, :], in0=pu[:, 0:128], in1=CTS, op=ALU.add), reads=[pu, cst], writes=[Um])
                kb.op("dve", lambda e, s0=s0: e.tensor_reduce(out=s0[:, 0:1], in_=Um[:, :], axis=AX.X, op=ALU.max), reads=[Um], writes=[s0])
                kb.op("dve", lambda e, s0=s0: e.tensor_tensor(out=mr[:, 1:2], in0=s0[:, 0:1], in1=mcol[:, 0:1], op=ALU.max), reads=[s0, mcol], writes=[mr])
                kb.op("dve", lambda e, bcol=bcol: e.tensor_tensor(out=mr[:, 0:1], in0=mr[:, 1:2], in1=bcol, op=ALU.add), reads=[mr, Bc], writes=[mr])
                pm = kb.ps()
                kb.op("pe", lambda e, pm=pm: e.matmul(pm[:, 0:2], lhsT=SEL127, rhs=mr[:, 0:2], start=True, stop=True), reads=[cst, mr], writes=[pm])
                kb.op("dve", lambda e, pm=pm, s0=s0: e.tensor_copy(out=s0[:, 1:3], in_=pm[:, 0:2]), reads=[pm], writes=[s0])
                if own:
                    d1 = dg[1]
                    kb.op("dve", lambda e, d1=d1: e.tensor_scalar(out=d1[:, :], in0=identf[:, :], scalar1=mr[:, 1:2], scalar2=None, op0=ALU.mult),
                          reads=[identf, mr], writes=[d1])
                    pr = kb.ps()
                    kb.op("pe", lambda e, pr=pr, d1=d1: e.matmul(pr[:, 0:128], lhsT=ONESF, rhs=d1[:, :], start=True, stop=True),
                          reads=[cst, d1], writes=[pr])
                    kb.op("dve", lambda e, pr=pr: e.tensor_tensor(out=Rm[:, :], in0=pr[:, 0:128], in1=CST, op=ALU.add), reads=[pr, cst], writes=[Rm])
                    kb.op("act", lambda e, ucol=ucol: e.activation(out=ET[:, :], in_=Rm[:, :], func=AF.Exp, scale=-1.0, bias=ucol),
                          reads=[Rm, U], writes=[ET])
                    pS = kb.ps()
                    for j in range(2):
                        kb.op("pe", lambda e, pS=pS, j=j, cs_=cs_: e.matmul(pS[:, 0:128], lhsT=kTh[:, j, cs_], rhs=qTh[:, j, cs_],
                                                                          start=(j == 0), stop=(j == 1)), reads=[kTh, qTh], writes=[pS])
                    kb.op("dve", lambda e, pS=pS: e.tensor_tensor(out=PT[:, :], in0=pS[:, 0:128], in1=ET[:, :], op=ALU.mult),
                          reads=[pS, ET], writes=[PT])
                    pnum = kb.ps()
                    kb.op("pe", lambda e, pnum=pnum, c=c: e.matmul(pnum[:, :], lhsT=PT[:, :], rhs=vh[:, c, :], start=True, stop=True),
                          reads=[PT, vh], writes=[pnum])
                    pden = kb.ps()
                    kb.op("pe", lambda e, pden=pden: e.matmul(pden[:, 0:1], lhsT=PT[:, :], rhs=onesb[:, 0:1], start=True, stop=True),
                          reads=[PT, onesb], writes=[pden])
                    pqc = kb.ps()
                    for j in range(2):
                        kb.op("pe", lambda e, pqc=pqc, j=j, cs_=cs_: e.matmul(pqc[:, :], lhsT=qTh[:, j, cs_], rhs=Cb[:, j, :],
                                                                            start=(j == 0), stop=(j == 1)), reads=[qTh, Cb], writes=[pqc])
                    for j in range(2):
                        kb.op("pe", lambda e, pden=pden, j=j, cs_=cs_: e.matmul(pden[:, 1:2], lhsT=qTh[:, j, cs_], rhs=nb_[:, j:j + 1],
                                                                              start=(j == 0), stop=(j == 1)), reads=[qTh, nb_], writes=[pden])
                    kb.op("dve", lambda e, s0=s0: e.tensor_tensor(out=s0[:, 3:4], in0=mcol[:, 0:1], in1=mr[:, 1:2], op=ALU.subtract),
                          reads=[mcol, mr], writes=[s0])
                    kb.op("act", lambda e, s0=s0: e.activation(out=s0[:, 3:4], in_=s0[:, 3:4], func=AF.Exp), reads=[s0], writes=[s0])
                    kb.op("act", lambda e, pqc=pqc, s0=s0: e.activation(out=tmpn[:, :], in_=pqc[:, :], func=AF.Copy, scale=s0[:, 3:4]),
                          reads=[pqc, s0], writes=[tmpn])
                    kb.op("dve", lambda e, pnum=pnum: e.tensor_tensor(out=num[:, :], in0=pnum[:, :], in1=tmpn[:, :], op=ALU.add),
                          reads=[pnum, tmpn], writes=[num])
                    kb.op("dve", lambda e, pden=pden, s0=s0: e.tensor_copy(out=s0[:, 9:11], in_=pden[:, 0:2]), reads=[pden], writes=[s0])
                    kb.op("dve", lambda e, s0=s0: e.scalar_tensor_tensor(out=s0[:, 4:5], in0=s0[:, 10:11], scalar=s0[:, 3:4], in1=s0[:, 9:10],
                                                                         op0=ALU.mult, op1=ALU.add), reads=[s0], writes=[s0])
                    kb.op("dve", lambda e, s0=s0: e.tensor_scalar(out=s0[:, 5:6], in0=s0[:, 4:5], scalar1=-1.0, scalar2=None, op0=ALU.mult),
                          reads=[s0], writes=[s0])
                    kb.op("dve", lambda e, s0=s0: e.tensor_tensor(out=s0[:, 5:6], in0=s0[:, 5:6], in1=s0[:, 4:5], op=ALU.max),
                          reads=[s0], writes=[s0])
                    kb.op("act", lambda e, s0=s0: e.activation(out=s0[:, 6:7], in_=mr[:, 0:1], func=AF.Exp, scale=-1.0), reads=[mr], writes=[s0])
                    kb.op("dve", lambda e, s0=s0: e.tensor_tensor(out=s0[:, 7:8], in0=s0[:, 5:6], in1=s0[:, 6:7], op=ALU.max), reads=[s0], writes=[s0])
                    kb.op("dve", lambda e, s0=s0: e.reciprocal(out=s0[:, 8:9], in_=s0[:, 7:8]), reads=[s0], writes=[s0])
                    kb.op("dve", lambda e, s0=s0: e.tensor_scalar(out=hh_[:, :], in0=num[:, :], scalar1=s0[:, 8:9], scalar2=None, op0=ALU.mult),
                          reads=[num, s0], writes=[hh_])
                    kb.op("act", lambda e, s0=s0: e.activation(out=junk5[:, :], in_=hh_[:, :], func=AF.Square, accum_out=s0[:, 11:12]),
                          reads=[hh_], writes=[junk5, s0])
                    kb.op("dve", lambda e, s0=s0: e.tensor_scalar(out=s0[:, 11:12], in0=s0[:, 11:12], scalar1=1.0 / 512, scalar2=EPS,
                                                                  op0=ALU.mult, op1=ALU.add), reads=[s0], writes=[s0])
                    kb.op("act", lambda e, s0=s0: e.activation(out=s0[:, 11:12], in_=s0[:, 11:12], func=AF.Sqrt), reads=[s0], writes=[s0])
                    kb.op("dve", lambda e, s0=s0: e.reciprocal(out=s0[:, 12:13], in_=s0[:, 11:12]), reads=[s0], writes=[s0])
                    tok0 = (c - NCH // 2) * 128
                    kb.dma("sp", ot[:, :], o_d[tok0:tok0 + 128, h * 512:(h + 1) * 512], writes=[ot])
                    kb.op("act", lambda e: e.activation(out=ot[:, :], in_=ot[:, :], func=AF.Sigmoid), reads=[ot], writes=[ot])
                    kb.op("dve", lambda e, h=h: e.tensor_tensor(out=ot[:, :], in0=ot[:, :], in1=mnwb[:, h * 512:(h + 1) * 512], op=ALU.mult),
                          reads=[ot, mnwb], writes=[ot])
                    ho = hout[c % 2]
                    kb.op("dve", lambda e, s0=s0, ho=ho: e.scalar_tensor_tensor(out=ho[:, :], in0=hh_[:, :], scalar=s0[:, 12:13], in1=ot[:, :],
                                                                               op0=ALU.mult, op1=ALU.mult), reads=[hh_, s0, ot], writes=[ho])
                    kb.dma("sp", hcat_d[tok0:tok0 + 128, h * 512:(h + 1) * 512], ho[:, :], reads=[ho])
                kb.op("dve", lambda e, s0=s0, ucol=ucol: e.tensor_tensor(out=s0[:, 13:14], in0=ucol, in1=s0[:, 2:3], op=ALU.subtract),
                      reads=[U, s0], writes=[s0])
                kb.op("dve", lambda e, s0=s0: e.tensor_tensor(out=s0[:, 14:15], in0=mcol[:, 0:1], in1=s0[:, 2:3], op=ALU.subtract),
                      reads=[mcol, s0], writes=[s0])
                kb.op("act", lambda e, s0=s0: e.activation(out=s0[:, 13:15], in_=s0[:, 13:15], func=AF.Exp), reads=[s0], writes=[s0])
                pk = kb.ps()
                for j in range(2):
                    kb.op("pe", lambda e, pk=pk, j=j, cs_=cs_: e.matmul(pk[:, j * 128:(j + 1) * 128], lhsT=kTh[:, j, cs_], rhs=identb[:, :],
                                                                      start=True, stop=True), reads=[kTh, identb], writes=[pk])
                kb.op("act", lambda e, pk=pk, s0=s0: e.activation(out=kw[:, :], in_=pk[:, 0:256], func=AF.Copy, scale=s0[:, 13:14]),
                      reads=[pk, s0], writes=[kw])
                pn = kb.ps()
                for j in range(2):
                    pkv = kb.ps()
                    kb.op("pe", lambda e, pkv=pkv, j=j, c=c: e.matmul(pkv[:, :], lhsT=kw[:, j * 128:(j + 1) * 128], rhs=vh[:, c, :],
                                                                     start=True, stop=True), reads=[kw, vh], writes=[pkv])
                    kb.op("pe", lambda e, pn=pn, j=j: e.matmul(pn[:, j:j + 1], lhsT=kw[:, j * 128:(j + 1) * 128], rhs=onesb[:, 0:1],
                                                              start=True, stop=True), reads=[kw, onesb], writes=[pn])
                    kb.op("dve", lambda e, pkv=pkv, j=j, s0=s0: e.scalar_tensor_tensor(out=Cst[:, j, :], in0=Cst[:, j, :], scalar=s0[:, 14:15],
                                                                                      in1=pkv[:, :], op0=ALU.mult, op1=ALU.add),
                          reads=[Cst, s0, pkv], writes=[Cst])
                kb.op("act", lambda e: e.activation(out=Cb[:, :, :], in_=Cst[:, :, :], func=AF.Copy), reads=[Cst], writes=[Cb])
                kb.op("dve", lambda e, pn=pn, s0=s0: e.scalar_tensor_tensor(out=nst_[:, :], in0=nst_[:, :], scalar=s0[:, 14:15], in1=pn[:, 0:2],
                                                                           op0=ALU.mult, op1=ALU.add), reads=[nst_, s0, pn], writes=[nst_])
                kb.op("dve", lambda e: e.tensor_copy(out=nb_[:, :], in_=nst_[:, :]), reads=[nst_], writes=[nb_])
                kb.op("dve", lambda e, s0=s0: e.tensor_copy(out=mcol[:, :], in_=s0[:, 1:2]), reads=[s0], writes=[mcol])

            for hp in range(2):
                for slot in range(2):
                    head_init(2 * hp + slot, sets[slot])
                for c in range(NCH):
                    for slot in range(2):
                        chunk(2 * hp + slot, c, sets[slot])


        NTq = TOK // 128
        NTk = T2 // 128
        SCL = 128 ** -0.5
        with kb.phase():
            kb.psum_rot = 6
            kb.pnext = 0
            pO, pL = kb.psum[6], kb.psum[7]
            cst_ = kb.sb([128, NTk, 128], F32)
            kb.dma("sp", cst_[:, :, :], cs_tab.rearrange("(i p) d -> p i d", p=128), writes=[cst_])
            pbt = kb.sb([128, NTq, NBP], F32)
            kb.dma("sp", pbt[:, :, :], pb_tab.rearrange("(i p) n -> p i n", p=128), writes=[pbt])
            ohb = kb.sb([NBP, NBP * 128], BF16)
            kb.dma("pool", ohb[:, :], oneh[:, :], writes=[ohb])
            tri01 = kb.sb([128, 128], BF16)
            kb.op("dve", lambda e: e.tensor_copy(out=tri01[:, :], in_=TRIu), reads=[cst], writes=[tri01])
            xq = kb.sb([128, NTq, 128], F32)
            xk = kb.sb([128, NTk, 128], F32)
            rq = kb.sb([128, NTk, 128], F32)
            t1 = kb.sb([128, NTk, 64], F32)
            qT = kb.sb([128, TOK], BF16)
            kT = kb.sb([128, T2], BF16)
            vk = kb.sb([128, NTk, 132], BF16)
            kb.op("dve", lambda e: e.memset(vk[:, :, :], 1.0), writes=[vk])
            kmT = kb.sb([128, NBP], BF16)
            kms = kb.sb([128, NBP], F32)
            gm = kb.sb([128, NBP], F32)
            t8 = kb.sb([128, 8], F32)
            selb = kb.sb([128, NBP], BF16)
            selT = kb.sb([NBP, TOK], BF16)
            PTm = [kb.sb([128, 512], BF16) for _ in range(3)]
            lrec = [kb.sb([128, 2], F32) for _ in range(2)]
            gmA = kb.sb([128, NTq, NBP], F32)
            t8A = kb.sb([128, NTq, 8], F32)
            selbA = kb.sb([128, NTq, NBP], BF16)
            st_m = {"rot": 0}
            pOs = [pO, pL]
            oo = [kb.sb([128, 128], BF16) for _ in range(2)]

            def rotary(X, R, nt):
                c_, s_ = cst_[:, NTk - nt:NTk, 0:64], cst_[:, NTk - nt:NTk, 64:128]
                x1, x2 = X[:, 0:nt, 0:64], X[:, 0:nt, 64:128]
                T1 = t1[:, 0:nt, :]
                kb.op("dve", lambda e: e.tensor_tensor(out=R[:, 0:nt, 0:64], in0=x1, in1=c_, op=ALU.mult), reads=[X, cst_], writes=[R])
                kb.op("dve", lambda e: e.tensor_tensor(out=T1, in0=x2, in1=s_, op=ALU.mult), reads=[X, cst_], writes=[t1])
                kb.op("dve", lambda e: e.tensor_tensor(out=R[:, 0:nt, 0:64], in0=R[:, 0:nt, 0:64], in1=T1, op=ALU.subtract), reads=[R, t1], writes=[R])
                kb.op("dve", lambda e: e.tensor_tensor(out=R[:, 0:nt, 64:128], in0=x2, in1=c_, op=ALU.mult), reads=[X, cst_], writes=[R])
                kb.op("dve", lambda e: e.tensor_tensor(out=T1, in0=x1, in1=s_, op=ALU.mult), reads=[X, cst_, t1], writes=[t1])
                kb.op("dve", lambda e: e.tensor_tensor(out=R[:, 0:nt, 64:128], in0=R[:, 0:nt, 64:128], in1=T1, op=ALU.add), reads=[R, t1], writes=[R])

            def transp(R, nt, dst):
                for i4 in range(nt // 4):
                    pt = kb.ps()
                    for q in range(4):
                        i = i4 * 4 + q
                        kb.op("pe", lambda e, pt=pt, q=q, i=i: e.matmul(pt[:, q * 128:(q + 1) * 128], lhsT=R[:, i, :], rhs=identf[:, :],
                                                                       start=True, stop=True), reads=[R, identf], writes=[pt])
                    ev_copy(i4, dst[:, i4 * 512:(i4 + 1) * 512], pt[:, :], [pt], [dst])

            nev = 0
            for h in range(16):
                hs = slice(h * 128, (h + 1) * 128)
                kb.dma("sp", xq[:, :, :], aq_d[:, hs].rearrange("(i p) d -> p i d", p=128), writes=[xq])
                kb.dma("sp", xk[:, :, :], ak_d[:, hs].rearrange("(i p) d -> p i d", p=128), writes=[xk])
                kb.dma("sp", vk[:, :, 0:128], av_d[:, hs].rearrange("(i p) d -> p i d", p=128), writes=[vk])
                rotary(xq, rq, NTq)
                transp(rq, NTq, qT)
                rotary(xk, rq, NTk)
                transp(rq, NTk, kT)
                kb.op("dve", lambda e: e.memset(kms[:, :], 0.0), writes=[kms])
                kb.op("dve", lambda e: e.tensor_reduce(out=kms[:, 0:NB], in_=kT[:, :].rearrange("p (n k) -> p n k", k=256), axis=AX.X, op=ALU.add),
                      reads=[kT], writes=[kms])
                kb.op("dve", lambda e: e.tensor_scalar(out=kmT[:, :], in0=kms[:, :], scalar1=1.0 / 256, scalar2=None, op0=ALU.mult),
                      reads=[kms], writes=[kmT])
                pg = kb.ps()
                for i in range(NTq):
                    kb.op("pe", lambda e, i=i: e.matmul(pg[:, i * NBP:(i + 1) * NBP], lhsT=qT[:, i * 128:(i + 1) * 128], rhs=kmT[:, :], start=True, stop=True),
                          reads=[qT, kmT], writes=[pg])
                kb.op("dve", lambda e: e.tensor_tensor(out=gmA[:, :, :].rearrange("p a b -> p (a b)"), in0=pg[:, 0:NTq * NBP],
                                                       in1=pbt[:, :, :].rearrange("p a b -> p (a b)"), op=ALU.add), reads=[pg, pbt], writes=[gmA])
                for i in range(NTq):
                    kb.op("dve", lambda e, i=i: e.max(out=t8A[:, i, :], in_=gmA[:, i, :]), reads=[gmA], writes=[t8A])
                kb.op("dve", lambda e: e.tensor_scalar(out=t8A[:, :, 2:3], in0=t8A[:, :, 2:3], scalar1=-1.0e29, scalar2=None, op0=ALU.max), reads=[t8A], writes=[t8A])
                for i in range(NTq):
                    kb.op("dve", lambda e, i=i: e.tensor_scalar(out=gmA[:, i, :], in0=gmA[:, i, :], scalar1=t8A[:, i, 2:3], scalar2=None, op0=ALU.is_ge),
                          reads=[gmA, t8A], writes=[gmA])
                kb.op("dve", lambda e: e.tensor_scalar(out=selbA[:, :, :], in0=gmA[:, :, :], scalar1=30000.0, scalar2=-30000.0, op0=ALU.mult, op1=ALU.add),
                      reads=[gmA], writes=[selbA])
                for i4 in range(NTq // 4):
                    pt = kb.ps()
                    for q in range(4):
                        i = i4 * 4 + q
                        kb.op("pe", lambda e, pt=pt, q=q, i=i: e.matmul(pt[0:NBP, q * 128:(q + 1) * 128], lhsT=selbA[:, i, :], rhs=identb[:, :], start=True, stop=True),
                              reads=[selbA, identb], writes=[pt])
                    kb.op("act", lambda e, pt=pt, i4=i4: e.activation(out=selT[:, i4 * 512:(i4 + 1) * 512], in_=pt[0:NBP, 0:512], func=AF.Copy), reads=[pt], writes=[selT])
                for jb in range(TOK // 256):
                    q2 = slice(jb * 256, (jb + 1) * 256)
                    gb_ = NB // 2 + jb
                    started = [False, False]

                    def score_past(n):
                        pS = kb.ps()
                        for hf in range(2):
                            ks_ = slice(n * 256 + hf * 128, n * 256 + hf * 128 + 128)
                            kb.op("pe", lambda e, pS=pS, hf=hf, ks_=ks_: e.matmul(pS[:, hf * 256:(hf + 1) * 256], lhsT=kT[:, ks_], rhs=qT[:, q2], start=True, stop=False),
                                  reads=[kT, qT], writes=[pS])
                            kb.op("pe", lambda e, pS=pS, hf=hf, n=n: e.matmul(pS[:, hf * 256:(hf + 1) * 256], lhsT=ohb[:, n * 128:(n + 1) * 128], rhs=selT[:, q2], start=False, stop=True),
                                  reads=[ohb, selT], writes=[pS])
                        P_ = PTm[st_m["rot"] % 3]
                        st_m["rot"] += 1
                        kb.op("act", lambda e, pS=pS, P_=P_: e.activation(out=P_[:, :], in_=pS[:, :], func=AF.Exp, scale=SCL), reads=[pS], writes=[P_])
                        return [(P_, hf * 256 + qt * 128, n * 2 + hf, qt) for hf in range(2) for qt in range(2)]

                    def score_own(qt, hf, tri):
                        pS = kb.ps()
                        ks_ = slice(gb_ * 256 + hf * 128, gb_ * 256 + hf * 128 + 128)
                        qs_ = slice(jb * 256 + qt * 128, jb * 256 + (qt + 1) * 128)
                        kb.op("pe", lambda e, pS=pS: e.matmul(pS[:, 0:128], lhsT=kT[:, ks_], rhs=qT[:, qs_], start=True, stop=True), reads=[kT, qT], writes=[pS])
                        P_ = PTm[st_m["rot"] % 3]
                        st_m["rot"] += 1
                        kb.op("act", lambda e, pS=pS, P_=P_: e.activation(out=P_[:, 0:128], in_=pS[:, 0:128], func=AF.Exp, scale=SCL), reads=[pS], writes=[P_])
                        if tri:
                            kb.op("dve", lambda e, P_=P_: e.tensor_tensor(out=P_[:, 0:128], in0=P_[:, 0:128], in1=tri01[:, :], op=ALU.mult), reads=[P_, tri01], writes=[P_])
                        return [(P_, 0, gb_ * 2 + hf, qt)]

                    def pv(lst, lastflags):
                        for (P_, c0_, ki, qt), last in zip(lst, lastflags):
                            first = not started[qt]
                            started[qt] = True
                            kb.op("pe", lambda e, P_=P_, c0_=c0_, ki=ki, qt=qt, first=first, last=last: e.matmul(
                                pOs[qt][:, 0:129], lhsT=P_[:, c0_:c0_ + 128], rhs=vk[:, ki, 0:129], start=first, stop=last), reads=[P_, vk], writes=[pOs[qt]])

                    prev = None
                    for n in range(gb_):
                        cur = score_past(n)
                        if prev is not None:
                            pv(prev, [False] * 4)
                        prev = cur
                    o0 = score_own(0, 0, True)
                    pv(prev, [False] * 4)
                    o1 = score_own(1, 0, False)
                    pv(o0, [True])
                    o2 = score_own(1, 1, True)
                    pv(o1, [False])
                    pv(o2, [True])
                    for qt in range(2):
                        i = jb * 2 + qt
                        lr = lrec[i % 2]
                        kb.op("dve", lambda e, qt=qt, lr=lr: e.reciprocal(out=lr[:, 0:1], in_=pOs[qt][:, 128:129]), reads=[pOs[qt]], writes=[lr])
                        o_ = oo[i % 2]
                        kb.op("act", lambda e, o_=o_, qt=qt, lr=lr: e.activation(out=o_[:, :], in_=pOs[qt][:, 0:128], func=AF.Copy, scale=lr[:, 0:1]), reads=[pOs[qt], lr], writes=[o_])
                        kb.dma("sp", hcat_d[i * 128:(i + 1) * 128, 2048 + h * 128:2048 + (h + 1) * 128], o_[:, :], reads=[o_])
            kb.psum_rot = 8
            kb.pnext = 0

        modflat = mod_d.rearrange("a b -> (a b)")

        def norm_T(x_t, junk_, xs_t, ssc, Acol, bias_fn, dstT, ti):
            kb.op("act", lambda e: e.activation(out=junk_[:, :], in_=x_t[:, :], func=AF.Square, accum_out=ssc[:, 0:1]), reads=[x_t], writes=[junk_, ssc])
            kb.op("dve", lambda e: e.tensor_scalar(out=ssc[:, 0:1], in0=ssc[:, 0:1], scalar1=1.0 / D, scalar2=EPS, op0=ALU.mult, op1=ALU.add), reads=[ssc], writes=[ssc])
            kb.op("act", lambda e: e.activation(out=ssc[:, 0:1], in_=ssc[:, 0:1], func=AF.Sqrt), reads=[ssc], writes=[ssc])
            kb.op("dve", lambda e: e.reciprocal(out=ssc[:, 1:2], in_=ssc[:, 0:1]), reads=[ssc], writes=[ssc])
            kb.op("dve", lambda e: e.tensor_scalar(out=xs_t[:, :], in0=x_t[:, :], scalar1=ssc[:, 1:2], scalar2=None, op0=ALU.mult), reads=[x_t, ssc], writes=[xs_t])
            for k4 in range(KC // 4):
                pt = kb.ps()
                for q in range(4):
                    kc = k4 * 4 + q
                    kb.op("pe", lambda e, pt=pt, q=q, kc=kc: e.matmul(pt[:, q * 128:(q + 1) * 128], lhsT=xs_t[:, kc * 128:(kc + 1) * 128], rhs=identb[:, :],
                                                                     start=True, stop=True), reads=[xs_t, identb], writes=[pt])
                for q in range(4):
                    kc = k4 * 4 + q
                    if Acol is None:
                        ev_copy(q, dstT[:, kc, ti * 128:(ti + 1) * 128], pt[:, q * 128:(q + 1) * 128], [pt], [dstT.ks[ti]])
                    else:
                        kb.op("act", lambda e, pt=pt, q=q, kc=kc: e.activation(out=dstT[:, kc, ti * 128:(ti + 1) * 128], in_=pt[:, q * 128:(q + 1) * 128],
                                                                              func=AF.Identity, scale=Acol[:, kc:kc + 1], bias=bias_fn(kc)),
                              reads=[pt, Acol, modT], writes=[dstT.ks[ti]])

        wov = w_out.rearrange("(kc p) n -> p kc n", p=128)
        with kb.phase():
            g1b = kb.sb([128, D], F32)
            kb.dma("sp", g1b[:, :], modflat[2 * D:3 * D].partition_broadcast(128), writes=[g1b])
            hT = kb.sb([128, KC, TG], BF16, n=4)
            hb = [kb.sb([128, D], BF16) for _ in range(2)]
            xg = [kb.sb([128, D], F32) for _ in range(4)]
            wb = [kb.sb([128, KC, 512], BF16) for _ in range(2)]
            tmpd = [kb.sb([128, 512], F32) for _ in range(2)]
            junk = hb[0]
            xs2 = hb[1]
            ssc = kb.sb([128, 2], F32)
            x2T = hT
            nw = 0
            for g in range(TOK // TG):
                for ti in range(4):
                    tok0 = g * TG + ti * 128
                    hb_ = hb[ti % 2]
                    kb.dma("sp", hb_[:, :], hcat_d[tok0:tok0 + 128, :], writes=[hb_])
                    kb.dma("sp", xg[ti][:, :], x_own[tok0:tok0 + 128, :], writes=[xg[ti]])
                    for k4 in range(KC // 4):
                        pt = kb.ps()
                        for q in range(4):
                            kc = k4 * 4 + q
                            kb.op("pe", lambda e, pt=pt, q=q, kc=kc, hb_=hb_: e.matmul(pt[:, q * 128:(q + 1) * 128], lhsT=hb_[:, kc * 128:(kc + 1) * 128],
                                                                                     rhs=identb[:, :], start=True, stop=True), reads=[hb_, identb], writes=[pt])
                        for q in range(4):
                            kc = k4 * 4 + q
                            ev_copy(q, hT[:, kc, ti * 128:(ti + 1) * 128], pt[:, q * 128:(q + 1) * 128], [pt], [hT.ks[ti]])
                for dc in range(8):
                    w = wb[nw % 2]
                    nw += 1
                    kb.dma("pool", w[:, :, :], wov[:, :, dc * 512:(dc + 1) * 512], writes=[w])
                    for ti in range(4):
                        pt = kb.ps()
                        for kc in range(KC):
                            kb.op("pe", lambda e, pt=pt, kc=kc, ti=ti, w=w: e.matmul(pt[:, :], lhsT=hT[:, kc, ti * 128:(ti + 1) * 128], rhs=w[:, kc, :],
                                                                                    start=(kc == 0), stop=(kc == KC - 1)), reads=[w, hT.ks[ti]], writes=[pt])
                        td = tmpd[ti % 2]
                        ds_ = slice(dc * 512, (dc + 1) * 512)
                        kb.op("dve", lambda e, pt=pt, td=td, ds_=ds_: e.tensor_tensor(out=td[:, :], in0=pt[:, :], in1=g1b[:, ds_], op=ALU.mult), reads=[pt, g1b], writes=[td])
                        kb.op("dve", lambda e, td=td, ds_=ds_, ti=ti: e.tensor_tensor(out=xg[ti][:, ds_], in0=xg[ti][:, ds_], in1=td[:, :], op=ALU.add),
                              reads=[xg[ti], td], writes=[xg[ti]])
                for ti in range(4):
                    tok0 = g * TG + ti * 128
                    kb.dma("sp", x2_d[tok0:tok0 + 128, :], xg[ti][:, :], reads=[xg[ti]])
                    norm_T(xg[ti], junk, xs2, ssc, A2, lambda kc: modT[:, 96 + kc:97 + kc], x2T, ti)
                kb.dma("sp", xn2T_d[:, :, g * TG:(g + 1) * TG], x2T[:, :, :], reads=x2T.ks)

        NGP = TOK // TG
        with kb.phase():
            xT = kb.sb([128, KC, TG], BF16)
            skb = kb.sb([128, 16, NK], BF16)
            kb.dma("pool", skb[:, :, :], skT[:, :, :], writes=[skb])
            wqb = [kb.sb([128, KC, 128], BF16) for _ in range(2)]
            qsb = [kb.sb([128, TG], BF16) for _ in range(2)]
            S2 = kb.sb([128, 4, 8, NK], F32)
            AA = kb.sb([128, 4, 8, NK], F32)
            NT = kb.sb([128, 4, 8, NK], F32)
            s1b = kb.sb([128, NK], F32)
            T1 = kb.sb([128, 16], F32)
            T2_ = kb.sb([128, 16], F32)
            cand = kb.sb([128, 256], F32)
            candb = kb.sb([128, 256], F32)
            c16 = kb.sb([128, 16], F32)
            e16 = kb.sb([128, 16], F32)
            zc = kb.sb([128, 4], F32)
            nwq = 0
            for g in range(NGP):
                kb.dma("sp", xT[:, :, :], xn2T_d[:, :, g * TG:(g + 1) * TG], writes=[xT])
                for hp in range(16):
                    h, half = hp // 2, hp % 2
                    w = wqb[nwq % 2]
                    qs_ = qsb[nwq % 2]
                    nwq += 1
                    kb.dma("pool", w[:, :, :], wqc[hp].rearrange("p (kc e) -> p kc e", e=128), writes=[w], max_dma_last_dim=4096)
                    pq = kb.ps()
                    for kc in range(KC):
                        kb.op("pe", lambda e, pq=pq, kc=kc, w=w: e.matmul(pq[:, :], lhsT=w[:, kc, :], rhs=xT[:, kc, :], start=(kc == 0), stop=(kc == KC - 1)),
                              reads=[w, xT], writes=[pq])
                    kb.op("act", lambda e, pq=pq, qs_=qs_: e.activation(out=qs_[:, :], in_=pq[:, :], func=AF.Copy), reads=[pq], writes=[qs_])
                    for ti in range(4):
                        psc = kb.ps()
                        kb.op("pe", lambda e, psc=psc, ti=ti, hp=hp, qs_=qs_: e.matmul(psc[:, 0:NK], lhsT=qs_[:, ti * 128:(ti + 1) * 128], rhs=skb[:, hp, :], start=True, stop=True),
                              reads=[qs_, skb], writes=[psc])
                        if half == 0:
                            kb.op("dve", lambda e, psc=psc, ti=ti, h=h: e.tensor_copy(out=AA[:, ti, h, :], in_=psc[:, 0:NK]), reads=[psc], writes=[AA])
                        else:
                            kb.op("dve", lambda e, psc=psc, ti=ti, h=h: e.tensor_copy(out=S2[:, ti, h, :], in_=psc[:, 0:NK]), reads=[psc], writes=[S2])
                for ti in range(4):
                    for h in range(8):
                        for (src, dstT_) in ((AA[:, ti, h, :], T1), (S2[:, ti, h, :], T2_)):
                            kb.op("dve", lambda e, src=src, dstT_=dstT_: e.max(out=dstT_[:, 0:8], in_=src), reads=[AA, S2], writes=[dstT_])
                            kb.op("dve", lambda e, src=src, dstT_=dstT_: e.match_replace(out=s1b[:, :], in_to_replace=dstT_[:, 0:8], in_values=src, imm_value=NEG),
                                  reads=[AA, S2, dstT_], writes=[s1b])
                            kb.op("dve", lambda e, dstT_=dstT_: e.max(out=dstT_[:, 8:16], in_=s1b[:, :]), reads=[s1b], writes=[dstT_])
                        for a_ in range(16):
                            kb.op("dve", lambda e, a_=a_: e.tensor_scalar(out=cand[:, a_ * 16:(a_ + 1) * 16], in0=T2_[:, :], scalar1=T1[:, a_:a_ + 1], scalar2=None, op0=ALU.add),
                                  reads=[T1, T2_], writes=[cand])
                        kb.op("dve", lambda e: e.max(out=c16[:, 0:8], in_=cand[:, :]), reads=[cand], writes=[c16])
                        kb.op("dve", lambda e: e.match_replace(out=candb[:, :], in_to_replace=c16[:, 0:8], in_values=cand[:, :], imm_value=NEG), reads=[cand, c16], writes=[candb])
                        kb.op("dve", lambda e: e.max(out=c16[:, 8:16], in_=candb[:, :]), reads=[candb], writes=[c16])
                        kb.op("dve", lambda e: e.tensor_scalar(out=zc[:, 0:1], in0=c16[:, 0:1], scalar1=-1.0, scalar2=None, op0=ALU.mult), reads=[c16], writes=[zc])
                        kb.op("act", lambda e: e.activation(out=e16[:, :], in_=c16[:, :], func=AF.Exp, bias=zc[:, 0:1], accum_out=zc[:, 1:2]), reads=[c16, zc], writes=[e16, zc])
                        kb.op("act", lambda e: e.activation(out=zc[:, 2:3], in_=zc[:, 1:2], func=AF.Ln), reads=[zc], writes=[zc])
                        kb.op("dve", lambda e: e.tensor_tensor(out=zc[:, 2:3], in0=zc[:, 0:1], in1=zc[:, 2:3], op=ALU.subtract), reads=[zc], writes=[zc])
                        kb.op("dve", lambda e, ti=ti, h=h: e.tensor_scalar(out=NT[:, ti, h, :], in0=AA[:, ti, h, :], scalar1=-1.0, scalar2=c16[:, 15:16], op0=ALU.mult, op1=ALU.add),
                              reads=[AA, c16], writes=[NT])
                        kb.op("dve", lambda e, ti=ti, h=h: e.tensor_scalar(out=AA[:, ti, h, :], in0=AA[:, ti, h, :], scalar1=zc[:, 2:3], scalar2=None, op0=ALU.add),
                              reads=[AA, zc], writes=[AA])
                for j_, t_ in enumerate((S2, AA, NT)):
                    kb.dma("sp", ans_d[g, j_, :, :], t_[:, :, :, :].rearrange("p a b c -> p (a b c)"), reads=[t_])

        with kb.phase():
            kb.psum_rot = 4
            kb.pnext = 0
            pA_, pG_ = [kb.psum[4], kb.psum[5]], [kb.psum[6], kb.psum[7]]
            xT = kb.sb([128, KC, TG], BF16)
            S2 = kb.sb([128, 4, 8, NK], F32)
            AA = kb.sb([128, 4, 8, NK], F32)
            NT = kb.sb([128, 4, 8, NK], F32)
            NUB, NVB, NEB = 2, 4, 5
            Ub = [kb.sb([128, KC, NK], BF16) for _ in range(NUB)]
            Vb = [kb.sb([NK, D], BF16) for _ in range(NVB)]
            Eb = [kb.sb([128, NK], BF16) for _ in range(NEB)]
            Gh = [kb.sb([128, NK], BF16) for _ in range(NEB)]
            gel = [kb.sb([NK, TG], BF16) for _ in range(4)]
            wT = [kb.sb([NK, TG], BF16) for _ in range(4)]
            ACC = kb.sb([128, 4, D], F32, n=32)
            NP_ = NK // 2
            st_ = {"rot": 0}

            def v_load(c):
                V_ = Vb[c % NVB]
                kb.dma("pool", V_[:, :], vv[c * NK:(c + 1) * NK, :], writes=[V_], max_dma_last_dim=4096)

            def u_load(c):
                U_ = Ub[c % NUB]
                kb.dma("pool", U_[:, :, :], uTc[c].rearrange("p (kc e) -> p kc e", e=NK), writes=[U_], max_dma_last_dim=4096)

            def block(ps2, ps1, ti, ppa):
                pend = []
                for k in range(8):
                    if ps2 is not None:
                        c0, c1 = 2 * ps2, 2 * ps2 + 1
                        po = kb.ps()
                        ds_ = slice(k * 512, (k + 1) * 512)
                        kb.op("pe", lambda e, po=po, ds_=ds_: e.matmul(po[:, :], lhsT=wT[c0 % 4][:, ti * 128:(ti + 1) * 128], rhs=Vb[c0 % NVB][:, ds_], start=True, stop=False),
                              reads=[wT[c0 % 4], Vb[c0 % NVB]], writes=[po])
                        kb.op("pe", lambda e, po=po, ds_=ds_: e.matmul(po[:, :], lhsT=wT[c1 % 4][:, ti * 128:(ti + 1) * 128], rhs=Vb[c1 % NVB][:, ds_], start=False, stop=True),
                              reads=[wT[c1 % 4], Vb[c1 % NVB]], writes=[po])
                        if ps2 == 0:
                            kb.op("dve", lambda e, po=po, ds_=ds_: e.tensor_copy(out=ACC[:, ti, ds_], in_=po[:, :]), reads=[po], writes=[ACC.ks[ti * 8 + k]])
                        else:
                            kb.op("dve", lambda e, po=po, ds_=ds_: e.tensor_tensor(out=ACC[:, ti, ds_], in0=ACC[:, ti, ds_], in1=po[:, :], op=ALU.add),
                                  reads=[po, ACC.ks[ti * 8 + k]], writes=[ACC.ks[ti * 8 + k]])
                    if ppa is not None:
                        q_, hf = ti // 2, ti % 2
                        c = 2 * ppa + q_
                        U_ = Ub[c % NUB]
                        pA = pA_[q_]
                        for kc in (hf * 16 + 2 * k, hf * 16 + 2 * k + 1):
                            kb.op("pe", lambda e, pA=pA, kc=kc, U_=U_: e.matmul(pA[0:NK, :], lhsT=U_[:, kc, :], rhs=xT[:, kc, :], start=(kc == 0), stop=(kc == KC - 1)),
                                  reads=[U_, xT], writes=[pA])
                    if ps1 is not None:
                        for fn in pend:
                            fn()
                        pend = []
                        h = k
                        for q in range(2):
                            i = 2 * ps1 + q
                            pG = pG_[q]
                            E_ = Eb[st_["rot"] % NEB]
                            G_ = Gh[st_["rot"] % NEB]
                            st_["rot"] += 1
                            kb.op("act", lambda e, E_=E_, h=h, i=i: e.activation(out=E_[:, :], in_=S2[:, ti, h, :], func=AF.Exp, bias=AA[:, ti, h, i:i + 1]),
                                  reads=[S2, AA], writes=[E_])
                            kb.op("dve", lambda e, E_=E_, G_=G_, h=h, i=i: e.scalar_tensor_tensor(out=G_[:, :], in0=S2[:, ti, h, :], scalar=NT[:, ti, h, i:i + 1], in1=E_[:, :],
                                                                                           op0=ALU.is_ge, op1=ALU.mult), reads=[S2, NT, E_], writes=[G_])
                            pend.append(lambda G_=G_, pG=pG, h=h: kb.op(
                                "pe", lambda e: e.matmul(pG[0:NK, ti * 128:(ti + 1) * 128], lhsT=G_[:, :], rhs=identb[:, :], start=(h == 0), stop=(h == 7)),
                                reads=[G_, identb], writes=[pG]))
                for fn in pend:
                    fn()
                if ppa is not None and ti % 2 == 1:
                    q_ = ti // 2
                    c = 2 * ppa + q_
                    g_ = gel[(ppa % 2) * 2 + q_]
                    kb.op("act", lambda e, q_=q_, g_=g_: e.activation(out=g_[:, :], in_=pA_[q_][0:NK, :], func=AF.Gelu), reads=[pA_[q_]], writes=[g_])
                    if c + 2 < NK:
                        u_load(c + 2)

            def s1_gbuild(p, ti):
                for q in range(2):
                    i = 2 * p + q
                    pG = pG_[q]
                    for h in range(8):
                        E_ = Eb[st_["rot"] % NEB]
                        G_ = Gh[st_["rot"] % NEB]
                        st_["rot"] += 1
                        kb.op("act", lambda e, E_=E_, h=h, i=i: e.activation(out=E_[:, :], in_=S2[:, ti, h, :], func=AF.Exp, bias=AA[:, ti, h, i:i + 1]),
                              reads=[S2, AA], writes=[E_])
                        kb.op("dve", lambda e, E_=E_, G_=G_, h=h, i=i: e.scalar_tensor_tensor(out=G_[:, :], in0=S2[:, ti, h, :], scalar=NT[:, ti, h, i:i + 1], in1=E_[:, :],
                                                                                       op0=ALU.is_ge, op1=ALU.mult), reads=[S2, NT, E_], writes=[G_])
                        kb.op("pe", lambda e, G_=G_, pG=pG, h=h: e.matmul(pG[0:NK, ti * 128:(ti + 1) * 128], lhsT=G_[:, :], rhs=identb[:, :], start=(h == 0), stop=(h == 7)),
                              reads=[G_, identb], writes=[pG])

            def s1_end(p):
                for q in range(2):
                    c = 2 * p + q
                    w_ = wT[c % 4]
                    g_ = gel[(p % 2) * 2 + q]
                    kb.op("dve", lambda e, w_=w_, q=q, g_=g_: e.tensor_tensor(out=w_[:, :], in0=pG_[q][0:NK, :], in1=g_[:, :], op=ALU.mult), reads=[pG_[q], g_], writes=[w_])

            def s2_(p, ti):
                c0, c1 = 2 * p, 2 * p + 1
                for dc in range(8):
                    po = kb.ps()
                    ds_ = slice(dc * 512, (dc + 1) * 512)
                    kb.op("pe", lambda e, po=po, ds_=ds_: e.matmul(po[:, :], lhsT=wT[c0 % 4][:, ti * 128:(ti + 1) * 128], rhs=Vb[c0 % NVB][:, ds_], start=True, stop=False),
                          reads=[wT[c0 % 4], Vb[c0 % NVB]], writes=[po])
                    kb.op("pe", lambda e, po=po, ds_=ds_: e.matmul(po[:, :], lhsT=wT[c1 % 4][:, ti * 128:(ti + 1) * 128], rhs=Vb[c1 % NVB][:, ds_], start=False, stop=True),
                          reads=[wT[c1 % 4], Vb[c1 % NVB]], writes=[po])
                    if p == 0:
                        kb.op("dve", lambda e, po=po, ds_=ds_: e.tensor_copy(out=ACC[:, ti, ds_], in_=po[:, :]), reads=[po], writes=[ACC.ks[ti]])
                    else:
                        kb.op("dve", lambda e, po=po, ds_=ds_: e.tensor_tensor(out=ACC[:, ti, ds_], in0=ACC[:, ti, ds_], in1=po[:, :], op=ALU.add),
                              reads=[po, ACC.ks[ti]], writes=[ACC.ks[ti]])

            for g in range(NGP):
                kb.dma("sp", xT[:, :, :], xn2T_d[:, :, g * TG:(g + 1) * TG], writes=[xT])
                for j_, t_ in enumerate((S2, AA, NT)):
                    kb.dma("sp", t_[:, :, :, :].rearrange("p a b c -> p (a b c)"), ans_d[g, j_, :, :], writes=[t_])
                u_load(0)
                u_load(1)
                v_load(0)
                v_load(1)
                for ti in range(4):
                    block(None, None, ti, 0)
                for ti in range(4):
                    block(None, 0, ti, 1 if NP_ > 1 else None)
                s1_end(0)
                for p in range(NP_):
                    nxt = p + 1 < NP_
                    if nxt:
                        v_load(2 * p + 2)
                        v_load(2 * p + 3)
                    for ti in range(4):
                        block(p, p + 1 if nxt else None, ti, p + 2 if p + 2 < NP_ else None)
                    if nxt:
                        s1_end(p + 1)
                for ti in range(4):
                    tok0 = g * TG + ti * 128
                    kb.dma("sp", peer_d[tok0:tok0 + 128, :], ACC[:, ti, :], reads=ACC.ks[ti * 8:(ti + 1) * 8])
            kb.psum_rot = 8
            kb.pnext = 0

        with kb.phase():
            g2b = kb.sb([128, D], F32)
            fnb = kb.sb([128, D], F32)
            kb.dma("sp", g2b[:, :], modflat[5 * D:6 * D].partition_broadcast(128), writes=[g2b])
            kb.dma("sp", fnb[:, :], fnw.partition_broadcast(128), writes=[fnb])
            xa = [kb.sb([128, D], F32) for _ in range(2)]
            pa = [kb.sb([128, D], F32) for _ in range(2)]
            junk = kb.sb([128, D], BF16)
            ssf = [kb.sb([128, 2], F32) for _ in range(2)]
            for it in range(TOK // 128):
                x_, p_, ss_ = xa[it % 2], pa[it % 2], ssf[it % 2]
                rs_ = slice(it * 128, (it + 1) * 128)
                kb.dma("sp", x_[:, :], x2_d[rs_, :], writes=[x_])
                kb.dma("sp", p_[:, :], peer_d[rs_, :], writes=[p_])
                kb.op("dve", lambda e, p_=p_: e.tensor_tensor(out=p_[:, :], in0=p_[:, :], in1=g2b[:, :], op=ALU.mult), reads=[p_, g2b], writes=[p_])
                kb.op("pool", lambda e, p_=p_, x_=x_: e.tensor_tensor(out=x_[:, :], in0=x_[:, :], in1=p_[:, :], op=ALU.add), reads=[p_, x_], writes=[x_])
                kb.op("act", lambda e, x_=x_, ss_=ss_: e.activation(out=junk[:, :], in_=x_[:, :], func=AF.Square, accum_out=ss_[:, 0:1]), reads=[x_], writes=[junk, ss_])
                kb.op("dve", lambda e, ss_=ss_: e.tensor_scalar(out=ss_[:, 0:1], in0=ss_[:, 0:1], scalar1=1.0 / D, scalar2=EPS, op0=ALU.mult, op1=ALU.add), reads=[ss_], writes=[ss_])
                kb.op("act", lambda e, ss_=ss_: e.activation(out=ss_[:, 0:1], in_=ss_[:, 0:1], func=AF.Sqrt), reads=[ss_], writes=[ss_])
                kb.op("dve", lambda e, ss_=ss_: e.reciprocal(out=ss_[:, 1:2], in_=ss_[:, 0:1]), reads=[ss_], writes=[ss_])
                kb.op("dve", lambda e, x_=x_, p_=p_, ss_=ss_: e.scalar_tensor_tensor(out=p_[:, :], in0=x_[:, :], scalar=ss_[:, 1:2], in1=fnb[:, :], op0=ALU.mult, op1=ALU.mult),
                      reads=[x_, ss_, fnb], writes=[p_])
                kb.dma("sp", y[rs_, :], p_[:, :], reads=[p_])


        kb.barrier()
    return nc


def host_shared(cfg, inp):
    TOK, T2, NK, NB, NCH = cfg.TOK, cfg.T2, cfg.NK, cfg.NB, cfg.NCH
    NBP = max(NB, 8)
    m = {}
    idx = np.arange(128)
    le = (idx[:, None] <= idx[None, :])
    consts = np.zeros((128, 5, 128), np.float32)
    consts[:, 0, :] = le.astype(np.float32)
    consts[:, 1, :] = np.where(le.T, 0.0, NEG)
    consts[:, 2, :] = np.where(le, 0.0, 1.0e30)
    consts[127, 3, :] = 1.0
    consts[:, 4, :] = 1.0
    m["consts"] = consts
    m["ident_f"] = np.eye(128, dtype=np.float32)
    m["w_ada"] = inp["w_ada"][0]
    m["b_adaT"] = np.ascontiguousarray(inp["b_ada"][0].reshape(192, 128).T)
    m["n1wT"] = np.ascontiguousarray(inp["norm1_w"][0].reshape(KC, 128).T)
    m["n2wT"] = np.ascontiguousarray(inp["norm2_w"][0].reshape(KC, 128).T)
    m["fnw"] = np.ascontiguousarray(inp["final_norm_w"])
    m["w_in"] = inp["w_in"][0]
    m["conv_wT"] = np.ascontiguousarray(inp["conv_w"][0].T.reshape(16, 128, 4).transpose(1, 0, 2))
    m["conv_bT"] = np.ascontiguousarray(inp["conv_b"][0].reshape(16, 128).T)
    gb = np.concatenate([inp["b_igate"][0], inp["b_fgate"][0]]).astype(np.float32)
    m["gbias"] = np.ascontiguousarray(np.broadcast_to(gb[None, None, :], (128, NCH, 8)))
    m["mnw"] = np.ascontiguousarray(inp["mlstm_norm_w"][0].reshape(2048))
    oneh = np.zeros((NBP, NBP, 128), np.float32)
    for n in range(NBP):
        oneh[n, n, :] = 1.0
    m["oneh"] = oneh.reshape(NBP, NBP * 128)
    m["w_out"] = inp["w_out"][0]
    m["wqc"] = np.ascontiguousarray(inp["peer_wq"][0].reshape(KC, 128, 16, 128).transpose(2, 1, 0, 3)).reshape(16, 128, KC * 128)
    m["skT"] = np.ascontiguousarray(inp["peer_subkeys"][0].reshape(16, NK, 128).transpose(2, 0, 1))
    m["uTc"] = np.ascontiguousarray(inp["peer_u"][0].reshape(NK, NK, KC, 128).transpose(0, 3, 2, 1)).reshape(NK, 128, KC * NK)
    m["vv"] = inp["peer_v"][0]
    return m


def host_inputs(cfg, core, b, s, inp, shared):
    TOK, T2, NB = cfg.TOK, cfg.T2, cfg.NB
    NBP = max(NB, 8)
    x = inp["x"]
    m = dict(shared)
    own0 = s * TOK
    m["x_own"] = np.ascontiguousarray(x[b, own0:own0 + TOK])
    m["x_past"] = np.ascontiguousarray(x[b, 0:TOK]) if s == 1 else np.zeros((TOK, D), np.float32)
    m["cT"] = np.ascontiguousarray(inp["c"][b].reshape(KC, 128).T)
    m["flag"] = np.full((128, 1), float(s), np.float32)
    pos = np.concatenate([np.arange(TOK), s * TOK + np.arange(TOK)]).astype(np.float32)
    half = 64
    inv = (np.float32(10000.0) ** (-np.arange(half, dtype=np.float32) / np.float32(half))).astype(np.float32)
    ang = (pos[:, None] * inv[None, :]).astype(np.float32)
    m["cs_tab"] = np.concatenate([np.cos(ang), np.sin(ang)], axis=1).astype(np.float32)
    pb = np.full((TOK, NBP), NEG, np.float32)
    for q0 in range(0, TOK, 256):
        jb = NB // 2 + q0 // 256
        lo = 0 if s == 1 else NB // 2
        pb[q0:q0 + 256, lo:jb] = 0.0
    m["pb_tab"] = pb
    return m


_CACHE = {}


def kernel(**inp):
    inp = {k: np.asarray(v) for k, v in inp.items()}
    cfg = Cfg(tok=2048, nk=128)
    if "nc" not in _CACHE:
        _CACHE["nc"] = build(cfg)
    nc = _CACHE["nc"]
    shared = host_shared(cfg, inp)
    maps = []
    for core in range(8):
        b, s = core // 2, core % 2
        maps.append(host_inputs(cfg, core, b, s, inp, shared))
    res = run_bass_kernel_spmd(nc, maps, core_ids=list(range(8)))
    out = np.zeros((4, 4096, D), np.float32)
    for core in range(8):
        b, s = core // 2, core % 2
        out[b, s * cfg.TOK:(s + 1) * cfg.TOK] = res.results[core]["y"]
    return out
```

```python
import numpy as np
from contextlib import ExitStack
import ml_dtypes
import concourse.bass as bass
import concourse.mybir as mybir
from concourse.bass_utils import run_bass_kernel_spmd

F32 = mybir.dt.float32
BF16 = mybir.dt.bfloat16
AF = mybir.ActivationFunctionType
ALU = mybir.AluOpType
AX = mybir.AxisListType

D = 4096
KC = 32
NDS = 24
EPS = 1e-6
NEG = -1.0e30


class Tk:
    __slots__ = ("w", "rs")

    def __init__(self):
        self.w = None
        self.rs = {}


class Tile:
    def __init__(self, t, n=1):
        self.t = t
        self.k = Tk()
        self.ks = [Tk() for _ in range(n)]

    def __getitem__(self, idx):
        return self.t[idx]


class KB:
    EPOCH = 20000

    def __init__(self, nc, st):
        self.nc = nc
        self.st = st
        self.eng = {"pe": nc.tensor, "act": nc.scalar, "dve": nc.vector, "pool": nc.gpsimd, "sp": nc.sync}
        self.cnt = {e: 0 for e in self.eng}
        self.sem = {e: self.newsem() for e in self.eng}
        self.last = {e: None for e in self.eng}
        self.seen = {}
        self.dsem = [self.newsem() for _ in range(NDS)]
        self.dval = [0] * NDS
        self.dnext = 0
        self.psum = []
        self.pnext = 0
        for i in range(8):
            self.psum.append(Tile(st.enter_context(nc.psum_tensor(f"ps{i}", [128, 512], F32))))
        self.pst = None

    def newsem(self):
        self._ns = getattr(self, "_ns", 0) + 1
        return self.st.enter_context(self.nc.semaphore(f"sem{self._ns}"))

    psum_rot = 8

    def ps(self):
        p = self.psum[self.pnext]
        self.pnext = (self.pnext + 1) % self.psum_rot
        return p

    def sb(self, shape, dtype, n=1, name=None):
        self._nm = getattr(self, "_nm", 0) + 1
        t = self.pst.enter_context(self.nc.sbuf_tensor(name or f"sb{self._nm}", list(shape), dtype))
        return Tile(t, n)

    def _wait(self, e, tk):
        sem, val = tk[0], tk[1]
        key = (e, id(sem))
        if self.seen.get(key, 0) >= val:
            return
        self.eng[e].wait_ge(sem, val)
        self.seen[key] = val

    def _deps(self, e, reads, writes):
        ticks = []
        for t in reads:
            if t.w is not None:
                ticks.append(t.w)
        for t in writes:
            if t.w is not None:
                ticks.append(t.w)
            ticks.extend(t.rs.values())
        for tk in ticks:
            if tk[2] == e and e == "pe":
                continue
            self._wait(e, tk)

    def _mark(self, tk, reads, writes):
        for t in reads:
            t.rs[id(tk[0])] = tk
        for t in writes:
            t.w = tk
            t.rs = {}

    @staticmethod
    def _tk(objs):
        out = []
        for o in objs:
            out.append(o.k if isinstance(o, Tile) else o)
        return out

    def op(self, e, fn, reads=(), writes=()):
        reads, writes = self._tk(reads), self._tk(writes)
        self._deps(e, reads, writes)
        if self.cnt[e] >= self.EPOCH:
            self.sem[e] = self.newsem()
            self.cnt[e] = 0
        self.cnt[e] += 1
        ins = fn(self.eng[e])
        ins.then_inc(self.sem[e], 1)
        tk = (self.sem[e], self.cnt[e], e)
        self.last[e] = tk
        self._mark(tk, reads, writes)

    def dma(self, q, out, in_, reads=(), writes=(), **kw):
        reads, writes = self._tk(reads), self._tk(writes)
        self._deps(q, reads, writes)
        i = self.dnext
        self.dnext = (self.dnext + 1) % NDS
        if self.dval[i] > 0:
            self._wait(q, (self.dsem[i], self.dval[i]))
        self.dval[i] += 16
        self.eng[q].dma_start(out=out, in_=in_, **kw).then_inc(self.dsem[i], 16)
        tk = (self.dsem[i], self.dval[i], "dma")
        self._mark(tk, reads, writes)

    def barrier(self):
        for e in self.eng:
            for e2 in self.eng:
                if e2 != e and self.last[e2] is not None:
                    self._wait(e, self.last[e2])
            for i in range(NDS):
                if self.dval[i] > 0:
                    self._wait(e, (self.dsem[i], self.dval[i]))

    def phase(self):
        kb = self

        class _P:
            def __enter__(s):
                kb.pst = ExitStack()
                kb.pst.__enter__()
                return kb

            def __exit__(s, *a):
                kb.barrier()
                kb.pst.__exit__(*a)
                kb.pst = None
                return False

        return _P()


class Cfg:
    def __init__(self, tok=2048, nk=128):
        self.TOK = tok
        self.T2 = 2 * tok
        self.NK = nk
        self.NE = nk * nk
        self.NB = self.T2 // 256
        self.NCH = self.T2 // 128


def build(cfg, upto=99):
    TOK, T2 = cfg.TOK, cfg.T2
    nc = bass.Bass("TRN2", target_bir_lowering=False)

    def din(name, shape, dt=F32):
        return nc.dram_tensor(name, list(shape), dt, kind="ExternalInput").ap()

    def dscr(name, shape, dt=F32):
        return nc.dram_tensor(name, list(shape), dt).ap()

    x_own = din("x_own", [TOK, D])
    x_past = din("x_past", [TOK, D])
    cT = din("cT", [128, KC])
    w_ada = din("w_ada", [D, 6 * D])
    b_adaT = din("b_adaT", [128, 192])
    n1wT = din("n1wT", [128, KC])
    n2wT = din("n2wT", [128, KC])
    fnw = din("fnw", [D])
    w_in = din("w_in", [D, 12296])
    flag = din("flag", [128, 1])
    ident_f = din("ident_f", [128, 128])
    y = nc.dram_tensor("y", [TOK, D], F32, kind="ExternalOutput").ap()
    NK, NE, NB, NCH = cfg.NK, cfg.NE, cfg.NB, cfg.NCH
    NBP = max(NB, 8)
    consts = din("consts", [128, 5, 128])
    conv_wT = din("conv_wT", [128, 16, 4])
    conv_bT = din("conv_bT", [128, 16])
    gbias = din("gbias", [128, NCH, 8])
    mnw = din("mnw", [2048])
    cs_tab = din("cs_tab", [T2, 128])
    pb_tab = din("pb_tab", [TOK, NBP])
    oneh = din("oneh", [NBP, NBP * 128])
    w_out = din("w_out", [D, D])
    wqc = din("wqc", [16, 128, KC * 128])
    skT = din("skT", [128, 16, NK])
    uTc = din("uTc", [NK, 128, KC * NK])
    vv = din("vv", [NE, D])
    qkc_d = dscr("qkc_d", [2048, T2], BF16)
    hcat_d = dscr("hcat_d", [TOK, D], BF16)
    x2_d = dscr("x2_d", [TOK, D])
    xn2T_d = dscr("xn2T_d", [128, KC, TOK], BF16)
    s_d = dscr("s_d", [TOK, 16, NK])
    peer_d = dscr("peer_d", [TOK, D])
    ans_d = dscr("ans_d", [TOK // 512, 3, 128, 4 * 8 * NK])

    mv_d = dscr("mv_d", [T2, 2048], BF16)
    o_d = dscr("o_d", [TOK, 2048])
    if_d = dscr("if_d", [T2, 8])
    aq_d = dscr("aq_d", [TOK, 2048])
    ak_d = dscr("ak_d", [T2, 2048])
    av_d = dscr("av_d", [T2, 2048], BF16)
    qkT_d = dscr("qkT_d", [2048, 4 + T2])
    mod_d = dscr("mod_d", [192, 128])
    dbg = {}

    with ExitStack() as st:
        kb = KB(nc, st)
        kb.pst = st
        modT = kb.sb([128, 192], F32, name="modT")
        A1 = kb.sb([128, KC], F32, name="A1")
        A1p = kb.sb([128, KC], F32, name="A1p")
        S1p = kb.sb([128, KC], F32, name="S1p")
        A2 = kb.sb([128, KC], F32, name="A2")
        identb = kb.sb([128, 128], BF16, name="identb")
        identf = kb.sb([128, 128], F32, name="identf")
        flg = kb.sb([128, 1], F32, name="flg")
        cst = kb.sb([128, 5, 128], F32, name="cst")
        onesb = kb.sb([128, 128], BF16, name="onesb")
        kb.pst = None
        kb.dma("sp", cst[:, :, :], consts[:, :, :], writes=[cst])
        kb.dma("pool", onesb[:, :], consts[:, 4, :], writes=[onesb])
        kb.dma("sp", identf[:, :], ident_f[:, :], writes=[identf])
        kb.dma("pool", identb[:, :], ident_f[:, :], writes=[identb])
        kb.dma("sp", flg[:, :], flag[:, :], writes=[flg])

        with kb.phase():
            c_sb = kb.sb([128, KC], F32)
            cs = kb.sb([128, KC], BF16)
            bad = kb.sb([128, 192], F32)
            n1 = kb.sb([128, KC], F32)
            n2 = kb.sb([128, KC], F32)
            wb = [kb.sb([128, KC, 512], BF16) for _ in range(2)]
            kb.dma("sp", c_sb[:, :], cT[:, :], writes=[c_sb])
            kb.dma("sp", bad[:, :], b_adaT[:, :], writes=[bad])
            kb.dma("sp", n1[:, :], n1wT[:, :], writes=[n1])
            kb.dma("sp", n2[:, :], n2wT[:, :], writes=[n2])
            kb.op("act", lambda e: e.activation(out=cs[:, :], in_=c_sb[:, :], func=AF.Silu), reads=[c_sb], writes=[cs])
            wv = w_ada.rearrange("(kc p) n -> p kc n", p=128)
            pacc = kb.ps()
            for cc in range(48):
                w = wb[cc % 2]
                kb.dma("pool", w[:, :, :], wv[:, :, cc * 512:(cc + 1) * 512], writes=[w])
                for sub in range(4):
                    j = cc * 4 + sub
                    for kc in range(KC):
                        kb.op("pe", lambda e, w=w, kc=kc, sub=sub, j=j: e.matmul(
                            pacc[:, j:j + 1], lhsT=w[:, kc, sub * 128:(sub + 1) * 128], rhs=cs[:, kc:kc + 1],
                            start=(kc == 0), stop=(kc == KC - 1)), reads=[w, cs], writes=[pacc])
            kb.op("dve", lambda e: e.tensor_tensor(out=modT[:, :], in0=pacc[:, 0:192], in1=bad[:, :], op=ALU.add),
                  reads=[pacc, bad], writes=[modT])
            kb.op("dve", lambda e: e.scalar_tensor_tensor(out=A1[:, :], in0=modT[:, 32:64], scalar=1.0, in1=n1[:, :],
                                                          op0=ALU.add, op1=ALU.mult), reads=[modT, n1], writes=[A1])
            kb.op("dve", lambda e: e.scalar_tensor_tensor(out=A2[:, :], in0=modT[:, 128:160], scalar=1.0, in1=n2[:, :],
                                                          op0=ALU.add, op1=ALU.mult), reads=[modT, n2], writes=[A2])
            kb.op("dve", lambda e: e.tensor_scalar(out=A1p[:, :], in0=A1[:, :], scalar1=flg[:, 0:1], scalar2=None,
                                                   op0=ALU.mult), reads=[A1, flg], writes=[A1p])
            kb.op("dve", lambda e: e.tensor_scalar(out=S1p[:, :], in0=modT[:, 0:32], scalar1=flg[:, 0:1], scalar2=None,
                                                   op0=ALU.mult), reads=[modT, flg], writes=[S1p])
            mrow = kb.sb([128, 2, 128], F32)
            for hh in range(2):
                pt = kb.ps()
                n = 128 if hh == 0 else 64
                kb.op("pe", lambda e, pt=pt, hh=hh, n=n: e.matmul(pt[0:n, 0:128], lhsT=modT[:, hh * 128:hh * 128 + n],
                                                                  rhs=identf[:, :], start=True, stop=True),
                      reads=[modT, identf], writes=[pt])
                kb.op("act", lambda e, pt=pt, hh=hh, n=n: e.activation(out=mrow[0:n, hh, :], in_=pt[0:n, 0:128], func=AF.Copy),
                      reads=[pt], writes=[mrow])
                kb.dma("sp", mod_d[hh * 128:hh * 128 + n, :], mrow[0:n, hh, :], reads=[mrow])
        if upto <= 1:
            dbg["modT"] = None

        TG = 512
        TGB = 1024 if TOK % 1024 == 0 else 512
        NG = T2 // TGB
        NTB = TGB // 128
        wiv = w_in.rearrange("(kc p) n -> p kc n", p=128)
        chunks = []
        for i in range(4):
            chunks.append((i * 512, 512, "qk"))
        for i in range(4):
            chunks.append((2048 + i * 512, 512, "mv"))
        for i in range(4):
            chunks.append((4096 + i * 512, 512, "o"))
        chunks.append((6144, 8, "if"))
        for i in range(4):
            chunks.append((6152 + i * 512, 512, "aq"))
        for i in range(4):
            chunks.append((8200 + i * 512, 512, "ak"))
        for i in range(4):
            chunks.append((10248 + i * 512, 512, "av"))
        with kb.phase():
            xt = [kb.sb([128, D], F32) for _ in range(2)]
            junk = kb.sb([128, D], BF16)
            xs = [kb.sb([128, D], BF16) for _ in range(2)]
            ss = [kb.sb([128, 1], F32) for _ in range(2)]
            xnT = kb.sb([128, KC, TGB], BF16, n=NTB)
            wb = [kb.sb([128, KC, 512], BF16) for _ in range(2)]
            stf = [kb.sb([128, 512], F32) for _ in range(3)]
            stb = [kb.sb([128, 512], BF16) for _ in range(3)]
            zpad = kb.sb([128, 4], F32)
            kb.op("dve", lambda e: e.memset(zpad[:, :], 0.0), writes=[zpad])
            for cc in range(16):
                kb.dma("sp", qkT_d[cc * 128:(cc + 1) * 128, 0:4], zpad[:, :], reads=[zpad])
            nst = 0
            nw = 0
            for g in range(NG):
                past = (g * TGB) < TOK
                for ti in range(NTB):
                    tok0 = g * TGB + ti * 128
                    src = x_past[tok0:tok0 + 128, :] if past else x_own[tok0 - TOK:tok0 - TOK + 128, :]
                    it = g * NTB + ti
                    x_t, xs_t, ss_t = xt[it % 2], xs[it % 2], ss[it % 2]
                    kb.dma("sp", x_t[:, :], src, writes=[x_t])
                    kb.op("act", lambda e, x_t=x_t, ss_t=ss_t: e.activation(out=junk[:, :], in_=x_t[:, :], func=AF.Square,
                                                                           accum_out=ss_t[:, 0:1]),
                          reads=[x_t], writes=[junk, ss_t])
                    kb.op("dve", lambda e, ss_t=ss_t: e.tensor_scalar(out=ss_t[:, :], in0=ss_t[:, :], scalar1=1.0 / D,
                                                                      scalar2=EPS, op0=ALU.mult, op1=ALU.add),
                          reads=[ss_t], writes=[ss_t])
                    kb.op("act", lambda e, ss_t=ss_t: e.activation(out=ss_t[:, :], in_=ss_t[:, :], func=AF.Sqrt),
                          reads=[ss_t], writes=[ss_t])
                    kb.op("dve", lambda e, ss_t=ss_t: e.reciprocal(out=ss_t[:, :], in_=ss_t[:, :]), reads=[ss_t], writes=[ss_t])
                    kb.op("dve", lambda e, x_t=x_t, xs_t=xs_t, ss_t=ss_t: e.tensor_scalar(
                        out=xs_t[:, :], in0=x_t[:, :], scalar1=ss_t[:, 0:1], scalar2=None, op0=ALU.mult),
                        reads=[x_t, ss_t], writes=[xs_t])
                    Asel, Ssel = (A1p, S1p) if past else (A1, None)
                    for k4 in range(KC // 4):
                        pt = kb.ps()
                        for q in range(4):
                            kc = k4 * 4 + q
                            kb.op("pe", lambda e, pt=pt, q=q, kc=kc, xs_t=xs_t: e.matmul(
                                pt[:, q * 128:(q + 1) * 128], lhsT=xs_t[:, kc * 128:(kc + 1) * 128], rhs=identb[:, :],
                                start=True, stop=True), reads=[xs_t, identb], writes=[pt])
                        for q in range(4):
                            kc = k4 * 4 + q
                            bias_ap = (S1p[:, kc:kc + 1] if past else modT[:, kc:kc + 1])
                            kb.op("act", lambda e, pt=pt, q=q, kc=kc, ti=ti, Asel=Asel, bias_ap=bias_ap: e.activation(
                                out=xnT[:, kc, ti * 128:(ti + 1) * 128], in_=pt[:, q * 128:(q + 1) * 128],
                                func=AF.Identity, scale=Asel[:, kc:kc + 1], bias=bias_ap),
                                reads=[pt, Asel, S1p if past else modT], writes=[xnT.ks[ti]])
                for (c0, ncol, kind) in chunks:
                    if past and kind in ("o", "aq"):
                        continue
                    w = wb[nw % 2]
                    nw += 1
                    kb.dma("pool", w[:, :, 0:ncol], wiv[:, :, c0:c0 + ncol], writes=[w])
                    if kind == "qk":
                        for sub, tb in [(a_, b_) for a_ in range(4) for b_ in range(TGB // 512)]:
                            pt = kb.ps()
                            for kc in range(KC):
                                kb.op("pe", lambda e, pt=pt, kc=kc, sub=sub, w=w, tb=tb: e.matmul(
                                    pt[:, :], lhsT=w[:, kc, sub * 128:(sub + 1) * 128], rhs=xnT[:, kc, tb * 512:(tb + 1) * 512],
                                    start=(kc == 0), stop=(kc == KC - 1)), reads=[w] + xnT.ks, writes=[pt])
                            s_t = stf[nst % 3]
                            ev = "act" if nst % 2 == 0 else "dve"
                            nst += 1
                            if ev == "act":
                                kb.op("act", lambda e, pt=pt, s_t=s_t: e.activation(out=s_t[:, :], in_=pt[:, :], func=AF.Copy),
                                      reads=[pt], writes=[s_t])
                            else:
                                kb.op("dve", lambda e, pt=pt, s_t=s_t: e.tensor_copy(out=s_t[:, :], in_=pt[:, :]),
                                      reads=[pt], writes=[s_t])
                            ch0 = c0 + sub * 128
                            kb.dma("sp", qkT_d[ch0:ch0 + 128, 4 + g * TGB + tb * 512:4 + g * TGB + (tb + 1) * 512], s_t[:, :], reads=[s_t])
                    else:
                        for ti in range(NTB):
                            tok0 = g * TGB + ti * 128
                            pt = kb.ps()
                            for kc in range(KC):
                                kb.op("pe", lambda e, pt=pt, kc=kc, ti=ti, w=w, ncol=ncol: e.matmul(
                                    pt[:, 0:ncol], lhsT=xnT[:, kc, ti * 128:(ti + 1) * 128], rhs=w[:, kc, 0:ncol],
                                    start=(kc == 0), stop=(kc == KC - 1)), reads=[w, xnT.ks[ti]], writes=[pt])
                            isb = kind in ("mv", "av")
                            s_t = (stb if isb else stf)[nst % 3]
                            ev = "act" if nst % 2 == 0 else "dve"
                            nst += 1
                            if ev == "act":
                                kb.op("act", lambda e, pt=pt, s_t=s_t, ncol=ncol: e.activation(out=s_t[:, 0:ncol], in_=pt[:, 0:ncol],
                                                                                              func=AF.Copy), reads=[pt], writes=[s_t])
                            else:
                                kb.op("dve", lambda e, pt=pt, s_t=s_t, ncol=ncol: e.tensor_copy(out=s_t[:, 0:ncol], in_=pt[:, 0:ncol]),
                                      reads=[pt], writes=[s_t])
                            if kind == "mv":
                                dst = mv_d[tok0:tok0 + 128, c0 - 2048:c0 - 2048 + 512]
                            elif kind == "o":
                                dst = o_d[tok0 - TOK:tok0 - TOK + 128, c0 - 4096:c0 - 4096 + 512]
                            elif kind == "if":
                                dst = if_d[tok0:tok0 + 128, 0:8]
                            elif kind == "aq":
                                dst = aq_d[tok0 - TOK:tok0 - TOK + 128, c0 - 6152:c0 - 6152 + 512]
                            elif kind == "ak":
                                dst = ak_d[tok0:tok0 + 128, c0 - 8200:c0 - 8200 + 512]
                            else:
                                dst = av_d[tok0:tok0 + 128, c0 - 10248:c0 - 10248 + 512]
                            kb.dma("sp", dst, s_t[:, 0:ncol], reads=[s_t])

        if upto <= 2:
            for nm, ap_, shp, dt in (("d_mod", mod_d, [192, 128], F32), ("d_qkT", qkT_d, [2048, 4 + T2], F32),
                                     ("d_if", if_d, [T2, 8], F32), ("d_ak", ak_d, [T2, 2048], F32),
                                     ("d_mv", mv_d, [T2, 2048], BF16), ("d_o", o_d, [TOK, 2048], F32)):
                o = nc.dram_tensor(nm, shp, dt, kind="ExternalOutput").ap()
                kb.dma("sp", o, ap_, )
            kb.barrier()
            return nc


        TRIu = cst[:, 0, :]
        CTS = cst[:, 1, :]
        CST = cst[:, 2, :]
        SEL127 = cst[:, 3, :]
        ONESF = cst[:, 4, :]

        def ev_copy(i, out, in_, rd, wr):
            if i % 2 == 0:
                kb.op("act", lambda e: e.activation(out=out, in_=in_, func=AF.Copy), reads=rd, writes=wr)
            else:
                kb.op("dve", lambda e: e.tensor_copy(out=out, in_=in_), reads=rd, writes=wr)

        with kb.phase():
            cw = kb.sb([128, 16, 4], F32)
            cb = kb.sb([128, 16], F32)
            kb.dma("sp", cw[:, :, :], conv_wT[:, :, :], writes=[cw])
            kb.dma("sp", cb[:, :], conv_bT[:, :], writes=[cb])
            ub = [kb.sb([128, 4 + T2], F32) for _ in range(2)]
            acc = kb.sb([128, T2], F32)
            ob = [kb.sb([128, T2], BF16) for _ in range(2)]
            for cc in range(16):
                u = ub[cc % 2]
                o_ = ob[cc % 2]
                kb.dma("sp", u[:, :], qkT_d[cc * 128:(cc + 1) * 128, :], writes=[u])
                kb.op("dve", lambda e, u=u, cc=cc: e.tensor_scalar(out=acc[:, :], in0=u[:, 1:1 + T2], scalar1=cw[:, cc, 0:1],
                                                                   scalar2=None, op0=ALU.mult), reads=[u, cw], writes=[acc])
                for j in range(1, 4):
                    kb.op("dve", lambda e, u=u, cc=cc, j=j: e.scalar_tensor_tensor(
                        out=acc[:, :], in0=u[:, 1 + j:1 + j + T2], scalar=cw[:, cc, j:j + 1], in1=acc[:, :],
                        op0=ALU.mult, op1=ALU.add), reads=[u, cw, acc], writes=[acc])
                kb.op("act", lambda e, o_=o_, cc=cc: e.activation(out=o_[:, :], in_=acc[:, :], func=AF.Silu, bias=cb[:, cc:cc + 1]),
                      reads=[acc, cb], writes=[o_])
                if cc < 8:
                    kb.op("dve", lambda e, o_=o_: e.tensor_scalar(out=o_[:, :], in0=o_[:, :], scalar1=0.0625, scalar2=None,
                                                                  op0=ALU.mult), reads=[o_], writes=[o_])
                kb.dma("sp", qkc_d[cc * 128:(cc + 1) * 128, :], o_[:, :], reads=[o_])

        with kb.phase():
            G = kb.sb([128, NCH, 8], F32)
            gb = kb.sb([128, NCH, 8], F32)
            LI = kb.sb([128, NCH, 4], F32)
            LF = kb.sb([128, NCH, 4], F32)
            Bc = kb.sb([128, NCH, 4], F32)
            U = kb.sb([128, NCH, 4], F32)
            kb.dma("sp", G[:, :, :], if_d.rearrange("(c p) g -> p c g", p=128), writes=[G])
            kb.dma("sp", gb[:, :, :], gbias[:, :, :], writes=[gb])
            kb.op("dve", lambda e: e.tensor_tensor(out=G[:, :, :], in0=G[:, :, :], in1=gb[:, :, :], op=ALU.add), reads=[G, gb], writes=[G])
            kb.op("act", lambda e: e.activation(out=G[:, :, :], in_=G[:, :, :], func=AF.Tanh, scale=1.0 / 15.0), reads=[G], writes=[G])
            kb.op("dve", lambda e: e.tensor_scalar(out=LI[:, :, :], in0=G[:, :, 0:4], scalar1=15.0, scalar2=None, op0=ALU.mult),
                  reads=[G], writes=[LI])
            kb.op("act", lambda e: e.activation(out=LF[:, :, :], in_=G[:, :, 4:8], func=AF.Exp, scale=-15.0), reads=[G], writes=[LF])
            kb.op("act", lambda e: e.activation(out=LF[:, :, :], in_=LF[:, :, :], func=AF.Ln, bias=1.0), reads=[LF], writes=[LF])
            kb.op("dve", lambda e: e.tensor_scalar(out=LF[:, :, :], in0=LF[:, :, :], scalar1=-1.0, scalar2=None, op0=ALU.mult),
                  reads=[LF], writes=[LF])
            pb_ = kb.ps()
            kb.op("pe", lambda e: e.matmul(pb_[:, 0:NCH * 4], lhsT=TRIu, rhs=LF[:, :, :].rearrange("p c h -> p (c h)"),
                                           start=True, stop=True), reads=[cst, LF], writes=[pb_])
            kb.op("dve", lambda e: e.tensor_copy(out=Bc[:, :, :].rearrange("p c h -> p (c h)"), in_=pb_[:, 0:NCH * 4]),
                  reads=[pb_], writes=[Bc])
            kb.op("dve", lambda e: e.tensor_tensor(out=U[:, :, :], in0=LI[:, :, :], in1=Bc[:, :, :], op=ALU.subtract),
                  reads=[LI, Bc], writes=[U])

            mnwb = kb.sb([128, 2048], F32)
            kb.dma("sp", mnwb[:, :], mnw.partition_broadcast(128), writes=[mnwb])
            def mkset():
                qTh = kb.sb([128, 2, T2], BF16)
                kTh = kb.sb([128, 2, T2], BF16)
                vh = kb.sb([128, NCH, 512], BF16)
                Cst = kb.sb([128, 2, 512], F32)
                Cb = kb.sb([128, 2, 512], BF16)
                nst_ = kb.sb([128, 2], F32)
                nb_ = kb.sb([128, 2], BF16)
                mcol = kb.sb([128, 1], F32)
                dg = [kb.sb([128, 128], F32) for _ in range(2)]
                Um = kb.sb([128, 128], F32)
                Rm = kb.sb([128, 128], F32)
                ET = kb.sb([128, 128], F32)
                PT = kb.sb([128, 128], BF16)
                sm = [kb.sb([128, 16], F32) for _ in range(2)]
                mr = kb.sb([128, 2], F32)
                kw = kb.sb([128, 256], BF16)
                tmpn = kb.sb([128, 512], F32)
                num = kb.sb([128, 512], F32)
                hh_ = kb.sb([128, 512], F32)
                junk5 = kb.sb([128, 512], BF16)
                ot = kb.sb([128, 512], F32)
                hout = [kb.sb([128, 512], BF16) for _ in range(2)]
                return dict(qTh=qTh, kTh=kTh, vh=vh, Cst=Cst, Cb=Cb, nst_=nst_, nb_=nb_, mcol=mcol, dg=dg, Um=Um, Rm=Rm, ET=ET, PT=PT, sm=sm, mr=mr, kw=kw, tmpn=tmpn, num=num, hh_=hh_, junk5=junk5, ot=ot, hout=hout, nit=0)

            sets = [mkset(), mkset()]

            def head_init(h, T):
                qTh, kTh, vh, Cst, Cb, nst_, nb_, mcol, dg, Um, Rm, ET, PT, sm, mr, kw, tmpn, num, hh_, junk5, ot, hout = T['qTh'], T['kTh'], T['vh'], T['Cst'], T['Cb'], T['nst_'], T['nb_'], T['mcol'], T['dg'], T['Um'], T['Rm'], T['ET'], T['PT'], T['sm'], T['mr'], T['kw'], T['tmpn'], T['num'], T['hh_'], T['junk5'], T['ot'], T['hout']
                kb.dma("sp", qTh[:, :, :], qkc_d[h * 256:(h + 1) * 256, :].rearrange("(j p) t -> p j t", p=128), writes=[qTh])
                kb.dma("sp", kTh[:, :, :], qkc_d[1024 + h * 256:1024 + (h + 1) * 256, :].rearrange("(j p) t -> p j t", p=128), writes=[kTh])
                kb.dma("sp", vh[:, :, :], mv_d[:, h * 512:(h + 1) * 512].rearrange("(c p) v -> p c v", p=128), writes=[vh])
                kb.op("dve", lambda e: e.memset(Cst[:, :, :], 0.0), writes=[Cst])
                kb.op("dve", lambda e: e.memset(Cb[:, :, :], 0.0), writes=[Cb])
                kb.op("dve", lambda e: e.memset(nst_[:, :], 0.0), writes=[nst_])
                kb.op("dve", lambda e: e.memset(nb_[:, :], 0.0), writes=[nb_])
                kb.op("dve", lambda e: e.memset(mcol[:, :], 0.0), writes=[mcol])

            def chunk(h, c, T):
                qTh, kTh, vh, Cst, Cb, nst_, nb_, mcol, dg, Um, Rm, ET, PT, sm, mr, kw, tmpn, num, hh_, junk5, ot, hout = T['qTh'], T['kTh'], T['vh'], T['Cst'], T['Cb'], T['nst_'], T['nb_'], T['mcol'], T['dg'], T['Um'], T['Rm'], T['ET'], T['PT'], T['sm'], T['mr'], T['kw'], T['tmpn'], T['num'], T['hh_'], T['junk5'], T['ot'], T['hout']
                own = c >= NCH // 2
                if c == NCH // 2:
                    kb.op("dve", lambda e: e.tensor_scalar(out=Cst[:, :, :], in0=Cst[:, :, :], scalar1=flg[:, 0:1], scalar2=None, op0=ALU.mult),
                          reads=[Cst, flg], writes=[Cst])
                    kb.op("dve", lambda e: e.tensor_copy(out=Cb[:, :, :], in_=Cst[:, :, :]), reads=[Cst], writes=[Cb])
                    kb.op("dve", lambda e: e.tensor_scalar(out=nst_[:, :], in0=nst_[:, :], scalar1=flg[:, 0:1], scalar2=None, op0=ALU.mult),
                          reads=[nst_, flg], writes=[nst_])
                    kb.op("dve", lambda e: e.tensor_copy(out=nb_[:, :], in_=nst_[:, :]), reads=[nst_], writes=[nb_])
                    kb.op("dve", lambda e: e.tensor_scalar(out=mcol[:, :], in0=mcol[:, :], scalar1=flg[:, 0:1], scalar2=None, op0=ALU.mult),
                          reads=[mcol, flg], writes=[mcol])
                cs_ = slice(c * 128, (c + 1) * 128)
                ucol = U[:, c, h:h + 1]
                bcol = Bc[:, c, h:h + 1]
                s0 = sm[T['nit'] % 2]
                T['nit'] += 1
                d0 = dg[0]
                kb.op("dve", lambda e, d0=d0, ucol=ucol: e.tensor_scalar(out=d0[:, :], in0=identf[:, :], scalar1=ucol, scalar2=None, op0=ALU.mult),
                      reads=[identf, U], writes=[d0])
                pu = kb.ps()
                kb.op("pe", lambda e, pu=pu, d0=d0: e.matmul(pu[:, 0:128], lhsT=ONESF, rhs=d0[:, :], start=True, stop=True),
                      reads=[cst, d0], writes=[pu])
                kb.op("dve", lambda e, pu=pu: e.tensor_tensor(out=Um[:, :], in0=pu[:, 0:128], in1=CTS, op=ALU.add), reads=[pu, cst], writes=[Um])
                kb.op("dve", lambda e, s0=s0: e.tensor_reduce(out=s0[:, 0:1], in_=Um[:, :], axis=AX.X, op=ALU.max), reads=[Um], writes=[s0])
                kb.op("dve", lambda e, s0=s0: e.tensor_tensor(out=mr[:, 1:2], in0=s0[:, 0:1], in1=mcol[:, 0:1], op=ALU.max), reads=[s0, mcol], writes=[mr])
                kb.op("dve", lambda e, bcol=bcol: e.tensor_tensor(out=mr[:, 0:1], in0=mr[:, 1:2], in1=bcol, op=ALU.add), reads=[mr, Bc], writes=[mr])
                pm = kb.ps()
                kb.op("pe", lambda e, pm=pm: e.matmul(pm[:, 0:2], lhsT=SEL127, rhs=mr[:, 0:2], start=True, stop=True), reads=[cst, mr], writes=[pm])
                kb.op("dve", lambda e, pm=pm, s0=s0: e.tensor_copy(out=s0[:, 1:3], in_=pm[:, 0:2]), reads=[pm], writes=[s0])
                if own:
                    d1 = dg[1]
                    kb.op("dve", lambda e, d1=d1: e.tensor_scalar(out=d1[:, :], in0=identf[:, :], scalar1=mr[:, 1:2], scalar2=None, op0=ALU.mult),
                          reads=[identf, mr], writes=[d1])
                    pr = kb.ps()
                    kb.op("pe", lambda e, pr=pr, d1=d1: e.matmul(pr[:, 0:128], lhsT=ONESF, rhs=d1[:, :], start=True, stop=True),
                          reads=[cst, d1], writes=[pr])
                    kb.op("dve", lambda e, pr=pr: e.tensor_tensor(out=Rm[:, :], in0=pr[:, 0:128], in1=CST, op=ALU.add), reads=[pr, cst], writes=[Rm])
                    kb.op("act", lambda e, ucol=ucol: e.activation(out=ET[:, :], in_=Rm[:, :], func=AF.Exp, scale=-1.0, bias=ucol),
                          reads=[Rm, U], writes=[ET])
                    pS = kb.ps()
                    for j in range(2):
                        kb.op("pe", lambda e, pS=pS, j=j, cs_=cs_: e.matmul(pS[:, 0:128], lhsT=kTh[:, j, cs_], rhs=qTh[:, j, cs_],
                                                                          start=(j == 0), stop=(j == 1)), reads=[kTh, qTh], writes=[pS])
                    kb.op("dve", lambda e, pS=pS: e.tensor_tensor(out=PT[:, :], in0=pS[:, 0:128], in1=ET[:, :], op=ALU.mult),
                          reads=[pS, ET], writes=[PT])
                    pnum = kb.ps()
                    kb.op("pe", lambda e, pnum=pnum, c=c: e.matmul(pnum[:, :], lhsT=PT[:, :], rhs=vh[:, c, :], start=True, stop=True),
                          reads=[PT, vh], writes=[pnum])
                    pden = kb.ps()
                    kb.op("pe", lambda e, pden=pden: e.matmul(pden[:, 0:1], lhsT=PT[:, :], rhs=onesb[:, 0:1], start=True, stop=True),
                          reads=[PT, onesb], writes=[pden])
                    pqc = kb.ps()
                    for j in range(2):
                        kb.op("pe", lambda e, pqc=pqc, j=j, cs_=cs_: e.matmul(pqc[:, :], lhsT=qTh[:, j, cs_], rhs=Cb[:, j, :],
                                                                            start=(j == 0), stop=(j == 1)), reads=[qTh, Cb], writes=[pqc])
                    for j in range(2):
                        kb.op("pe", lambda e, pden=pden, j=j, cs_=cs_: e.matmul(pden[:, 1:2], lhsT=qTh[:, j, cs_], rhs=nb_[:, j:j + 1],
                                                                              start=(j == 0), stop=(j == 1)), reads=[qTh, nb_], writes=[pden])
                    kb.op("dve", lambda e, s0=s0: e.tensor_tensor(out=s0[:, 3:4], in0=mcol[:, 0:1], in1=mr[:, 1:2], op=ALU.subtract),
                          reads=[mcol, mr], writes=[s0])
                    kb.op("act", lambda e, s0=s0: e.activation(out=s0[:, 3:4], in_=s0[:, 3:4], func=AF.Exp), reads=[s0], writes=[s0])
                    kb.op("act", lambda e, pqc=pqc, s0=s0: e.activation(out=tmpn[:, :], in_=pqc[:, :], func=AF.Copy, scale=s0[:, 3:4]),
                          reads=[pqc, s0], writes=[tmpn])
                    kb.op("dve", lambda e, pnum=pnum: e.tensor_tensor(out=num[:, :], in0=pnum[:, :], in1=tmpn[:, :], op=ALU.add),
                          reads=[pnum, tmpn], writes=[num])
                    kb.op("dve", lambda e, pden=pden, s0=s0: e.tensor_copy(out=s0[:, 9:11], in_=pden[:, 0:2]), reads=[pden], writes=[s0])
                    kb.op("dve", lambda e, s0=s0: e.scalar_tensor_tensor(out=s0[:, 4:5], in0=s0[:, 10:11], scalar=s0[:, 3:4], in1=s0[:, 9:10],
                                                                         op0=ALU.mult, op1=ALU.add), reads=[s0], writes=[s0])
                    kb.op("dve", lambda e, s0=s0: e.tensor_scalar(out=s0[:, 5:6], in0=s0[:, 4:5], scalar1=-1.0, scalar2=None, op0=ALU.mult),
                          reads=[s0], writes=[s0])
                    kb.op("dve", lambda e, s0=s0: e.tensor_tensor(out=s0[:, 5:6], in0=s0[:, 5:6], in1=s0[:, 4:5], op=ALU.max),
                          reads=[s0], writes=[s0])
                    kb.op("act", lambda e, s0=s0: e.activation(out=s0[:, 6:7], in_=mr[:, 0:1], func=AF.Exp, scale=-1.0), reads=[mr], writes=[s0])
                    kb.op("dve", lambda e, s0=s0: e.tensor_tensor(out=s0[:, 7:8], in0=s0[:, 5:6], in1=s0[:, 6:7], op=ALU.max), reads=[s0], writes=[s0])
                    kb.op("dve", lambda e, s0=s0: e.reciprocal(out=s0[:, 8:9], in_=s0[:, 7:8]), reads=[s0], writes=[s0])
                    kb.op("dve", lambda e, s0=s0: e.tensor_scalar(out=hh_[:, :], in0=num[:, :], scalar1=s0[:, 8:9], scalar2=None, op0=ALU.mult),
                          reads=[num, s0], writes=[hh_])
                    kb.op("act", lambda e, s0=s0: e.activation(out=junk5[:, :], in_=hh_[:, :], func=AF.Square, accum_out=s0[:, 11:12]),
                          reads=[hh_], writes=[junk5, s0])
                    kb.op("dve", lambda e, s0=s0: e.tensor_scalar(out=s0[:, 11:12], in0=s0[:, 11:12], scalar1=1.0 / 512, scalar2=EPS,
                                                                  op0=ALU.mult, op1=ALU.add), reads=[s0], writes=[s0])
                    kb.op("act", lambda e, s0=s0: e.activation(out=s0[:, 11:12], in_=s0[:, 11:12], func=AF.Sqrt), reads=[s0], writes=[s0])
                    kb.op("dve", lambda e, s0=s0: e.reciprocal(out=s0[:, 12:13], in_=s0[:, 11:12]), reads=[s0], writes=[s0])
                    tok0 = (c - NCH // 2) * 128
                    kb.dma("sp", ot[:, :], o_d[tok0:tok0 + 128, h * 512:(h + 1) * 512], writes=[ot])
                    kb.op("act", lambda e: e.activation(out=ot[:, :], in_=ot[:, :], func=AF.Sigmoid), reads=[ot], writes=[ot])
                    kb.op("dve", lambda e, h=h: e.tensor_tensor(out=ot[:, :], in0=ot[:, :], in1=mnwb[:, h * 512:(h + 1) * 512], op=ALU.mult),
                          reads=[ot, mnwb], writes=[ot])
                    ho = hout[c % 2]
                    kb.op("dve", lambda e, s0=s0, ho=ho: e.scalar_tensor_tensor(out=ho[:, :], in0=hh_[:, :], scalar=s0[:, 12:13], in1=ot[:, :],
                                                                               op0=ALU.mult, op1=ALU.mult), reads=[hh_, s0, ot], writes=[ho])
                    kb.dma("sp", hcat_d[tok0:tok0 + 128, h * 512:(h + 1) * 512], ho[:, :], reads=[ho])
                kb.op("dve", lambda e, s0=s0, ucol=ucol: e.tensor_tensor(out=s0[:, 13:14], in0=ucol, in1=s0[:, 2:3], op=ALU.subtract),
                      reads=[U, s0], writes=[s0])
                kb.op("dve", lambda e, s0=s0: e.tensor_tensor(out=s0[:, 14:15], in0=mcol[:, 0:1], in1=s0[:, 2:3], op=ALU.subtract),
                      reads=[mcol, s0], writes=[s0])
                kb.op("act", lambda e, s0=s0: e.activation(out=s0[:, 13:15], in_=s0[:, 13:15], func=AF.Exp), reads=[s0], writes=[s0])
                pk = kb.ps()
                for j in range(2):
                    kb.op("pe", lambda e, pk=pk, j=j, cs_=cs_: e.matmul(pk[:, j * 128:(j + 1) * 128], lhsT=kTh[:, j, cs_], rhs=identb[:, :],
                                                                      start=True, stop=True), reads=[kTh, identb], writes=[pk])
                kb.op("act", lambda e, pk=pk, s0=s0: e.activation(out=kw[:, :], in_=pk[:, 0:256], func=AF.Copy, scale=s0[:, 13:14]),
                      reads=[pk, s0], writes=[kw])
                pn = kb.ps()
                for j in range(2):
                    pkv = kb.ps()
                    kb.op("pe", lambda e, pkv=pkv, j=j, c=c: e.matmul(pkv[:, :], lhsT=kw[:, j * 128:(j + 1) * 128], rhs=vh[:, c, :],
                                                                     start=True, stop=True), reads=[kw, vh], writes=[pkv])
                    kb.op("pe", lambda e, pn=pn, j=j: e.matmul(pn[:, j:j + 1], lhsT=kw[:, j * 128:(j + 1) * 128], rhs=onesb[:, 0:1],
                                                              start=True, stop=True), reads=[kw, onesb], writes=[pn])
                    kb.op("dve", lambda e, pkv=pkv, j=j, s0=s0: e.scalar_tensor_tensor(out=Cst[:, j, :], in0=Cst[:, j, :], scalar=s0[:, 14:15],
                                                                                      in1=pkv[:, :], op0=ALU.mult, op1=ALU.add),
                          reads=[Cst, s0, pkv], writes=[Cst])
                kb.op("act", lambda e: e.activation(out=Cb[:, :, :], in_=Cst[:, :, :], func=AF.Copy), reads=[Cst], writes=[Cb])
                kb.op("dve", lambda e, pn=pn, s0=s0: e.scalar_tensor_tensor(out=nst_[:, :], in0=nst_[:, :], scalar=s0[:, 14:15], in1=pn[:, 0:2],
                                                                           op0=ALU.mult, op1=ALU.add), reads=[nst_, s0, pn], writes=[nst_])
                kb.op("dve", lambda e: e.tensor_copy(out=nb_[:, :], in_=nst_[:, :]), reads=[nst_], writes=[nb_])
                kb.op("dve", lambda e, s0=s0: e.tensor_copy(out=mcol[:, :], in_=s0[:, 1:2]), reads=[s0], writes=[mcol])

            for hp in range(2):
                for slot in range(2):
                    head_init(2 * hp + slot, sets[slot])
                for c in range(NCH):
                    for slot in range(2):
                        chunk(2 * hp + slot, c, sets[slot])


        NTq = TOK // 128
        NTk = T2 // 128
        SCL = 128 ** -0.5
        with kb.phase():
            kb.psum_rot = 6
            kb.pnext = 0
            pO, pL = kb.psum[6], kb.psum[7]
            cst_ = kb.sb([128, NTk, 128], F32)
            kb.dma("sp", cst_[:, :, :], cs_tab.rearrange("(i p) d -> p i d", p=128), writes=[cst_])
            pbt = kb.sb([128, NTq, NBP], F32)
            kb.dma("sp", pbt[:, :, :], pb_tab.rearrange("(i p) n -> p i n", p=128), writes=[pbt])
            ohb = kb.sb([NBP, NBP * 128], BF16)
            kb.dma("pool", ohb[:, :], oneh[:, :], writes=[ohb])
            tri01 = kb.sb([128, 128], BF16)
            kb.op("dve", lambda e: e.tensor_copy(out=tri01[:, :], in_=TRIu), reads=[cst], writes=[tri01])
            xq = kb.sb([128, NTq, 128], F32)
            xk = kb.sb([128, NTk, 128], F32)
            rq = kb.sb([128, NTk, 128], F32)
            t1 = kb.sb([128, NTk, 64], F32)
            qT = kb.sb([128, TOK], BF16)
            kT = kb.sb([128, T2], BF16)
            vk = kb.sb([128, NTk, 132], BF16)
            kb.op("dve", lambda e: e.memset(vk[:, :, :], 1.0), writes=[vk])
            kmT = kb.sb([128, NBP], BF16)
            kms = kb.sb([128, NBP], F32)
            gm = kb.sb([128, NBP], F32)
            t8 = kb.sb([128, 8], F32)
            selb = kb.sb([128, NBP], BF16)
            selT = kb.sb([NBP, TOK], BF16)
            PTm = [kb.sb([128, 512], BF16) for _ in range(3)]
            lrec = [kb.sb([128, 2], F32) for _ in range(2)]
            gmA = kb.sb([128, NTq, NBP], F32)
            t8A = kb.sb([128, NTq, 8], F32)
            selbA = kb.sb([128, NTq, NBP], BF16)
            st_m = {"rot": 0}
            pOs = [pO, pL]
            oo = [kb.sb([128, 128], BF16) for _ in range(2)]

            def rotary(X, R, nt):
                c_, s_ = cst_[:, NTk - nt:NTk, 0:64], cst_[:, NTk - nt:NTk, 64:128]
                x1, x2 = X[:, 0:nt, 0:64], X[:, 0:nt, 64:128]
                T1 = t1[:, 0:nt, :]
                kb.op("dve", lambda e: e.tensor_tensor(out=R[:, 0:nt, 0:64], in0=x1, in1=c_, op=ALU.mult), reads=[X, cst_], writes=[R])
                kb.op("dve", lambda e: e.tensor_tensor(out=T1, in0=x2, in1=s_, op=ALU.mult), reads=[X, cst_], writes=[t1])
                kb.op("dve", lambda e: e.tensor_tensor(out=R[:, 0:nt, 0:64], in0=R[:, 0:nt, 0:64], in1=T1, op=ALU.subtract), reads=[R, t1], writes=[R])
                kb.op("dve", lambda e: e.tensor_tensor(out=R[:, 0:nt, 64:128], in0=x2, in1=c_, op=ALU.mult), reads=[X, cst_], writes=[R])
                kb.op("dve", lambda e: e.tensor_tensor(out=T1, in0=x1, in1=s_, op=ALU.mult), reads=[X, cst_, t1], writes=[t1])
                kb.op("dve", lambda e: e.tensor_tensor(out=R[:, 0:nt, 64:128], in0=R[:, 0:nt, 64:128], in1=T1, op=ALU.add), reads=[R, t1], writes=[R])

            def transp(R, nt, dst):
                for i4 in range(nt // 4):
                    pt = kb.ps()
                    for q in range(4):
                        i = i4 * 4 + q
                        kb.op("pe", lambda e, pt=pt, q=q, i=i: e.matmul(pt[:, q * 128:(q + 1) * 128], lhsT=R[:, i, :], rhs=identf[:, :],
                                                                       start=True, stop=True), reads=[R, identf], writes=[pt])
                    ev_copy(i4, dst[:, i4 * 512:(i4 + 1) * 512], pt[:, :], [pt], [dst])

            nev = 0
            for h in range(16):
                hs = slice(h * 128, (h + 1) * 128)
                kb.dma("sp", xq[:, :, :], aq_d[:, hs].rearrange("(i p) d -> p i d", p=128), writes=[xq])
                kb.dma("sp", xk[:, :, :], ak_d[:, hs].rearrange("(i p) d -> p i d", p=128), writes=[xk])
                kb.dma("sp", vk[:, :, 0:128], av_d[:, hs].rearrange("(i p) d -> p i d", p=128), writes=[vk])
                rotary(xq, rq, NTq)
                transp(rq, NTq, qT)
                rotary(xk, rq, NTk)
                transp(rq, NTk, kT)
                kb.op("dve", lambda e: e.memset(kms[:, :], 0.0), writes=[kms])
                kb.op("dve", lambda e: e.tensor_reduce(out=kms[:, 0:NB], in_=kT[:, :].rearrange("p (n k) -> p n k", k=256), axis=AX.X, op=ALU.add),
                      reads=[kT], writes=[kms])
                kb.op("dve", lambda e: e.tensor_scalar(out=kmT[:, :], in0=kms[:, :], scalar1=1.0 / 256, scalar2=None, op0=ALU.mult),
                      reads=[kms], writes=[kmT])
                pg = kb.ps()
                for i in range(NTq):
                    kb.op("pe", lambda e, i=i: e.matmul(pg[:, i * NBP:(i + 1) * NBP], lhsT=qT[:, i * 128:(i + 1) * 128], rhs=kmT[:, :], start=True, stop=True),
                          reads=[qT, kmT], writes=[pg])
                kb.op("dve", lambda e: e.tensor_tensor(out=gmA[:, :, :].rearrange("p a b -> p (a b)"), in0=pg[:, 0:NTq * NBP],
                                                       in1=pbt[:, :, :].rearrange("p a b -> p (a b)"), op=ALU.add), reads=[pg, pbt], writes=[gmA])
                for i in range(NTq):
                    kb.op("dve", lambda e, i=i: e.max(out=t8A[:, i, :], in_=gmA[:, i, :]), reads=[gmA], writes=[t8A])
                kb.op("dve", lambda e: e.tensor_scalar(out=t8A[:, :, 2:3], in0=t8A[:, :, 2:3], scalar1=-1.0e29, scalar2=None, op0=ALU.max), reads=[t8A], writes=[t8A])
                for i in range(NTq):
                    kb.op("dve", lambda e, i=i: e.tensor_scalar(out=gmA[:, i, :], in0=gmA[:, i, :], scalar1=t8A[:, i, 2:3], scalar2=None, op0=ALU.is_ge),
                          reads=[gmA, t8A], writes=[gmA])
                kb.op("dve", lambda e: e.tensor_scalar(out=selbA[:, :, :], in0=gmA[:, :, :], scalar1=30000.0, scalar2=-30000.0, op0=ALU.mult, op1=ALU.add),
                      reads=[gmA], writes=[selbA])
                for i4 in range(NTq // 4):
                    pt = kb.ps()
                    for q in range(4):
                        i = i4 * 4 + q
                        kb.op("pe", lambda e, pt=pt, q=q, i=i: e.matmul(pt[0:NBP, q * 128:(q + 1) * 128], lhsT=selbA[:, i, :], rhs=identb[:, :], start=True, stop=True),
                              reads=[selbA, identb], writes=[pt])
                    kb.op("act", lambda e, pt=pt, i4=i4: e.activation(out=selT[:, i4 * 512:(i4 + 1) * 512], in_=pt[0:NBP, 0:512], func=AF.Copy), reads=[pt], writes=[selT])
                for jb in range(TOK // 256):
                    q2 = slice(jb * 256, (jb + 1) * 256)
                    gb_ = NB // 2 + jb
                    started = [False, False]

                    def score_past(n):
                        pS = kb.ps()
                        for hf in range(2):
                            ks_ = slice(n * 256 + hf * 128, n * 256 + hf * 128 + 128)
                            kb.op("pe", lambda e, pS=pS, hf=hf, ks_=ks_: e.matmul(pS[:, hf * 256:(hf + 1) * 256], lhsT=kT[:, ks_], rhs=qT[:, q2], start=True, stop=False),
                                  reads=[kT, qT], writes=[pS])
                            kb.op("pe", lambda e, pS=pS, hf=hf, n=n: e.matmul(pS[:, hf * 256:(hf + 1) * 256], lhsT=ohb[:, n * 128:(n + 1) * 128], rhs=selT[:, q2], start=False, stop=True),
                                  reads=[ohb, selT], writes=[pS])
                        P_ = PTm[st_m["rot"] % 3]
                        st_m["rot"] += 1
                        kb.op("act", lambda e, pS=pS, P_=P_: e.activation(out=P_[:, :], in_=pS[:, :], func=AF.Exp, scale=SCL), reads=[pS], writes=[P_])
                        return [(P_, hf * 256 + qt * 128, n * 2 + hf, qt) for hf in range(2) for qt in range(2)]

                    def score_own(qt, hf, tri):
                        pS = kb.ps()
                        ks_ = slice(gb_ * 256 + hf * 128, gb_ * 256 + hf * 128 + 128)
                        qs_ = slice(jb * 256 + qt * 128, jb * 256 + (qt + 1) * 128)
                        kb.op("pe", lambda e, pS=pS: e.matmul(pS[:, 0:128], lhsT=kT[:, ks_], rhs=qT[:, qs_], start=True, stop=True), reads=[kT, qT], writes=[pS])
                        P_ = PTm[st_m["rot"] % 3]
                        st_m["rot"] += 1
                        kb.op("act", lambda e, pS=pS, P_=P_: e.activation(out=P_[:, 0:128], in_=pS[:, 0:128], func=AF.Exp, scale=SCL), reads=[pS], writes=[P_])
                        if tri:
                            kb.op("dve", lambda e, P_=P_: e.tensor_tensor(out=P_[:, 0:128], in0=P_[:, 0:128], in1=tri01[:, :], op=ALU.mult), reads=[P_, tri01], writes=[P_])
                        return [(P_, 0, gb_ * 2 + hf, qt)]

                    def pv(lst, lastflags):
                        for (P_, c0_, ki, qt), last in zip(lst, lastflags):
                            first = not started[qt]
                            started[qt] = True
                            kb.op("pe", lambda e, P_=P_, c0_=c0_, ki=ki, qt=qt, first=first, last=last: e.matmul(
                                pOs[qt][:, 0:129], lhsT=P_[:, c0_:c0_ + 128], rhs=vk[:, ki, 0:129], start=first, stop=last), reads=[P_, vk], writes=[pOs[qt]])

                    prev = None
                    for n in range(gb_):
                        cur = score_past(n)
                        if prev is not None:
                            pv(prev, [False] * 4)
                        prev = cur
                    o0 = score_own(0, 0, True)
                    pv(prev, [False] * 4)
                    o1 = score_own(1, 0, False)
                    pv(o0, [True])
                    o2 = score_own(1, 1, True)
                    pv(o1, [False])
                    pv(o2, [True])
                    for qt in range(2):
                        i = jb * 2 + qt
                        lr = lrec[i % 2]
                        kb.op("dve", lambda e, qt=qt, lr=lr: e.reciprocal(out=lr[:, 0:1], in_=pOs[qt][:, 128:129]), reads=[pOs[qt]], writes=[lr])
                        o_ = oo[i % 2]
                        kb.op("act", lambda e, o_=o_, qt=qt, lr=lr: e.activation(out=o_[:, :], in_=pOs[qt][:, 0:128], func=AF.Copy, scale=lr[:, 0:1]), reads=[pOs[qt], lr], writes=[o_])
                        kb.dma("sp", hcat_d[i * 128:(i + 1) * 128, 2048 + h * 128:2048 + (h + 1) * 128], o_[:, :], reads=[o_])
            kb.psum_rot = 8
            kb.pnext = 0

        modflat = mod_d.rearrange("a b -> (a b)")

        def norm_T(x_t, junk_, xs_t, ssc, Acol, bias_fn, dstT, ti):
            kb.op("act", lambda e: e.activation(out=junk_[:, :], in_=x_t[:, :], func=AF.Square, accum_out=ssc[:, 0:1]), reads=[x_t], writes=[junk_, ssc])
            kb.op("dve", lambda e: e.tensor_scalar(out=ssc[:, 0:1], in0=ssc[:, 0:1], scalar1=1.0 / D, scalar2=EPS, op0=ALU.mult, op1=ALU.add), reads=[ssc], writes=[ssc])
            kb.op("act", lambda e: e.activation(out=ssc[:, 0:1], in_=ssc[:, 0:1], func=AF.Sqrt), reads=[ssc], writes=[ssc])
            kb.op("dve", lambda e: e.reciprocal(out=ssc[:, 1:2], in_=ssc[:, 0:1]), reads=[ssc], writes=[ssc])
            kb.op("dve", lambda e: e.tensor_scalar(out=xs_t[:, :], in0=x_t[:, :], scalar1=ssc[:, 1:2], scalar2=None, op0=ALU.mult), reads=[x_t, ssc], writes=[xs_t])
            for k4 in range(KC // 4):
                pt = kb.ps()
                for q in range(4):
                    kc = k4 * 4 + q
                    kb.op("pe", lambda e, pt=pt, q=q, kc=kc: e.matmul(pt[:, q * 128:(q + 1) * 128], lhsT=xs_t[:, kc * 128:(kc + 1) * 128], rhs=identb[:, :],
                                                                     start=True, stop=True), reads=[xs_t, identb], writes=[pt])
                for q in range(4):
                    kc = k4 * 4 + q
                    if Acol is None:
                        ev_copy(q, dstT[:, kc, ti * 128:(ti + 1) * 128], pt[:, q * 128:(q + 1) * 128], [pt], [dstT.ks[ti]])
                    else:
                        kb.op("act", lambda e, pt=pt, q=q, kc=kc: e.activation(out=dstT[:, kc, ti * 128:(ti + 1) * 128], in_=pt[:, q * 128:(q + 1) * 128],
                                                                              func=AF.Identity, scale=Acol[:, kc:kc + 1], bias=bias_fn(kc)),
                              reads=[pt, Acol, modT], writes=[dstT.ks[ti]])

        wov = w_out.rearrange("(kc p) n -> p kc n", p=128)
        with kb.phase():
            g1b = kb.sb([128, D], F32)
            kb.dma("sp", g1b[:, :], modflat[2 * D:3 * D].partition_broadcast(128), writes=[g1b])
            hT = kb.sb([128, KC, TG], BF16, n=4)
            hb = [kb.sb([128, D], BF16) for _ in range(2)]
            xg = [kb.sb([128, D], F32) for _ in range(4)]
            wb = [kb.sb([128, KC, 512], BF16) for _ in range(2)]
            tmpd = [kb.sb([128, 512], F32) for _ in range(2)]
            junk = hb[0]
            xs2 = hb[1]
            ssc = kb.sb([128, 2], F32)
            x2T = hT
            nw = 0
            for g in range(TOK // TG):
                for ti in range(4):
                    tok0 = g * TG + ti * 128
                    hb_ = hb[ti % 2]
                    kb.dma("sp", hb_[:, :], hcat_d[tok0:tok0 + 128, :], writes=[hb_])
                    kb.dma("sp", xg[ti][:, :], x_own[tok0:tok0 + 128, :], writes=[xg[ti]])
                    for k4 in range(KC // 4):
                        pt = kb.ps()
                        for q in range(4):
                            kc = k4 * 4 + q
                            kb.op("pe", lambda e, pt=pt, q=q, kc=kc, hb_=hb_: e.matmul(pt[:, q * 128:(q + 1) * 128], lhsT=hb_[:, kc * 128:(kc + 1) * 128],
                                                                                     rhs=identb[:, :], start=True, stop=True), reads=[hb_, identb], writes=[pt])
                        for q in range(4):
                            kc = k4 * 4 + q
                            ev_copy(q, hT[:, kc, ti * 128:(ti + 1) * 128], pt[:, q * 128:(q + 1) * 128], [pt], [hT.ks[ti]])
                for dc in range(8):
                    w = wb[nw % 2]
                    nw += 1
                    kb.dma("pool", w[:, :, :], wov[:, :, dc * 512:(dc + 1) * 512], writes=[w])
                    for ti in range(4):
                        pt = kb.ps()
                        for kc in range(KC):
                            kb.op("pe", lambda e, pt=pt, kc=kc, ti=ti, w=w: e.matmul(pt[:, :], lhsT=hT[:, kc, ti * 128:(ti + 1) * 128], rhs=w[:, kc, :],
                                                                                    start=(kc == 0), stop=(kc == KC - 1)), reads=[w, hT.ks[ti]], writes=[pt])
                        td = tmpd[ti % 2]
                        ds_ = slice(dc * 512, (dc + 1) * 512)
                        kb.op("dve", lambda e, pt=pt, td=td, ds_=ds_: e.tensor_tensor(out=td[:, :], in0=pt[:, :], in1=g1b[:, ds_], op=ALU.mult), reads=[pt, g1b], writes=[td])
                        kb.op("dve", lambda e, td=td, ds_=ds_, ti=ti: e.tensor_tensor(out=xg[ti][:, ds_], in0=xg[ti][:, ds_], in1=td[:, :], op=ALU.add),
                              reads=[xg[ti], td], writes=[xg[ti]])
                for ti in range(4):
                    tok0 = g * TG + ti * 128
                    kb.dma("sp", x2_d[tok0:tok0 + 128, :], xg[ti][:, :], reads=[xg[ti]])
                    norm_T(xg[ti], junk, xs2, ssc, A2, lambda kc: modT[:, 96 + kc:97 + kc], x2T, ti)
                kb.dma("sp", xn2T_d[:, :, g * TG:(g + 1) * TG], x2T[:, :, :], reads=x2T.ks)

        NGP = TOK // TG
        with kb.phase():
            xT = kb.sb([128, KC, TG], BF16)
            skb = kb.sb([128, 16, NK], BF16)
            kb.dma("pool", skb[:, :, :], skT[:, :, :], writes=[skb])
            wqb = [kb.sb([128, KC, 128], BF16) for _ in range(2)]
            qsb = [kb.sb([128, TG], BF16) for _ in range(2)]
            S2 = kb.sb([128, 4, 8, NK], F32)
            AA = kb.sb([128, 4, 8, NK], F32)
            NT = kb.sb([128, 4, 8, NK], F32)
            s1b = kb.sb([128, NK], F32)
            T1 = kb.sb([128, 16], F32)
            T2_ = kb.sb([128, 16], F32)
            cand = kb.sb([128, 256], F32)
            candb = kb.sb([128, 256], F32)
            c16 = kb.sb([128, 16], F32)
            e16 = kb.sb([128, 16], F32)
            zc = kb.sb([128, 4], F32)
            nwq = 0
            for g in range(NGP):
                kb.dma("sp", xT[:, :, :], xn2T_d[:, :, g * TG:(g + 1) * TG], writes=[xT])
                for hp in range(16):
                    h, half = hp // 2, hp % 2
                    w = wqb[nwq % 2]
                    qs_ = qsb[nwq % 2]
                    nwq += 1
                    kb.dma("pool", w[:, :, :], wqc[hp].rearrange("p (kc e) -> p kc e", e=128), writes=[w], max_dma_last_dim=4096)
                    pq = kb.ps()
                    for kc in range(KC):
                        kb.op("pe", lambda e, pq=pq, kc=kc, w=w: e.matmul(pq[:, :], lhsT=w[:, kc, :], rhs=xT[:, kc, :], start=(kc == 0), stop=(kc == KC - 1)),
                              reads=[w, xT], writes=[pq])
                    kb.op("act", lambda e, pq=pq, qs_=qs_: e.activation(out=qs_[:, :], in_=pq[:, :], func=AF.Copy), reads=[pq], writes=[qs_])
                    for ti in range(4):
                        psc = kb.ps()
                        kb.op("pe", lambda e, psc=psc, ti=ti, hp=hp, qs_=qs_: e.matmul(psc[:, 0:NK], lhsT=qs_[:, ti * 128:(ti + 1) * 128], rhs=skb[:, hp, :], start=True, stop=True),
                              reads=[qs_, skb], writes=[psc])
                        if half == 0:
                            kb.op("dve", lambda e, psc=psc, ti=ti, h=h: e.tensor_copy(out=AA[:, ti, h, :], in_=psc[:, 0:NK]), reads=[psc], writes=[AA])
                        else:
                            kb.op("dve", lambda e, psc=psc, ti=ti, h=h: e.tensor_copy(out=S2[:, ti, h, :], in_=psc[:, 0:NK]), reads=[psc], writes=[S2])
                for ti in range(4):
                    for h in range(8):
                        for (src, dstT_) in ((AA[:, ti, h, :], T1), (S2[:, ti, h, :], T2_)):
                            kb.op("dve", lambda e, src=src, dstT_=dstT_: e.max(out=dstT_[:, 0:8], in_=src), reads=[AA, S2], writes=[dstT_])
                            kb.op("dve", lambda e, src=src, dstT_=dstT_: e.match_replace(out=s1b[:, :], in_to_replace=dstT_[:, 0:8], in_values=src, imm_value=NEG),
                                  reads=[AA, S2, dstT_], writes=[s1b])
                            kb.op("dve", lambda e, dstT_=dstT_: e.max(out=dstT_[:, 8:16], in_=s1b[:, :]), reads=[s1b], writes=[dstT_])
                        kb.op("dve", lambda e: e.tensor_tensor(
                            out=cand[:, :].rearrange("p (a b) -> p a b", a=16),
                            in0=T2_[:, 0:16].rearrange("p (o b) -> p o b", o=1).broadcast_to([128, 16, 16]),
                            in1=T1[:, 0:16].rearrange("p (a o) -> p a o", o=1).broadcast_to([128, 16, 16]), op=ALU.add),
                            reads=[T1, T2_], writes=[cand])
                        kb.op("dve", lambda e: e.max(out=c16[:, 0:8], in_=cand[:, :]), reads=[cand], writes=[c16])
                        kb.op("dve", lambda e: e.match_replace(out=candb[:, :], in_to_replace=c16[:, 0:8], in_values=cand[:, :], imm_value=NEG), reads=[cand, c16], writes=[candb])
                        kb.op("dve", lambda e: e.max(out=c16[:, 8:16], in_=candb[:, :]), reads=[candb], writes=[c16])
                        kb.op("dve", lambda e: e.tensor_scalar(out=zc[:, 0:1], in0=c16[:, 0:1], scalar1=-1.0, scalar2=None, op0=ALU.mult), reads=[c16], writes=[zc])
                        kb.op("act", lambda e: e.activation(out=e16[:, :], in_=c16[:, :], func=AF.Exp, bias=zc[:, 0:1], accum_out=zc[:, 1:2]), reads=[c16, zc], writes=[e16, zc])
                        kb.op("act", lambda e: e.activation(out=zc[:, 2:3], in_=zc[:, 1:2], func=AF.Ln), reads=[zc], writes=[zc])
                        kb.op("dve", lambda e: e.tensor_tensor(out=zc[:, 2:3], in0=zc[:, 0:1], in1=zc[:, 2:3], op=ALU.subtract), reads=[zc], writes=[zc])
                        kb.op("dve", lambda e, ti=ti, h=h: e.tensor_scalar(out=NT[:, ti, h, :], in0=AA[:, ti, h, :], scalar1=-1.0, scalar2=c16[:, 15:16], op0=ALU.mult, op1=ALU.add),
                              reads=[AA, c16], writes=[NT])
                        kb.op("dve", lambda e, ti=ti, h=h: e.tensor_scalar(out=AA[:, ti, h, :], in0=AA[:, ti, h, :], scalar1=zc[:, 2:3], scalar2=None, op0=ALU.add),
                              reads=[AA, zc], writes=[AA])
                for j_, t_ in enumerate((S2, AA, NT)):
                    kb.dma("sp", ans_d[g, j_, :, :], t_[:, :, :, :].rearrange("p a b c -> p (a b c)"), reads=[t_])

        with kb.phase():
            kb.psum_rot = 4
            kb.pnext = 0
            pA_, pG_ = [kb.psum[4], kb.psum[5]], [kb.psum[6], kb.psum[7]]
            xT = kb.sb([128, KC, TG], BF16)
            S2 = kb.sb([128, 4, 8, NK], F32)
            AA = kb.sb([128, 4, 8, NK], F32)
            NT = kb.sb([128, 4, 8, NK], F32)
            NUB, NVB, NEB = 2, 4, 5
            Ub = [kb.sb([128, KC, NK], BF16) for _ in range(NUB)]
            Vb = [kb.sb([NK, D], BF16) for _ in range(NVB)]
            Eb = [kb.sb([128, NK], BF16) for _ in range(NEB)]
            Gh = [kb.sb([128, NK], BF16) for _ in range(NEB)]
            gel = [kb.sb([NK, TG], BF16) for _ in range(4)]
            wT = [kb.sb([NK, TG], BF16) for _ in range(4)]
            ACC = kb.sb([128, 4, D], F32, n=32)
            NP_ = NK // 2
            st_ = {"rot": 0}

            def v_load(c):
                V_ = Vb[c % NVB]
                kb.dma("pool", V_[:, :], vv[c * NK:(c + 1) * NK, :], writes=[V_], max_dma_last_dim=4096)

            def u_load(c):
                U_ = Ub[c % NUB]
                kb.dma("pool", U_[:, :, :], uTc[c].rearrange("p (kc e) -> p kc e", e=NK), writes=[U_], max_dma_last_dim=4096)

            def block(ps2, ps1, ti, ppa):
                pend = []
                for k in range(8):
                    if ps2 is not None:
                        c0, c1 = 2 * ps2, 2 * ps2 + 1
                        po = kb.ps()
                        ds_ = slice(k * 512, (k + 1) * 512)
                        kb.op("pe", lambda e, po=po, ds_=ds_: e.matmul(po[:, :], lhsT=wT[c0 % 4][:, ti * 128:(ti + 1) * 128], rhs=Vb[c0 % NVB][:, ds_], start=True, stop=False),
                              reads=[wT[c0 % 4], Vb[c0 % NVB]], writes=[po])
                        kb.op("pe", lambda e, po=po, ds_=ds_: e.matmul(po[:, :], lhsT=wT[c1 % 4][:, ti * 128:(ti + 1) * 128], rhs=Vb[c1 % NVB][:, ds_], start=False, stop=True),
                              reads=[wT[c1 % 4], Vb[c1 % NVB]], writes=[po])
                        if ps2 == 0:
                            kb.op("dve", lambda e, po=po, ds_=ds_: e.tensor_copy(out=ACC[:, ti, ds_], in_=po[:, :]), reads=[po], writes=[ACC.ks[ti * 8 + k]])
                        else:
                            kb.op("dve", lambda e, po=po, ds_=ds_: e.tensor_tensor(out=ACC[:, ti, ds_], in0=ACC[:, ti, ds_], in1=po[:, :], op=ALU.add),
                                  reads=[po, ACC.ks[ti * 8 + k]], writes=[ACC.ks[ti * 8 + k]])
                    if ppa is not None:
                        q_, hf = ti // 2, ti % 2
                        c = 2 * ppa + q_
                        U_ = Ub[c % NUB]
                        pA = pA_[q_]
                        for kc in (hf * 16 + 2 * k, hf * 16 + 2 * k + 1):
                            kb.op("pe", lambda e, pA=pA, kc=kc, U_=U_: e.matmul(pA[0:NK, :], lhsT=U_[:, kc, :], rhs=xT[:, kc, :], start=(kc == 0), stop=(kc == KC - 1)),
                                  reads=[U_, xT], writes=[pA])
                    if ps1 is not None:
                        for fn in pend:
                            fn()
                        pend = []
                        h = k
                        for q in range(2):
                            i = 2 * ps1 + q
                            pG = pG_[q]
                            E_ = Eb[st_["rot"] % NEB]
                            G_ = Gh[st_["rot"] % NEB]
                            st_["rot"] += 1
                            kb.op("act", lambda e, E_=E_, h=h, i=i: e.activation(out=E_[:, :], in_=S2[:, ti, h, :], func=AF.Exp, bias=AA[:, ti, h, i:i + 1]),
                                  reads=[S2, AA], writes=[E_])
                            kb.op("dve", lambda e, E_=E_, G_=G_, h=h, i=i: e.scalar_tensor_tensor(out=G_[:, :], in0=S2[:, ti, h, :], scalar=NT[:, ti, h, i:i + 1], in1=E_[:, :],
                                                                                           op0=ALU.is_ge, op1=ALU.mult), reads=[S2, NT, E_], writes=[G_])
                            pend.append(lambda G_=G_, pG=pG, h=h: kb.op(
                                "pe", lambda e: e.matmul(pG[0:NK, ti * 128:(ti + 1) * 128], lhsT=G_[:, :], rhs=identb[:, :], start=(h == 0), stop=(h == 7)),
                                reads=[G_, identb], writes=[pG]))
                for fn in pend:
                    fn()
                if ppa is not None and ti % 2 == 1:
                    q_ = ti // 2
                    c = 2 * ppa + q_
                    g_ = gel[(ppa % 2) * 2 + q_]
                    kb.op("act", lambda e, q_=q_, g_=g_: e.activation(out=g_[:, :], in_=pA_[q_][0:NK, :], func=AF.Gelu), reads=[pA_[q_]], writes=[g_])
                    if c + 2 < NK:
                        u_load(c + 2)

            def s1_gbuild(p, ti):
                for q in range(2):
                    i = 2 * p + q
                    pG = pG_[q]
                    for h in range(8):
                        E_ = Eb[st_["rot"] % NEB]
                        G_ = Gh[st_["rot"] % NEB]
                        st_["rot"] += 1
                        kb.op("act", lambda e, E_=E_, h=h, i=i: e.activation(out=E_[:, :], in_=S2[:, ti, h, :], func=AF.Exp, bias=AA[:, ti, h, i:i + 1]),
                              reads=[S2, AA], writes=[E_])
                        kb.op("dve", lambda e, E_=E_, G_=G_, h=h, i=i: e.scalar_tensor_tensor(out=G_[:, :], in0=S2[:, ti, h, :], scalar=NT[:, ti, h, i:i + 1], in1=E_[:, :],
                                                                                       op0=ALU.is_ge, op1=ALU.mult), reads=[S2, NT, E_], writes=[G_])
                        kb.op("pe", lambda e, G_=G_, pG=pG, h=h: e.matmul(pG[0:NK, ti * 128:(ti + 1) * 128], lhsT=G_[:, :], rhs=identb[:, :], start=(h == 0), stop=(h == 7)),
                              reads=[G_, identb], writes=[pG])

            def s1_end(p):
                for q in range(2):
                    c = 2 * p + q
                    w_ = wT[c % 4]
                    g_ = gel[(p % 2) * 2 + q]
                    kb.op("dve", lambda e, w_=w_, q=q, g_=g_: e.tensor_tensor(out=w_[:, :], in0=pG_[q][0:NK, :], in1=g_[:, :], op=ALU.mult), reads=[pG_[q], g_], writes=[w_])

            def s2_(p, ti):
                c0, c1 = 2 * p, 2 * p + 1
                for dc in range(8):
                    po = kb.ps()
                    ds_ = slice(dc * 512, (dc + 1) * 512)
                    kb.op("pe", lambda e, po=po, ds_=ds_: e.matmul(po[:, :], lhsT=wT[c0 % 4][:, ti * 128:(ti + 1) * 128], rhs=Vb[c0 % NVB][:, ds_], start=True, stop=False),
                          reads=[wT[c0 % 4], Vb[c0 % NVB]], writes=[po])
                    kb.op("pe", lambda e, po=po, ds_=ds_: e.matmul(po[:, :], lhsT=wT[c1 % 4][:, ti * 128:(ti + 1) * 128], rhs=Vb[c1 % NVB][:, ds_], start=False, stop=True),
                          reads=[wT[c1 % 4], Vb[c1 % NVB]], writes=[po])
                    if p == 0:
                        kb.op("dve", lambda e, po=po, ds_=ds_: e.tensor_copy(out=ACC[:, ti, ds_], in_=po[:, :]), reads=[po], writes=[ACC.ks[ti]])
                    else:
                        kb.op("dve", lambda e, po=po, ds_=ds_: e.tensor_tensor(out=ACC[:, ti, ds_], in0=ACC[:, ti, ds_], in1=po[:, :], op=ALU.add),
                              reads=[po, ACC.ks[ti]], writes=[ACC.ks[ti]])

            for g in range(NGP):
                kb.dma("sp", xT[:, :, :], xn2T_d[:, :, g * TG:(g + 1) * TG], writes=[xT])
                for j_, t_ in enumerate((S2, AA, NT)):
                    kb.dma("sp", t_[:, :, :, :].rearrange("p a b c -> p (a b c)"), ans_d[g, j_, :, :], writes=[t_])
                u_load(0)
                u_load(1)
                v_load(0)
                v_load(1)
                for ti in range(4):
                    block(None, None, ti, 0)
                for ti in range(4):
                    block(None, 0, ti, 1 if NP_ > 1 else None)
                s1_end(0)
                for p in range(NP_):
                    nxt = p + 1 < NP_
                    if nxt:
                        v_load(2 * p + 2)
                        v_load(2 * p + 3)
                    for ti in range(4):
                        block(p, p + 1 if nxt else None, ti, p + 2 if p + 2 < NP_ else None)
                    if nxt:
                        s1_end(p + 1)
                for ti in range(4):
                    tok0 = g * TG + ti * 128
                    kb.dma("sp", peer_d[tok0:tok0 + 128, :], ACC[:, ti, :], reads=ACC.ks[ti * 8:(ti + 1) * 8])
            kb.psum_rot = 8
            kb.pnext = 0

        with kb.phase():
            g2b = kb.sb([128, D], F32)
            fnb = kb.sb([128, D], F32)
            kb.dma("sp", g2b[:, :], modflat[5 * D:6 * D].partition_broadcast(128), writes=[g2b])
            kb.dma("sp", fnb[:, :], fnw.partition_broadcast(128), writes=[fnb])
            xa = [kb.sb([128, D], F32) for _ in range(2)]
            pa = [kb.sb([128, D], F32) for _ in range(2)]
            junk = kb.sb([128, D], BF16)
            ssf = [kb.sb([128, 2], F32) for _ in range(2)]
            for it in range(TOK // 128):
                x_, p_, ss_ = xa[it % 2], pa[it % 2], ssf[it % 2]
                rs_ = slice(it * 128, (it + 1) * 128)
                kb.dma("sp", x_[:, :], x2_d[rs_, :], writes=[x_])
                kb.dma("sp", p_[:, :], peer_d[rs_, :], writes=[p_])
                kb.op("dve", lambda e, p_=p_: e.tensor_tensor(out=p_[:, :], in0=p_[:, :], in1=g2b[:, :], op=ALU.mult), reads=[p_, g2b], writes=[p_])
                kb.op("pool", lambda e, p_=p_, x_=x_: e.tensor_tensor(out=x_[:, :], in0=x_[:, :], in1=p_[:, :], op=ALU.add), reads=[p_, x_], writes=[x_])
                kb.op("act", lambda e, x_=x_, ss_=ss_: e.activation(out=junk[:, :], in_=x_[:, :], func=AF.Square, accum_out=ss_[:, 0:1]), reads=[x_], writes=[junk, ss_])
                kb.op("dve", lambda e, ss_=ss_: e.tensor_scalar(out=ss_[:, 0:1], in0=ss_[:, 0:1], scalar1=1.0 / D, scalar2=EPS, op0=ALU.mult, op1=ALU.add), reads=[ss_], writes=[ss_])
                kb.op("act", lambda e, ss_=ss_: e.activation(out=ss_[:, 0:1], in_=ss_[:, 0:1], func=AF.Sqrt), reads=[ss_], writes=[ss_])
                kb.op("dve", lambda e, ss_=ss_: e.reciprocal(out=ss_[:, 1:2], in_=ss_[:, 0:1]), reads=[ss_], writes=[ss_])
                kb.op("dve", lambda e, x_=x_, p_=p_, ss_=ss_: e.scalar_tensor_tensor(out=p_[:, :], in0=x_[:, :], scalar=ss_[:, 1:2], in1=fnb[:, :], op0=ALU.mult, op1=ALU.mult),
                      reads=[x_, ss_, fnb], writes=[p_])
                kb.dma("sp", y[rs_, :], p_[:, :], reads=[p_])


        kb.barrier()
    return nc


def host_shared(cfg, inp):
    TOK, T2, NK, NB, NCH = cfg.TOK, cfg.T2, cfg.NK, cfg.NB, cfg.NCH
    NBP = max(NB, 8)
    m = {}
    idx = np.arange(128)
    le = (idx[:, None] <= idx[None, :])
    consts = np.zeros((128, 5, 128), np.float32)
    consts[:, 0, :] = le.astype(np.float32)
    consts[:, 1, :] = np.where(le.T, 0.0, NEG)
    consts[:, 2, :] = np.where(le, 0.0, 1.0e30)
    consts[127, 3, :] = 1.0
    consts[:, 4, :] = 1.0
    m["consts"] = consts
    m["ident_f"] = np.eye(128, dtype=np.float32)
    m["w_ada"] = inp["w_ada"][0]
    m["b_adaT"] = np.ascontiguousarray(inp["b_ada"][0].reshape(192, 128).T)
    m["n1wT"] = np.ascontiguousarray(inp["norm1_w"][0].reshape(KC, 128).T)
    m["n2wT"] = np.ascontiguousarray(inp["norm2_w"][0].reshape(KC, 128).T)
    m["fnw"] = np.ascontiguousarray(inp["final_norm_w"])
    m["w_in"] = inp["w_in"][0]
    m["conv_wT"] = np.ascontiguousarray(inp["conv_w"][0].T.reshape(16, 128, 4).transpose(1, 0, 2))
    m["conv_bT"] = np.ascontiguousarray(inp["conv_b"][0].reshape(16, 128).T)
    gb = np.concatenate([inp["b_igate"][0], inp["b_fgate"][0]]).astype(np.float32)
    m["gbias"] = np.ascontiguousarray(np.broadcast_to(gb[None, None, :], (128, NCH, 8)))
    m["mnw"] = np.ascontiguousarray(inp["mlstm_norm_w"][0].reshape(2048))
    oneh = np.zeros((NBP, NBP, 128), np.float32)
    for n in range(NBP):
        oneh[n, n, :] = 1.0
    m["oneh"] = oneh.reshape(NBP, NBP * 128)
    m["w_out"] = inp["w_out"][0]
    m["wqc"] = np.ascontiguousarray(inp["peer_wq"][0].reshape(KC, 128, 16, 128).transpose(2, 1, 0, 3)).reshape(16, 128, KC * 128)
    m["skT"] = np.ascontiguousarray(inp["peer_subkeys"][0].reshape(16, NK, 128).transpose(2, 0, 1))
    m["uTc"] = np.ascontiguousarray(inp["peer_u"][0].reshape(NK, NK, KC, 128).transpose(0, 3, 2, 1)).reshape(NK, 128, KC * NK)
    m["vv"] = inp["peer_v"][0]
    return m


def host_inputs(cfg, core, b, s, inp, shared):
    TOK, T2, NB = cfg.TOK, cfg.T2, cfg.NB
    NBP = max(NB, 8)
    x = inp["x"]
    m = dict(shared)
    own0 = s * TOK
    m["x_own"] = np.ascontiguousarray(x[b, own0:own0 + TOK])
    m["x_past"] = np.ascontiguousarray(x[b, 0:TOK]) if s == 1 else np.zeros((TOK, D), np.float32)
    m["cT"] = np.ascontiguousarray(inp["c"][b].reshape(KC, 128).T)
    m["flag"] = np.full((128, 1), float(s), np.float32)
    pos = np.concatenate([np.arange(TOK), s * TOK + np.arange(TOK)]).astype(np.float32)
    half = 64
    inv = (np.float32(10000.0) ** (-np.arange(half, dtype=np.float32) / np.float32(half))).astype(np.float32)
    ang = (pos[:, None] * inv[None, :]).astype(np.float32)
    m["cs_tab"] = np.concatenate([np.cos(ang), np.sin(ang)], axis=1).astype(np.float32)
    pb = np.full((TOK, NBP), NEG, np.float32)
    for q0 in range(0, TOK, 256):
        jb = NB // 2 + q0 // 256
        lo = 0 if s == 1 else NB // 2
        pb[q0:q0 + 256, lo:jb] = 0.0
    m["pb_tab"] = pb
    return m


_CACHE = {}


def kernel(**inp):
    inp = {k: np.asarray(v) for k, v in inp.items()}
    cfg = Cfg(tok=2048, nk=128)
    if "nc" not in _CACHE:
        _CACHE["nc"] = build(cfg)
    nc = _CACHE["nc"]
    shared = host_shared(cfg, inp)
    maps = []
    for core in range(8):
        b, s = core // 2, core % 2
        maps.append(host_inputs(cfg, core, b, s, inp, shared))
    res = run_bass_kernel_spmd(nc, maps, core_ids=list(range(8)))
    out = np.zeros((4, 4096, D), np.float32)
    for core in range(8):
        b, s = core // 2, core % 2
        out[b, s * cfg.TOK:(s + 1) * cfg.TOK] = res.results[core]["y"]
    return out
```
